# Optimizing a Trainium2 kernel written in Bass

```python
import math
import jax, jax.numpy as jnp
from jax import lax
import numpy as np

D_MODEL = 1024
BATCH = 8
SEQ = 2048
DEPTH = 4

CHUNK = 64
Q_BLOCK = 128
N_MIXERS = 4
ALPHA = (2 * DEPTH) ** 0.25
BETA = (8 * DEPTH) ** -0.25
LN_EPS = 1e-5
RMS_EPS = 1e-6
NEG_INF = -1e30

DIFF_HEAD_DIM = 64
DIFF_HEADS = D_MODEL // (2 * DIFF_HEAD_DIM)
DIFF_LAMBDA_STD = 0.1

CA_HEAD_DIM = 64
CA_HEADS = D_MODEL // CA_HEAD_DIM
CA_LEFT_CHUNKS = 8
CA_BAND = (CA_LEFT_CHUNKS + 1) * CHUNK
CA_REL_CLIP = 128

MLA_HEADS = D_MODEL // 64
MLA_NOPE_DIM = 64
MLA_ROPE_DIM = 32
MLA_V_DIM = 64
MLA_Q_RANK = 384
MLA_KV_RANK = 256
ROPE_THETA = 10000.0

GMLP_CHUNK = 128
GMLP_GROUPS = 8
GMLP_WIDTH = D_MODEL

D_FF = 2816
N_EXPERTS = 8
TOP_K = 2
D_FF_EXPERT = 3584

N_A = (DEPTH + 3) // 4
N_B = (DEPTH + 2) // 4
N_C = (DEPTH + 1) // 4
N_D = DEPTH // 4
N_DENSE = (DEPTH + 1) // 2
N_MOE = DEPTH // 2

kernel_name = "hybrid_chunk_causal_deepnorm_trunk"


def layer_norm(x, g, b):
    xf = x.astype(jnp.float32)
    mu = jnp.mean(xf, axis=-1, keepdims=True)
    xc = xf - mu
    var = jnp.mean(xc * xc, axis=-1, keepdims=True)
    return (xc * lax.rsqrt(var + LN_EPS) * g + b).astype(x.dtype)


def rms_norm(x, g, eps=RMS_EPS):
    xf = x.astype(jnp.float32)
    ms = jnp.mean(xf * xf, axis=-1, keepdims=True)
    return (xf * lax.rsqrt(ms + eps) * g).astype(x.dtype)


def rotary(x, pos):
    half = x.shape[-1] // 2
    inv_freq = ROPE_THETA ** (-jnp.arange(half, dtype=jnp.float32) / half)
    ang = pos.astype(jnp.float32)[:, None] * inv_freq[None, :]
    cos = jnp.cos(ang)[:, None, :]
    sin = jnp.sin(ang)[:, None, :]
    xf = x.astype(jnp.float32)
    x1, x2 = xf[..., :half], xf[..., half:]
    return jnp.concatenate([x1 * cos - x2 * sin, x2 * cos + x1 * sin], axis=-1).astype(x.dtype)


def chunk_causal_attention(q, k, v, coef, scale):
    seq = q.shape[1]
    coef = coef.astype(jnp.float32)
    outs = []
    for blk in range(seq // Q_BLOCK):
        q0 = blk * Q_BLOCK
        k_end = q0 + Q_BLOCK
        s = jnp.einsum('bqmhd,bkmhd->bmhqk', q[:, q0:k_end], k[:, :k_end],
                       preferred_element_type=jnp.float32) * scale
        q_chunk = (q0 + jnp.arange(Q_BLOCK)) // CHUNK
        k_chunk = jnp.arange(k_end) // CHUNK
        visible = k_chunk[None, :] <= q_chunk[:, None]
        p = jax.nn.softmax(jnp.where(visible, s, NEG_INF), axis=-1)
        p = jnp.einsum('m,bmhqk->bhqk', coef, p)
        outs.append(jnp.einsum('bhqk,bkhd->bqhd', p.astype(v.dtype), v[:, :k_end]))
    return jnp.concatenate(outs, axis=1)


def diff_attention(x, wq, wk, wv, lq1, lk1, lq2, lk2, sub_g, wo, lambda_init):
    b, s, _ = x.shape
    q = (x @ wq).reshape(b, s, DIFF_HEADS, 2, DIFF_HEAD_DIM).transpose(0, 1, 3, 2, 4)
    k = (x @ wk).reshape(b, s, DIFF_HEADS, 2, DIFF_HEAD_DIM).transpose(0, 1, 3, 2, 4)
    v = (x @ wv).reshape(b, s, DIFF_HEADS, 2 * DIFF_HEAD_DIM)
    lam = (jnp.exp(jnp.sum(lq1.astype(jnp.float32) * lk1.astype(jnp.float32)))
           - jnp.exp(jnp.sum(lq2.astype(jnp.float32) * lk2.astype(jnp.float32))) + lambda_init)
    coef = jnp.stack([jnp.ones_like(lam), -lam])
    o = chunk_causal_attention(q, k, v, coef, DIFF_HEAD_DIM ** -0.5)
    o = rms_norm(o, sub_g, eps=1e-5) * (1.0 - lambda_init)
    return o.reshape(b, s, D_MODEL) @ wo


def band_chunk_attention(x, w_qkv, rel_bias, wo):
    b, s, _ = x.shape
    nc = s // CHUNK
    qkv = (x @ w_qkv).reshape(b, s, 3, CA_HEADS, CA_HEAD_DIM)
    q, k, v = qkv[:, :, 0], qkv[:, :, 1], qkv[:, :, 2]
    pad = CA_LEFT_CHUNKS * CHUNK
    k_pad = jnp.pad(k, ((0, 0), (pad, 0), (0, 0), (0, 0)))
    v_pad = jnp.pad(v, ((0, 0), (pad, 0), (0, 0), (0, 0)))
    band = jnp.arange(nc)[:, None] * CHUNK + jnp.arange(CA_BAND)[None, :]
    k_band = k_pad[:, band]
    v_band = v_pad[:, band]
    qc = q.reshape(b, nc, CHUNK, CA_HEADS, CA_HEAD_DIM)
    sc = jnp.einsum('bcqhd,bckhd->bhcqk', qc, k_band,
                    preferred_element_type=jnp.float32) * (CA_HEAD_DIM ** -0.5)
    rel = jnp.arange(CHUNK)[:, None] - jnp.arange(CA_BAND)[None, :] + pad
    rel_idx = jnp.clip(rel, -CA_REL_CLIP, CA_REL_CLIP) + CA_REL_CLIP
    bias = rel_bias[:, rel_idx].astype(jnp.float32)
    valid = band >= pad
    sc = jnp.where(valid[None, None, :, None, :], sc + bias[None, :, None], NEG_INF)
    p = jax.nn.softmax(sc, axis=-1)
    o = jnp.einsum('bhcqk,bckhd->bcqhd', p.astype(v.dtype), v_band).reshape(b, s, D_MODEL)
    return o @ wo


def mla_attention(x, w_dq, q_norm_g, w_uq, w_dkv, kv_norm_g, w_ukv, wo, pos):
    b, s, _ = x.shape
    cq = rms_norm(x @ w_dq, q_norm_g)
    q = (cq @ w_uq).reshape(b, s, MLA_HEADS, MLA_NOPE_DIM + MLA_ROPE_DIM)
    q_nope, q_rope = q[..., :MLA_NOPE_DIM], rotary(q[..., MLA_NOPE_DIM:], pos)
    ckv = x @ w_dkv
    c_kv = rms_norm(ckv[..., :MLA_KV_RANK], kv_norm_g)
    k_rope = rotary(ckv[..., MLA_KV_RANK:][:, :, None, :], pos)
    kv = (c_kv @ w_ukv).reshape(b, s, MLA_HEADS, MLA_NOPE_DIM + MLA_V_DIM)
    k_nope, v = kv[..., :MLA_NOPE_DIM], kv[..., MLA_NOPE_DIM:]
    q_full = jnp.concatenate([q_nope, q_rope], axis=-1)[:, :, None]
    k_full = jnp.concatenate(
        [k_nope, jnp.broadcast_to(k_rope, (b, s, MLA_HEADS, MLA_ROPE_DIM))], axis=-1)[:, :, None]
    o = chunk_causal_attention(q_full, k_full, v, jnp.ones((1,), jnp.float32),
                               (MLA_NOPE_DIM + MLA_ROPE_DIM) ** -0.5)
    return o.reshape(b, s, MLA_HEADS * MLA_V_DIM) @ wo


def chunk_spatial_gating(x, w_in, v_norm_g, v_norm_b, w_s, b_s, w_out):
    b, s, _ = x.shape
    nc = s // GMLP_CHUNK
    h = jax.nn.gelu(x @ w_in)
    u, v = h[..., :GMLP_WIDTH], h[..., GMLP_WIDTH:]
    v = layer_norm(v, v_norm_g, v_norm_b)
    v = v.reshape(b, nc, GMLP_CHUNK, GMLP_GROUPS, GMLP_WIDTH // GMLP_GROUPS)
    sub = jnp.arange(GMLP_CHUNK) // CHUNK
    mask = sub[None, :] <= sub[:, None]
    w = jnp.where(mask[None], w_s, 0.0)
    mixed = jnp.einsum('gij,bcjgd->bcigd', w, v) + b_s.T[None, None, :, :, None]
    return (u * mixed.reshape(b, s, GMLP_WIDTH)) @ w_out


def swiglu(x, w1, w3, w2):
    return (jax.nn.silu(x @ w1) * (x @ w3)) @ w2


def moe_swiglu(x, w_router, w1, w3, w2):
    b, s, d = x.shape
    t = x.reshape(b * s, d)
    logits = (t @ w_router).astype(jnp.float32)
    top_val, top_idx = lax.top_k(logits, TOP_K)
    gates = jax.nn.softmax(top_val, axis=-1)
    combine = jnp.sum(jax.nn.one_hot(top_idx, N_EXPERTS, dtype=jnp.float32) * gates[..., None], axis=1)
    y = jnp.zeros_like(t)
    for e in range(N_EXPERTS):
        y = y + combine[:, e:e + 1].astype(t.dtype) * swiglu(t, w1[e], w3[e], w2[e])
    return y.reshape(b, s, d)


def setup_inputs(seed: int = 0) -> dict:
    key = jax.random.key(seed)
    keys = iter(jax.random.split(key, 48))

    def nrm(shape, scale):
        return jax.random.normal(next(keys), shape, jnp.float32) * scale

    def gain(shape):
        return 1.0 + nrm(shape, 0.02)

    d = D_MODEL
    return {
        "x": nrm((BATCH, SEQ, d), 1.0),
        "ln_mix_g": gain((DEPTH, d)),
        "ln_mix_b": nrm((DEPTH, d), 0.02),
        "ln_ffn_g": gain((DEPTH, d)),
        "ln_ffn_b": nrm((DEPTH, d), 0.02),
        "diff_wq": nrm((N_A, d, d), d ** -0.5),
        "diff_wk": nrm((N_A, d, d), d ** -0.5),
        "diff_wv": nrm((N_A, d, d), d ** -0.5),
        "diff_lq1": nrm((N_A, DIFF_HEAD_DIM), DIFF_LAMBDA_STD),
        "diff_lk1": nrm((N_A, DIFF_HEAD_DIM), DIFF_LAMBDA_STD),
        "diff_lq2": nrm((N_A, DIFF_HEAD_DIM), DIFF_LAMBDA_STD),
        "diff_lk2": nrm((N_A, DIFF_HEAD_DIM), DIFF_LAMBDA_STD),
        "diff_sub_g": gain((N_A, 2 * DIFF_HEAD_DIM)),
        "diff_wo": nrm((N_A, d, d), d ** -0.5 * BETA),
        "ca_w_qkv": nrm((N_B, d, 3 * d), d ** -0.5),
        "ca_rel_bias": nrm((N_B, CA_HEADS, 2 * CA_REL_CLIP + 1), 0.2),
        "ca_wo": nrm((N_B, d, d), d ** -0.5 * BETA),
        "mla_w_dq": nrm((N_C, d, MLA_Q_RANK), d ** -0.5),
        "mla_q_norm_g": gain((N_C, MLA_Q_RANK)),
        "mla_w_uq": nrm((N_C, MLA_Q_RANK, MLA_HEADS * (MLA_NOPE_DIM + MLA_ROPE_DIM)), MLA_Q_RANK ** -0.5),
        "mla_w_dkv": nrm((N_C, d, MLA_KV_RANK + MLA_ROPE_DIM), d ** -0.5),
        "mla_kv_norm_g": gain((N_C, MLA_KV_RANK)),
        "mla_w_ukv": nrm((N_C, MLA_KV_RANK, MLA_HEADS * (MLA_NOPE_DIM + MLA_V_DIM)), MLA_KV_RANK ** -0.5),
        "mla_wo": nrm((N_C, MLA_HEADS * MLA_V_DIM, d), (MLA_HEADS * MLA_V_DIM) ** -0.5 * BETA),
        "sg_w_in": nrm((N_D, d, 2 * GMLP_WIDTH), d ** -0.5),
        "sg_v_norm_g": gain((N_D, GMLP_WIDTH)),
        "sg_v_norm_b": nrm((N_D, GMLP_WIDTH), 0.02),
        "sg_w_s": nrm((N_D, GMLP_GROUPS, GMLP_CHUNK, GMLP_CHUNK), GMLP_CHUNK ** -0.5),
        "sg_b_s": 1.0 + nrm((N_D, GMLP_GROUPS, GMLP_CHUNK), 0.01),
        "sg_w_out": nrm((N_D, GMLP_WIDTH, d), GMLP_WIDTH ** -0.5 * BETA),
        "ffn_w1": nrm((N_DENSE, d, D_FF), d ** -0.5),
        "ffn_w3": nrm((N_DENSE, d, D_FF), d ** -0.5),
        "ffn_w2": nrm((N_DENSE, D_FF, d), D_FF ** -0.5 * BETA),
        "moe_w_router": nrm((N_MOE, d, N_EXPERTS), d ** -0.5),
        "moe_w1": nrm((N_MOE, N_EXPERTS, d, D_FF_EXPERT), d ** -0.5),
        "moe_w3": nrm((N_MOE, N_EXPERTS, d, D_FF_EXPERT), d ** -0.5),
        "moe_w2": nrm((N_MOE, N_EXPERTS, D_FF_EXPERT, d), D_FF_EXPERT ** -0.5 * BETA),
    }


def reference(x, ln_mix_g, ln_mix_b, ln_ffn_g, ln_ffn_b,
              diff_wq, diff_wk, diff_wv, diff_lq1, diff_lk1, diff_lq2, diff_lk2, diff_sub_g, diff_wo,
              ca_w_qkv, ca_rel_bias, ca_wo,
              mla_w_dq, mla_q_norm_g, mla_w_uq, mla_w_dkv, mla_kv_norm_g, mla_w_ukv, mla_wo,
              sg_w_in, sg_v_norm_g, sg_v_norm_b, sg_w_s, sg_b_s, sg_w_out,
              ffn_w1, ffn_w3, ffn_w2,
              moe_w_router, moe_w1, moe_w3, moe_w2):
    pos = jnp.arange(x.shape[1])
    h = x
    for i in range(DEPTH):
        kind, j = i % N_MIXERS, i // N_MIXERS
        if kind == 0:
            lambda_init = 0.8 - 0.6 * math.exp(-0.3 * i)
            m = diff_attention(h, diff_wq[j], diff_wk[j], diff_wv[j], diff_lq1[j], diff_lk1[j],
                               diff_lq2[j], diff_lk2[j], diff_sub_g[j], diff_wo[j], lambda_init)
        elif kind == 1:
            m = band_chunk_attention(h, ca_w_qkv[j], ca_rel_bias[j], ca_wo[j])
        elif kind == 2:
            m = mla_attention(h, mla_w_dq[j], mla_q_norm_g[j], mla_w_uq[j], mla_w_dkv[j],
                              mla_kv_norm_g[j], mla_w_ukv[j], mla_wo[j], pos)
        else:
            m = chunk_spatial_gating(h, sg_w_in[j], sg_v_norm_g[j], sg_v_norm_b[j],
                                     sg_w_s[j], sg_b_s[j], sg_w_out[j])
        h = layer_norm(ALPHA * h + m, ln_mix_g[i], ln_mix_b[i])
        if i % 2 == 0:
            f = swiglu(h, ffn_w1[i // 2], ffn_w3[i // 2], ffn_w2[i // 2])
        else:
            f = moe_swiglu(h, moe_w_router[i // 2], moe_w1[i // 2], moe_w3[i // 2], moe_w2[i // 2])
        h = layer_norm(ALPHA * h + f, ln_ffn_g[i], ln_ffn_b[i])
    return h
```

```python
import math
from contextlib import ExitStack

import numpy as np
import concourse.bass as bass
import concourse.mybir as mybir
from concourse.bass_utils import run_bass_kernel_spmd

F32 = mybir.dt.float32
BF16 = mybir.dt.bfloat16
AF = mybir.ActivationFunctionType
ALU = mybir.AluOpType
AX = mybir.AxisListType

S = 2048
D = 1024
NT = S // 128
KC = D // 128
DEPTH = 4
ALPHA = (2 * DEPTH) ** 0.25
LN_EPS = 1e-5
D_FF = 2816
D_FFE = 3584
NE = 8
NDS = 16


class Buf:
    def __init__(self, ap, name=""):
        self.ap = ap
        self.w = None
        self.r = {}
        self.name = name

    def __getitem__(self, idx):
        return View(self.ap[idx], self)

    def v(self):
        return View(self.ap, self)


class View:
    def __init__(self, ap, buf):
        self.ap = ap
        self.buf = buf

    def __getitem__(self, idx):
        return View(self.ap[idx], self.buf)


def _ap(x):
    return x.ap if isinstance(x, View) else x


class K:
    def __init__(self):
        nc = bass.Bass("TRN2", target_bir_lowering=False)
        self.nc = nc
        self.eng = {"pe": nc.tensor, "act": nc.scalar, "dve": nc.vector, "pool": nc.gpsimd, "sp": nc.sync}
        self.sem = {e: nc.alloc_semaphore("s_" + e) for e in ("pe", "act", "dve", "pool")}
        self.cnt = {e: 0 for e in self.sem}
        self.dsem = [nc.alloc_semaphore(f"s_d{i}") for i in range(NDS)]
        self.dcnt = [0] * NDS
        self.dnext = 0
        self.dnext_pool = 0
        self.seen = {e: {} for e in self.eng}
        self.n_inst = 0

    def _semh(self, key):
        return self.sem[key] if isinstance(key, str) else self.dsem[key[1]]

    def _wait(self, e, key, val):
        if self.seen[e].get(key, 0) >= val:
            return
        self.eng[e].wait_ge(self._semh(key), val)
        self.seen[e][key] = val

    def _deps(self, e, reads, writes):
        deps = {}

        def add(t):
            if t is None:
                return
            k, v = t
            if deps.get(k, 0) < v:
                deps[k] = v

        for v in reads:
            add(v.buf.w)
        for v in writes:
            add(v.buf.w)
            for kk, val in v.buf.r.items():
                add((kk, val))
        for kk, val in deps.items():
            if kk == e and e == "pe":
                continue
            self._wait(e, kk, val)

    def _done(self, key, val, reads, writes):
        for v in writes:
            v.buf.w = (key, val)
            v.buf.r = {}
        for v in reads:
            if v.buf.r.get(key, 0) < val:
                v.buf.r[key] = val

    def op(self, e, fn, reads, writes):
        reads = [r for r in reads if isinstance(r, View)]
        self._deps(e, reads, writes)
        inst = fn()
        self.cnt[e] += 1
        inst.then_inc(self.sem[e], 1)
        self._done(e, self.cnt[e], reads, writes)
        self.n_inst += 1
        return inst

    def dma(self, q, out, in_, **kw):
        self._deps(q, [in_], [out])
        half = NDS // 2
        if q == "pool":
            i = half + self.dnext_pool
            self.dnext_pool = (self.dnext_pool + 1) % half
        else:
            i = self.dnext
            self.dnext = (self.dnext + 1) % half
        key = ("d", i)
        if self.dcnt[i] > 0:
            self._wait(q, key, 16 * self.dcnt[i])
        inst = self.eng[q].dma_start(out=out.ap, in_=in_.ap, **kw)
        self.dcnt[i] += 1
        inst.then_inc(self.dsem[i], 16)
        self._done(key, 16 * self.dcnt[i], [in_], [out])
        self.n_inst += 1

    def dma_indirect(self, out, out_off, in_, in_off):
        q = "pool"
        idx = in_off if in_off is not None else out_off
        self._deps(q, [in_, idx], [out])
        half = NDS // 2
        i = half + self.dnext_pool
        self.dnext_pool = (self.dnext_pool + 1) % half
        key = ("d", i)
        if self.dcnt[i] > 0:
            self._wait(q, key, 16 * self.dcnt[i])
        oo = bass.IndirectOffsetOnAxis(ap=out_off.ap, axis=0) if out_off is not None else None
        io = bass.IndirectOffsetOnAxis(ap=in_off.ap, axis=0) if in_off is not None else None
        inst = self.nc.gpsimd.indirect_dma_start(out=out.ap, out_offset=oo, in_=in_.ap, in_offset=io)
        self.dcnt[i] += 1
        inst.then_inc(self.dsem[i], 16)
        self._done(key, 16 * self.dcnt[i], [in_, idx], [out])
        self.n_inst += 1

    def barrier(self):
        keys = [(e, self.cnt[e]) for e in self.sem] + [(("d", i), 16 * self.dcnt[i]) for i in range(NDS)]
        for e in self.eng:
            for kk, val in keys:
                if val > 0 and kk != e:
                    self._wait(e, kk, val)

    def final_wait(self):
        for i in range(NDS):
            if self.dcnt[i] > 0:
                self._wait("sp", ("d", i), 16 * self.dcnt[i])

    def mm(self, out, lhsT, rhs, start=True, stop=True, **kw):
        return self.op("pe", lambda: self.nc.tensor.matmul(out.ap, lhsT.ap, rhs.ap, start=start, stop=stop, **kw),
                       [lhsT, rhs], [out])

    def tr(self, out, in_, ident):
        return self.op("pe", lambda: self.nc.tensor.transpose(out.ap, in_.ap, ident.ap), [in_, ident], [out])

    def act(self, out, in_, func, bias=None, scale=None, accum=None):
        kw = {}
        reads = [in_]
        writes = [out]
        if bias is not None:
            kw["bias"] = _ap(bias)
            reads.append(bias)
        if scale is not None:
            kw["scale"] = _ap(scale)
            reads.append(scale)
        if accum is not None:
            kw["accum_out"] = accum.ap
            writes.append(accum)
        return self.op("act", lambda: self.nc.scalar.activation(out=out.ap, in_=in_.ap, func=func, **kw), reads, writes)

    def tt(self, e, out, in0, in1, op):
        return self.op(e, lambda: self.eng[e].tensor_tensor(out=out.ap, in0=in0.ap, in1=in1.ap, op=op), [in0, in1], [out])

    def ts(self, e, out, in0, s1, op0, s2=None, op1=None, accum=None):
        kw = {}
        writes = [out]
        if op1 is not None:
            kw["op1"] = op1
        if accum is not None:
            kw["accum_out"] = accum.ap
            writes.append(accum)
        return self.op(e, lambda: self.eng[e].tensor_scalar(out=out.ap, in0=in0.ap, scalar1=_ap(s1), scalar2=_ap(s2), op0=op0, **kw),
                       [in0, s1, s2], writes)

    def stt(self, e, out, in0, scalar, in1, op0, op1, accum=None):
        kw = {}
        writes = [out]
        if accum is not None:
            kw["accum_out"] = accum.ap
            writes.append(accum)
        return self.op(e, lambda: self.eng[e].scalar_tensor_tensor(out=out.ap, in0=in0.ap, scalar=_ap(scalar), in1=in1.ap, op0=op0, op1=op1, **kw),
                       [in0, scalar, in1], writes)

    def copy(self, e, out, in_):
        if e == "act":
            return self.act(out, in_, AF.Copy)
        return self.op(e, lambda: self.eng[e].tensor_copy(out=out.ap, in_=in_.ap), [in_], [out])

    def memset(self, e, out, val):
        return self.op(e, lambda: self.eng[e].memset(out.ap, val), [], [out])

    def recip(self, out, in_):
        return self.op("dve", lambda: self.nc.vector.reciprocal(out=out.ap, in_=in_.ap), [in_], [out])

    def sb(self, st, name, shape, dtype):
        self.n_alloc = getattr(self, "n_alloc", 0) + 1
        return st.enter_context(self.nc.sbuf_tensor(f"{name}_{self.n_alloc}", list(shape), dtype))

    def dram_in(self, name, shape, dtype=F32):
        return Buf(self.nc.dram_tensor(name, list(shape), dtype, kind="ExternalInput").ap(), name)

    def dram_out(self, name, shape, dtype=F32):
        return Buf(self.nc.dram_tensor(name, list(shape), dtype, kind="ExternalOutput").ap(), name)

    def dram_tmp(self, name, shape, dtype=F32):
        return Buf(self.nc.dram_tensor(name, list(shape), dtype, kind="Internal").ap(), name)


class Prog:
    def __init__(self, k, plan):
        self.k = k
        nc = k.nc
        self.plan = plan
        self.st = ExitStack()
        st = self.st
        hT_t = k.sb(st, "hT", [128, KC, S], BF16)
        self.hT = [Buf(hT_t[:, :, tb * 512:(tb + 1) * 512], f"hT{tb}") for tb in range(4)]
        ident_t = k.sb(st, "ident", [128, 128], BF16)
        self.ident = Buf(ident_t[:, :], "ident")
        ones_t = k.sb(st, "ones", [128, 128], BF16)
        self.ones = Buf(ones_t[:, :], "ones")
        small_t = k.sb(st, "small", [128, 64], F32)
        self.small_t = small_t
        self.ps = [Buf(nc.alloc_psum_tensor(f"ps{i}", [128, 512], F32)[:, :], f"ps{i}") for i in range(7)]
        pst = nc.alloc_psum_tensor("pstr", [128, 1024], BF16)
        self.ps_tr = Buf(pst[:, :], "pstr")
        self.x = k.dram_in("x", [S, D])
        self.y = k.dram_out("y", [S, D])
        self.hres = k.dram_tmp("hres", [S, D])
        self.ident_d = k.dram_in("ident_c", [128, 128])
        self.ins = {}

    def inp(self, name, shape):
        if name not in self.ins:
            self.ins[name] = self.k.dram_in(name, shape)
        return self.ins[name]


def emit_consts(P):
    k = P.k
    k.dma("pool", P.ident.v(), P.ident_d.v())
    k.memset("pool", P.ones.v(), 1.0)


def emit_ln_tail(P, st, t, xs, gb, bb, dst, tmp):
    k = P.k
    ts_ = tmp["sets"][t % len(tmp["sets"])]
    stats, mv, sc, xn, xb = ts_["stats"], ts_["mv"], ts_["sc"], ts_["xn"], ts_["xb"]
    for hf in range(2):
        k.op("dve", lambda hf=hf: k.nc.vector.bn_stats(out=stats.ap[:, hf * 6:(hf + 1) * 6], in_=xs.ap[:, hf * 512:(hf + 1) * 512]),
             [xs.v()], [stats.v()])
    k.op("dve", lambda: k.nc.vector.bn_aggr(out=mv.ap, in_=stats.ap), [stats.v()], [mv.v()])
    k.act(sc[:, 0:1], mv[:, 1:2], AF.Sqrt, bias=tmp["eps"][:, 0:1])
    k.recip(sc[:, 1:2], sc[:, 0:1])
    k.stt("dve", sc[:, 2:3], mv[:, 0:1], -1.0, sc[:, 1:2], ALU.mult, ALU.mult)
    k.act(xn.v(), xs.v(), AF.Identity, scale=sc[:, 1:2], bias=sc[:, 2:3])
    k.tt("pool", xn.v(), xn.v(), gb.v(), ALU.mult)
    k.tt("pool", xn.v(), xn.v(), bb.v(), ALU.add)
    k.dma("sp", dst[t * 128:(t + 1) * 128, :], xn.v())
    k.copy("act", xb.v(), xn.v())

    def part_b(t=t, xb=xb):
        for kc in range(KC):
            k.tr(P.ps_tr[:, kc * 128:(kc + 1) * 128], xb[:, kc * 128:(kc + 1) * 128], P.ident.v())
        tb, tt_ = t // 4, t % 4
        k.copy("dve", P.hT[tb][:, :, tt_ * 128:(tt_ + 1) * 128],
               View(P.ps_tr.ap.rearrange("p (k c) -> p k c", c=128), P.ps_tr))

    flush_ln_tail(P, tmp)
    tmp["pending"] = part_b
    return xn


def flush_ln_tail(P, tmp):
    if tmp.get("pending") is not None:
        fn = tmp["pending"]
        tmp["pending"] = None
        fn()


def alloc_ln_tmp(P, st, pfx, nset=2):
    k = P.k
    tmp = {"sets": [], "pending": None}
    for i in range(nset):
        d = {}
        d["stats"] = Buf(k.sb(st, pfx + f"stats{i}", [128, 12], F32)[:, :])
        d["mv"] = Buf(k.sb(st, pfx + f"mv{i}", [128, 2], F32)[:, :])
        d["sc"] = Buf(k.sb(st, pfx + f"sc{i}", [128, 4], F32)[:, :])
        d["xn"] = Buf(k.sb(st, pfx + f"xn{i}", [128, D], F32)[:, :])
        d["xb"] = Buf(k.sb(st, pfx + f"xb{i}", [128, D], BF16)[:, :])
        tmp["sets"].append(d)
    tmp["eps"] = Buf(k.sb(st, pfx + "eps", [128, 1], F32)[:, :])
    k.memset("pool", tmp["eps"].v(), LN_EPS)
    return tmp


def emit_prologue(P):
    k = P.k
    with ExitStack() as st:
        xin = [Buf(k.sb(st, f"pro_x{i}", [128, D], F32)[:, :]) for i in range(2)]
        xb = [Buf(k.sb(st, f"pro_xb{i}", [128, D], BF16)[:, :]) for i in range(2)]
        for t in range(NT):
            xi = xin[t % 2]
            k.dma("sp", xi.v(), P.x[t * 128:(t + 1) * 128, :])
            k.dma("sp", P.hres[t * 128:(t + 1) * 128, :], xi.v())
            k.copy("act", xb[t % 2].v(), xi.v())
            for kc in range(KC):
                k.tr(P.ps_tr[:, kc * 128:(kc + 1) * 128], xb[t % 2][:, kc * 128:(kc + 1) * 128], P.ident.v())
            tb, tt_ = t // 4, t % 4
            k.copy("dve", P.hT[tb][:, :, tt_ * 128:(tt_ + 1) * 128],
                   View(P.ps_tr.ap.rearrange("p (k c) -> p k c", c=128), P.ps_tr))
        k.barrier()


def load_ln_params(P, st, g_d, b_d, pfx):
    k = P.k
    gb = Buf(k.sb(st, pfx + "g", [128, D], F32)[:, :])
    bb = Buf(k.sb(st, pfx + "b", [128, D], F32)[:, :])
    k.dma("sp", gb.v(), View(g_d.ap.partition_broadcast(128), g_d.buf))
    k.dma("sp", bb.v(), View(b_d.ap.partition_broadcast(128), b_d.buf))
    return gb, bb


def emit_ffn(P, li, moe, dst):
    k = P.k
    nc = k.nc
    j = li // 2
    if moe:
        E, F = NE, D_FFE
        w1 = P.inp("moe_w1", [2, NE, D, D_FFE])
        w3 = P.inp("moe_w3", [2, NE, D, D_FFE])
        w2 = P.inp("moe_w2", [2, NE, D_FFE, D])
        wr = P.inp("moe_wrT", [2, NE, D])
        w1v = lambda e: View(w1.ap[j, e], w1)
        w3v = lambda e: View(w3.ap[j, e], w3)
        w2v = lambda e: View(w2.ap[j, e], w2)
    else:
        E, F = 1, D_FF
        w1 = P.inp("ffn_w1", [2, D, D_FF])
        w3 = P.inp("ffn_w3", [2, D, D_FF])
        w2 = P.inp("ffn_w2", [2, D_FF, D])
        w1v = lambda e: View(w1.ap[j], w1)
        w3v = lambda e: View(w3.ap[j], w3)
        w2v = lambda e: View(w2.ap[j], w2)
    g_d = P.inp("ln_ffn_g", [DEPTH, D])
    b_d = P.inp("ln_ffn_b", [DEPTH, D])
    nj = F // 128
    G = 4
    units = [(e, j0, min(G, nj - j0)) for e in range(E) for j0 in range(0, nj, G)]
    U = len(units)
    with ExitStack() as st:
        comb_t = k.sb(st, "comb", [128, NT, NE], F32)
        comb = Buf(comb_t[:, :, :], "comb")
        if moe:
            with ExitStack() as st2:
                wrb = Buf(k.sb(st2, "wrb", [128, NE, D], F32)[:, :, :], "wrb")
                k.dma("sp", wrb.v(), View(wr.ap[j].partition_broadcast(128), wr))
                hin = [Buf(k.sb(st2, f"rt_h{i}", [128, D], F32)[:, :]) for i in range(2)]
                junk = Buf(k.sb(st2, "rt_junk", [128, D], F32)[:, :])
                lg = Buf(k.sb(st2, "rt_lg", [128, NE], F32)[:, :])
                l2 = Buf(k.sb(st2, "rt_l2", [128, NE], F32)[:, :])
                m1 = Buf(k.sb(st2, "rt_m1", [128, NE], F32)[:, :])
                m2 = Buf(k.sb(st2, "rt_m2", [128, NE], F32)[:, :])
                sc = Buf(k.sb(st2, "rt_sc", [128, 8], F32)[:, :])
                for t in range(NT):
                    hi = hin[t % 2]
                    k.dma("sp", hi.v(), P.hres[t * 128:(t + 1) * 128, :])
                    for e in range(NE):
                        k.stt("dve", junk.v(), hi.v(), 1.0, wrb[:, e, :], ALU.mult, ALU.mult, accum=lg[:, e:e + 1])
                    k.op("dve", lambda: nc.vector.reduce_max(out=sc.ap[:, 0:1], in_=lg.ap, axis=AX.X), [lg.v()], [sc.v()])
                    k.ts("dve", m1.v(), lg.v(), sc[:, 0:1], ALU.is_equal)
                    k.stt("dve", l2.v(), m1.v(), -1e30, lg.v(), ALU.mult, ALU.add)
                    k.op("dve", lambda: nc.vector.reduce_max(out=sc.ap[:, 1:2], in_=l2.ap, axis=AX.X), [l2.v()], [sc.v()])
                    k.ts("dve", m2.v(), l2.v(), sc[:, 1:2], ALU.is_equal)
                    k.tt("dve", sc[:, 2:3], sc[:, 1:2], sc[:, 0:1], ALU.subtract)
                    k.act(sc[:, 3:4], sc[:, 2:3], AF.Exp)
                    k.ts("dve", sc[:, 4:5], sc[:, 3:4], 1.0, ALU.add)
                    k.recip(sc[:, 5:6], sc[:, 4:5])
                    k.tt("dve", sc[:, 6:7], sc[:, 3:4], sc[:, 5:6], ALU.mult)
                    k.ts("dve", m1.v(), m1.v(), sc[:, 5:6], ALU.mult)
                    k.stt("dve", comb[:, t, :], m2.v(), sc[:, 6:7], m1.v(), ALU.mult, ALU.add)
                k.barrier()
        yacc_t = k.sb(st, "yacc", [128, NT, D], F32)
        yacc = [[Buf(yacc_t[:, t, hf * 512:(hf + 1) * 512]) for hf in range(2)] for t in range(NT)]
        stc = ExitStack()
        htg_t = [k.sb(stc, f"htg{i}", [128, G, S], BF16) for i in range(2)]
        htg = [[[Buf(htg_t[i][:, jj, tb * 512:(tb + 1) * 512]) for tb in range(4)] for jj in range(G)] for i in range(2)]
        w1g = [Buf(k.sb(stc, f"w1g{i}", [128, KC, G * 128], BF16)[:, :, :]) for i in range(2)]
        w3g = [Buf(k.sb(stc, f"w3g{i}", [128, KC, G * 128], BF16)[:, :, :]) for i in range(2)]
        w2g = [Buf(k.sb(stc, f"w2g{i}", [128, G, D], BF16)[:, :, :]) for i in range(2)]
        sil = [Buf(k.sb(stc, f"sil{i}", [128, 512], F32)[:, :]) for i in range(2)]

        def loadA(u):
            e, j0, n = units[u]
            s = u % 2
            k.dma("pool", w1g[s][:, :, 0:n * 128],
                  View(w1v(e).ap.rearrange("(kc p) f -> p kc f", p=128)[:, :, j0 * 128:(j0 + n) * 128], w1))
            k.dma("pool", w3g[s][:, :, 0:n * 128],
                  View(w3v(e).ap.rearrange("(kc p) f -> p kc f", p=128)[:, :, j0 * 128:(j0 + n) * 128], w3))

        def loadB(u):
            e, j0, n = units[u]
            s = u % 2
            k.dma("pool", w2g[s][:, 0:n, :],
                  View(w2v(e).ap[j0 * 128:(j0 + n) * 128, :].rearrange("(j p) d -> p j d", p=128), w2))

        cnt1 = [0]

        def phase1(u):
            e, j0, n = units[u]
            s = u % 2
            for jj in range(n):
                for tb in range(4):
                    c = cnt1[0] % 2
                    cnt1[0] += 1
                    pa, pb = P.ps[2 * c], P.ps[2 * c + 1]
                    for (W, pp) in ((w1g[s], pa), (w3g[s], pb)):
                        for kc in range(KC):
                            k.mm(pp.v(), W[:, kc, jj * 128:(jj + 1) * 128], P.hT[tb][:, kc, :], start=(kc == 0), stop=(kc == KC - 1))
                    k.act(sil[c].v(), pa.v(), AF.Silu)
                    k.tt("dve", htg[s][jj][tb].v(), sil[c].v(), pb.v(), ALU.mult)

        cnt2 = [0]

        def phase2(u):
            e, j0, n = units[u]
            s = u % 2
            for t in range(NT):
                tb, tt_ = t // 4, t % 4
                for hf in range(2):
                    py = P.ps[4 + cnt2[0] % 3]
                    cnt2[0] += 1
                    for jj in range(n):
                        k.mm(py.v(), htg[s][jj][tb][:, tt_ * 128:(tt_ + 1) * 128], w2g[s][:, jj, hf * 512:(hf + 1) * 512],
                             start=(jj == 0), stop=(jj == n - 1))
                    ya = yacc[t][hf]
                    if moe:
                        if u == 0:
                            k.ts("dve", ya.v(), py.v(), comb[:, t, e:e + 1], ALU.mult)
                        else:
                            k.stt("dve", ya.v(), py.v(), comb[:, t, e:e + 1], ya.v(), ALU.mult, ALU.add)
                    else:
                        if u == 0:
                            k.copy("dve", ya.v(), py.v())
                        else:
                            k.tt("dve", ya.v(), py.v(), ya.v(), ALU.add)

        loadA(0)
        loadB(0)
        if U > 1:
            loadA(1)
            loadB(1)
        phase1(0)
        for u in range(U):
            if u + 1 < U:
                phase1(u + 1)
            if u + 2 < U:
                loadA(u + 2)
            phase2(u)
            if u + 2 < U:
                loadB(u + 2)
        k.barrier()
        stc.close()
        gb, bb = load_ln_params(P, st, View(g_d.ap[li:li + 1, :], g_d), View(b_d.ap[li:li + 1, :], b_d), "ffn_ln")
        tmp = alloc_ln_tmp(P, st, "ffn_")
        xin = [Buf(k.sb(st, f"ffn_xin{i}", [128, D], F32)[:, :]) for i in range(2)]
        k.dma("sp", xin[0].v(), P.hres[0:128, :])
        for t in range(NT):
            if t + 1 < NT:
                k.dma("sp", xin[(t + 1) % 2].v(), P.hres[(t + 1) * 128:(t + 2) * 128, :])
            xi = xin[t % 2]
            for hf in range(2):
                k.stt("dve", xi[:, hf * 512:(hf + 1) * 512], xi[:, hf * 512:(hf + 1) * 512], ALPHA, yacc[t][hf].v(), ALU.mult, ALU.add)
            emit_ln_tail(P, st, t, xi, gb, bb, dst, tmp)
        flush_ln_tail(P, tmp)
        k.barrier()


NG = 7
NST = 16
WROW = 2048


def emit_ffn_sparse(P, li, dst):
    k = P.k
    nc = k.nc
    j = li // 2
    I32 = mybir.dt.int32
    nrows = 2 * NE * NG * 128 * 2
    w1r = P.inp("moe_w1r", [nrows, WROW])
    w3r = P.inp("moe_w3r", [nrows, WROW])
    w2r = P.inp("moe_w2r", [nrows, WROW])
    wr = P.inp("moe_wrT", [2, NE, D])
    tri_d = P.inp("tri_c", [128, 128])
    io_d = P.inp("iota2_c", [128, 1])
    g_d = P.inp("ln_ffn_g", [DEPTH, D])
    b_d = P.inp("ln_ffn_b", [DEPTH, D])
    if not hasattr(P, "xs_d"):
        P.xs_d = k.dram_tmp("xs_sorted", [NST * 512, D], BF16)
        P.ys_d = k.dram_tmp("ys_sorted", [NST * 512, D], F32)
    xs_d, ys_d = P.xs_d, P.ys_d
    with ExitStack() as st:
        gs = Buf(k.sb(st, "gs", [128, NT, 2], F32)[:, :, :], "gs")
        slot_i = Buf(k.sb(st, "slot_i", [128, NT, 2], I32)[:, :, :], "slot_i")
        idxw = Buf(k.sb(st, "idxw", [128, NST, NG, 2], I32)[:, :, :, :], "idxw")
        with ExitStack() as st2:
            wrb = Buf(k.sb(st2, "wrb", [128, NE, D], F32)[:, :, :], "wrb")
            k.dma("sp", wrb.v(), View(wr.ap[j].partition_broadcast(128), wr))
            tri = Buf(k.sb(st2, "tri", [128, 128], BF16)[:, :], "tri")
            k.dma("pool", tri.v(), tri_d.v())
            io2 = Buf(k.sb(st2, "io2", [128, 1], F32)[:, :], "io2")
            k.dma("sp", io2.v(), io_d.v())
            zt = Buf(k.sb(st2, "zt", [128, 4096], BF16)[:, :], "zt")
            k.memset("pool", zt.v(), 0.0)
            xs_flat = View(xs_d.ap.rearrange("(p r) d -> p (r d)", p=128), xs_d)
            for i in range(16):
                k.dma("sp", xs_flat[:, i * 4096:(i + 1) * 4096], zt.v())
            m1s = Buf(k.sb(st2, "m1s", [128, NT, NE], F32)[:, :, :], "m1s")
            m2s = Buf(k.sb(st2, "m2s", [128, NT, NE], F32)[:, :, :], "m2s")
            abf = Buf(k.sb(st2, "abf", [128, NT, NE], BF16)[:, :, :], "abf")
            hin = [Buf(k.sb(st2, f"rt_h{i}", [128, D], F32)[:, :]) for i in range(2)]
            xbt_t = k.sb(st2, "rt_xb", [128, NT, D], BF16)
            xbt = [Buf(xbt_t[:, t, :]) for t in range(NT)]
            junk = Buf(k.sb(st2, "rt_junk", [128, D], F32)[:, :])
            lg = Buf(k.sb(st2, "rt_lg", [128, NE], F32)[:, :])
            l2 = Buf(k.sb(st2, "rt_l2", [128, NE], F32)[:, :])
            sc = Buf(k.sb(st2, "rt_sc", [128, 8], F32)[:, :])
            for t in range(NT):
                hi = hin[t % 2]
                k.dma("sp", hi.v(), P.hres[t * 128:(t + 1) * 128, :])
                k.copy("act", xbt[t].v(), hi.v())
                for e in range(NE):
                    k.stt("dve", junk.v(), hi.v(), 1.0, wrb[:, e, :], ALU.mult, ALU.mult, accum=lg[:, e:e + 1])
                k.op("dve", lambda: nc.vector.reduce_max(out=sc.ap[:, 0:1], in_=lg.ap, axis=AX.X), [lg.v()], [sc.v()])
                k.ts("dve", m1s[:, t, :], lg.v(), sc[:, 0:1], ALU.is_equal)
                k.stt("dve", l2.v(), m1s[:, t, :], -1e30, lg.v(), ALU.mult, ALU.add)
                k.op("dve", lambda: nc.vector.reduce_max(out=sc.ap[:, 1:2], in_=l2.ap, axis=AX.X), [l2.v()], [sc.v()])
                k.ts("dve", m2s[:, t, :], l2.v(), sc[:, 1:2], ALU.is_equal)
                k.tt("dve", sc[:, 2:3], sc[:, 1:2], sc[:, 0:1], ALU.subtract)
                k.act(sc[:, 3:4], sc[:, 2:3], AF.Exp)
                k.ts("dve", sc[:, 4:5], sc[:, 3:4], 1.0, ALU.add)
                k.recip(gs[:, t, 0:1], sc[:, 4:5])
                k.tt("dve", gs[:, t, 1:2], sc[:, 3:4], gs[:, t, 0:1], ALU.mult)
                k.tt("dve", abf[:, t, :], m1s[:, t, :], m2s[:, t, :], ALU.add)
            pp = P.ps[0]
            for t in range(NT):
                for tp in range(t):
                    k.mm(pp[:, t * 8:(t + 1) * 8], P.ones.v(), abf[:, tp, :], start=(tp == 0), stop=False)
                k.mm(pp[:, t * 8:(t + 1) * 8], tri.v(), abf[:, t, :], start=(t == 0), stop=True)
            for t in range(NT):
                k.mm(pp[:, 128:136], P.ones.v(), abf[:, t, :], start=(t == 0), stop=(t == NT - 1))
            posf = Buf(k.sb(st2, "posf", [128, 136], F32)[:, :], "posf")
            k.copy("dve", posf.v(), pp[:, 0:136])
            w8 = Buf(k.sb(st2, "w8", [128, 8, 8], F32)[:, :, :], "w8")
            for m in range(4):
                k.ts("dve", w8[:, m, :], posf[:, 128:136], 512.0 * m, ALU.is_gt)
            k.tt("dve", w8[:, 0, :], w8[:, 0, :], w8[:, 1, :], ALU.add)
            k.tt("dve", w8[:, 2, :], w8[:, 2, :], w8[:, 3, :], ALU.add)
            k.tt("dve", w8[:, 0, :], w8[:, 0, :], w8[:, 2, :], ALU.add)
            k.ts("dve", w8[:, 4, :], w8[:, 0, :], 512.0, ALU.mult)
            k.memset("pool", w8[:, 5, :], 0.0)
            for e in range(1, NE):
                k.tt("dve", w8[:, 5, e:e + 1], w8[:, 5, e - 1:e], w8[:, 4, e - 1:e], ALU.add)
            k.tt("dve", w8[:, 6, :], w8[:, 5, :], w8[:, 4, :], ALU.add)
            sf = Buf(k.sb(st2, "sf", [128, NT, NE], F32)[:, :, :], "sf")
            slotf = Buf(k.sb(st2, "slotf", [128, NT, 2], F32)[:, :, :], "slotf")
            j8 = Buf(k.sb(st2, "j8", [128, NE], F32)[:, :], "j8")
            for t in range(NT):
                k.tt("dve", sf[:, t, :], posf[:, t * 8:(t + 1) * 8], w8[:, 5, :], ALU.add)
                k.stt("dve", j8.v(), m1s[:, t, :], 1.0, sf[:, t, :], ALU.mult, ALU.mult, accum=slotf[:, t, 0:1])
                k.stt("dve", j8.v(), m2s[:, t, :], 1.0, sf[:, t, :], ALU.mult, ALU.mult, accum=slotf[:, t, 1:2])
            k.copy("dve", slot_i.v(), slotf.v())
            esf = Buf(k.sb(st2, "esf", [128, NST], F32)[:, :], "esf")
            for s_ in range(NST):
                k.ts("dve", j8.v(), w8[:, 6, :], 512.0 * s_, ALU.is_le, s2=0.0, op1=ALU.add, accum=esf[:, s_:s_ + 1])
            k.ts("dve", esf.v(), esf.v(), float(NE - 1), ALU.min)
            k.ts("dve", esf.v(), esf.v(), float(NG * 128 * 2), ALU.mult, s2=io2[:, 0:1], op1=ALU.add)
            idxf = Buf(k.sb(st2, "idxf", [128, NST, NG, 2], F32)[:, :, :, :], "idxf")
            for g in range(NG):
                for hf in range(2):
                    k.ts("dve", idxf[:, :, g, hf], esf.v(), float(((j * NE) * NG + g) * 128 * 2 + hf), ALU.add)
            k.copy("dve", idxw.v(), idxf.v())
            k.barrier()
            for t in range(NT):
                for sl in range(2):
                    k.dma_indirect(out=xs_d.v(), out_off=slot_i[:, t, sl:sl + 1], in_=xbt[t].v(), in_off=None)
            k.barrier()
        with ExitStack() as st3:
            G = 4
            xsT = [Buf(k.sb(st3, f"xsT{i}", [128, KC, 512], BF16)[:, :, :]) for i in range(2)]
            xrow = [Buf(k.sb(st3, f"xrow{i}", [128, D], BF16)[:, :]) for i in range(2)]
            yacc_t = [k.sb(st3, f"yacc{i}", [128, 4, D], F32) for i in range(2)]
            yacc = [[[Buf(yacc_t[i][:, r, hf * 512:(hf + 1) * 512]) for hf in range(2)] for r in range(4)] for i in range(2)]
            htg = [[Buf(k.sb(st3, f"htg{i}_{jj}", [128, 512], BF16)[:, :]) for jj in range(G)] for i in range(2)]
            w1g = [Buf(k.sb(st3, f"w1g{i}", [128, KC, 512], BF16)[:, :, :]) for i in range(2)]
            w3g = [Buf(k.sb(st3, f"w3g{i}", [128, KC, 512], BF16)[:, :, :]) for i in range(2)]
            w2g = [Buf(k.sb(st3, f"w2g{i}", [128, G, D], BF16)[:, :, :]) for i in range(2)]
            sil = [Buf(k.sb(st3, f"sil{i}", [128, 512], F32)[:, :]) for i in range(2)]
            units = [(s_, g) for s_ in range(NST) for g in range(NG)]
            U = len(units)
            ptr3 = View(P.ps_tr.ap.rearrange("p (k c) -> p k c", c=128), P.ps_tr)
            xc = [0]

            def load_x(s_):
                for r in range(4):
                    xr = xrow[xc[0] % 2]
                    xc[0] += 1
                    k.dma("sp", xr.v(), xs_d[s_ * 512 + r * 128:s_ * 512 + (r + 1) * 128, :])
                    for kc in range(KC):
                        k.tr(P.ps_tr[:, kc * 128:(kc + 1) * 128], xr[:, kc * 128:(kc + 1) * 128], P.ident.v())
                    k.copy("dve", xsT[s_ % 2][:, :, r * 128:(r + 1) * 128], ptr3)

            def loadA(u):
                s_, g = units[u]
                sl = u % 2
                for hf in range(2):
                    k.dma_indirect(out=View(w1g[sl].ap[:, hf * 4:(hf + 1) * 4, :].rearrange("p k f -> p (k f)"), w1g[sl]),
                                   out_off=None, in_=w1r.v(), in_off=idxw[:, s_, g, hf:hf + 1])
                    k.dma_indirect(out=View(w3g[sl].ap[:, hf * 4:(hf + 1) * 4, :].rearrange("p k f -> p (k f)"), w3g[sl]),
                                   out_off=None, in_=w3r.v(), in_off=idxw[:, s_, g, hf:hf + 1])

            def loadB(u):
                s_, g = units[u]
                sl = u % 2
                for hf in range(2):
                    k.dma_indirect(out=View(w2g[sl].ap[:, hf * 2:(hf + 1) * 2, :].rearrange("p k f -> p (k f)"), w2g[sl]),
                                   out_off=None, in_=w2r.v(), in_off=idxw[:, s_, g, hf:hf + 1])

            cnt1 = [0]

            def phase1(u):
                s_, g = units[u]
                sl = u % 2
                for jj in range(G):
                    c = cnt1[0] % 2
                    cnt1[0] += 1
                    pa, pb = P.ps[2 * c], P.ps[2 * c + 1]
                    for (W, pq) in ((w1g[sl], pa), (w3g[sl], pb)):
                        for kc in range(KC):
                            k.mm(pq.v(), W[:, kc, jj * 128:(jj + 1) * 128], xsT[s_ % 2][:, kc, :], start=(kc == 0), stop=(kc == KC - 1))
                    k.act(sil[c].v(), pa.v(), AF.Silu)
                    k.tt("dve", htg[sl][jj].v(), sil[c].v(), pb.v(), ALU.mult)

            cnt2 = [0]

            def phase2(u):
                s_, g = units[u]
                sl = u % 2
                for r in range(4):
                    for hf in range(2):
                        py = P.ps[4 + cnt2[0] % 3]
                        cnt2[0] += 1
                        for jj in range(G):
                            k.mm(py.v(), htg[sl][jj][:, r * 128:(r + 1) * 128], w2g[sl][:, jj, hf * 512:(hf + 1) * 512],
                                 start=(jj == 0), stop=(jj == G - 1))
                        ya = yacc[s_ % 2][r][hf]
                        if g == 0:
                            k.copy("dve", ya.v(), py.v())
                        else:
                            k.tt("dve", ya.v(), py.v(), ya.v(), ALU.add)
                if g == NG - 1:
                    for r in range(4):
                        k._deps("sp", [yacc[s_ % 2][r][1].v()], [])
                        k.dma("sp", ys_d[s_ * 512 + r * 128:s_ * 512 + (r + 1) * 128, :],
                              View(yacc_t[s_ % 2][:, r, :], yacc[s_ % 2][r][0]))

            load_x(0)
            loadA(0)
            loadB(0)
            loadA(1)
            loadB(1)
            phase1(0)
            for u in range(U):
                s_, g = units[u]
                if g == 2 and s_ + 1 < NST:
                    load_x(s_ + 1)
                if u + 1 < U:
                    phase1(u + 1)
                if u + 2 < U:
                    loadA(u + 2)
                phase2(u)
                if u + 2 < U:
                    loadB(u + 2)
            k.barrier()
        with ExitStack() as st4:
            gb, bb = load_ln_params(P, st4, View(g_d.ap[li:li + 1, :], g_d), View(b_d.ap[li:li + 1, :], b_d), "ffn_ln")
            tmp = alloc_ln_tmp(P, st4, "ffn_")
            xin = [Buf(k.sb(st4, f"ffn_xin{i}", [128, D], F32)[:, :]) for i in range(2)]
            y1 = [Buf(k.sb(st4, f"ffn_y1{i}", [128, D], F32)[:, :]) for i in range(2)]
            y2 = [Buf(k.sb(st4, f"ffn_y2{i}", [128, D], F32)[:, :]) for i in range(2)]

            def fetch(t):
                k.dma("sp", xin[t % 2].v(), P.hres[t * 128:(t + 1) * 128, :])
                k.dma_indirect(out=y1[t % 2].v(), out_off=None, in_=ys_d.v(), in_off=slot_i[:, t, 0:1])
                k.dma_indirect(out=y2[t % 2].v(), out_off=None, in_=ys_d.v(), in_off=slot_i[:, t, 1:2])

            fetch(0)
            for t in range(NT):
                if t + 1 < NT:
                    fetch(t + 1)
                xi, a, b = xin[t % 2], y1[t % 2], y2[t % 2]
                k.ts("pool", a.v(), a.v(), gs[:, t, 0:1], ALU.mult)
                k.stt("dve", a.v(), b.v(), gs[:, t, 1:2], a.v(), ALU.mult, ALU.add)
                k.stt("dve", xi.v(), xi.v(), ALPHA, a.v(), ALU.mult, ALU.add)
                emit_ln_tail(P, st4, t, xi, gb, bb, dst, tmp)
            flush_ln_tail(P, tmp)
            k.barrier()


def load_w_bf16(P, st, name, src_view, kchunks, ncols, col0=0):
    k = P.k
    b = Buf(k.sb(st, name, [128, kchunks, ncols], BF16)[:, :, :], name)
    k.dma("pool", b.v(), View(src_view.ap.rearrange("(kc p) f -> p kc f", p=128)[:, :, col0:col0 + ncols], src_view.buf))
    return b


def emit_mix_out(P, li, oT, wo_view, dst):
    k = P.k
    g_d = P.inp("ln_mix_g", [DEPTH, D])
    b_d = P.inp("ln_mix_b", [DEPTH, D])
    with ExitStack() as st:
        wo = load_w_bf16(P, st, "wo_sb", wo_view, KC, D)
        gb, bb = load_ln_params(P, st, View(g_d.ap[li:li + 1, :], g_d), View(b_d.ap[li:li + 1, :], b_d), "mix_ln")
        tmp = alloc_ln_tmp(P, st, "mix_")
        xin = [Buf(k.sb(st, f"mix_xin{i}", [128, D], F32)[:, :]) for i in range(2)]
        k.dma("sp", xin[0].v(), P.hres[0:128, :])
        c = 0
        for t in range(NT):
            if t + 1 < NT:
                k.dma("sp", xin[(t + 1) % 2].v(), P.hres[(t + 1) * 128:(t + 2) * 128, :])
            xi = xin[t % 2]
            for hf in range(2):
                py = P.ps[c % 4]
                c += 1
                for kc in range(KC):
                    k.mm(py.v(), oT[kc][:, t * 128:(t + 1) * 128], wo[:, kc, hf * 512:(hf + 1) * 512], start=(kc == 0), stop=(kc == KC - 1))
                k.stt("dve", xi[:, hf * 512:(hf + 1) * 512], xi[:, hf * 512:(hf + 1) * 512], ALPHA, py.v(), ALU.mult, ALU.add)
            emit_ln_tail(P, st, t, xi, gb, bb, dst, tmp)
        flush_ln_tail(P, tmp)
        k.barrier()


def emit_mix_gmlp(P, li, dst):
    k = P.k
    nc = k.nc
    j = li // 4
    w_in = P.inp("sg_w_in", [1, D, 2 * D])
    vg_d = P.inp("sg_v_norm_g", [1, D])
    vb_d = P.inp("sg_v_norm_b", [1, D])
    ws_d = P.inp("sg_w_s", [1, 8, 128, 128])
    bs_d = P.inp("sg_b_s", [1, 8, 128])
    wout = P.inp("sg_w_out", [1, D, D])
    with ExitStack() as st:
        uT_t = k.sb(st, "uT", [128, KC, S], BF16)
        uT = [[Buf(uT_t[:, c, tb * 512:(tb + 1) * 512]) for tb in range(4)] for c in range(KC)]
        vln_t = k.sb(st, "vln", [128, NT, D], BF16)
        vln = [Buf(vln_t[:, t, :]) for t in range(NT)]
        wsT = Buf(k.sb(st, "wsT", [128, 8, 128], BF16)[:, :, :], "wsT")
        bs4 = Buf(k.sb(st, "bs4", [1, 8, 512], BF16)[:, :, :], "bs4")
        with ExitStack() as st2:
            win = load_w_bf16(P, st2, "w_in_sb", View(w_in.ap[j], w_in), KC, 2 * D)
            ws_sb = Buf(k.sb(st2, "ws_sb", [128, 8, 128], BF16)[:, :, :])
            k.dma("pool", ws_sb.v(), View(ws_d.ap[j].rearrange("g i j -> i g j"), ws_d))
            for g in range(8):
                k.tr(P.ps_tr[:, g * 128:(g + 1) * 128], ws_sb[:, g, :], P.ident.v())
            k.copy("dve", wsT.v(), View(P.ps_tr.ap.rearrange("p (k c) -> p k c", c=128), P.ps_tr))
            k.memset("pool", wsT[64:128, :, 0:64], 0.0)
            bs_f = Buf(k.sb(st2, "bs_f", [1, 8, 128], F32)[:, :, :])
            k.dma("sp", bs_f.v(), View(bs_d.ap[j:j + 1], bs_d))
            for r in range(4):
                k.copy("dve", bs4[:, :, r * 128:(r + 1) * 128], bs_f.v())
            vg, vb = load_ln_params(P, st2, View(vg_d.ap[j:j + 1, :], vg_d), View(vb_d.ap[j:j + 1, :], vb_d), "sg_ln")
            c2 = 0
            for c in range(KC):
                for tb in range(4):
                    pp = P.ps[c2 % 4]
                    c2 += 1
                    for kc in range(KC):
                        k.mm(pp.v(), win[:, kc, c * 128:(c + 1) * 128], P.hT[tb][:, kc, :], start=(kc == 0), stop=(kc == KC - 1))
                    k.act(uT[c][tb].v(), pp.v(), AF.Gelu_apprx_tanh)
            vt = [Buf(k.sb(st2, f"sg_v{i}", [128, D], F32)[:, :]) for i in range(2)]
            stats = Buf(k.sb(st2, "sg_stats", [128, 12], F32)[:, :])
            mv = Buf(k.sb(st2, "sg_mv", [128, 2], F32)[:, :])
            sc = Buf(k.sb(st2, "sg_sc", [128, 4], F32)[:, :])
            eps = Buf(k.sb(st2, "sg_eps", [128, 1], F32)[:, :])
            k.memset("pool", eps.v(), LN_EPS)
            for t in range(NT):
                tb, tt_ = t // 4, t % 4
                v = vt[t % 2]
                for hf in range(2):
                    pp = P.ps[4 + c2 % 3]
                    c2 += 1
                    for kc in range(KC):
                        k.mm(pp.v(), P.hT[tb][:, kc, tt_ * 128:(tt_ + 1) * 128], win[:, kc, D + hf * 512:D + (hf + 1) * 512],
                             start=(kc == 0), stop=(kc == KC - 1))
                    k.act(v[:, hf * 512:(hf + 1) * 512], pp.v(), AF.Gelu_apprx_tanh)
                    k.op("dve", lambda hf=hf, v=v: nc.vector.bn_stats(out=stats.ap[:, hf * 6:(hf + 1) * 6], in_=v.ap[:, hf * 512:(hf + 1) * 512]),
                         [v.v()], [stats.v()])
                k.op("dve", lambda: nc.vector.bn_aggr(out=mv.ap, in_=stats.ap), [stats.v()], [mv.v()])
                k.act(sc[:, 0:1], mv[:, 1:2], AF.Sqrt, bias=eps[:, 0:1])
                k.recip(sc[:, 1:2], sc[:, 0:1])
                k.stt("dve", sc[:, 2:3], mv[:, 0:1], -1.0, sc[:, 1:2], ALU.mult, ALU.mult)
                k.act(v.v(), v.v(), AF.Identity, scale=sc[:, 1:2], bias=sc[:, 2:3])
                k.tt("pool", v.v(), v.v(), vg.v(), ALU.mult)
                k.tt("pool", vln[t].v(), v.v(), vb.v(), ALU.add)
            k.barrier()
        c3 = 0
        for g in range(8):
            for tb in range(4):
                pp = P.ps[c3 % 4]
                c3 += 1
                k.mm(pp.v(), P.ones[0:1, :], bs4[0:1, g, :], start=True, stop=False)
                for r in range(4):
                    t = tb * 4 + r
                    k.mm(pp[:, r * 128:(r + 1) * 128], vln[t][:, g * 128:(g + 1) * 128], wsT[:, g, :], start=False, stop=(r == 3))
                k.tt("dve", uT[g][tb].v(), uT[g][tb].v(), pp.v(), ALU.mult)
        sT = [Buf(uT_t[:, c, :]) for c in range(KC)]
        k.barrier()
        emit_mix_out(P, li, sT, View(wout.ap[j], wout), dst)


class Rot:
    def __init__(self, n):
        self.n = n
        self.i = 0

    def nxt(self):
        v = self.i % self.n
        self.i += 1
        return v


class AttnPipe:
    def __init__(self, depth=2):
        self.depth = depth
        self.q = []
        self.deferred = []

    def push(self, cfn, after=None):
        self.q.append((cfn, after))
        ready = [fn for (n, fn) in self.deferred if n <= 1]
        self.deferred = [(n - 1, fn) for (n, fn) in self.deferred if n > 1]
        for fn in ready:
            fn()
        while len(self.q) > self.depth:
            self._pop()

    def _pop(self):
        cfn, after = self.q.pop(0)
        cfn()
        if after is not None:
            after()

    def defer(self, n, fn):
        self.deferred.append((n, fn))

    def flush(self):
        while self.q:
            self._pop()
        while self.deferred:
            d = self.deferred
            self.deferred = []
            for (_, fn) in d:
                fn()


def run_blocks(P, pipe, blocks, q_of, k_of, v_of, ones_v, psO, psD, pts, rs, rp, scale, bias_of=None, after=None):
    k = P.k
    n = len(blocks)
    for bi, (kb, c0, N, zero, extra) in enumerate(blocks):
        pS = P.ps[rs.nxt()]
        k.mm(pS[:, 0:N], k_of(kb), q_of(c0, N), start=True, stop=(bias_of is None))
        if bias_of is not None:
            k.mm(pS[:, 0:N], P.ident.v(), bias_of(extra, N), start=False, stop=True)
        pt = pts[rp.nxt()]
        k.act(pt[:, 0:N], pS[:, 0:N], AF.Exp, scale=scale)
        if zero is not None:
            (r0, r1, z0, z1) = zero
            k.memset("pool", pt[r0:r1, z0:z1], 0.0)

        def cfn(o=psO[:, c0:c0 + N], dn=psD[:, c0:c0 + N], vv=v_of(kb), pv=pt[:, 0:N], st_=(bi == 0), sp_=(bi == n - 1)):
            k.mm(o, vv, pv, start=st_, stop=sp_)
            k.mm(dn, ones_v, pv, start=st_, stop=sp_)

        pipe.push(cfn, after if bi == n - 1 else None)


def causal_blocks(qb):
    bl = []
    for kb in range(4 * qb + 4):
        r = kb - 4 * qb
        if r <= 0:
            bl.append((kb, 0, 512, (64, 128, 0, 64) if r == 0 else None, None))
        else:
            bl.append((kb, 128 * r, 512 - 128 * r, (64, 128, 0, 64), None))
    return bl


def emit_mix_diff(P, li, dst):
    k = P.k
    nc = k.nc
    j = li // 4
    lam_init = 0.8 - 0.6 * math.exp(-0.3 * li)
    wq = P.inp("diff_wq", [1, D, D])
    wk = P.inp("diff_wk", [1, D, D])
    wv = P.inp("diff_wv", [1, D, D])
    wo = P.inp("diff_wo", [1, D, D])
    lqk = [P.inp(n, [1, 64]) for n in ("diff_lq1", "diff_lk1", "diff_lq2", "diff_lk2")]
    subg = P.inp("diff_sub_g", [1, 128])
    scale = 64 ** -0.5
    with ExitStack() as st:
        oT_t = k.sb(st, "oT", [128, KC, S], BF16)
        oTb = [[Buf(oT_t[:, c, qb * 512:(qb + 1) * 512]) for qb in range(4)] for c in range(KC)]
        V_t = k.sb(st, "Vall", [128, NT, D], BF16)
        V = [Buf(V_t[:, t, :]) for t in range(NT)]
        nlam = Buf(k.sb(st, "nlam", [128, 1], F32)[:, :])
        gsc = Buf(k.sb(st, "gsc", [128, 1], F32)[:, :])
        eps5 = Buf(k.sb(st, "eps5", [128, 1], F32)[:, :])
        k.memset("pool", eps5.v(), 1e-5)
        with ExitStack() as st2:
            lt = Buf(k.sb(st2, "lqk", [128, 4, 64], F32)[:, :, :])
            for i in range(4):
                k.dma("sp", lt[:, i, :], View(lqk[i].ap[j:j + 1, :].partition_broadcast(128), lqk[i]))
            junk = Buf(k.sb(st2, "ljunk", [128, 64], F32)[:, :])
            acc = Buf(k.sb(st2, "lacc", [128, 4], F32)[:, :])
            k.stt("dve", junk.v(), lt[:, 0, :], 1.0, lt[:, 1, :], ALU.mult, ALU.mult, accum=acc[:, 0:1])
            k.stt("dve", junk.v(), lt[:, 2, :], 1.0, lt[:, 3, :], ALU.mult, ALU.mult, accum=acc[:, 1:2])
            k.act(acc[:, 2:3], acc[:, 0:1], AF.Exp)
            k.act(acc[:, 3:4], acc[:, 1:2], AF.Exp)
            k.tt("dve", nlam.v(), acc[:, 3:4], acc[:, 2:3], ALU.subtract)
            k.ts("dve", nlam.v(), nlam.v(), -lam_init, ALU.add)
            k.dma("sp", gsc.v(), View(subg.ap[j:j + 1, :].rearrange("o d -> d o"), subg))
            k.ts("dve", gsc.v(), gsc.v(), 1.0 - lam_init, ALU.mult)
            wv_sb = load_w_bf16(P, st2, "wv_sb", View(wv.ap[j], wv), KC, D)
            c = 0
            for t in range(NT):
                tb, tt_ = t // 4, t % 4
                for hf in range(2):
                    pp = P.ps[c % 4]
                    c += 1
                    for kc in range(KC):
                        k.mm(pp.v(), P.hT[tb][:, kc, tt_ * 128:(tt_ + 1) * 128], wv_sb[:, kc, hf * 512:(hf + 1) * 512],
                             start=(kc == 0), stop=(kc == KC - 1))
                    k.copy("act", V[t][:, hf * 512:(hf + 1) * 512], pp.v())
            k.barrier()
        with ExitStack() as st3:
            wqh = [Buf(k.sb(st3, f"wqh{i}", [128, KC, 128], BF16)[:, :, :]) for i in range(2)]
            wkh = [Buf(k.sb(st3, f"wkh{i}", [128, KC, 128], BF16)[:, :, :]) for i in range(2)]
            QT_t = [k.sb(st3, f"QT{i}", [128, S], BF16) for i in range(2)]
            KT_t = [k.sb(st3, f"KT{i}", [128, S], BF16) for i in range(2)]
            QT = [[Buf(QT_t[i][:, tb * 512:(tb + 1) * 512]) for tb in range(4)] for i in range(2)]
            KT = [[Buf(KT_t[i][:, tb * 512:(tb + 1) * 512]) for tb in range(4)] for i in range(2)]
            pts = [Buf(k.sb(st3, f"pt{i}", [128, 512], BF16)[:, :]) for i in range(4)]
            rd = Buf(k.sb(st3, "f_rd", [128, 512], F32)[:, :])
            o0 = Buf(k.sb(st3, "f_o0", [128, 512], F32)[:, :])
            o1 = Buf(k.sb(st3, "f_o1", [128, 512], F32)[:, :])
            sq = Buf(k.sb(st3, "f_sq", [128, 512], BF16)[:, :])
            rs, rp = Rot(3), Rot(4)

            def load_head(h):
                s_ = h % 2
                k.dma("pool", wqh[s_].v(), View(wq.ap[j].rearrange("(kc p) f -> p kc f", p=128)[:, :, h * 128:(h + 1) * 128], wq))
                k.dma("pool", wkh[s_].v(), View(wk.ap[j].rearrange("(kc p) f -> p kc f", p=128)[:, :, h * 128:(h + 1) * 128], wk))

            def proj_head(h):
                s_ = h % 2
                for (W, T) in ((wqh[s_], QT[s_]), (wkh[s_], KT[s_])):
                    for tb in range(4):
                        pp = P.ps[rs.nxt()]
                        for kc in range(KC):
                            k.mm(pp.v(), W[:, kc, :], P.hT[tb][:, kc, :], start=(kc == 0), stop=(kc == KC - 1))
                        k.copy("dve", T[tb].v(), pp.v())

            pipe = AttnPipe(2)
            unit = [0]
            o0b = [Buf(k.sb(st3, f"f_o0b{i}", [128, 512], F32)[:, :]) for i in range(2)]
            sqb = [Buf(k.sb(st3, f"f_sqb{i}", [128, 512], BF16)[:, :]) for i in range(2)]
            rdb = [Buf(k.sb(st3, f"f_rdb{i}", [128, 512], F32)[:, :]) for i in range(2)]

            def attn_head(h):
                s_ = h % 2
                for qb in range(4):
                    bl = causal_blocks(qb)
                    par = (h * 4 + qb) % 2
                    for m in range(2):
                        r0, r1 = m * 64, (m + 1) * 64
                        u = unit[0] % 2
                        unit[0] += 1
                        psO, psD = P.ps[3 + 2 * u], P.ps[4 + 2 * u]

                        def fin(m=m, psO=psO, psD=psD, par=par, h=h, qb=qb):
                            if m == 0:
                                k.recip(rd.v(), psD.v())
                                k.tt("dve", o0b[par].v(), psO.v(), rd.v(), ALU.mult)
                            else:
                                k.recip(rd.v(), psD.v())
                                k.tt("dve", o1.v(), psO.v(), rd.v(), ALU.mult)
                                k.stt("dve", o0b[par].v(), o1.v(), nlam[:, 0:1], o0b[par].v(), ALU.mult, ALU.add)
                                k.act(sqb[par].v(), o0b[par].v(), AF.Square)

                                def fin2():
                                    pS = P.ps[rs.nxt()]
                                    k.mm(pS.v(), P.ones.v(), sqb[par].v(), start=True, stop=True)
                                    k.act(rdb[par].v(), pS.v(), AF.Sqrt, scale=1.0 / 128.0, bias=eps5[:, 0:1])
                                    k.recip(rdb[par].v(), rdb[par].v())
                                    k.stt("dve", oTb[h][qb].v(), o0b[par].v(), gsc[:, 0:1], rdb[par].v(), ALU.mult, ALU.mult)

                                pipe.defer(4, fin2)

                        run_blocks(P, pipe, bl,
                                   q_of=lambda c0, N: QT[s_][qb][r0:r1, c0:c0 + N],
                                   k_of=lambda kb: KT[s_][kb // 4][r0:r1, (kb % 4) * 128:(kb % 4 + 1) * 128],
                                   v_of=lambda kb: V[kb][:, h * 128:(h + 1) * 128],
                                   ones_v=P.ones.v(), psO=psO, psD=psD, pts=pts, rs=rs, rp=rp, scale=scale, after=fin)

            load_head(0)
            load_head(1)
            proj_head(0)
            for h in range(8):
                if h + 1 < 8:
                    proj_head(h + 1)
                if h + 2 < 8:
                    load_head(h + 2)
                attn_head(h)
            pipe.flush()
            k.barrier()
        oT = [Buf(oT_t[:, c, :]) for c in range(KC)]
        emit_mix_out(P, li, oT, View(wo.ap[j], wo), dst)


def band_blocks(qb):
    bl = []
    for r in (4, 5, 6, 7, 3, 2, 1, 0):
        if r >= 4:
            rp_ = r - 4
            bl.append((4 * qb + rp_, 128 * rp_, 512 - 128 * rp_, (64, 128, 0, 64), 0))
        elif qb > 0:
            N = 128 * (r + 1)
            bl.append((4 * qb - 4 + r, 0, N, (0, 64, N - 64, N), 512 - 128 * r))
    return bl


def emit_mix_band(P, li, dst):
    k = P.k
    j = li // 4
    wqkv = P.inp("ca_w_qkv", [1, D, 3 * D])
    bt_d = P.inp("ca_bt", [16, 128, 640])
    wo = P.inp("ca_wo", [1, D, D])
    scale = 64 ** -0.5
    with ExitStack() as st:
        oT_t = k.sb(st, "oT", [128, KC, S], BF16)
        oTb = [[Buf(oT_t[:, c, qb * 512:(qb + 1) * 512]) for qb in range(4)] for c in range(KC)]
        V_t = k.sb(st, "Vall", [128, NT, D], BF16)
        V = [Buf(V_t[:, t, :]) for t in range(NT)]
        BT = Buf(k.sb(st, "BT", [128, 16, 640], BF16)[:, :, :], "BT")
        k.dma("pool", BT.v(), View(bt_d.ap.rearrange("h p x -> p h x"), bt_d))
        k.ts("pool", BT.v(), BT.v(), 1.0 / scale, ALU.mult)
        with ExitStack() as st2:
            wv_sb = load_w_bf16(P, st2, "wv_sb", View(wqkv.ap[j], wqkv), KC, D, col0=2 * D)
            c = 0
            for t in range(NT):
                tb, tt_ = t // 4, t % 4
                for hf in range(2):
                    pp = P.ps[c % 4]
                    c += 1
                    for kc in range(KC):
                        k.mm(pp.v(), P.hT[tb][:, kc, tt_ * 128:(tt_ + 1) * 128], wv_sb[:, kc, hf * 512:(hf + 1) * 512],
                             start=(kc == 0), stop=(kc == KC - 1))
                    k.copy("act", V[t][:, hf * 512:(hf + 1) * 512], pp.v())
            k.barrier()
        with ExitStack() as st3:
            wqh = [Buf(k.sb(st3, f"wqh{i}", [128, KC, 128], BF16)[:, :, :]) for i in range(2)]
            wkh = [Buf(k.sb(st3, f"wkh{i}", [128, KC, 128], BF16)[:, :, :]) for i in range(2)]
            QT_t = [k.sb(st3, f"QT{i}", [128, S], BF16) for i in range(2)]
            KT_t = [k.sb(st3, f"KT{i}", [128, S], BF16) for i in range(2)]
            QT = [[Buf(QT_t[i][:, tb * 512:(tb + 1) * 512]) for tb in range(4)] for i in range(2)]
            KT = [[Buf(KT_t[i][:, tb * 512:(tb + 1) * 512]) for tb in range(4)] for i in range(2)]
            pts = [Buf(k.sb(st3, f"pt{i}", [128, 512], BF16)[:, :]) for i in range(4)]
            rd = [Buf(k.sb(st3, f"f_rd{i}", [128, 512], F32)[:, :]) for i in range(2)]
            rs, rp = Rot(3), Rot(4)
            wq_v = View(wqkv.ap[j].rearrange("(kc p) f -> p kc f", p=128), wqkv)

            def load_pair(p):
                s_ = p % 2
                k.dma("pool", wqh[s_].v(), wq_v[:, :, p * 128:(p + 1) * 128])
                k.dma("pool", wkh[s_].v(), wq_v[:, :, D + p * 128:D + (p + 1) * 128])

            def proj_pair(p):
                s_ = p % 2
                for (W, T) in ((wqh[s_], QT[s_]), (wkh[s_], KT[s_])):
                    for tb in range(4):
                        pp = P.ps[rs.nxt()]
                        for kc in range(KC):
                            k.mm(pp.v(), W[:, kc, :], P.hT[tb][:, kc, :], start=(kc == 0), stop=(kc == KC - 1))
                        k.copy("dve", T[tb].v(), pp.v())

            unit = [0]
            pipe = AttnPipe(2)

            def attn_pair(p):
                s_ = p % 2
                for hh in range(2):
                    h = 2 * p + hh
                    r0, r1 = hh * 64, (hh + 1) * 64
                    for qb in range(4):
                        u = unit[0] % 2
                        unit[0] += 1
                        psO, psD = P.ps[3 + 2 * u], P.ps[4 + 2 * u]
                        def fin(u=u, psO=psO, psD=psD, r0=r0, r1=r1, p=p, qb=qb):
                            k.recip(rd[u][r0:r1, :], psD[r0:r1, :])
                            k.tt("dve", oTb[p][qb][r0:r1, :], psO[r0:r1, :], rd[u][r0:r1, :], ALU.mult)

                        run_blocks(P, pipe, band_blocks(qb),
                                   q_of=lambda c0, N: QT[s_][qb][r0:r1, c0:c0 + N],
                                   k_of=lambda kb: KT[s_][kb // 4][r0:r1, (kb % 4) * 128:(kb % 4 + 1) * 128],
                                   v_of=lambda kb: V[kb][:, p * 128:(p + 1) * 128],
                                   ones_v=P.ones.v(), psO=psO, psD=psD, pts=pts, rs=rs, rp=rp, scale=scale,
                                   bias_of=lambda off, N: BT[:, h, off:off + N], after=fin)

            load_pair(0)
            load_pair(1)
            proj_pair(0)
            for p in range(8):
                if p + 1 < 8:
                    proj_pair(p + 1)
                if p + 2 < 8:
                    load_pair(p + 2)
                attn_pair(p)
            pipe.flush()
            k.barrier()
        oT = [Buf(oT_t[:, c, :]) for c in range(KC)]
        emit_mix_out(P, li, oT, View(wo.ap[j], wo), dst)


MLA_STAGE = [9]


def emit_mix_mla(P, li, dst):
    k = P.k
    nc = k.nc
    j = li // 4
    QR, KVR, NH = 384, 256, 16
    w_dq = P.inp("mla_w_dq", [1, D, QR])
    qg_d = P.inp("mla_q_norm_g", [1, QR])
    w_uq = P.inp("mla_w_uq", [1, QR, NH * 96])
    w_dkv = P.inp("mla_w_dkv", [1, D, KVR + 32])
    kvg_d = P.inp("mla_kv_norm_g", [1, KVR])
    w_ukv = P.inp("mla_w_ukv", [1, KVR, NH * 128])
    wo = P.inp("mla_wo", [1, D, D])
    cs_d = P.inp("rope_cs", [2, 32, S])
    scale = 96 ** -0.5
    with ExitStack() as st:
        oT_t = k.sb(st, "oT", [128, KC, S], BF16)
        oTb = [[Buf(oT_t[:, c, qb * 512:(qb + 1) * 512]) for qb in range(4)] for c in range(KC)]
        V_t = k.sb(st, "Vall", [128, NT, D], BF16)
        V = [Buf(V_t[:, t, :]) for t in range(NT)]
        cqT_t = k.sb(st, "cqT", [128, 3, S], BF16)
        cqT = [Buf(cqT_t[:, :, tb * 512:(tb + 1) * 512]) for tb in range(4)]
        ckvT_t = k.sb(st, "ckvT", [128, 2, S], BF16)
        ckvT = [Buf(ckvT_t[:, :, tb * 512:(tb + 1) * 512]) for tb in range(4)]
        cs = Buf(k.sb(st, "cs", [128, 2, S], F32)[:, :, :], "cs")
        k.dma("sp", cs[64:96, :, :], View(cs_d.ap.rearrange("c p s -> p c s"), cs_d))
        wuq = Buf(k.sb(st, "wuq", [128, 3, NH * 96 + 32], BF16)[:, :, :], "wuq")
        wuqR = Buf(k.sb(st, "wuqR", [128, 3, NH * 96 + 32], BF16)[:, :, :], "wuqR")
        wkn = Buf(k.sb(st, "wkn", [128, 2, NH * 64 + 64], BF16)[:, :, :], "wkn")
        k.memset("pool", wuq.v(), 0.0)
        k.memset("pool", wkn.v(), 0.0)
        KR_t = k.sb(st, "KR", [128, S], BF16)
        KR = [Buf(KR_t[:, tb * 512:(tb + 1) * 512]) for tb in range(4)]
        eps6 = Buf(k.sb(st, "eps6", [128, 1], F32)[:, :])
        k.memset("pool", eps6.v(), 1e-6)
        with ExitStack() as st2:
            wdq = load_w_bf16(P, st2, "wdq", View(w_dq.ap[j], w_dq), KC, QR)
            wdkc = load_w_bf16(P, st2, "wdkc", View(w_dkv.ap[j], w_dkv), KC, KVR)
            wvv = Buf(k.sb(st2, "wvv", [128, 2, NH * 64], BF16)[:, :, :], "wvv")
            wkr = Buf(k.sb(st2, "wkr", [128, KC, 128], BF16)[:, :, :], "wkr")
            wkrR = Buf(k.sb(st2, "wkrR", [128, KC, 128], BF16)[:, :, :], "wkrR")
            wkst = Buf(k.sb(st2, "wkst", [128, KC, 32], F32)[:, :, :], "wkst")
            dkv_v = View(w_dkv.ap[j].rearrange("(kc p) f -> p kc f", p=128), w_dkv)
            k.memset("pool", wkr.v(), 0.0)
            k.memset("pool", wkrR.v(), 0.0)
            k.dma("sp", wkst.v(), dkv_v[:, :, KVR:KVR + 32])
            k.copy("pool", wkr[:, :, 64:96], wkst.v())
            k.ts("pool", wkrR[:, :, 64:80], wkst[:, :, 16:32], -1.0, ALU.mult)
            k.copy("pool", wkrR[:, :, 80:96], wkst[:, :, 0:16])
            gq = Buf(k.sb(st2, "gq", [128, 3], F32)[:, :])
            gkv = Buf(k.sb(st2, "gkv", [128, 2], F32)[:, :])
            for kc in range(3):
                k.dma("sp", gq[:, kc:kc + 1], View(qg_d.ap[j:j + 1, kc * 128:(kc + 1) * 128].rearrange("o d -> d o"), qg_d))
            for kc in range(2):
                k.dma("sp", gkv[:, kc:kc + 1], View(kvg_d.ap[j:j + 1, kc * 128:(kc + 1) * 128].rearrange("o d -> d o"), kvg_d))
            with ExitStack() as st2a:
                stg = Buf(k.sb(st2a, "stg_uq", [128, 3, NH * 96], F32)[:, :, :])
                k.dma("sp", stg.v(), View(w_uq.ap[j].rearrange("(kc p) f -> p kc f", p=128), w_uq))
                for kc in range(3):
                    k.ts("pool", wuq[:, kc, 0:NH * 96], stg[:, kc, :], gq[:, kc:kc + 1], ALU.mult)
                k.memset("pool", wuqR.v(), 0.0)
                w4 = View(wuq.ap[:, :, 0:NH * 96].rearrange("p k (h c) -> p k h c", c=96), wuq)
                r4 = View(wuqR.ap[:, :, 0:NH * 96].rearrange("p k (h c) -> p k h c", c=96), wuqR)
                for kc in range(3):
                    k.ts("pool", r4[:, kc, :, 64:80], w4[:, kc, :, 80:96], -1.0, ALU.mult)
                    k.copy("pool", r4[:, kc, :, 80:96], w4[:, kc, :, 64:80])
                k.barrier()
            with ExitStack() as st2b:
                stg = Buf(k.sb(st2b, "stg_ukv", [128, 2, NH * 128], F32)[:, :, :])
                k.dma("sp", stg.v(), View(w_ukv.ap[j].rearrange("(kc p) f -> p kc f", p=128), w_ukv))
                s4 = View(stg.ap.rearrange("p k (h c) -> p k h c", c=128), stg)
                kn4 = View(wkn.ap[:, :, 0:NH * 64].rearrange("p k (h c) -> p k h c", c=64), wkn)
                vv4 = View(wvv.ap.rearrange("p k (h c) -> p k h c", c=64), wvv)
                for kc in range(2):
                    k.ts("pool", kn4[:, kc, :, :], s4[:, kc, :, 0:64], gkv[:, kc:kc + 1], ALU.mult)
                    k.ts("pool", vv4[:, kc, :, :], s4[:, kc, :, 64:128], gkv[:, kc:kc + 1], ALU.mult)
                k.barrier()
            junk = Buf(k.sb(st2, "mjunk", [128, QR], F32)[:, :])
            NT_A = NT if MLA_STAGE[0] >= 2 else 0
            cqn = [Buf(k.sb(st2, f"cqn{i}", [128, QR], BF16)[:, :]) for i in range(2)]
            cqf = [Buf(k.sb(st2, f"cqf{i}", [128, QR], F32)[:, :]) for i in range(2)]
            ckf = [Buf(k.sb(st2, f"ckf{i}", [128, KVR], F32)[:, :]) for i in range(2)]
            ckn = [Buf(k.sb(st2, f"ckn{i}", [128, KVR], BF16)[:, :]) for i in range(2)]
            sc = [Buf(k.sb(st2, f"msc{i}", [128, 8], F32)[:, :]) for i in range(2)]
            ptr3 = View(P.ps_tr.ap.rearrange("p (k c) -> p k c", c=128), P.ps_tr)
            for t in range(NT_A):
                tb, tt_ = t // 4, t % 4
                u = t % 2
                pq, pk = P.ps[2 * u], P.ps[2 * u + 1]
                for kc in range(KC):
                    k.mm(pq[:, 0:QR], P.hT[tb][:, kc, tt_ * 128:(tt_ + 1) * 128], wdq[:, kc, :], start=(kc == 0), stop=(kc == KC - 1))
                for kc in range(KC):
                    k.mm(pk[:, 0:KVR], P.hT[tb][:, kc, tt_ * 128:(tt_ + 1) * 128], wdkc[:, kc, :], start=(kc == 0), stop=(kc == KC - 1))
                k.copy("act", cqf[u].v(), pq[:, 0:QR])
                k.copy("act", ckf[u].v(), pk[:, 0:KVR])
                k.stt("dve", junk[:, 0:QR], cqf[u].v(), 1.0, cqf[u].v(), ALU.mult, ALU.mult, accum=sc[u][:, 0:1])
                k.stt("dve", junk[:, 0:KVR], ckf[u].v(), 1.0, ckf[u].v(), ALU.mult, ALU.mult, accum=sc[u][:, 1:2])
                k.act(sc[u][:, 2:3], sc[u][:, 0:1], AF.Sqrt, scale=1.0 / QR, bias=eps6[:, 0:1])
                k.act(sc[u][:, 3:4], sc[u][:, 1:2], AF.Sqrt, scale=1.0 / KVR, bias=eps6[:, 0:1])
                k.recip(sc[u][:, 4:6], sc[u][:, 2:4])
                k.ts("dve", cqn[u].v(), cqf[u].v(), sc[u][:, 4:5], ALU.mult)
                k.ts("dve", ckn[u].v(), ckf[u].v(), sc[u][:, 5:6], ALU.mult)
                for kc in range(3):
                    k.tr(P.ps_tr[:, kc * 128:(kc + 1) * 128], cqn[u][:, kc * 128:(kc + 1) * 128], P.ident.v())
                for kc in range(2):
                    k.tr(P.ps_tr[:, (3 + kc) * 128:(4 + kc) * 128], ckn[u][:, kc * 128:(kc + 1) * 128], P.ident.v())
                k.copy("dve", cqT[tb][:, :, tt_ * 128:(tt_ + 1) * 128], ptr3[:, 0:3, :])
                k.copy("dve", ckvT[tb][:, :, tt_ * 128:(tt_ + 1) * 128], ptr3[:, 3:5, :])
            ta = Buf(k.sb(st2, "rk_a", [128, 512], F32)[:, :])
            tb_ = Buf(k.sb(st2, "rk_b", [128, 512], F32)[:, :])
            for tb in range(4 if MLA_STAGE[0] >= 3 else 0):
                p1, p2 = P.ps[4], P.ps[5]
                for kc in range(KC):
                    k.mm(p1.v(), wkr[:, kc, :], P.hT[tb][:, kc, :], start=(kc == 0), stop=(kc == KC - 1))
                for kc in range(KC):
                    k.mm(p2.v(), wkrR[:, kc, :], P.hT[tb][:, kc, :], start=(kc == 0), stop=(kc == KC - 1))
                k.tt("dve", ta[64:96, :], p1[64:96, :], cs[64:96, 0, tb * 512:(tb + 1) * 512], ALU.mult)
                k.tt("dve", tb_[64:96, :], p2[64:96, :], cs[64:96, 1, tb * 512:(tb + 1) * 512], ALU.mult)
                k.tt("dve", KR[tb][64:96, :], ta[64:96, :], tb_[64:96, :], ALU.add)
            c = 0
            for t in range(NT if MLA_STAGE[0] >= 4 else 0):
                tb, tt_ = t // 4, t % 4
                for hf in range(2):
                    pp = P.ps[c % 4]
                    c += 1
                    for kc in range(2):
                        k.mm(pp.v(), ckvT[tb][:, kc, tt_ * 128:(tt_ + 1) * 128], wvv[:, kc, hf * 512:(hf + 1) * 512],
                             start=(kc == 0), stop=(kc == 1))
                    k.copy("act", V[t][:, hf * 512:(hf + 1) * 512], pp.v())
            k.barrier()
        with ExitStack() as st3:
            QT_t = [k.sb(st3, f"QT{i}", [128, S], BF16) for i in range(2)]
            KT_t = [k.sb(st3, f"KT{i}", [128, S], BF16) for i in range(2)]
            QT = [[Buf(QT_t[i][:, tb * 512:(tb + 1) * 512]) for tb in range(4)] for i in range(2)]
            KT = [[Buf(KT_t[i][:, tb * 512:(tb + 1) * 512]) for tb in range(4)] for i in range(2)]
            pts = [Buf(k.sb(st3, f"pt{i}", [128, 512], BF16)[:, :]) for i in range(4)]
            rd = [Buf(k.sb(st3, f"f_rd{i}", [128, 512], F32)[:, :]) for i in range(2)]
            ta = Buf(k.sb(st3, "rq_a", [128, 512], F32)[:, :])
            tb2 = Buf(k.sb(st3, "rq_b", [128, 512], F32)[:, :])
            rs, rp = Rot(3), Rot(4)
            for i in range(2):
                for tb in range(4):
                    k.memset("pool", QT[i][tb].v(), 0.0)
                    k.memset("pool", KT[i][tb].v(), 0.0)
                    k.copy("pool", KT[i][tb][64:96, :], KR[tb][64:96, :])

            def proj_head(h):
                s_ = h % 2
                for tb in range(4):
                    p1 = P.ps[rs.nxt()]
                    for kc in range(3):
                        k.mm(p1.v(), wuq[:, kc, h * 96:h * 96 + 128], cqT[tb][:, kc, :], start=(kc == 0), stop=(kc == 2))
                    p2 = P.ps[rs.nxt()]
                    for kc in range(3):
                        k.mm(p2.v(), wuqR[:, kc, h * 96:h * 96 + 128], cqT[tb][:, kc, :], start=(kc == 0), stop=(kc == 2))
                    k.copy("dve", QT[s_][tb][0:64, :], p1[0:64, :])
                    k.tt("dve", ta[64:96, :], p1[64:96, :], cs[64:96, 0, tb * 512:(tb + 1) * 512], ALU.mult)
                    k.tt("dve", tb2[64:96, :], p2[64:96, :], cs[64:96, 1, tb * 512:(tb + 1) * 512], ALU.mult)
                    k.tt("dve", QT[s_][tb][64:96, :], ta[64:96, :], tb2[64:96, :], ALU.add)
                    p3 = P.ps[rs.nxt()]
                    for kc in range(2):
                        k.mm(p3.v(), wkn[:, kc, h * 64:h * 64 + 128], ckvT[tb][:, kc, :], start=(kc == 0), stop=(kc == 1))
                    k.copy("dve", KT[s_][tb][0:64, :], p3[0:64, :])

            unit = [0]
            pipe = AttnPipe(2)

            def attn_head(h):
                s_ = h % 2
                p, hh = h // 2, h % 2
                r0, r1 = hh * 64, (hh + 1) * 64
                for qb in range(4):
                    u = unit[0] % 2
                    unit[0] += 1
                    psO, psD = P.ps[3 + 2 * u], P.ps[4 + 2 * u]
                    def fin(u=u, psO=psO, psD=psD, r0=r0, r1=r1, p=p, qb=qb):
                        k.recip(rd[u][r0:r1, :], psD[r0:r1, :])
                        k.tt("dve", oTb[p][qb][r0:r1, :], psO[r0:r1, :], rd[u][r0:r1, :], ALU.mult)

                    run_blocks(P, pipe, causal_blocks(qb),
                               q_of=lambda c0, N: QT[s_][qb][:, c0:c0 + N],
                               k_of=lambda kb: KT[s_][kb // 4][:, (kb % 4) * 128:(kb % 4 + 1) * 128],
                               v_of=lambda kb: V[kb][:, p * 128:(p + 1) * 128],
                               ones_v=P.ones.v(), psO=psO, psD=psD, pts=pts, rs=rs, rp=rp, scale=scale, after=fin)

            NHX = NH if MLA_STAGE[0] >= 6 else (1 if MLA_STAGE[0] >= 5 else 0)
            if NHX:
                proj_head(0)
            for h in range(NHX):
                if h + 1 < NHX:
                    proj_head(h + 1)
                if MLA_STAGE[0] != 5:
                    attn_head(h)
            pipe.flush()
            k.barrier()
        oT = [Buf(oT_t[:, c, :]) for c in range(KC)]
        emit_mix_out(P, li, oT, View(wo.ap[j], wo), dst)


def build(plan):
    k = K()
    P = Prog(k, plan)
    emit_consts(P)
    emit_prologue(P)
    n = len(plan)
    for i, name in enumerate(plan):
        dst = P.y if i == n - 1 else P.hres
        kind, li = name[:3], int(name[3:])
        if kind == "ffn":
            if li % 2 == 1 and SPARSE_MOE:
                emit_ffn_sparse(P, li, dst)
            else:
                emit_ffn(P, li, moe=(li % 2 == 1), dst=dst)
        elif kind == "mix":
            [emit_mix_diff, emit_mix_band, emit_mix_mla, emit_mix_gmlp][li % 4](P, li, dst)
        else:
            raise ValueError(name)
    k.final_wait()
    return k, P


def host_inputs(P, inputs):
    m = {}
    for name in P.ins:
        if name == "ca_bt":
            rb = np.asarray(inputs["ca_rel_bias"], np.float32)[0]
            idx = np.minimum(np.arange(640)[None, :] - np.arange(128)[:, None] + 128, 256)
            m[name] = np.ascontiguousarray(rb[:, idx])
        elif name == "rope_cs":
            half = 16
            inv_freq = (10000.0 ** (-np.arange(half, dtype=np.float32) / half)).astype(np.float32)
            ang = np.arange(S, dtype=np.float32)[None, :] * inv_freq[:, None]
            cos = np.concatenate([np.cos(ang), np.cos(ang)], axis=0)
            sin = np.concatenate([np.sin(ang), np.sin(ang)], axis=0)
            m[name] = np.ascontiguousarray(np.stack([cos, sin], axis=0).astype(np.float32))
        elif name in ("moe_w1r", "moe_w3r"):
            w = np.asarray(inputs["moe_w1" if name == "moe_w1r" else "moe_w3"], np.float32)
            w = w.reshape(2, NE, 2, 4, 128, NG, 512).transpose(0, 1, 5, 4, 2, 3, 6)
            m[name] = np.ascontiguousarray(w).reshape(2 * NE * NG * 128 * 2, WROW)
        elif name == "moe_w2r":
            w = np.asarray(inputs["moe_w2"], np.float32)
            w = w.reshape(2, NE, NG, 2, 2, 128, D).transpose(0, 1, 2, 5, 3, 4, 6)
            m[name] = np.ascontiguousarray(w).reshape(2 * NE * NG * 128 * 2, WROW)
        elif name == "tri_c":
            m[name] = np.triu(np.ones((128, 128), np.float32), 1)
        elif name == "iota2_c":
            m[name] = (2.0 * np.arange(128, dtype=np.float32)).reshape(128, 1)
        elif name == "moe_wrT":
            m[name] = np.ascontiguousarray(np.transpose(np.asarray(inputs["moe_w_router"], np.float32), (0, 2, 1)))
        else:
            m[name] = np.ascontiguousarray(np.asarray(inputs[name], np.float32))
    m["ident_c"] = np.eye(128, dtype=np.float32)
    return m


SPARSE_MOE = True
FULL_PLAN = ["mix0", "ffn0", "mix1", "ffn1", "mix2", "ffn2", "mix3", "ffn3"]


def run_plan(plan, inputs, xs, trace=False):
    k, P = build(plan)
    shared = host_inputs(P, inputs)
    in_maps = []
    for xc in xs:
        mcore = dict(shared)
        mcore["x"] = np.ascontiguousarray(xc, dtype=np.float32)
        in_maps.append(mcore)
    res = run_bass_kernel_spmd(k.nc, in_maps, core_ids=list(range(len(xs))), trace=trace)
    return [r["y"] for r in res.results], res


def kernel(**inputs):
    x = np.asarray(inputs["x"], np.float32)
    outs, _ = run_plan(FULL_PLAN, inputs, [x[b] for b in range(x.shape[0])])
    return np.stack(outs, axis=0).astype(np.float32)
```

```python
import math
from contextlib import ExitStack

import numpy as np
import concourse.bass as bass
import concourse.mybir as mybir
from concourse.bass_utils import run_bass_kernel_spmd

F32 = mybir.dt.float32
BF16 = mybir.dt.bfloat16
AF = mybir.ActivationFunctionType
ALU = mybir.AluOpType
AX = mybir.AxisListType

S = 2048
D = 1024
NT = S // 128
KC = D // 128
DEPTH = 4
ALPHA = (2 * DEPTH) ** 0.25
LN_EPS = 1e-5
D_FF = 2816
D_FFE = 3584
NE = 8
NDS = 16


class Buf:
    def __init__(self, ap, name=""):
        self.ap = ap
        self.w = None
        self.r = {}
        self.name = name

    def __getitem__(self, idx):
        return View(self.ap[idx], self)

    def v(self):
        return View(self.ap, self)


class View:
    def __init__(self, ap, buf):
        self.ap = ap
        self.buf = buf

    def __getitem__(self, idx):
        return View(self.ap[idx], self.buf)


def _ap(x):
    return x.ap if isinstance(x, View) else x


class K:
    def __init__(self):
        nc = bass.Bass("TRN2", target_bir_lowering=False)
        self.nc = nc
        self.eng = {"pe": nc.tensor, "act": nc.scalar, "dve": nc.vector, "pool": nc.gpsimd, "sp": nc.sync}
        self.sem = {e: nc.alloc_semaphore("s_" + e) for e in ("pe", "act", "dve", "pool")}
        self.cnt = {e: 0 for e in self.sem}
        self.dsem = [nc.alloc_semaphore(f"s_d{i}") for i in range(NDS)]
        self.dcnt = [0] * NDS
        self.dnext = 0
        self.dnext_pool = 0
        self.seen = {e: {} for e in self.eng}
        self.n_inst = 0

    def _semh(self, key):
        return self.sem[key] if isinstance(key, str) else self.dsem[key[1]]

    def _wait(self, e, key, val):
        if self.seen[e].get(key, 0) >= val:
            return
        self.eng[e].wait_ge(self._semh(key), val)
        self.seen[e][key] = val

    def _deps(self, e, reads, writes):
        deps = {}

        def add(t):
            if t is None:
                return
            k, v = t
            if deps.get(k, 0) < v:
                deps[k] = v

        for v in reads:
            add(v.buf.w)
        for v in writes:
            add(v.buf.w)
            for kk, val in v.buf.r.items():
                add((kk, val))
        for kk, val in deps.items():
            if kk == e and e == "pe":
                continue
            self._wait(e, kk, val)

    def _done(self, key, val, reads, writes):
        for v in writes:
            v.buf.w = (key, val)
            v.buf.r = {}
        for v in reads:
            if v.buf.r.get(key, 0) < val:
                v.buf.r[key] = val

    def op(self, e, fn, reads, writes):
        reads = [r for r in reads if isinstance(r, View)]
        self._deps(e, reads, writes)
        inst = fn()
        self.cnt[e] += 1
        inst.then_inc(self.sem[e], 1)
        self._done(e, self.cnt[e], reads, writes)
        self.n_inst += 1
        return inst

    def dma(self, q, out, in_, **kw):
        self._deps(q, [in_], [out])
        half = NDS // 2
        if q == "pool":
            i = half + self.dnext_pool
            self.dnext_pool = (self.dnext_pool + 1) % half
        else:
            i = self.dnext
            self.dnext = (self.dnext + 1) % half
        key = ("d", i)
        if self.dcnt[i] > 0:
            self._wait(q, key, 16 * self.dcnt[i])
        inst = self.eng[q].dma_start(out=out.ap, in_=in_.ap, **kw)
        self.dcnt[i] += 1
        inst.then_inc(self.dsem[i], 16)
        self._done(key, 16 * self.dcnt[i], [in_], [out])
        self.n_inst += 1

    def dma_indirect(self, out, out_off, in_, in_off):
        q = "pool"
        idx = in_off if in_off is not None else out_off
        self._deps(q, [in_, idx], [out])
        half = NDS // 2
        i = half + self.dnext_pool
        self.dnext_pool = (self.dnext_pool + 1) % half
        key = ("d", i)
        if self.dcnt[i] > 0:
            self._wait(q, key, 16 * self.dcnt[i])
        oo = bass.IndirectOffsetOnAxis(ap=out_off.ap, axis=0) if out_off is not None else None
        io = bass.IndirectOffsetOnAxis(ap=in_off.ap, axis=0) if in_off is not None else None
        inst = self.nc.gpsimd.indirect_dma_start(out=out.ap, out_offset=oo, in_=in_.ap, in_offset=io)
        self.dcnt[i] += 1
        inst.then_inc(self.dsem[i], 16)
        self._done(key, 16 * self.dcnt[i], [in_, idx], [out])
        self.n_inst += 1

    def barrier(self):
        keys = [(e, self.cnt[e]) for e in self.sem] + [(("d", i), 16 * self.dcnt[i]) for i in range(NDS)]
        for e in self.eng:
            for kk, val in keys:
                if val > 0 and kk != e:
                    self._wait(e, kk, val)

    def final_wait(self):
        for i in range(NDS):
            if self.dcnt[i] > 0:
                self._wait("sp", ("d", i), 16 * self.dcnt[i])

    def mm(self, out, lhsT, rhs, start=True, stop=True, **kw):
        return self.op("pe", lambda: self.nc.tensor.matmul(out.ap, lhsT.ap, rhs.ap, start=start, stop=stop, **kw),
                       [lhsT, rhs], [out])

    def tr(self, out, in_, ident):
        return self.op("pe", lambda: self.nc.tensor.transpose(out.ap, in_.ap, ident.ap), [in_, ident], [out])

    def act(self, out, in_, func, bias=None, scale=None, accum=None):
        kw = {}
        reads = [in_]
        writes = [out]
        if bias is not None:
            kw["bias"] = _ap(bias)
            reads.append(bias)
        if scale is not None:
            kw["scale"] = _ap(scale)
            reads.append(scale)
        if accum is not None:
            kw["accum_out"] = accum.ap
            writes.append(accum)
        return self.op("act", lambda: self.nc.scalar.activation(out=out.ap, in_=in_.ap, func=func, **kw), reads, writes)

    def tt(self, e, out, in0, in1, op):
        return self.op(e, lambda: self.eng[e].tensor_tensor(out=out.ap, in0=in0.ap, in1=in1.ap, op=op), [in0, in1], [out])

    def ts(self, e, out, in0, s1, op0, s2=None, op1=None, accum=None):
        kw = {}
        writes = [out]
        if op1 is not None:
            kw["op1"] = op1
        if accum is not None:
            kw["accum_out"] = accum.ap
            writes.append(accum)
        return self.op(e, lambda: self.eng[e].tensor_scalar(out=out.ap, in0=in0.ap, scalar1=_ap(s1), scalar2=_ap(s2), op0=op0, **kw),
                       [in0, s1, s2], writes)

    def stt(self, e, out, in0, scalar, in1, op0, op1, accum=None):
        kw = {}
        writes = [out]
        if accum is not None:
            kw["accum_out"] = accum.ap
            writes.append(accum)
        return self.op(e, lambda: self.eng[e].scalar_tensor_tensor(out=out.ap, in0=in0.ap, scalar=_ap(scalar), in1=in1.ap, op0=op0, op1=op1, **kw),
                       [in0, scalar, in1], writes)

    def copy(self, e, out, in_):
        if e == "act":
            return self.act(out, in_, AF.Copy)
        return self.op(e, lambda: self.eng[e].tensor_copy(out=out.ap, in_=in_.ap), [in_], [out])

    def memset(self, e, out, val):
        return self.op(e, lambda: self.eng[e].memset(out.ap, val), [], [out])

    def recip(self, out, in_):
        return self.op("dve", lambda: self.nc.vector.reciprocal(out=out.ap, in_=in_.ap), [in_], [out])

    def sb(self, st, name, shape, dtype):
        self.n_alloc = getattr(self, "n_alloc", 0) + 1
        return st.enter_context(self.nc.sbuf_tensor(f"{name}_{self.n_alloc}", list(shape), dtype))

    def dram_in(self, name, shape, dtype=F32):
        return Buf(self.nc.dram_tensor(name, list(shape), dtype, kind="ExternalInput").ap(), name)

    def dram_out(self, name, shape, dtype=F32):
        return Buf(self.nc.dram_tensor(name, list(shape), dtype, kind="ExternalOutput").ap(), name)

    def dram_tmp(self, name, shape, dtype=F32):
        return Buf(self.nc.dram_tensor(name, list(shape), dtype, kind="Internal").ap(), name)


class Prog:
    def __init__(self, k, plan):
        self.k = k
        nc = k.nc
        self.plan = plan
        self.st = ExitStack()
        st = self.st
        hT_t = k.sb(st, "hT", [128, KC, S], BF16)
        self.hT = [Buf(hT_t[:, :, tb * 512:(tb + 1) * 512], f"hT{tb}") for tb in range(4)]
        ident_t = k.sb(st, "ident", [128, 128], BF16)
        self.ident = Buf(ident_t[:, :], "ident")
        ones_t = k.sb(st, "ones", [128, 128], BF16)
        self.ones = Buf(ones_t[:, :], "ones")
        small_t = k.sb(st, "small", [128, 64], F32)
        self.small_t = small_t
        self.ps = [Buf(nc.alloc_psum_tensor(f"ps{i}", [128, 512], F32)[:, :], f"ps{i}") for i in range(7)]
        pst = nc.alloc_psum_tensor("pstr", [128, 1024], BF16)
        self.ps_tr = Buf(pst[:, :], "pstr")
        self.x = k.dram_in("x", [S, D])
        self.y = k.dram_out("y", [S, D])
        self.hres = k.dram_tmp("hres", [S, D])
        self.ident_d = k.dram_in("ident_c", [128, 128])
        self.ins = {}

    def inp(self, name, shape):
        if name not in self.ins:
            self.ins[name] = self.k.dram_in(name, shape)
        return self.ins[name]


def emit_consts(P):
    k = P.k
    k.dma("pool", P.ident.v(), P.ident_d.v())
    k.memset("pool", P.ones.v(), 1.0)


def emit_ln_tail(P, st, t, xs, gb, bb, dst, tmp):
    k = P.k
    ts_ = tmp["sets"][t % len(tmp["sets"])]
    stats, mv, sc, xn, xb = ts_["stats"], ts_["mv"], ts_["sc"], ts_["xn"], ts_["xb"]
    for hf in range(2):
        k.op("dve", lambda hf=hf: k.nc.vector.bn_stats(out=stats.ap[:, hf * 6:(hf + 1) * 6], in_=xs.ap[:, hf * 512:(hf + 1) * 512]),
             [xs.v()], [stats.v()])
    k.op("dve", lambda: k.nc.vector.bn_aggr(out=mv.ap, in_=stats.ap), [stats.v()], [mv.v()])
    k.act(sc[:, 0:1], mv[:, 1:2], AF.Sqrt, bias=tmp["eps"][:, 0:1])
    k.recip(sc[:, 1:2], sc[:, 0:1])
    k.stt("dve", sc[:, 2:3], mv[:, 0:1], -1.0, sc[:, 1:2], ALU.mult, ALU.mult)
    k.act(xn.v(), xs.v(), AF.Identity, scale=sc[:, 1:2], bias=sc[:, 2:3])
    k.tt("pool", xn.v(), xn.v(), gb.v(), ALU.mult)
    k.tt("pool", xn.v(), xn.v(), bb.v(), ALU.add)
    k.dma("sp", dst[t * 128:(t + 1) * 128, :], xn.v())
    k.copy("act", xb.v(), xn.v())

    def part_b(t=t, xb=xb):
        for kc in range(KC):
            k.tr(P.ps_tr[:, kc * 128:(kc + 1) * 128], xb[:, kc * 128:(kc + 1) * 128], P.ident.v())
        tb, tt_ = t // 4, t % 4
        k.copy("dve", P.hT[tb][:, :, tt_ * 128:(tt_ + 1) * 128],
               View(P.ps_tr.ap.rearrange("p (k c) -> p k c", c=128), P.ps_tr))

    flush_ln_tail(P, tmp)
    tmp["pending"] = part_b
    return xn


def flush_ln_tail(P, tmp):
    if tmp.get("pending") is not None:
        fn = tmp["pending"]
        tmp["pending"] = None
        fn()


def alloc_ln_tmp(P, st, pfx, nset=2):
    k = P.k
    tmp = {"sets": [], "pending": None}
    for i in range(nset):
        d = {}
        d["stats"] = Buf(k.sb(st, pfx + f"stats{i}", [128, 12], F32)[:, :])
        d["mv"] = Buf(k.sb(st, pfx + f"mv{i}", [128, 2], F32)[:, :])
        d["sc"] = Buf(k.sb(st, pfx + f"sc{i}", [128, 4], F32)[:, :])
        d["xn"] = Buf(k.sb(st, pfx + f"xn{i}", [128, D], F32)[:, :])
        d["xb"] = Buf(k.sb(st, pfx + f"xb{i}", [128, D], BF16)[:, :])
        tmp["sets"].append(d)
    tmp["eps"] = Buf(k.sb(st, pfx + "eps", [128, 1], F32)[:, :])
    k.memset("pool", tmp["eps"].v(), LN_EPS)
    return tmp


def emit_prologue(P):
    k = P.k
    with ExitStack() as st:
        xin = [Buf(k.sb(st, f"pro_x{i}", [128, D], F32)[:, :]) for i in range(2)]
        xb = [Buf(k.sb(st, f"pro_xb{i}", [128, D], BF16)[:, :]) for i in range(2)]
        for t in range(NT):
            xi = xin[t % 2]
            k.dma("sp", xi.v(), P.x[t * 128:(t + 1) * 128, :])
            k.dma("sp", P.hres[t * 128:(t + 1) * 128, :], xi.v())
            k.copy("act", xb[t % 2].v(), xi.v())
            for kc in range(KC):
                k.tr(P.ps_tr[:, kc * 128:(kc + 1) * 128], xb[t % 2][:, kc * 128:(kc + 1) * 128], P.ident.v())
            tb, tt_ = t // 4, t % 4
            k.copy("dve", P.hT[tb][:, :, tt_ * 128:(tt_ + 1) * 128],
                   View(P.ps_tr.ap.rearrange("p (k c) -> p k c", c=128), P.ps_tr))
        k.barrier()


def load_ln_params(P, st, g_d, b_d, pfx):
    k = P.k
    gb = Buf(k.sb(st, pfx + "g", [128, D], F32)[:, :])
    bb = Buf(k.sb(st, pfx + "b", [128, D], F32)[:, :])
    k.dma("sp", gb.v(), View(g_d.ap.partition_broadcast(128), g_d.buf))
    k.dma("sp", bb.v(), View(b_d.ap.partition_broadcast(128), b_d.buf))
    return gb, bb


def emit_ffn(P, li, moe, dst):
    k = P.k
    nc = k.nc
    j = li // 2
    if moe:
        E, F = NE, D_FFE
        w1 = P.inp("moe_w1", [2, NE, D, D_FFE])
        w3 = P.inp("moe_w3", [2, NE, D, D_FFE])
        w2 = P.inp("moe_w2", [2, NE, D_FFE, D])
        wr = P.inp("moe_wrT", [2, NE, D])
        w1v = lambda e: View(w1.ap[j, e], w1)
        w3v = lambda e: View(w3.ap[j, e], w3)
        w2v = lambda e: View(w2.ap[j, e], w2)
    else:
        E, F = 1, D_FF
        w1 = P.inp("ffn_w1", [2, D, D_FF])
        w3 = P.inp("ffn_w3", [2, D, D_FF])
        w2 = P.inp("ffn_w2", [2, D_FF, D])
        w1v = lambda e: View(w1.ap[j], w1)
        w3v = lambda e: View(w3.ap[j], w3)
        w2v = lambda e: View(w2.ap[j], w2)
    g_d = P.inp("ln_ffn_g", [DEPTH, D])
    b_d = P.inp("ln_ffn_b", [DEPTH, D])
    nj = F // 128
    G = 4
    units = [(e, j0, min(G, nj - j0)) for e in range(E) for j0 in range(0, nj, G)]
    U = len(units)
    with ExitStack() as st:
        comb_t = k.sb(st, "comb", [128, NT, NE], F32)
        comb = Buf(comb_t[:, :, :], "comb")
        if moe:
            with ExitStack() as st2:
                wrb = Buf(k.sb(st2, "wrb", [128, NE, D], F32)[:, :, :], "wrb")
                k.dma("sp", wrb.v(), View(wr.ap[j].partition_broadcast(128), wr))
                hin = [Buf(k.sb(st2, f"rt_h{i}", [128, D], F32)[:, :]) for i in range(2)]
                junk = Buf(k.sb(st2, "rt_junk", [128, D], F32)[:, :])
                lg = Buf(k.sb(st2, "rt_lg", [128, NE], F32)[:, :])
                l2 = Buf(k.sb(st2, "rt_l2", [128, NE], F32)[:, :])
                m1 = Buf(k.sb(st2, "rt_m1", [128, NE], F32)[:, :])
                m2 = Buf(k.sb(st2, "rt_m2", [128, NE], F32)[:, :])
                sc = Buf(k.sb(st2, "rt_sc", [128, 8], F32)[:, :])
                for t in range(NT):
                    hi = hin[t % 2]
                    k.dma("sp", hi.v(), P.hres[t * 128:(t + 1) * 128, :])
                    for e in range(NE):
                        k.stt("dve", junk.v(), hi.v(), 1.0, wrb[:, e, :], ALU.mult, ALU.mult, accum=lg[:, e:e + 1])
                    k.op("dve", lambda: nc.vector.reduce_max(out=sc.ap[:, 0:1], in_=lg.ap, axis=AX.X), [lg.v()], [sc.v()])
                    k.ts("dve", m1.v(), lg.v(), sc[:, 0:1], ALU.is_equal)
                    k.stt("dve", l2.v(), m1.v(), -1e30, lg.v(), ALU.mult, ALU.add)
                    k.op("dve", lambda: nc.vector.reduce_max(out=sc.ap[:, 1:2], in_=l2.ap, axis=AX.X), [l2.v()], [sc.v()])
                    k.ts("dve", m2.v(), l2.v(), sc[:, 1:2], ALU.is_equal)
                    k.tt("dve", sc[:, 2:3], sc[:, 1:2], sc[:, 0:1], ALU.subtract)
                    k.act(sc[:, 3:4], sc[:, 2:3], AF.Exp)
                    k.ts("dve", sc[:, 4:5], sc[:, 3:4], 1.0, ALU.add)
                    k.recip(sc[:, 5:6], sc[:, 4:5])
                    k.tt("dve", sc[:, 6:7], sc[:, 3:4], sc[:, 5:6], ALU.mult)
                    k.ts("dve", m1.v(), m1.v(), sc[:, 5:6], ALU.mult)
                    k.stt("dve", comb[:, t, :], m2.v(), sc[:, 6:7], m1.v(), ALU.mult, ALU.add)
                k.barrier()
        yacc_t = k.sb(st, "yacc", [128, NT, D], F32)
        yacc = [[Buf(yacc_t[:, t, hf * 512:(hf + 1) * 512]) for hf in range(2)] for t in range(NT)]
        stc = ExitStack()
        htg_t = [k.sb(stc, f"htg{i}", [128, G, S], BF16) for i in range(2)]
        htg = [[[Buf(htg_t[i][:, jj, tb * 512:(tb + 1) * 512]) for tb in range(4)] for jj in range(G)] for i in range(2)]
        w1g = [Buf(k.sb(stc, f"w1g{i}", [128, KC, G * 128], BF16)[:, :, :]) for i in range(2)]
        w3g = [Buf(k.sb(stc, f"w3g{i}", [128, KC, G * 128], BF16)[:, :, :]) for i in range(2)]
        w2g = [Buf(k.sb(stc, f"w2g{i}", [128, G, D], BF16)[:, :, :]) for i in range(2)]
        sil = [Buf(k.sb(stc, f"sil{i}", [128, 512], F32)[:, :]) for i in range(2)]

        def loadA(u):
            e, j0, n = units[u]
            s = u % 2
            k.dma("pool", w1g[s][:, :, 0:n * 128],
                  View(w1v(e).ap.rearrange("(kc p) f -> p kc f", p=128)[:, :, j0 * 128:(j0 + n) * 128], w1))
            k.dma("pool", w3g[s][:, :, 0:n * 128],
                  View(w3v(e).ap.rearrange("(kc p) f -> p kc f", p=128)[:, :, j0 * 128:(j0 + n) * 128], w3))

        def loadB(u):
            e, j0, n = units[u]
            s = u % 2
            k.dma("pool", w2g[s][:, 0:n, :],
                  View(w2v(e).ap[j0 * 128:(j0 + n) * 128, :].rearrange("(j p) d -> p j d", p=128), w2))

        cnt1 = [0]

        def phase1(u):
            e, j0, n = units[u]
            s = u % 2
            for jj in range(n):
                for tb in range(4):
                    c = cnt1[0] % 2
                    cnt1[0] += 1
                    pa, pb = P.ps[2 * c], P.ps[2 * c + 1]
                    for (W, pp) in ((w1g[s], pa), (w3g[s], pb)):
                        for kc in range(KC):
                            k.mm(pp.v(), W[:, kc, jj * 128:(jj + 1) * 128], P.hT[tb][:, kc, :], start=(kc == 0), stop=(kc == KC - 1))
                    k.act(sil[c].v(), pa.v(), AF.Silu)
                    k.tt("dve", htg[s][jj][tb].v(), sil[c].v(), pb.v(), ALU.mult)

        cnt2 = [0]

        def phase2(u):
            e, j0, n = units[u]
            s = u % 2
            for t in range(NT):
                tb, tt_ = t // 4, t % 4
                for hf in range(2):
                    py = P.ps[4 + cnt2[0] % 3]
                    cnt2[0] += 1
                    for jj in range(n):
                        k.mm(py.v(), htg[s][jj][tb][:, tt_ * 128:(tt_ + 1) * 128], w2g[s][:, jj, hf * 512:(hf + 1) * 512],
                             start=(jj == 0), stop=(jj == n - 1))
                    ya = yacc[t][hf]
                    if moe:
                        if u == 0:
                            k.ts("dve", ya.v(), py.v(), comb[:, t, e:e + 1], ALU.mult)
                        else:
                            k.stt("dve", ya.v(), py.v(), comb[:, t, e:e + 1], ya.v(), ALU.mult, ALU.add)
                    else:
                        if u == 0:
                            k.copy("dve", ya.v(), py.v())
                        else:
                            k.tt("dve", ya.v(), py.v(), ya.v(), ALU.add)

        loadA(0)
        loadB(0)
        if U > 1:
            loadA(1)
            loadB(1)
        phase1(0)
        for u in range(U):
            if u + 1 < U:
                phase1(u + 1)
            if u + 2 < U:
                loadA(u + 2)
            phase2(u)
            if u + 2 < U:
                loadB(u + 2)
        k.barrier()
        stc.close()
        gb, bb = load_ln_params(P, st, View(g_d.ap[li:li + 1, :], g_d), View(b_d.ap[li:li + 1, :], b_d), "ffn_ln")
        tmp = alloc_ln_tmp(P, st, "ffn_")
        xin = [Buf(k.sb(st, f"ffn_xin{i}", [128, D], F32)[:, :]) for i in range(2)]
        k.dma("sp", xin[0].v(), P.hres[0:128, :])
        for t in range(NT):
            if t + 1 < NT:
                k.dma("sp", xin[(t + 1) % 2].v(), P.hres[(t + 1) * 128:(t + 2) * 128, :])
            xi = xin[t % 2]
            for hf in range(2):
                k.stt("dve", xi[:, hf * 512:(hf + 1) * 512], xi[:, hf * 512:(hf + 1) * 512], ALPHA, yacc[t][hf].v(), ALU.mult, ALU.add)
            emit_ln_tail(P, st, t, xi, gb, bb, dst, tmp)
        flush_ln_tail(P, tmp)
        k.barrier()


NG = 7
NST = 16
WROW = 2048


def emit_ffn_sparse(P, li, dst):
    k = P.k
    nc = k.nc
    j = li // 2
    I32 = mybir.dt.int32
    nrows = 2 * NE * NG * 128 * 2
    w1r = P.inp("moe_w1r", [nrows, WROW])
    w3r = P.inp("moe_w3r", [nrows, WROW])
    w2r = P.inp("moe_w2r", [nrows, WROW])
    wr = P.inp("moe_wrT", [2, NE, D])
    tri_d = P.inp("tri_c", [128, 128])
    io_d = P.inp("iota2_c", [128, 1])
    g_d = P.inp("ln_ffn_g", [DEPTH, D])
    b_d = P.inp("ln_ffn_b", [DEPTH, D])
    if not hasattr(P, "xs_d"):
        P.xs_d = k.dram_tmp("xs_sorted", [NST * 512, D], BF16)
        P.ys_d = k.dram_tmp("ys_sorted", [NST * 512, D], F32)
    xs_d, ys_d = P.xs_d, P.ys_d
    with ExitStack() as st:
        gs = Buf(k.sb(st, "gs", [128, NT, 2], F32)[:, :, :], "gs")
        slot_i = Buf(k.sb(st, "slot_i", [128, NT, 2], I32)[:, :, :], "slot_i")
        idxw = Buf(k.sb(st, "idxw", [128, NST, NG, 2], I32)[:, :, :, :], "idxw")
        with ExitStack() as st2:
            wrb = Buf(k.sb(st2, "wrb", [128, NE, D], F32)[:, :, :], "wrb")
            k.dma("sp", wrb.v(), View(wr.ap[j].partition_broadcast(128), wr))
            tri = Buf(k.sb(st2, "tri", [128, 128], BF16)[:, :], "tri")
            k.dma("pool", tri.v(), tri_d.v())
            io2 = Buf(k.sb(st2, "io2", [128, 1], F32)[:, :], "io2")
            k.dma("sp", io2.v(), io_d.v())
            zt = Buf(k.sb(st2, "zt", [128, 4096], BF16)[:, :], "zt")
            k.memset("pool", zt.v(), 0.0)
            xs_flat = View(xs_d.ap.rearrange("(p r) d -> p (r d)", p=128), xs_d)
            for i in range(16):
                k.dma("sp", xs_flat[:, i * 4096:(i + 1) * 4096], zt.v())
            m1s = Buf(k.sb(st2, "m1s", [128, NT, NE], F32)[:, :, :], "m1s")
            m2s = Buf(k.sb(st2, "m2s", [128, NT, NE], F32)[:, :, :], "m2s")
            abf = Buf(k.sb(st2, "abf", [128, NT, NE], BF16)[:, :, :], "abf")
            hin = [Buf(k.sb(st2, f"rt_h{i}", [128, D], F32)[:, :]) for i in range(2)]
            xbt_t = k.sb(st2, "rt_xb", [128, NT, D], BF16)
            xbt = [Buf(xbt_t[:, t, :]) for t in range(NT)]
            junk = Buf(k.sb(st2, "rt_junk", [128, D], F32)[:, :])
            lg = Buf(k.sb(st2, "rt_lg", [128, NE], F32)[:, :])
            l2 = Buf(k.sb(st2, "rt_l2", [128, NE], F32)[:, :])
            sc = Buf(k.sb(st2, "rt_sc", [128, 8], F32)[:, :])
            lga = Buf(k.sb(st2, "rt_lga", [128, NT, NE], F32)[:, :, :], "lga")
            l2a = Buf(k.sb(st2, "rt_l2a", [128, NT, NE], F32)[:, :, :], "l2a")
            mxa = Buf(k.sb(st2, "rt_mxa", [128, 4, NT], F32)[:, :, :], "mxa")
            for t in range(NT):
                hi = hin[t % 2]
                k.dma("sp", hi.v(), P.hres[t * 128:(t + 1) * 128, :])
                k.copy("act", xbt[t].v(), hi.v())
                for e in range(NE):
                    k.stt("dve", junk.v(), hi.v(), 1.0, wrb[:, e, :], ALU.mult, ALU.mult, accum=lga[:, t, e:e + 1])
            bc = lambda v: View(v.ap.unsqueeze(2).broadcast_to([128, NT, NE]), v.buf)
            k.op("dve", lambda: nc.vector.reduce_max(out=mxa.ap[:, 0, :], in_=lga.ap, axis=AX.X), [lga.v()], [mxa.v()])
            k.tt("dve", m1s.v(), lga.v(), bc(mxa[:, 0, :]), ALU.is_equal)
            k.stt("dve", l2a.v(), m1s.v(), -1e30, lga.v(), ALU.mult, ALU.add)
            k.op("dve", lambda: nc.vector.reduce_max(out=mxa.ap[:, 1, :], in_=l2a.ap, axis=AX.X), [l2a.v()], [mxa.v()])
            k.tt("dve", m2s.v(), l2a.v(), bc(mxa[:, 1, :]), ALU.is_equal)
            k.tt("dve", mxa[:, 2, :], mxa[:, 1, :], mxa[:, 0, :], ALU.subtract)
            k.act(mxa[:, 2, :], mxa[:, 2, :], AF.Exp)
            k.ts("dve", mxa[:, 3, :], mxa[:, 2, :], 1.0, ALU.add)
            k.recip(gs[:, :, 0], mxa[:, 3, :])
            k.tt("dve", gs[:, :, 1], mxa[:, 2, :], gs[:, :, 0], ALU.mult)
            k.tt("dve", abf.v(), m1s.v(), m2s.v(), ALU.add)
            pp = P.ps[0]
            for t in range(NT):
                for tp in range(t):
                    k.mm(pp[:, t * 8:(t + 1) * 8], P.ones.v(), abf[:, tp, :], start=(tp == 0), stop=False)
                k.mm(pp[:, t * 8:(t + 1) * 8], tri.v(), abf[:, t, :], start=(t == 0), stop=True)
            for t in range(NT):
                k.mm(pp[:, 128:136], P.ones.v(), abf[:, t, :], start=(t == 0), stop=(t == NT - 1))
            posf = Buf(k.sb(st2, "posf", [128, 136], F32)[:, :], "posf")
            k.copy("dve", posf.v(), pp[:, 0:136])
            w8 = Buf(k.sb(st2, "w8", [128, 8, 8], F32)[:, :, :], "w8")
            for m in range(4):
                k.ts("dve", w8[:, m, :], posf[:, 128:136], 512.0 * m, ALU.is_gt)
            k.tt("dve", w8[:, 0, :], w8[:, 0, :], w8[:, 1, :], ALU.add)
            k.tt("dve", w8[:, 2, :], w8[:, 2, :], w8[:, 3, :], ALU.add)
            k.tt("dve", w8[:, 0, :], w8[:, 0, :], w8[:, 2, :], ALU.add)
            k.ts("dve", w8[:, 4, :], w8[:, 0, :], 512.0, ALU.mult)
            k.memset("pool", w8[:, 5, :], 0.0)
            for e in range(1, NE):
                k.tt("dve", w8[:, 5, e:e + 1], w8[:, 5, e - 1:e], w8[:, 4, e - 1:e], ALU.add)
            k.tt("dve", w8[:, 6, :], w8[:, 5, :], w8[:, 4, :], ALU.add)
            sf = Buf(k.sb(st2, "sf", [128, NT, NE], F32)[:, :, :], "sf")
            slotf = Buf(k.sb(st2, "slotf", [128, 2, NT], F32)[:, :, :], "slotf")
            j8 = Buf(k.sb(st2, "j8", [128, NE], F32)[:, :], "j8")
            pos3 = View(posf.ap[:, 0:128].rearrange("p (t e) -> p t e", e=NE), posf)
            k.tt("dve", sf.v(), pos3, View(w8.ap[:, 5, :].unsqueeze(1).broadcast_to([128, NT, NE]), w8), ALU.add)
            k.tt("dve", l2a.v(), m1s.v(), sf.v(), ALU.mult)
            k.op("dve", lambda: nc.vector.reduce_sum(out=slotf.ap[:, 0, :], in_=l2a.ap, axis=AX.X), [l2a.v()], [slotf.v()])
            k.tt("dve", l2a.v(), m2s.v(), sf.v(), ALU.mult)
            k.op("dve", lambda: nc.vector.reduce_sum(out=slotf.ap[:, 1, :], in_=l2a.ap, axis=AX.X), [l2a.v()], [slotf.v()])
            k.copy("dve", slot_i[:, :, 0], slotf[:, 0, :])
            k.copy("dve", slot_i[:, :, 1], slotf[:, 1, :])
            esf = Buf(k.sb(st2, "esf", [128, NST], F32)[:, :], "esf")
            for s_ in range(NST):
                k.ts("dve", j8.v(), w8[:, 6, :], 512.0 * s_, ALU.is_le, s2=0.0, op1=ALU.add, accum=esf[:, s_:s_ + 1])
            k.ts("dve", esf.v(), esf.v(), float(NE - 1), ALU.min)
            k.ts("dve", esf.v(), esf.v(), float(NG * 128 * 2), ALU.mult, s2=io2[:, 0:1], op1=ALU.add)
            idxf = Buf(k.sb(st2, "idxf", [128, NST, NG, 2], F32)[:, :, :, :], "idxf")
            for g in range(NG):
                for hf in range(2):
                    k.ts("dve", idxf[:, :, g, hf], esf.v(), float(((j * NE) * NG + g) * 128 * 2 + hf), ALU.add)
            k.copy("dve", idxw.v(), idxf.v())
            k.barrier()
            for t in range(NT):
                for sl in range(2):
                    k.dma_indirect(out=xs_d.v(), out_off=slot_i[:, t, sl:sl + 1], in_=xbt[t].v(), in_off=None)
            k.barrier()
        with ExitStack() as st3:
            G = 4
            xsT = [Buf(k.sb(st3, f"xsT{i}", [128, KC, 512], BF16)[:, :, :]) for i in range(2)]
            xrow = [Buf(k.sb(st3, f"xrow{i}", [128, D], BF16)[:, :]) for i in range(2)]
            yacc_t = [k.sb(st3, f"yacc{i}", [128, 4, D], F32) for i in range(2)]
            yacc = [[[Buf(yacc_t[i][:, r, hf * 512:(hf + 1) * 512]) for hf in range(2)] for r in range(4)] for i in range(2)]
            htg = [[Buf(k.sb(st3, f"htg{i}_{jj}", [128, 512], BF16)[:, :]) for jj in range(G)] for i in range(2)]
            w1g = [Buf(k.sb(st3, f"w1g{i}", [128, KC, 512], BF16)[:, :, :]) for i in range(2)]
            w3g = [Buf(k.sb(st3, f"w3g{i}", [128, KC, 512], BF16)[:, :, :]) for i in range(2)]
            w2g = [Buf(k.sb(st3, f"w2g{i}", [128, G, D], BF16)[:, :, :]) for i in range(2)]
            sil = [Buf(k.sb(st3, f"sil{i}", [128, 512], F32)[:, :]) for i in range(2)]
            units = [(s_, g) for s_ in range(NST) for g in range(NG)]
            U = len(units)
            ptr3 = View(P.ps_tr.ap.rearrange("p (k c) -> p k c", c=128), P.ps_tr)
            xc = [0]

            def load_x(s_):
                for r in range(4):
                    xr = xrow[xc[0] % 2]
                    xc[0] += 1
                    k.dma("sp", xr.v(), xs_d[s_ * 512 + r * 128:s_ * 512 + (r + 1) * 128, :])
                    for kc in range(KC):
                        k.tr(P.ps_tr[:, kc * 128:(kc + 1) * 128], xr[:, kc * 128:(kc + 1) * 128], P.ident.v())
                    k.copy("dve", xsT[s_ % 2][:, :, r * 128:(r + 1) * 128], ptr3)

            def loadA(u):
                s_, g = units[u]
                sl = u % 2
                for hf in range(2):
                    k.dma_indirect(out=View(w1g[sl].ap[:, hf * 4:(hf + 1) * 4, :].rearrange("p k f -> p (k f)"), w1g[sl]),
                                   out_off=None, in_=w1r.v(), in_off=idxw[:, s_, g, hf:hf + 1])
                    k.dma_indirect(out=View(w3g[sl].ap[:, hf * 4:(hf + 1) * 4, :].rearrange("p k f -> p (k f)"), w3g[sl]),
                                   out_off=None, in_=w3r.v(), in_off=idxw[:, s_, g, hf:hf + 1])

            def loadB(u):
                s_, g = units[u]
                sl = u % 2
                for hf in range(2):
                    k.dma_indirect(out=View(w2g[sl].ap[:, hf * 2:(hf + 1) * 2, :].rearrange("p k f -> p (k f)"), w2g[sl]),
                                   out_off=None, in_=w2r.v(), in_off=idxw[:, s_, g, hf:hf + 1])

            cnt1 = [0]

            def phase1(u):
                s_, g = units[u]
                sl = u % 2
                for jj in range(G):
                    c = cnt1[0] % 2
                    cnt1[0] += 1
                    pa, pb = P.ps[2 * c], P.ps[2 * c + 1]
                    for (W, pq) in ((w1g[sl], pa), (w3g[sl], pb)):
                        for kc in range(KC):
                            k.mm(pq.v(), W[:, kc, jj * 128:(jj + 1) * 128], xsT[s_ % 2][:, kc, :], start=(kc == 0), stop=(kc == KC - 1))
                    k.act(sil[c].v(), pa.v(), AF.Silu)
                    k.tt("dve", htg[sl][jj].v(), sil[c].v(), pb.v(), ALU.mult)

            cnt2 = [0]

            def phase2(u):
                s_, g = units[u]
                sl = u % 2
                for r in range(4):
                    for hf in range(2):
                        py = P.ps[4 + cnt2[0] % 3]
                        cnt2[0] += 1
                        for jj in range(G):
                            k.mm(py.v(), htg[sl][jj][:, r * 128:(r + 1) * 128], w2g[sl][:, jj, hf * 512:(hf + 1) * 512],
                                 start=(jj == 0), stop=(jj == G - 1))
                        ya = yacc[s_ % 2][r][hf]
                        if g == 0:
                            k.copy("dve", ya.v(), py.v())
                        else:
                            k.tt("dve", ya.v(), py.v(), ya.v(), ALU.add)
                if g == NG - 1:
                    for r in range(4):
                        k._deps("sp", [yacc[s_ % 2][r][1].v()], [])
                        k.dma("sp", ys_d[s_ * 512 + r * 128:s_ * 512 + (r + 1) * 128, :],
                              View(yacc_t[s_ % 2][:, r, :], yacc[s_ % 2][r][0]))

            load_x(0)
            loadA(0)
            loadB(0)
            loadA(1)
            loadB(1)
            phase1(0)
            for u in range(U):
                s_, g = units[u]
                if g == 2 and s_ + 1 < NST:
                    load_x(s_ + 1)
                if u + 1 < U:
                    phase1(u + 1)
                if u + 2 < U:
                    loadA(u + 2)
                phase2(u)
                if u + 2 < U:
                    loadB(u + 2)
            k.barrier()
        with ExitStack() as st4:
            gb, bb = load_ln_params(P, st4, View(g_d.ap[li:li + 1, :], g_d), View(b_d.ap[li:li + 1, :], b_d), "ffn_ln")
            tmp = alloc_ln_tmp(P, st4, "ffn_")
            xin = [Buf(k.sb(st4, f"ffn_xin{i}", [128, D], F32)[:, :]) for i in range(2)]
            y1 = [Buf(k.sb(st4, f"ffn_y1{i}", [128, D], F32)[:, :]) for i in range(2)]
            y2 = [Buf(k.sb(st4, f"ffn_y2{i}", [128, D], F32)[:, :]) for i in range(2)]

            def fetch(t):
                k.dma("sp", xin[t % 2].v(), P.hres[t * 128:(t + 1) * 128, :])
                k.dma_indirect(out=y1[t % 2].v(), out_off=None, in_=ys_d.v(), in_off=slot_i[:, t, 0:1])
                k.dma_indirect(out=y2[t % 2].v(), out_off=None, in_=ys_d.v(), in_off=slot_i[:, t, 1:2])

            fetch(0)
            for t in range(NT):
                if t + 1 < NT:
                    fetch(t + 1)
                xi, a, b = xin[t % 2], y1[t % 2], y2[t % 2]
                k.act(a.v(), a.v(), AF.Identity, scale=gs[:, t, 0:1])
                k.stt("dve", a.v(), b.v(), gs[:, t, 1:2], a.v(), ALU.mult, ALU.add)
                k.stt("dve", xi.v(), xi.v(), ALPHA, a.v(), ALU.mult, ALU.add)
                emit_ln_tail(P, st4, t, xi, gb, bb, dst, tmp)
            flush_ln_tail(P, tmp)
            k.barrier()


def load_w_bf16(P, st, name, src_view, kchunks, ncols, col0=0):
    k = P.k
    b = Buf(k.sb(st, name, [128, kchunks, ncols], BF16)[:, :, :], name)
    k.dma("pool", b.v(), View(src_view.ap.rearrange("(kc p) f -> p kc f", p=128)[:, :, col0:col0 + ncols], src_view.buf))
    return b


def emit_mix_out(P, li, oT, wo_view, dst):
    k = P.k
    g_d = P.inp("ln_mix_g", [DEPTH, D])
    b_d = P.inp("ln_mix_b", [DEPTH, D])
    with ExitStack() as st:
        wo = load_w_bf16(P, st, "wo_sb", wo_view, KC, D)
        gb, bb = load_ln_params(P, st, View(g_d.ap[li:li + 1, :], g_d), View(b_d.ap[li:li + 1, :], b_d), "mix_ln")
        tmp = alloc_ln_tmp(P, st, "mix_")
        xin = [Buf(k.sb(st, f"mix_xin{i}", [128, D], F32)[:, :]) for i in range(2)]
        k.dma("sp", xin[0].v(), P.hres[0:128, :])
        c = 0
        for t in range(NT):
            if t + 1 < NT:
                k.dma("sp", xin[(t + 1) % 2].v(), P.hres[(t + 1) * 128:(t + 2) * 128, :])
            xi = xin[t % 2]
            for hf in range(2):
                py = P.ps[c % 4]
                c += 1
                for kc in range(KC):
                    k.mm(py.v(), oT[kc][:, t * 128:(t + 1) * 128], wo[:, kc, hf * 512:(hf + 1) * 512], start=(kc == 0), stop=(kc == KC - 1))
                k.stt("dve", xi[:, hf * 512:(hf + 1) * 512], xi[:, hf * 512:(hf + 1) * 512], ALPHA, py.v(), ALU.mult, ALU.add)
            emit_ln_tail(P, st, t, xi, gb, bb, dst, tmp)
        flush_ln_tail(P, tmp)
        k.barrier()


def emit_mix_gmlp(P, li, dst):
    k = P.k
    nc = k.nc
    j = li // 4
    w_in = P.inp("sg_w_in", [1, D, 2 * D])
    vg_d = P.inp("sg_v_norm_g", [1, D])
    vb_d = P.inp("sg_v_norm_b", [1, D])
    ws_d = P.inp("sg_w_s", [1, 8, 128, 128])
    bs_d = P.inp("sg_b_s", [1, 8, 128])
    wout = P.inp("sg_w_out", [1, D, D])
    with ExitStack() as st:
        uT_t = k.sb(st, "uT", [128, KC, S], BF16)
        uT = [[Buf(uT_t[:, c, tb * 512:(tb + 1) * 512]) for tb in range(4)] for c in range(KC)]
        vln_t = k.sb(st, "vln", [128, NT, D], BF16)
        vln = [Buf(vln_t[:, t, :]) for t in range(NT)]
        wsT = Buf(k.sb(st, "wsT", [128, 8, 128], BF16)[:, :, :], "wsT")
        bs4 = Buf(k.sb(st, "bs4", [1, 8, 512], BF16)[:, :, :], "bs4")
        with ExitStack() as st2:
            win = load_w_bf16(P, st2, "w_in_sb", View(w_in.ap[j], w_in), KC, 2 * D)
            ws_sb = Buf(k.sb(st2, "ws_sb", [128, 8, 128], BF16)[:, :, :])
            k.dma("pool", ws_sb.v(), View(ws_d.ap[j].rearrange("g i j -> i g j"), ws_d))
            for g in range(8):
                k.tr(P.ps_tr[:, g * 128:(g + 1) * 128], ws_sb[:, g, :], P.ident.v())
            k.copy("dve", wsT.v(), View(P.ps_tr.ap.rearrange("p (k c) -> p k c", c=128), P.ps_tr))
            k.memset("pool", wsT[64:128, :, 0:64], 0.0)
            bs_f = Buf(k.sb(st2, "bs_f", [1, 8, 128], F32)[:, :, :])
            k.dma("sp", bs_f.v(), View(bs_d.ap[j:j + 1], bs_d))
            for r in range(4):
                k.copy("dve", bs4[:, :, r * 128:(r + 1) * 128], bs_f.v())
            vg, vb = load_ln_params(P, st2, View(vg_d.ap[j:j + 1, :], vg_d), View(vb_d.ap[j:j + 1, :], vb_d), "sg_ln")
            c2 = 0
            for c in range(KC):
                for tb in range(4):
                    pp = P.ps[c2 % 4]
                    c2 += 1
                    for kc in range(KC):
                        k.mm(pp.v(), win[:, kc, c * 128:(c + 1) * 128], P.hT[tb][:, kc, :], start=(kc == 0), stop=(kc == KC - 1))
                    k.act(uT[c][tb].v(), pp.v(), AF.Gelu_apprx_tanh)
            vt = [Buf(k.sb(st2, f"sg_v{i}", [128, D], F32)[:, :]) for i in range(2)]
            stats = Buf(k.sb(st2, "sg_stats", [128, 12], F32)[:, :])
            mv = Buf(k.sb(st2, "sg_mv", [128, 2], F32)[:, :])
            sc = Buf(k.sb(st2, "sg_sc", [128, 4], F32)[:, :])
            eps = Buf(k.sb(st2, "sg_eps", [128, 1], F32)[:, :])
            k.memset("pool", eps.v(), LN_EPS)
            for t in range(NT):
                tb, tt_ = t // 4, t % 4
                v = vt[t % 2]
                for hf in range(2):
                    pp = P.ps[4 + c2 % 3]
                    c2 += 1
                    for kc in range(KC):
                        k.mm(pp.v(), P.hT[tb][:, kc, tt_ * 128:(tt_ + 1) * 128], win[:, kc, D + hf * 512:D + (hf + 1) * 512],
                             start=(kc == 0), stop=(kc == KC - 1))
                    k.act(v[:, hf * 512:(hf + 1) * 512], pp.v(), AF.Gelu_apprx_tanh)
                    k.op("dve", lambda hf=hf, v=v: nc.vector.bn_stats(out=stats.ap[:, hf * 6:(hf + 1) * 6], in_=v.ap[:, hf * 512:(hf + 1) * 512]),
                         [v.v()], [stats.v()])
                k.op("dve", lambda: nc.vector.bn_aggr(out=mv.ap, in_=stats.ap), [stats.v()], [mv.v()])
                k.act(sc[:, 0:1], mv[:, 1:2], AF.Sqrt, bias=eps[:, 0:1])
                k.recip(sc[:, 1:2], sc[:, 0:1])
                k.stt("dve", sc[:, 2:3], mv[:, 0:1], -1.0, sc[:, 1:2], ALU.mult, ALU.mult)
                k.act(v.v(), v.v(), AF.Identity, scale=sc[:, 1:2], bias=sc[:, 2:3])
                k.tt("pool", v.v(), v.v(), vg.v(), ALU.mult)
                k.tt("pool", vln[t].v(), v.v(), vb.v(), ALU.add)
            k.barrier()
        c3 = 0
        for g in range(8):
            for tb in range(4):
                pp = P.ps[c3 % 4]
                c3 += 1
                k.mm(pp.v(), P.ones[0:1, :], bs4[0:1, g, :], start=True, stop=False)
                for r in range(4):
                    t = tb * 4 + r
                    k.mm(pp[:, r * 128:(r + 1) * 128], vln[t][:, g * 128:(g + 1) * 128], wsT[:, g, :], start=False, stop=(r == 3))
                k.tt("dve", uT[g][tb].v(), uT[g][tb].v(), pp.v(), ALU.mult)
        sT = [Buf(uT_t[:, c, :]) for c in range(KC)]
        k.barrier()
        emit_mix_out(P, li, sT, View(wout.ap[j], wout), dst)


class Rot:
    def __init__(self, n):
        self.n = n
        self.i = 0

    def nxt(self):
        v = self.i % self.n
        self.i += 1
        return v


class AttnPipe:
    def __init__(self, depth=2):
        self.depth = depth
        self.q = []
        self.deferred = []

    def push(self, cfn, after=None):
        self.q.append((cfn, after))
        ready = [fn for (n, fn) in self.deferred if n <= 1]
        self.deferred = [(n - 1, fn) for (n, fn) in self.deferred if n > 1]
        for fn in ready:
            fn()
        while len(self.q) > self.depth:
            self._pop()

    def _pop(self):
        cfn, after = self.q.pop(0)
        cfn()
        if after is not None:
            after()

    def defer(self, n, fn):
        self.deferred.append((n, fn))

    def flush(self):
        while self.q:
            self._pop()
        while self.deferred:
            d = self.deferred
            self.deferred = []
            for (_, fn) in d:
                fn()


def run_blocks(P, pipe, blocks, q_of, k_of, v_of, ones_v, psO, psD, pts, rs, rp, scale, bias_of=None, after=None):
    k = P.k
    n = len(blocks)
    for bi, (kb, c0, N, zero, extra) in enumerate(blocks):
        pS = P.ps[rs.nxt()]
        k.mm(pS[:, 0:N], k_of(kb), q_of(c0, N), start=True, stop=(bias_of is None))
        if bias_of is not None:
            k.mm(pS[:, 0:N], P.ident.v(), bias_of(extra, N), start=False, stop=True)
        pt = pts[rp.nxt()]
        k.act(pt[:, 0:N], pS[:, 0:N], AF.Exp, scale=scale)
        if zero is not None:
            (r0, r1, z0, z1) = zero
            k.memset("pool", pt[r0:r1, z0:z1], 0.0)

        def cfn(o=psO[:, c0:c0 + N], dn=psD[:, c0:c0 + N], vv=v_of(kb), pv=pt[:, 0:N], st_=(bi == 0), sp_=(bi == n - 1)):
            k.mm(o, vv, pv, start=st_, stop=sp_)
            k.mm(dn, ones_v, pv, start=st_, stop=sp_)

        pipe.push(cfn, after if bi == n - 1 else None)


def causal_blocks(qb):
    bl = []
    for kb in range(4 * qb + 4):
        r = kb - 4 * qb
        if r <= 0:
            bl.append((kb, 0, 512, (64, 128, 0, 64) if r == 0 else None, None))
        else:
            bl.append((kb, 128 * r, 512 - 128 * r, (64, 128, 0, 64), None))
    return bl


def emit_mix_diff(P, li, dst):
    k = P.k
    nc = k.nc
    j = li // 4
    lam_init = 0.8 - 0.6 * math.exp(-0.3 * li)
    wq = P.inp("diff_wq", [1, D, D])
    wk = P.inp("diff_wk", [1, D, D])
    wv = P.inp("diff_wv", [1, D, D])
    wo = P.inp("diff_wo", [1, D, D])
    lqk = [P.inp(n, [1, 64]) for n in ("diff_lq1", "diff_lk1", "diff_lq2", "diff_lk2")]
    subg = P.inp("diff_sub_g", [1, 128])
    scale = 64 ** -0.5
    with ExitStack() as st:
        oT_t = k.sb(st, "oT", [128, KC, S], BF16)
        oTb = [[Buf(oT_t[:, c, qb * 512:(qb + 1) * 512]) for qb in range(4)] for c in range(KC)]
        V_t = k.sb(st, "Vall", [128, NT, D], BF16)
        V = [Buf(V_t[:, t, :]) for t in range(NT)]
        nlam = Buf(k.sb(st, "nlam", [128, 1], F32)[:, :])
        gsc = Buf(k.sb(st, "gsc", [128, 1], F32)[:, :])
        eps5 = Buf(k.sb(st, "eps5", [128, 1], F32)[:, :])
        k.memset("pool", eps5.v(), 1e-5)
        with ExitStack() as st2:
            lt = Buf(k.sb(st2, "lqk", [128, 4, 64], F32)[:, :, :])
            for i in range(4):
                k.dma("sp", lt[:, i, :], View(lqk[i].ap[j:j + 1, :].partition_broadcast(128), lqk[i]))
            junk = Buf(k.sb(st2, "ljunk", [128, 64], F32)[:, :])
            acc = Buf(k.sb(st2, "lacc", [128, 4], F32)[:, :])
            k.stt("dve", junk.v(), lt[:, 0, :], 1.0, lt[:, 1, :], ALU.mult, ALU.mult, accum=acc[:, 0:1])
            k.stt("dve", junk.v(), lt[:, 2, :], 1.0, lt[:, 3, :], ALU.mult, ALU.mult, accum=acc[:, 1:2])
            k.act(acc[:, 2:3], acc[:, 0:1], AF.Exp)
            k.act(acc[:, 3:4], acc[:, 1:2], AF.Exp)
            k.tt("dve", nlam.v(), acc[:, 3:4], acc[:, 2:3], ALU.subtract)
            k.ts("dve", nlam.v(), nlam.v(), -lam_init, ALU.add)
            k.dma("sp", gsc.v(), View(subg.ap[j:j + 1, :].rearrange("o d -> d o"), subg))
            k.ts("dve", gsc.v(), gsc.v(), 1.0 - lam_init, ALU.mult)
            wv_sb = load_w_bf16(P, st2, "wv_sb", View(wv.ap[j], wv), KC, D)
            c = 0
            for t in range(NT):
                tb, tt_ = t // 4, t % 4
                for hf in range(2):
                    pp = P.ps[c % 4]
                    c += 1
                    for kc in range(KC):
                        k.mm(pp.v(), P.hT[tb][:, kc, tt_ * 128:(tt_ + 1) * 128], wv_sb[:, kc, hf * 512:(hf + 1) * 512],
                             start=(kc == 0), stop=(kc == KC - 1))
                    k.copy("act", V[t][:, hf * 512:(hf + 1) * 512], pp.v())
            k.barrier()
        with ExitStack() as st3:
            wqh = [Buf(k.sb(st3, f"wqh{i}", [128, KC, 128], BF16)[:, :, :]) for i in range(2)]
            wkh = [Buf(k.sb(st3, f"wkh{i}", [128, KC, 128], BF16)[:, :, :]) for i in range(2)]
            QT_t = [k.sb(st3, f"QT{i}", [128, S], BF16) for i in range(2)]
            KT_t = [k.sb(st3, f"KT{i}", [128, S], BF16) for i in range(2)]
            QT = [[Buf(QT_t[i][:, tb * 512:(tb + 1) * 512]) for tb in range(4)] for i in range(2)]
            KT = [[Buf(KT_t[i][:, tb * 512:(tb + 1) * 512]) for tb in range(4)] for i in range(2)]
            pts = [Buf(k.sb(st3, f"pt{i}", [128, 512], BF16)[:, :]) for i in range(4)]
            rd = Buf(k.sb(st3, "f_rd", [128, 512], F32)[:, :])
            o0 = Buf(k.sb(st3, "f_o0", [128, 512], F32)[:, :])
            o1 = Buf(k.sb(st3, "f_o1", [128, 512], F32)[:, :])
            sq = Buf(k.sb(st3, "f_sq", [128, 512], BF16)[:, :])
            rs, rp = Rot(3), Rot(4)

            def load_head(h):
                s_ = h % 2
                k.dma("pool", wqh[s_].v(), View(wq.ap[j].rearrange("(kc p) f -> p kc f", p=128)[:, :, h * 128:(h + 1) * 128], wq))
                k.dma("pool", wkh[s_].v(), View(wk.ap[j].rearrange("(kc p) f -> p kc f", p=128)[:, :, h * 128:(h + 1) * 128], wk))

            def proj_head(h):
                s_ = h % 2
                for (W, T) in ((wqh[s_], QT[s_]), (wkh[s_], KT[s_])):
                    for tb in range(4):
                        pp = P.ps[rs.nxt()]
                        for kc in range(KC):
                            k.mm(pp.v(), W[:, kc, :], P.hT[tb][:, kc, :], start=(kc == 0), stop=(kc == KC - 1))
                        k.copy("dve", T[tb].v(), pp.v())

            pipe = AttnPipe(2)
            unit = [0]
            o0b = [Buf(k.sb(st3, f"f_o0b{i}", [128, 512], F32)[:, :]) for i in range(2)]
            sqb = [Buf(k.sb(st3, f"f_sqb{i}", [128, 512], BF16)[:, :]) for i in range(2)]
            rdb = [Buf(k.sb(st3, f"f_rdb{i}", [128, 512], F32)[:, :]) for i in range(2)]

            def attn_head(h):
                s_ = h % 2
                for qb in range(4):
                    bl = causal_blocks(qb)
                    par = (h * 4 + qb) % 2
                    for m in range(2):
                        r0, r1 = m * 64, (m + 1) * 64
                        u = unit[0] % 2
                        unit[0] += 1
                        psO, psD = P.ps[3 + 2 * u], P.ps[4 + 2 * u]

                        def fin(m=m, psO=psO, psD=psD, par=par, h=h, qb=qb):
                            if m == 0:
                                k.recip(rd.v(), psD.v())
                                k.tt("dve", o0b[par].v(), psO.v(), rd.v(), ALU.mult)
                            else:
                                k.recip(rd.v(), psD.v())
                                k.tt("dve", o1.v(), psO.v(), rd.v(), ALU.mult)
                                k.stt("dve", o0b[par].v(), o1.v(), nlam[:, 0:1], o0b[par].v(), ALU.mult, ALU.add)
                                k.tt("dve", sqb[par].v(), o0b[par].v(), o0b[par].v(), ALU.mult)

                                def fin2():
                                    pS = P.ps[rs.nxt()]
                                    k.mm(pS.v(), P.ones.v(), sqb[par].v(), start=True, stop=True)
                                    k.act(rdb[par].v(), pS.v(), AF.Ln, scale=1.0 / 128.0, bias=eps5[:, 0:1])
                                    k.act(rdb[par].v(), rdb[par].v(), AF.Exp, scale=-0.5)
                                    k.stt("dve", oTb[h][qb].v(), o0b[par].v(), gsc[:, 0:1], rdb[par].v(), ALU.mult, ALU.mult)

                                pipe.defer(4, fin2)

                        run_blocks(P, pipe, bl,
                                   q_of=lambda c0, N: QT[s_][qb][r0:r1, c0:c0 + N],
                                   k_of=lambda kb: KT[s_][kb // 4][r0:r1, (kb % 4) * 128:(kb % 4 + 1) * 128],
                                   v_of=lambda kb: V[kb][:, h * 128:(h + 1) * 128],
                                   ones_v=P.ones.v(), psO=psO, psD=psD, pts=pts, rs=rs, rp=rp, scale=scale, after=fin)

            load_head(0)
            load_head(1)
            proj_head(0)
            for h in range(8):
                if h + 1 < 8:
                    proj_head(h + 1)
                if h + 2 < 8:
                    load_head(h + 2)
                attn_head(h)
            pipe.flush()
            k.barrier()
        oT = [Buf(oT_t[:, c, :]) for c in range(KC)]
        emit_mix_out(P, li, oT, View(wo.ap[j], wo), dst)


def band_blocks(qb):
    bl = []
    for r in (4, 5, 6, 7, 3, 2, 1, 0):
        if r >= 4:
            rp_ = r - 4
            bl.append((4 * qb + rp_, 128 * rp_, 512 - 128 * rp_, (64, 128, 0, 64), 0))
        elif qb > 0:
            N = 128 * (r + 1)
            bl.append((4 * qb - 4 + r, 0, N, (0, 64, N - 64, N), 512 - 128 * r))
    return bl


def emit_mix_band(P, li, dst):
    k = P.k
    j = li // 4
    wqkv = P.inp("ca_w_qkv", [1, D, 3 * D])
    bt_d = P.inp("ca_bt", [16, 128, 640])
    wo = P.inp("ca_wo", [1, D, D])
    scale = 64 ** -0.5
    with ExitStack() as st:
        oT_t = k.sb(st, "oT", [128, KC, S], BF16)
        oTb = [[Buf(oT_t[:, c, qb * 512:(qb + 1) * 512]) for qb in range(4)] for c in range(KC)]
        V_t = k.sb(st, "Vall", [128, NT, D], BF16)
        V = [Buf(V_t[:, t, :]) for t in range(NT)]
        BT = Buf(k.sb(st, "BT", [128, 16, 640], BF16)[:, :, :], "BT")
        k.dma("pool", BT.v(), View(bt_d.ap.rearrange("h p x -> p h x"), bt_d))
        k.ts("pool", BT.v(), BT.v(), 1.0 / scale, ALU.mult)
        with ExitStack() as st2:
            wv_sb = load_w_bf16(P, st2, "wv_sb", View(wqkv.ap[j], wqkv), KC, D, col0=2 * D)
            c = 0
            for t in range(NT):
                tb, tt_ = t // 4, t % 4
                for hf in range(2):
                    pp = P.ps[c % 4]
                    c += 1
                    for kc in range(KC):
                        k.mm(pp.v(), P.hT[tb][:, kc, tt_ * 128:(tt_ + 1) * 128], wv_sb[:, kc, hf * 512:(hf + 1) * 512],
                             start=(kc == 0), stop=(kc == KC - 1))
                    k.copy("act", V[t][:, hf * 512:(hf + 1) * 512], pp.v())
            k.barrier()
        with ExitStack() as st3:
            wqh = [Buf(k.sb(st3, f"wqh{i}", [128, KC, 128], BF16)[:, :, :]) for i in range(2)]
            wkh = [Buf(k.sb(st3, f"wkh{i}", [128, KC, 128], BF16)[:, :, :]) for i in range(2)]
            QT_t = [k.sb(st3, f"QT{i}", [128, S], BF16) for i in range(2)]
            KT_t = [k.sb(st3, f"KT{i}", [128, S], BF16) for i in range(2)]
            QT = [[Buf(QT_t[i][:, tb * 512:(tb + 1) * 512]) for tb in range(4)] for i in range(2)]
            KT = [[Buf(KT_t[i][:, tb * 512:(tb + 1) * 512]) for tb in range(4)] for i in range(2)]
            pts = [Buf(k.sb(st3, f"pt{i}", [128, 512], BF16)[:, :]) for i in range(4)]
            rd = [Buf(k.sb(st3, f"f_rd{i}", [128, 512], F32)[:, :]) for i in range(2)]
            rs, rp = Rot(3), Rot(4)
            wq_v = View(wqkv.ap[j].rearrange("(kc p) f -> p kc f", p=128), wqkv)

            def load_pair(p):
                s_ = p % 2
                k.dma("pool", wqh[s_].v(), wq_v[:, :, p * 128:(p + 1) * 128])
                k.dma("pool", wkh[s_].v(), wq_v[:, :, D + p * 128:D + (p + 1) * 128])

            def proj_pair(p):
                s_ = p % 2
                for (W, T) in ((wqh[s_], QT[s_]), (wkh[s_], KT[s_])):
                    for tb in range(4):
                        pp = P.ps[rs.nxt()]
                        for kc in range(KC):
                            k.mm(pp.v(), W[:, kc, :], P.hT[tb][:, kc, :], start=(kc == 0), stop=(kc == KC - 1))
                        k.copy("dve", T[tb].v(), pp.v())

            unit = [0]
            pipe = AttnPipe(2)

            def attn_pair(p):
                s_ = p % 2
                for hh in range(2):
                    h = 2 * p + hh
                    r0, r1 = hh * 64, (hh + 1) * 64
                    for qb in range(4):
                        u = unit[0] % 2
                        unit[0] += 1
                        psO, psD = P.ps[3 + 2 * u], P.ps[4 + 2 * u]
                        def fin(u=u, psO=psO, psD=psD, r0=r0, r1=r1, p=p, qb=qb):
                            k.recip(rd[u][r0:r1, :], psD[r0:r1, :])
                            k.tt("dve", oTb[p][qb][r0:r1, :], psO[r0:r1, :], rd[u][r0:r1, :], ALU.mult)

                        run_blocks(P, pipe, band_blocks(qb),
                                   q_of=lambda c0, N: QT[s_][qb][r0:r1, c0:c0 + N],
                                   k_of=lambda kb: KT[s_][kb // 4][r0:r1, (kb % 4) * 128:(kb % 4 + 1) * 128],
                                   v_of=lambda kb: V[kb][:, p * 128:(p + 1) * 128],
                                   ones_v=P.ones.v(), psO=psO, psD=psD, pts=pts, rs=rs, rp=rp, scale=scale,
                                   bias_of=lambda off, N: BT[:, h, off:off + N], after=fin)

            load_pair(0)
            load_pair(1)
            proj_pair(0)
            for p in range(8):
                if p + 1 < 8:
                    proj_pair(p + 1)
                if p + 2 < 8:
                    load_pair(p + 2)
                attn_pair(p)
            pipe.flush()
            k.barrier()
        oT = [Buf(oT_t[:, c, :]) for c in range(KC)]
        emit_mix_out(P, li, oT, View(wo.ap[j], wo), dst)


MLA_STAGE = [9]


def emit_mix_mla(P, li, dst):
    k = P.k
    nc = k.nc
    j = li // 4
    QR, KVR, NH = 384, 256, 16
    w_dq = P.inp("mla_w_dq", [1, D, QR])
    qg_d = P.inp("mla_q_norm_g", [1, QR])
    w_uq = P.inp("mla_w_uq", [1, QR, NH * 96])
    w_dkv = P.inp("mla_w_dkv", [1, D, KVR + 32])
    kvg_d = P.inp("mla_kv_norm_g", [1, KVR])
    w_ukv = P.inp("mla_w_ukv", [1, KVR, NH * 128])
    wo = P.inp("mla_wo", [1, D, D])
    cs_d = P.inp("rope_cs", [2, 32, S])
    scale = 96 ** -0.5
    with ExitStack() as st:
        oT_t = k.sb(st, "oT", [128, KC, S], BF16)
        oTb = [[Buf(oT_t[:, c, qb * 512:(qb + 1) * 512]) for qb in range(4)] for c in range(KC)]
        V_t = k.sb(st, "Vall", [128, NT, D], BF16)
        V = [Buf(V_t[:, t, :]) for t in range(NT)]
        cqT_t = k.sb(st, "cqT", [128, 3, S], BF16)
        cqT = [Buf(cqT_t[:, :, tb * 512:(tb + 1) * 512]) for tb in range(4)]
        ckvT_t = k.sb(st, "ckvT", [128, 2, S], BF16)
        ckvT = [Buf(ckvT_t[:, :, tb * 512:(tb + 1) * 512]) for tb in range(4)]
        cs = Buf(k.sb(st, "cs", [128, 2, S], F32)[:, :, :], "cs")
        k.dma("sp", cs[64:96, :, :], View(cs_d.ap.rearrange("c p s -> p c s"), cs_d))
        wuq = Buf(k.sb(st, "wuq", [128, 3, NH * 96 + 32], BF16)[:, :, :], "wuq")
        wuqR = Buf(k.sb(st, "wuqR", [128, 3, NH * 96 + 32], BF16)[:, :, :], "wuqR")
        wkn = Buf(k.sb(st, "wkn", [128, 2, NH * 64 + 64], BF16)[:, :, :], "wkn")
        k.memset("pool", wuq.v(), 0.0)
        k.memset("pool", wkn.v(), 0.0)
        KR_t = k.sb(st, "KR", [128, S], BF16)
        KR = [Buf(KR_t[:, tb * 512:(tb + 1) * 512]) for tb in range(4)]
        eps6 = Buf(k.sb(st, "eps6", [128, 1], F32)[:, :])
        k.memset("pool", eps6.v(), 1e-6)
        with ExitStack() as st2:
            wdq = load_w_bf16(P, st2, "wdq", View(w_dq.ap[j], w_dq), KC, QR)
            wdkc = load_w_bf16(P, st2, "wdkc", View(w_dkv.ap[j], w_dkv), KC, KVR)
            wvv = Buf(k.sb(st2, "wvv", [128, 2, NH * 64], BF16)[:, :, :], "wvv")
            wkr = Buf(k.sb(st2, "wkr", [128, KC, 128], BF16)[:, :, :], "wkr")
            wkrR = Buf(k.sb(st2, "wkrR", [128, KC, 128], BF16)[:, :, :], "wkrR")
            wkst = Buf(k.sb(st2, "wkst", [128, KC, 32], F32)[:, :, :], "wkst")
            dkv_v = View(w_dkv.ap[j].rearrange("(kc p) f -> p kc f", p=128), w_dkv)
            k.memset("pool", wkr.v(), 0.0)
            k.memset("pool", wkrR.v(), 0.0)
            k.dma("sp", wkst.v(), dkv_v[:, :, KVR:KVR + 32])
            k.copy("pool", wkr[:, :, 64:96], wkst.v())
            k.ts("pool", wkrR[:, :, 64:80], wkst[:, :, 16:32], -1.0, ALU.mult)
            k.copy("pool", wkrR[:, :, 80:96], wkst[:, :, 0:16])
            gq = Buf(k.sb(st2, "gq", [128, 3], F32)[:, :])
            gkv = Buf(k.sb(st2, "gkv", [128, 2], F32)[:, :])
            for kc in range(3):
                k.dma("sp", gq[:, kc:kc + 1], View(qg_d.ap[j:j + 1, kc * 128:(kc + 1) * 128].rearrange("o d -> d o"), qg_d))
            for kc in range(2):
                k.dma("sp", gkv[:, kc:kc + 1], View(kvg_d.ap[j:j + 1, kc * 128:(kc + 1) * 128].rearrange("o d -> d o"), kvg_d))
            with ExitStack() as st2a:
                stg = Buf(k.sb(st2a, "stg_uq", [128, 3, NH * 96], F32)[:, :, :])
                k.dma("sp", stg.v(), View(w_uq.ap[j].rearrange("(kc p) f -> p kc f", p=128), w_uq))
                for kc in range(3):
                    k.ts("pool", wuq[:, kc, 0:NH * 96], stg[:, kc, :], gq[:, kc:kc + 1], ALU.mult)
                k.memset("pool", wuqR.v(), 0.0)
                w4 = View(wuq.ap[:, :, 0:NH * 96].rearrange("p k (h c) -> p k h c", c=96), wuq)
                r4 = View(wuqR.ap[:, :, 0:NH * 96].rearrange("p k (h c) -> p k h c", c=96), wuqR)
                for kc in range(3):
                    k.ts("pool", r4[:, kc, :, 64:80], w4[:, kc, :, 80:96], -1.0, ALU.mult)
                    k.copy("pool", r4[:, kc, :, 80:96], w4[:, kc, :, 64:80])
                k.barrier()
            with ExitStack() as st2b:
                stg = Buf(k.sb(st2b, "stg_ukv", [128, 2, NH * 128], F32)[:, :, :])
                k.dma("sp", stg.v(), View(w_ukv.ap[j].rearrange("(kc p) f -> p kc f", p=128), w_ukv))
                s4 = View(stg.ap.rearrange("p k (h c) -> p k h c", c=128), stg)
                kn4 = View(wkn.ap[:, :, 0:NH * 64].rearrange("p k (h c) -> p k h c", c=64), wkn)
                vv4 = View(wvv.ap.rearrange("p k (h c) -> p k h c", c=64), wvv)
                for kc in range(2):
                    k.ts("pool", kn4[:, kc, :, :], s4[:, kc, :, 0:64], gkv[:, kc:kc + 1], ALU.mult)
                    k.ts("pool", vv4[:, kc, :, :], s4[:, kc, :, 64:128], gkv[:, kc:kc + 1], ALU.mult)
                k.barrier()
            junk = Buf(k.sb(st2, "mjunk", [128, QR], F32)[:, :])
            NT_A = NT if MLA_STAGE[0] >= 2 else 0
            cqn = [Buf(k.sb(st2, f"cqn{i}", [128, QR], BF16)[:, :]) for i in range(2)]
            cqf = [Buf(k.sb(st2, f"cqf{i}", [128, QR], F32)[:, :]) for i in range(2)]
            ckf = [Buf(k.sb(st2, f"ckf{i}", [128, KVR], F32)[:, :]) for i in range(2)]
            ckn = [Buf(k.sb(st2, f"ckn{i}", [128, KVR], BF16)[:, :]) for i in range(2)]
            sc = [Buf(k.sb(st2, f"msc{i}", [128, 8], F32)[:, :]) for i in range(2)]
            ptr3 = View(P.ps_tr.ap.rearrange("p (k c) -> p k c", c=128), P.ps_tr)
            for t in range(NT_A):
                tb, tt_ = t // 4, t % 4
                u = t % 2
                pq, pk = P.ps[2 * u], P.ps[2 * u + 1]
                for kc in range(KC):
                    k.mm(pq[:, 0:QR], P.hT[tb][:, kc, tt_ * 128:(tt_ + 1) * 128], wdq[:, kc, :], start=(kc == 0), stop=(kc == KC - 1))
                for kc in range(KC):
                    k.mm(pk[:, 0:KVR], P.hT[tb][:, kc, tt_ * 128:(tt_ + 1) * 128], wdkc[:, kc, :], start=(kc == 0), stop=(kc == KC - 1))
                k.copy("act", cqf[u].v(), pq[:, 0:QR])
                k.copy("act", ckf[u].v(), pk[:, 0:KVR])
                k.stt("dve", junk[:, 0:QR], cqf[u].v(), 1.0, cqf[u].v(), ALU.mult, ALU.mult, accum=sc[u][:, 0:1])
                k.stt("dve", junk[:, 0:KVR], ckf[u].v(), 1.0, ckf[u].v(), ALU.mult, ALU.mult, accum=sc[u][:, 1:2])
                k.act(sc[u][:, 2:3], sc[u][:, 0:1], AF.Sqrt, scale=1.0 / QR, bias=eps6[:, 0:1])
                k.act(sc[u][:, 3:4], sc[u][:, 1:2], AF.Sqrt, scale=1.0 / KVR, bias=eps6[:, 0:1])
                k.recip(sc[u][:, 4:6], sc[u][:, 2:4])
                k.ts("dve", cqn[u].v(), cqf[u].v(), sc[u][:, 4:5], ALU.mult)
                k.ts("dve", ckn[u].v(), ckf[u].v(), sc[u][:, 5:6], ALU.mult)
                for kc in range(3):
                    k.tr(P.ps_tr[:, kc * 128:(kc + 1) * 128], cqn[u][:, kc * 128:(kc + 1) * 128], P.ident.v())
                for kc in range(2):
                    k.tr(P.ps_tr[:, (3 + kc) * 128:(4 + kc) * 128], ckn[u][:, kc * 128:(kc + 1) * 128], P.ident.v())
                k.copy("dve", cqT[tb][:, :, tt_ * 128:(tt_ + 1) * 128], ptr3[:, 0:3, :])
                k.copy("dve", ckvT[tb][:, :, tt_ * 128:(tt_ + 1) * 128], ptr3[:, 3:5, :])
            ta = Buf(k.sb(st2, "rk_a", [128, 512], F32)[:, :])
            tb_ = Buf(k.sb(st2, "rk_b", [128, 512], F32)[:, :])
            for tb in range(4 if MLA_STAGE[0] >= 3 else 0):
                p1, p2 = P.ps[4], P.ps[5]
                for kc in range(KC):
                    k.mm(p1.v(), wkr[:, kc, :], P.hT[tb][:, kc, :], start=(kc == 0), stop=(kc == KC - 1))
                for kc in range(KC):
                    k.mm(p2.v(), wkrR[:, kc, :], P.hT[tb][:, kc, :], start=(kc == 0), stop=(kc == KC - 1))
                k.tt("dve", ta[64:96, :], p1[64:96, :], cs[64:96, 0, tb * 512:(tb + 1) * 512], ALU.mult)
                k.tt("dve", tb_[64:96, :], p2[64:96, :], cs[64:96, 1, tb * 512:(tb + 1) * 512], ALU.mult)
                k.tt("dve", KR[tb][64:96, :], ta[64:96, :], tb_[64:96, :], ALU.add)
            c = 0
            for t in range(NT if MLA_STAGE[0] >= 4 else 0):
                tb, tt_ = t // 4, t % 4
                for hf in range(2):
                    pp = P.ps[c % 4]
                    c += 1
                    for kc in range(2):
                        k.mm(pp.v(), ckvT[tb][:, kc, tt_ * 128:(tt_ + 1) * 128], wvv[:, kc, hf * 512:(hf + 1) * 512],
                             start=(kc == 0), stop=(kc == 1))
                    k.copy("act", V[t][:, hf * 512:(hf + 1) * 512], pp.v())
            k.barrier()
        with ExitStack() as st3:
            QT_t = [k.sb(st3, f"QT{i}", [128, S], BF16) for i in range(2)]
            KT_t = [k.sb(st3, f"KT{i}", [128, S], BF16) for i in range(2)]
            QT = [[Buf(QT_t[i][:, tb * 512:(tb + 1) * 512]) for tb in range(4)] for i in range(2)]
            KT = [[Buf(KT_t[i][:, tb * 512:(tb + 1) * 512]) for tb in range(4)] for i in range(2)]
            pts = [Buf(k.sb(st3, f"pt{i}", [128, 512], BF16)[:, :]) for i in range(4)]
            rd = [Buf(k.sb(st3, f"f_rd{i}", [128, 512], F32)[:, :]) for i in range(2)]
            ta = Buf(k.sb(st3, "rq_a", [128, 512], F32)[:, :])
            tb2 = Buf(k.sb(st3, "rq_b", [128, 512], F32)[:, :])
            rs, rp = Rot(3), Rot(4)
            for i in range(2):
                for tb in range(4):
                    k.memset("pool", QT[i][tb].v(), 0.0)
                    k.memset("pool", KT[i][tb].v(), 0.0)
                    k.copy("pool", KT[i][tb][64:96, :], KR[tb][64:96, :])

            def proj_head(h):
                s_ = h % 2
                for tb in range(4):
                    p1 = P.ps[rs.nxt()]
                    for kc in range(3):
                        k.mm(p1.v(), wuq[:, kc, h * 96:h * 96 + 128], cqT[tb][:, kc, :], start=(kc == 0), stop=(kc == 2))
                    p2 = P.ps[rs.nxt()]
                    for kc in range(3):
                        k.mm(p2.v(), wuqR[:, kc, h * 96:h * 96 + 128], cqT[tb][:, kc, :], start=(kc == 0), stop=(kc == 2))
                    k.copy("dve", QT[s_][tb][0:64, :], p1[0:64, :])
                    k.tt("dve", ta[64:96, :], p1[64:96, :], cs[64:96, 0, tb * 512:(tb + 1) * 512], ALU.mult)
                    k.tt("dve", tb2[64:96, :], p2[64:96, :], cs[64:96, 1, tb * 512:(tb + 1) * 512], ALU.mult)
                    k.tt("dve", QT[s_][tb][64:96, :], ta[64:96, :], tb2[64:96, :], ALU.add)
                    p3 = P.ps[rs.nxt()]
                    for kc in range(2):
                        k.mm(p3.v(), wkn[:, kc, h * 64:h * 64 + 128], ckvT[tb][:, kc, :], start=(kc == 0), stop=(kc == 1))
                    k.copy("dve", KT[s_][tb][0:64, :], p3[0:64, :])

            unit = [0]
            pipe = AttnPipe(2)

            def attn_head(h):
                s_ = h % 2
                p, hh = h // 2, h % 2
                r0, r1 = hh * 64, (hh + 1) * 64
                for qb in range(4):
                    u = unit[0] % 2
                    unit[0] += 1
                    psO, psD = P.ps[3 + 2 * u], P.ps[4 + 2 * u]
                    def fin(u=u, psO=psO, psD=psD, r0=r0, r1=r1, p=p, qb=qb):
                        k.recip(rd[u][r0:r1, :], psD[r0:r1, :])
                        k.tt("dve", oTb[p][qb][r0:r1, :], psO[r0:r1, :], rd[u][r0:r1, :], ALU.mult)

                    run_blocks(P, pipe, causal_blocks(qb),
                               q_of=lambda c0, N: QT[s_][qb][:, c0:c0 + N],
                               k_of=lambda kb: KT[s_][kb // 4][:, (kb % 4) * 128:(kb % 4 + 1) * 128],
                               v_of=lambda kb: V[kb][:, p * 128:(p + 1) * 128],
                               ones_v=P.ones.v(), psO=psO, psD=psD, pts=pts, rs=rs, rp=rp, scale=scale, after=fin)

            NHX = NH if MLA_STAGE[0] >= 6 else (1 if MLA_STAGE[0] >= 5 else 0)
            if NHX:
                proj_head(0)
            for h in range(NHX):
                if h + 1 < NHX:
                    proj_head(h + 1)
                if MLA_STAGE[0] != 5:
                    attn_head(h)
            pipe.flush()
            k.barrier()
        oT = [Buf(oT_t[:, c, :]) for c in range(KC)]
        emit_mix_out(P, li, oT, View(wo.ap[j], wo), dst)


def build(plan):
    k = K()
    P = Prog(k, plan)
    emit_consts(P)
    emit_prologue(P)
    n = len(plan)
    for i, name in enumerate(plan):
        dst = P.y if i == n - 1 else P.hres
        kind, li = name[:3], int(name[3:])
        if kind == "ffn":
            if li % 2 == 1 and SPARSE_MOE:
                emit_ffn_sparse(P, li, dst)
            else:
                emit_ffn(P, li, moe=(li % 2 == 1), dst=dst)
        elif kind == "mix":
            [emit_mix_diff, emit_mix_band, emit_mix_mla, emit_mix_gmlp][li % 4](P, li, dst)
        else:
            raise ValueError(name)
    k.final_wait()
    return k, P


def host_inputs(P, inputs):
    m = {}
    for name in P.ins:
        if name == "ca_bt":
            rb = np.asarray(inputs["ca_rel_bias"], np.float32)[0]
            idx = np.minimum(np.arange(640)[None, :] - np.arange(128)[:, None] + 128, 256)
            m[name] = np.ascontiguousarray(rb[:, idx])
        elif name == "rope_cs":
            half = 16
            inv_freq = (10000.0 ** (-np.arange(half, dtype=np.float32) / half)).astype(np.float32)
            ang = np.arange(S, dtype=np.float32)[None, :] * inv_freq[:, None]
            cos = np.concatenate([np.cos(ang), np.cos(ang)], axis=0)
            sin = np.concatenate([np.sin(ang), np.sin(ang)], axis=0)
            m[name] = np.ascontiguousarray(np.stack([cos, sin], axis=0).astype(np.float32))
        elif name in ("moe_w1r", "moe_w3r"):
            w = np.asarray(inputs["moe_w1" if name == "moe_w1r" else "moe_w3"], np.float32)
            w = w.reshape(2, NE, 2, 4, 128, NG, 512).transpose(0, 1, 5, 4, 2, 3, 6)
            m[name] = np.ascontiguousarray(w).reshape(2 * NE * NG * 128 * 2, WROW)
        elif name == "moe_w2r":
            w = np.asarray(inputs["moe_w2"], np.float32)
            w = w.reshape(2, NE, NG, 2, 2, 128, D).transpose(0, 1, 2, 5, 3, 4, 6)
            m[name] = np.ascontiguousarray(w).reshape(2 * NE * NG * 128 * 2, WROW)
        elif name == "tri_c":
            m[name] = np.triu(np.ones((128, 128), np.float32), 1)
        elif name == "iota2_c":
            m[name] = (2.0 * np.arange(128, dtype=np.float32)).reshape(128, 1)
        elif name == "moe_wrT":
            m[name] = np.ascontiguousarray(np.transpose(np.asarray(inputs["moe_w_router"], np.float32), (0, 2, 1)))
        else:
            m[name] = np.ascontiguousarray(np.asarray(inputs[name], np.float32))
    m["ident_c"] = np.eye(128, dtype=np.float32)
    return m


SPARSE_MOE = True
FULL_PLAN = ["mix0", "ffn0", "mix1", "ffn1", "mix2", "ffn2", "mix3", "ffn3"]


def run_plan(plan, inputs, xs, trace=False):
    k, P = build(plan)
    shared = host_inputs(P, inputs)
    in_maps = []
    for xc in xs:
        mcore = dict(shared)
        mcore["x"] = np.ascontiguousarray(xc, dtype=np.float32)
        in_maps.append(mcore)
    res = run_bass_kernel_spmd(k.nc, in_maps, core_ids=list(range(len(xs))), trace=trace)
    return [r["y"] for r in res.results], res


def kernel(**inputs):
    x = np.asarray(inputs["x"], np.float32)
    outs, _ = run_plan(FULL_PLAN, inputs, [x[b] for b in range(x.shape[0])])
    return np.stack(outs, axis=0).astype(np.float32)
```

```python
import math
from contextlib import ExitStack

import numpy as np
import concourse.bass as bass
import concourse.mybir as mybir
from concourse.bass_utils import run_bass_kernel_spmd

F32 = mybir.dt.float32
BF16 = mybir.dt.bfloat16
AF = mybir.ActivationFunctionType
ALU = mybir.AluOpType
AX = mybir.AxisListType

S = 2048
D = 1024
NT = S // 128
KC = D // 128
DEPTH = 4
ALPHA = (2 * DEPTH) ** 0.25
LN_EPS = 1e-5
D_FF = 2816
D_FFE = 3584
NE = 8
NDS = 16


class Buf:
    def __init__(self, ap, name=""):
        self.ap = ap
        self.w = None
        self.r = {}
        self.name = name

    def __getitem__(self, idx):
        return View(self.ap[idx], self)

    def v(self):
        return View(self.ap, self)


class View:
    def __init__(self, ap, buf):
        self.ap = ap
        self.buf = buf

    def __getitem__(self, idx):
        return View(self.ap[idx], self.buf)


def _ap(x):
    return x.ap if isinstance(x, View) else x


class K:
    def __init__(self):
        nc = bass.Bass("TRN2", target_bir_lowering=False)
        self.nc = nc
        self.eng = {"pe": nc.tensor, "act": nc.scalar, "dve": nc.vector, "pool": nc.gpsimd, "sp": nc.sync}
        self.sem = {e: nc.alloc_semaphore("s_" + e) for e in ("pe", "act", "dve", "pool")}
        self.cnt = {e: 0 for e in self.sem}
        self.dsem = [nc.alloc_semaphore(f"s_d{i}") for i in range(NDS)]
        self.dcnt = [0] * NDS
        self.dnext = 0
        self.dnext_pool = 0
        self.seen = {e: {} for e in self.eng}
        self.n_inst = 0

    def _semh(self, key):
        return self.sem[key] if isinstance(key, str) else self.dsem[key[1]]

    def _wait(self, e, key, val):
        if self.seen[e].get(key, 0) >= val:
            return
        self.eng[e].wait_ge(self._semh(key), val)
        self.seen[e][key] = val

    def _deps(self, e, reads, writes):
        deps = {}

        def add(t):
            if t is None:
                return
            k, v = t
            if deps.get(k, 0) < v:
                deps[k] = v

        for v in reads:
            add(v.buf.w)
        for v in writes:
            add(v.buf.w)
            for kk, val in v.buf.r.items():
                add((kk, val))
        for kk, val in deps.items():
            if kk == e and e == "pe":
                continue
            self._wait(e, kk, val)

    def _done(self, key, val, reads, writes):
        for v in writes:
            v.buf.w = (key, val)
            v.buf.r = {}
        for v in reads:
            if v.buf.r.get(key, 0) < val:
                v.buf.r[key] = val

    def op(self, e, fn, reads, writes):
        reads = [r for r in reads if isinstance(r, View)]
        self._deps(e, reads, writes)
        inst = fn()
        self.cnt[e] += 1
        inst.then_inc(self.sem[e], 1)
        self._done(e, self.cnt[e], reads, writes)
        self.n_inst += 1
        return inst

    def dma(self, q, out, in_, **kw):
        self._deps(q, [in_], [out])
        half = NDS // 2
        if q == "pool":
            i = half + self.dnext_pool
            self.dnext_pool = (self.dnext_pool + 1) % half
        else:
            i = self.dnext
            self.dnext = (self.dnext + 1) % half
        key = ("d", i)
        if self.dcnt[i] > 0:
            self._wait(q, key, 16 * self.dcnt[i])
        inst = self.eng[q].dma_start(out=out.ap, in_=in_.ap, **kw)
        self.dcnt[i] += 1
        inst.then_inc(self.dsem[i], 16)
        self._done(key, 16 * self.dcnt[i], [in_], [out])
        self.n_inst += 1

    def dma_indirect(self, out, out_off, in_, in_off):
        q = "pool"
        idx = in_off if in_off is not None else out_off
        self._deps(q, [in_, idx], [out])
        half = NDS // 2
        i = half + self.dnext_pool
        self.dnext_pool = (self.dnext_pool + 1) % half
        key = ("d", i)
        if self.dcnt[i] > 0:
            self._wait(q, key, 16 * self.dcnt[i])
        oo = bass.IndirectOffsetOnAxis(ap=out_off.ap, axis=0) if out_off is not None else None
        io = bass.IndirectOffsetOnAxis(ap=in_off.ap, axis=0) if in_off is not None else None
        inst = self.nc.gpsimd.indirect_dma_start(out=out.ap, out_offset=oo, in_=in_.ap, in_offset=io)
        self.dcnt[i] += 1
        inst.then_inc(self.dsem[i], 16)
        self._done(key, 16 * self.dcnt[i], [in_, idx], [out])
        self.n_inst += 1

    def barrier(self):
        keys = [(e, self.cnt[e]) for e in self.sem] + [(("d", i), 16 * self.dcnt[i]) for i in range(NDS)]
        for e in self.eng:
            for kk, val in keys:
                if val > 0 and kk != e:
                    self._wait(e, kk, val)

    def final_wait(self):
        for i in range(NDS):
            if self.dcnt[i] > 0:
                self._wait("sp", ("d", i), 16 * self.dcnt[i])

    def mm(self, out, lhsT, rhs, start=True, stop=True, **kw):
        return self.op("pe", lambda: self.nc.tensor.matmul(out.ap, lhsT.ap, rhs.ap, start=start, stop=stop, **kw),
                       [lhsT, rhs], [out])

    def tr(self, out, in_, ident):
        return self.op("pe", lambda: self.nc.tensor.transpose(out.ap, in_.ap, ident.ap), [in_, ident], [out])

    def act(self, out, in_, func, bias=None, scale=None, accum=None):
        kw = {}
        reads = [in_]
        writes = [out]
        if bias is not None:
            kw["bias"] = _ap(bias)
            reads.append(bias)
        if scale is not None:
            kw["scale"] = _ap(scale)
            reads.append(scale)
        if accum is not None:
            kw["accum_out"] = accum.ap
            writes.append(accum)
        return self.op("act", lambda: self.nc.scalar.activation(out=out.ap, in_=in_.ap, func=func, **kw), reads, writes)

    def tt(self, e, out, in0, in1, op):
        return self.op(e, lambda: self.eng[e].tensor_tensor(out=out.ap, in0=in0.ap, in1=in1.ap, op=op), [in0, in1], [out])

    def ts(self, e, out, in0, s1, op0, s2=None, op1=None, accum=None):
        kw = {}
        writes = [out]
        if op1 is not None:
            kw["op1"] = op1
        if accum is not None:
            kw["accum_out"] = accum.ap
            writes.append(accum)
        return self.op(e, lambda: self.eng[e].tensor_scalar(out=out.ap, in0=in0.ap, scalar1=_ap(s1), scalar2=_ap(s2), op0=op0, **kw),
                       [in0, s1, s2], writes)

    def stt(self, e, out, in0, scalar, in1, op0, op1, accum=None):
        kw = {}
        writes = [out]
        if accum is not None:
            kw["accum_out"] = accum.ap
            writes.append(accum)
        return self.op(e, lambda: self.eng[e].scalar_tensor_tensor(out=out.ap, in0=in0.ap, scalar=_ap(scalar), in1=in1.ap, op0=op0, op1=op1, **kw),
                       [in0, scalar, in1], writes)

    def copy(self, e, out, in_):
        if e == "act":
            return self.act(out, in_, AF.Copy)
        return self.op(e, lambda: self.eng[e].tensor_copy(out=out.ap, in_=in_.ap), [in_], [out])

    def memset(self, e, out, val):
        return self.op(e, lambda: self.eng[e].memset(out.ap, val), [], [out])

    def recip(self, out, in_):
        return self.op("dve", lambda: self.nc.vector.reciprocal(out=out.ap, in_=in_.ap), [in_], [out])

    def sb(self, st, name, shape, dtype):
        self.n_alloc = getattr(self, "n_alloc", 0) + 1
        return st.enter_context(self.nc.sbuf_tensor(f"{name}_{self.n_alloc}", list(shape), dtype))

    def dram_in(self, name, shape, dtype=F32):
        return Buf(self.nc.dram_tensor(name, list(shape), dtype, kind="ExternalInput").ap(), name)

    def dram_out(self, name, shape, dtype=F32):
        return Buf(self.nc.dram_tensor(name, list(shape), dtype, kind="ExternalOutput").ap(), name)

    def dram_tmp(self, name, shape, dtype=F32):
        return Buf(self.nc.dram_tensor(name, list(shape), dtype, kind="Internal").ap(), name)


class Prog:
    def __init__(self, k, plan):
        self.k = k
        nc = k.nc
        self.plan = plan
        self.st = ExitStack()
        st = self.st
        hT_t = k.sb(st, "hT", [128, KC, S], BF16)
        self.hT = [Buf(hT_t[:, :, tb * 512:(tb + 1) * 512], f"hT{tb}") for tb in range(4)]
        ident_t = k.sb(st, "ident", [128, 128], BF16)
        self.ident = Buf(ident_t[:, :], "ident")
        ones_t = k.sb(st, "ones", [128, 128], BF16)
        self.ones = Buf(ones_t[:, :], "ones")
        small_t = k.sb(st, "small", [128, 64], F32)
        self.small_t = small_t
        self.ps = [Buf(nc.alloc_psum_tensor(f"ps{i}", [128, 512], F32)[:, :], f"ps{i}") for i in range(7)]
        pst = nc.alloc_psum_tensor("pstr", [128, 1024], BF16)
        self.ps_tr = Buf(pst[:, :], "pstr")
        self.x = k.dram_in("x", [S, D])
        self.y = k.dram_out("y", [S, D])
        self.hres = k.dram_tmp("hres", [S, D])
        self.ident_d = k.dram_in("ident_c", [128, 128])
        self.ins = {}

    def inp(self, name, shape):
        if name not in self.ins:
            self.ins[name] = self.k.dram_in(name, shape)
        return self.ins[name]


def emit_consts(P):
    k = P.k
    k.dma("pool", P.ident.v(), P.ident_d.v())
    k.memset("pool", P.ones.v(), 1.0)


def emit_ln_tail(P, st, t, xs, gb, bb, dst, tmp):
    k = P.k
    ts_ = tmp["sets"][t % len(tmp["sets"])]
    stats, mv, sc, xn, xb = ts_["stats"], ts_["mv"], ts_["sc"], ts_["xn"], ts_["xb"]
    for hf in range(2):
        k.op("dve", lambda hf=hf: k.nc.vector.bn_stats(out=stats.ap[:, hf * 6:(hf + 1) * 6], in_=xs.ap[:, hf * 512:(hf + 1) * 512]),
             [xs.v()], [stats.v()])
    k.op("dve", lambda: k.nc.vector.bn_aggr(out=mv.ap, in_=stats.ap), [stats.v()], [mv.v()])
    k.act(sc[:, 0:1], mv[:, 1:2], AF.Sqrt, bias=tmp["eps"][:, 0:1])
    k.recip(sc[:, 1:2], sc[:, 0:1])
    k.stt("dve", sc[:, 2:3], mv[:, 0:1], -1.0, sc[:, 1:2], ALU.mult, ALU.mult)
    k.act(xn.v(), xs.v(), AF.Identity, scale=sc[:, 1:2], bias=sc[:, 2:3])
    k.tt("pool", xn.v(), xn.v(), gb.v(), ALU.mult)
    k.tt("pool", xn.v(), xn.v(), bb.v(), ALU.add)
    k.dma("sp", dst[t * 128:(t + 1) * 128, :], xn.v())
    k.copy("act", xb.v(), xn.v())

    def part_b(t=t, xb=xb):
        for kc in range(KC):
            k.tr(P.ps_tr[:, kc * 128:(kc + 1) * 128], xb[:, kc * 128:(kc + 1) * 128], P.ident.v())
        tb, tt_ = t // 4, t % 4
        k.copy("dve", P.hT[tb][:, :, tt_ * 128:(tt_ + 1) * 128],
               View(P.ps_tr.ap.rearrange("p (k c) -> p k c", c=128), P.ps_tr))

    tmp["pending"].append(part_b)
    while len(tmp["pending"]) > len(tmp["sets"]) - 1:
        tmp["pending"].pop(0)()
    return xn


def flush_ln_tail(P, tmp):
    while tmp["pending"]:
        tmp["pending"].pop(0)()


def alloc_ln_tmp(P, st, pfx, nset=4):
    k = P.k
    tmp = {"sets": [], "pending": []}
    for i in range(nset):
        d = {}
        d["stats"] = Buf(k.sb(st, pfx + f"stats{i}", [128, 12], F32)[:, :])
        d["mv"] = Buf(k.sb(st, pfx + f"mv{i}", [128, 2], F32)[:, :])
        d["sc"] = Buf(k.sb(st, pfx + f"sc{i}", [128, 4], F32)[:, :])
        d["xn"] = Buf(k.sb(st, pfx + f"xn{i}", [128, D], F32)[:, :])
        d["xb"] = Buf(k.sb(st, pfx + f"xb{i}", [128, D], BF16)[:, :])
        tmp["sets"].append(d)
    tmp["eps"] = Buf(k.sb(st, pfx + "eps", [128, 1], F32)[:, :])
    k.memset("pool", tmp["eps"].v(), LN_EPS)
    return tmp


def emit_prologue(P):
    k = P.k
    with ExitStack() as st:
        xin = [Buf(k.sb(st, f"pro_x{i}", [128, D], F32)[:, :]) for i in range(2)]
        xb = [Buf(k.sb(st, f"pro_xb{i}", [128, D], BF16)[:, :]) for i in range(2)]
        for t in range(NT):
            xi = xin[t % 2]
            k.dma("sp", xi.v(), P.x[t * 128:(t + 1) * 128, :])
            k.dma("sp", P.hres[t * 128:(t + 1) * 128, :], xi.v())
            k.copy("act", xb[t % 2].v(), xi.v())
            for kc in range(KC):
                k.tr(P.ps_tr[:, kc * 128:(kc + 1) * 128], xb[t % 2][:, kc * 128:(kc + 1) * 128], P.ident.v())
            tb, tt_ = t // 4, t % 4
            k.copy("dve", P.hT[tb][:, :, tt_ * 128:(tt_ + 1) * 128],
                   View(P.ps_tr.ap.rearrange("p (k c) -> p k c", c=128), P.ps_tr))
        k.barrier()


def load_ln_params(P, st, g_d, b_d, pfx):
    k = P.k
    gb = Buf(k.sb(st, pfx + "g", [128, D], F32)[:, :])
    bb = Buf(k.sb(st, pfx + "b", [128, D], F32)[:, :])
    k.dma("sp", gb.v(), View(g_d.ap.partition_broadcast(128), g_d.buf))
    k.dma("sp", bb.v(), View(b_d.ap.partition_broadcast(128), b_d.buf))
    return gb, bb


def emit_ffn(P, li, moe, dst):
    k = P.k
    nc = k.nc
    j = li // 2
    if moe:
        E, F = NE, D_FFE
        w1 = P.inp("moe_w1", [2, NE, D, D_FFE])
        w3 = P.inp("moe_w3", [2, NE, D, D_FFE])
        w2 = P.inp("moe_w2", [2, NE, D_FFE, D])
        wr = P.inp("moe_wrT", [2, NE, D])
        w1v = lambda e: View(w1.ap[j, e], w1)
        w3v = lambda e: View(w3.ap[j, e], w3)
        w2v = lambda e: View(w2.ap[j, e], w2)
    else:
        E, F = 1, D_FF
        w1 = P.inp("ffn_w1", [2, D, D_FF])
        w3 = P.inp("ffn_w3", [2, D, D_FF])
        w2 = P.inp("ffn_w2", [2, D_FF, D])
        w1v = lambda e: View(w1.ap[j], w1)
        w3v = lambda e: View(w3.ap[j], w3)
        w2v = lambda e: View(w2.ap[j], w2)
    g_d = P.inp("ln_ffn_g", [DEPTH, D])
    b_d = P.inp("ln_ffn_b", [DEPTH, D])
    nj = F // 128
    G = 4
    units = [(e, j0, min(G, nj - j0)) for e in range(E) for j0 in range(0, nj, G)]
    U = len(units)
    with ExitStack() as st:
        comb_t = k.sb(st, "comb", [128, NT, NE], F32)
        comb = Buf(comb_t[:, :, :], "comb")
        if moe:
            with ExitStack() as st2:
                wrb = Buf(k.sb(st2, "wrb", [128, NE, D], F32)[:, :, :], "wrb")
                k.dma("sp", wrb.v(), View(wr.ap[j].partition_broadcast(128), wr))
                hin = [Buf(k.sb(st2, f"rt_h{i}", [128, D], F32)[:, :]) for i in range(2)]
                junk = Buf(k.sb(st2, "rt_junk", [128, D], F32)[:, :])
                lg = Buf(k.sb(st2, "rt_lg", [128, NE], F32)[:, :])
                l2 = Buf(k.sb(st2, "rt_l2", [128, NE], F32)[:, :])
                m1 = Buf(k.sb(st2, "rt_m1", [128, NE], F32)[:, :])
                m2 = Buf(k.sb(st2, "rt_m2", [128, NE], F32)[:, :])
                sc = Buf(k.sb(st2, "rt_sc", [128, 8], F32)[:, :])
                for t in range(NT):
                    hi = hin[t % 2]
                    k.dma("sp", hi.v(), P.hres[t * 128:(t + 1) * 128, :])
                    for e in range(NE):
                        k.stt("dve", junk.v(), hi.v(), 1.0, wrb[:, e, :], ALU.mult, ALU.mult, accum=lg[:, e:e + 1])
                    k.op("dve", lambda: nc.vector.reduce_max(out=sc.ap[:, 0:1], in_=lg.ap, axis=AX.X), [lg.v()], [sc.v()])
                    k.ts("dve", m1.v(), lg.v(), sc[:, 0:1], ALU.is_equal)
                    k.stt("dve", l2.v(), m1.v(), -1e30, lg.v(), ALU.mult, ALU.add)
                    k.op("dve", lambda: nc.vector.reduce_max(out=sc.ap[:, 1:2], in_=l2.ap, axis=AX.X), [l2.v()], [sc.v()])
                    k.ts("dve", m2.v(), l2.v(), sc[:, 1:2], ALU.is_equal)
                    k.tt("dve", sc[:, 2:3], sc[:, 1:2], sc[:, 0:1], ALU.subtract)
                    k.act(sc[:, 3:4], sc[:, 2:3], AF.Exp)
                    k.ts("dve", sc[:, 4:5], sc[:, 3:4], 1.0, ALU.add)
                    k.recip(sc[:, 5:6], sc[:, 4:5])
                    k.tt("dve", sc[:, 6:7], sc[:, 3:4], sc[:, 5:6], ALU.mult)
                    k.ts("dve", m1.v(), m1.v(), sc[:, 5:6], ALU.mult)
                    k.stt("dve", comb[:, t, :], m2.v(), sc[:, 6:7], m1.v(), ALU.mult, ALU.add)
                k.barrier()
        yacc_t = k.sb(st, "yacc", [128, NT, D], F32)
        yacc = [[Buf(yacc_t[:, t, hf * 512:(hf + 1) * 512]) for hf in range(2)] for t in range(NT)]
        stc = ExitStack()
        htg_t = [k.sb(stc, f"htg{i}", [128, G, S], BF16) for i in range(2)]
        htg = [[[Buf(htg_t[i][:, jj, tb * 512:(tb + 1) * 512]) for tb in range(4)] for jj in range(G)] for i in range(2)]
        w1g = [Buf(k.sb(stc, f"w1g{i}", [128, KC, G * 128], BF16)[:, :, :]) for i in range(2)]
        w3g = [Buf(k.sb(stc, f"w3g{i}", [128, KC, G * 128], BF16)[:, :, :]) for i in range(2)]
        w2g = [Buf(k.sb(stc, f"w2g{i}", [128, G, D], BF16)[:, :, :]) for i in range(2)]
        sil = [Buf(k.sb(stc, f"sil{i}", [128, 512], F32)[:, :]) for i in range(2)]

        def loadA(u):
            e, j0, n = units[u]
            s = u % 2
            k.dma("pool", w1g[s][:, :, 0:n * 128],
                  View(w1v(e).ap.rearrange("(kc p) f -> p kc f", p=128)[:, :, j0 * 128:(j0 + n) * 128], w1))
            k.dma("pool", w3g[s][:, :, 0:n * 128],
                  View(w3v(e).ap.rearrange("(kc p) f -> p kc f", p=128)[:, :, j0 * 128:(j0 + n) * 128], w3))

        def loadB(u):
            e, j0, n = units[u]
            s = u % 2
            k.dma("pool", w2g[s][:, 0:n, :],
                  View(w2v(e).ap[j0 * 128:(j0 + n) * 128, :].rearrange("(j p) d -> p j d", p=128), w2))

        cnt1 = [0]

        def phase1(u):
            e, j0, n = units[u]
            s = u % 2
            for jj in range(n):
                for tb in range(4):
                    c = cnt1[0] % 2
                    cnt1[0] += 1
                    pa, pb = P.ps[2 * c], P.ps[2 * c + 1]
                    for (W, pp) in ((w1g[s], pa), (w3g[s], pb)):
                        for kc in range(KC):
                            k.mm(pp.v(), W[:, kc, jj * 128:(jj + 1) * 128], P.hT[tb][:, kc, :], start=(kc == 0), stop=(kc == KC - 1))
                    k.act(sil[c].v(), pa.v(), AF.Silu)
                    k.tt("dve", htg[s][jj][tb].v(), sil[c].v(), pb.v(), ALU.mult)

        cnt2 = [0]

        def phase2(u):
            e, j0, n = units[u]
            s = u % 2
            for t in range(NT):
                tb, tt_ = t // 4, t % 4
                for hf in range(2):
                    py = P.ps[4 + cnt2[0] % 3]
                    cnt2[0] += 1
                    for jj in range(n):
                        k.mm(py.v(), htg[s][jj][tb][:, tt_ * 128:(tt_ + 1) * 128], w2g[s][:, jj, hf * 512:(hf + 1) * 512],
                             start=(jj == 0), stop=(jj == n - 1))
                    ya = yacc[t][hf]
                    if moe:
                        if u == 0:
                            k.ts("dve", ya.v(), py.v(), comb[:, t, e:e + 1], ALU.mult)
                        else:
                            k.stt("dve", ya.v(), py.v(), comb[:, t, e:e + 1], ya.v(), ALU.mult, ALU.add)
                    else:
                        if u == 0:
                            k.copy("dve", ya.v(), py.v())
                        else:
                            k.tt("dve", ya.v(), py.v(), ya.v(), ALU.add)

        loadA(0)
        loadB(0)
        if U > 1:
            loadA(1)
            loadB(1)
        phase1(0)
        for u in range(U):
            if u + 1 < U:
                phase1(u + 1)
            if u + 2 < U:
                loadA(u + 2)
            phase2(u)
            if u + 2 < U:
                loadB(u + 2)
        k.barrier()
        stc.close()
        gb, bb = load_ln_params(P, st, View(g_d.ap[li:li + 1, :], g_d), View(b_d.ap[li:li + 1, :], b_d), "ffn_ln")
        tmp = alloc_ln_tmp(P, st, "ffn_")
        xin = [Buf(k.sb(st, f"ffn_xin{i}", [128, D], F32)[:, :]) for i in range(3)]
        k.dma("sp", xin[0].v(), P.hres[0:128, :])
        k.dma("sp", xin[1].v(), P.hres[128:256, :])
        for t in range(NT):
            if t + 2 < NT:
                k.dma("sp", xin[(t + 2) % 3].v(), P.hres[(t + 2) * 128:(t + 3) * 128, :])
            xi = xin[t % 3]
            for hf in range(2):
                k.stt("dve", xi[:, hf * 512:(hf + 1) * 512], xi[:, hf * 512:(hf + 1) * 512], ALPHA, yacc[t][hf].v(), ALU.mult, ALU.add)
            emit_ln_tail(P, st, t, xi, gb, bb, dst, tmp)
        flush_ln_tail(P, tmp)
        k.barrier()


NG = 7
NST = 15
WROW = 2048


def emit_ffn_sparse(P, li, dst):
    k = P.k
    nc = k.nc
    j = li // 2
    I32 = mybir.dt.int32
    nrows = 2 * NE * NG * 128 * 2
    w1r = P.inp("moe_w1r", [nrows, WROW])
    w3r = P.inp("moe_w3r", [nrows, WROW])
    w2r = P.inp("moe_w2r", [nrows, WROW])
    wr = P.inp("moe_wrT", [2, NE, D])
    tri_d = P.inp("tri_c", [128, 128])
    io_d = P.inp("iota2_c", [128, 1])
    g_d = P.inp("ln_ffn_g", [DEPTH, D])
    b_d = P.inp("ln_ffn_b", [DEPTH, D])
    if not hasattr(P, "xs_d"):
        P.xs_d = k.dram_tmp("xs_sorted", [NST * 512, D], BF16)
        P.ys_d = k.dram_tmp("ys_sorted", [NST * 512, D], F32)
    xs_d, ys_d = P.xs_d, P.ys_d
    with ExitStack() as st:
        gs = Buf(k.sb(st, "gs", [128, NT, 2], F32)[:, :, :], "gs")
        slot_i = Buf(k.sb(st, "slot_i", [128, NT, 2], I32)[:, :, :], "slot_i")
        idxw = Buf(k.sb(st, "idxw", [128, NST, NG, 2], I32)[:, :, :, :], "idxw")
        with ExitStack() as st2:
            wrb = Buf(k.sb(st2, "wrb", [128, NE, D], F32)[:, :, :], "wrb")
            k.dma("sp", wrb.v(), View(wr.ap[j].partition_broadcast(128), wr))
            tri = Buf(k.sb(st2, "tri", [128, 128], BF16)[:, :], "tri")
            k.dma("pool", tri.v(), tri_d.v())
            io2 = Buf(k.sb(st2, "io2", [128, 1], F32)[:, :], "io2")
            k.dma("sp", io2.v(), io_d.v())
            zt = Buf(k.sb(st2, "zt", [128, 4096], BF16)[:, :], "zt")
            k.memset("pool", zt.v(), 0.0)
            xs_flat = View(xs_d.ap.rearrange("(p r) d -> p (r d)", p=128), xs_d)
            for i in range(NST * 4 * D // 4096):
                k.dma("sp", xs_flat[:, i * 4096:(i + 1) * 4096], zt.v())
            m1s = Buf(k.sb(st2, "m1s", [128, NT, NE], F32)[:, :, :], "m1s")
            m2s = Buf(k.sb(st2, "m2s", [128, NT, NE], F32)[:, :, :], "m2s")
            abf = Buf(k.sb(st2, "abf", [128, NT, NE], BF16)[:, :, :], "abf")
            hin = [Buf(k.sb(st2, f"rt_h{i}", [128, D], F32)[:, :]) for i in range(2)]
            xbt_t = k.sb(st2, "rt_xb", [128, NT, D], BF16)
            xbt = [Buf(xbt_t[:, t, :]) for t in range(NT)]
            junk = Buf(k.sb(st2, "rt_junk", [128, D], F32)[:, :])
            lg = Buf(k.sb(st2, "rt_lg", [128, NE], F32)[:, :])
            l2 = Buf(k.sb(st2, "rt_l2", [128, NE], F32)[:, :])
            sc = Buf(k.sb(st2, "rt_sc", [128, 8], F32)[:, :])
            lga = Buf(k.sb(st2, "rt_lga", [128, NT, NE], F32)[:, :, :], "lga")
            l2a = Buf(k.sb(st2, "rt_l2a", [128, NT, NE], F32)[:, :, :], "l2a")
            mxa = Buf(k.sb(st2, "rt_mxa", [128, 4, NT], F32)[:, :, :], "mxa")
            for t in range(NT):
                hi = hin[t % 2]
                k.dma("sp", hi.v(), P.hres[t * 128:(t + 1) * 128, :])
                k.copy("act", xbt[t].v(), hi.v())
                for e in range(NE):
                    k.stt("dve", junk.v(), hi.v(), 1.0, wrb[:, e, :], ALU.mult, ALU.mult, accum=lga[:, t, e:e + 1])
            bc = lambda v: View(v.ap.unsqueeze(2).broadcast_to([128, NT, NE]), v.buf)
            k.op("dve", lambda: nc.vector.reduce_max(out=mxa.ap[:, 0, :], in_=lga.ap, axis=AX.X), [lga.v()], [mxa.v()])
            k.tt("dve", m1s.v(), lga.v(), bc(mxa[:, 0, :]), ALU.is_equal)
            k.stt("dve", l2a.v(), m1s.v(), -1e30, lga.v(), ALU.mult, ALU.add)
            k.op("dve", lambda: nc.vector.reduce_max(out=mxa.ap[:, 1, :], in_=l2a.ap, axis=AX.X), [l2a.v()], [mxa.v()])
            k.tt("dve", m2s.v(), l2a.v(), bc(mxa[:, 1, :]), ALU.is_equal)
            k.tt("dve", mxa[:, 2, :], mxa[:, 1, :], mxa[:, 0, :], ALU.subtract)
            k.act(mxa[:, 2, :], mxa[:, 2, :], AF.Exp)
            k.ts("dve", mxa[:, 3, :], mxa[:, 2, :], 1.0, ALU.add)
            k.recip(gs[:, :, 0], mxa[:, 3, :])
            k.tt("dve", gs[:, :, 1], mxa[:, 2, :], gs[:, :, 0], ALU.mult)
            k.tt("dve", abf.v(), m1s.v(), m2s.v(), ALU.add)
            pp = P.ps[0]
            for t in range(NT):
                for tp in range(t):
                    k.mm(pp[:, t * 8:(t + 1) * 8], P.ones.v(), abf[:, tp, :], start=(tp == 0), stop=False)
                k.mm(pp[:, t * 8:(t + 1) * 8], tri.v(), abf[:, t, :], start=(t == 0), stop=True)
            for t in range(NT):
                k.mm(pp[:, 128:136], P.ones.v(), abf[:, t, :], start=(t == 0), stop=(t == NT - 1))
            posf = Buf(k.sb(st2, "posf", [128, 136], F32)[:, :], "posf")
            k.copy("dve", posf.v(), pp[:, 0:136])
            w8 = Buf(k.sb(st2, "w8", [128, 8, 8], F32)[:, :, :], "w8")
            for m in range(4):
                k.ts("dve", w8[:, m, :], posf[:, 128:136], 512.0 * m, ALU.is_gt)
            k.tt("dve", w8[:, 0, :], w8[:, 0, :], w8[:, 1, :], ALU.add)
            k.tt("dve", w8[:, 2, :], w8[:, 2, :], w8[:, 3, :], ALU.add)
            k.tt("dve", w8[:, 0, :], w8[:, 0, :], w8[:, 2, :], ALU.add)
            k.ts("dve", w8[:, 4, :], w8[:, 0, :], 512.0, ALU.mult)
            k.memset("pool", w8[:, 5, :], 0.0)
            for e in range(1, NE):
                k.tt("dve", w8[:, 5, e:e + 1], w8[:, 5, e - 1:e], w8[:, 4, e - 1:e], ALU.add)
            k.tt("dve", w8[:, 6, :], w8[:, 5, :], w8[:, 4, :], ALU.add)
            sf = Buf(k.sb(st2, "sf", [128, NT, NE], F32)[:, :, :], "sf")
            slotf = Buf(k.sb(st2, "slotf", [128, 2, NT], F32)[:, :, :], "slotf")
            j8 = Buf(k.sb(st2, "j8", [128, NE], F32)[:, :], "j8")
            pos3 = View(posf.ap[:, 0:128].rearrange("p (t e) -> p t e", e=NE), posf)
            k.tt("dve", sf.v(), pos3, View(w8.ap[:, 5, :].unsqueeze(1).broadcast_to([128, NT, NE]), w8), ALU.add)
            k.tt("dve", l2a.v(), m1s.v(), sf.v(), ALU.mult)
            k.op("dve", lambda: nc.vector.reduce_sum(out=slotf.ap[:, 0, :], in_=l2a.ap, axis=AX.X), [l2a.v()], [slotf.v()])
            k.tt("dve", l2a.v(), m2s.v(), sf.v(), ALU.mult)
            k.op("dve", lambda: nc.vector.reduce_sum(out=slotf.ap[:, 1, :], in_=l2a.ap, axis=AX.X), [l2a.v()], [slotf.v()])
            k.copy("dve", slot_i[:, :, 0], slotf[:, 0, :])
            k.copy("dve", slot_i[:, :, 1], slotf[:, 1, :])
            esf = Buf(k.sb(st2, "esf", [128, NST], F32)[:, :], "esf")
            for s_ in range(NST):
                k.ts("dve", j8.v(), w8[:, 6, :], 512.0 * s_, ALU.is_le, s2=0.0, op1=ALU.add, accum=esf[:, s_:s_ + 1])
            k.ts("dve", esf.v(), esf.v(), float(NE - 1), ALU.min)
            k.ts("dve", esf.v(), esf.v(), float(NG * 128 * 2), ALU.mult, s2=io2[:, 0:1], op1=ALU.add)
            idxf = Buf(k.sb(st2, "idxf", [128, NST, NG, 2], F32)[:, :, :, :], "idxf")
            for g in range(NG):
                for hf in range(2):
                    k.ts("dve", idxf[:, :, g, hf], esf.v(), float(((j * NE) * NG + g) * 128 * 2 + hf), ALU.add)
            k.copy("dve", idxw.v(), idxf.v())
            k.barrier()
            for t in range(NT):
                for sl in range(2):
                    k.dma_indirect(out=xs_d.v(), out_off=slot_i[:, t, sl:sl + 1], in_=xbt[t].v(), in_off=None)
            k.barrier()
        with ExitStack() as st3:
            G = 4
            xsT = [Buf(k.sb(st3, f"xsT{i}", [128, KC, 512], BF16)[:, :, :]) for i in range(2)]
            xrow = [Buf(k.sb(st3, f"xrow{i}", [128, D], BF16)[:, :]) for i in range(2)]
            yacc_t = [k.sb(st3, f"yacc{i}", [128, 4, D], F32) for i in range(2)]
            yacc = [[[Buf(yacc_t[i][:, r, hf * 512:(hf + 1) * 512]) for hf in range(2)] for r in range(4)] for i in range(2)]
            htg = [[Buf(k.sb(st3, f"htg{i}_{jj}", [128, 512], BF16)[:, :]) for jj in range(G)] for i in range(2)]
            w1g = [Buf(k.sb(st3, f"w1g{i}", [128, KC, 512], BF16)[:, :, :]) for i in range(3)]
            w3g = [Buf(k.sb(st3, f"w3g{i}", [128, KC, 512], BF16)[:, :, :]) for i in range(3)]
            w2g = [Buf(k.sb(st3, f"w2g{i}", [128, G, D], BF16)[:, :, :]) for i in range(2)]
            sil = [Buf(k.sb(st3, f"sil{i}", [128, 512], F32)[:, :]) for i in range(2)]
            units = [(s_, g) for s_ in range(NST) for g in range(NG)]
            U = len(units)
            ptr3 = View(P.ps_tr.ap.rearrange("p (k c) -> p k c", c=128), P.ps_tr)
            xc = [0]

            def load_x(s_):
                for r in range(4):
                    xr = xrow[xc[0] % 2]
                    xc[0] += 1
                    k.dma("sp", xr.v(), xs_d[s_ * 512 + r * 128:s_ * 512 + (r + 1) * 128, :])
                    for kc in range(KC):
                        k.tr(P.ps_tr[:, kc * 128:(kc + 1) * 128], xr[:, kc * 128:(kc + 1) * 128], P.ident.v())
                    k.copy("dve", xsT[s_ % 2][:, :, r * 128:(r + 1) * 128], ptr3)

            def loadA(u):
                s_, g = units[u]
                sl = u % 3
                for hf in range(2):
                    k.dma_indirect(out=View(w1g[sl].ap[:, hf * 4:(hf + 1) * 4, :].rearrange("p k f -> p (k f)"), w1g[sl]),
                                   out_off=None, in_=w1r.v(), in_off=idxw[:, s_, g, hf:hf + 1])
                    k.dma_indirect(out=View(w3g[sl].ap[:, hf * 4:(hf + 1) * 4, :].rearrange("p k f -> p (k f)"), w3g[sl]),
                                   out_off=None, in_=w3r.v(), in_off=idxw[:, s_, g, hf:hf + 1])

            def loadB(u):
                s_, g = units[u]
                sl = u % 2
                for hf in range(2):
                    k.dma_indirect(out=View(w2g[sl].ap[:, hf * 2:(hf + 1) * 2, :].rearrange("p k f -> p (k f)"), w2g[sl]),
                                   out_off=None, in_=w2r.v(), in_off=idxw[:, s_, g, hf:hf + 1])

            cnt1 = [0]

            def phase1(u):
                s_, g = units[u]
                sl = u % 2
                sa = u % 3
                for jj in range(G):
                    c = cnt1[0] % 2
                    cnt1[0] += 1
                    pa, pb = P.ps[2 * c], P.ps[2 * c + 1]
                    for (W, pq) in ((w1g[sa], pa), (w3g[sa], pb)):
                        for kc in range(KC):
                            k.mm(pq.v(), W[:, kc, jj * 128:(jj + 1) * 128], xsT[s_ % 2][:, kc, :], start=(kc == 0), stop=(kc == KC - 1))
                    k.act(sil[c].v(), pa.v(), AF.Silu)
                    k.tt("dve", htg[sl][jj].v(), sil[c].v(), pb.v(), ALU.mult)

            cnt2 = [0]

            def phase2(u):
                s_, g = units[u]
                sl = u % 2
                for r in range(4):
                    for hf in range(2):
                        py = P.ps[4 + cnt2[0] % 3]
                        cnt2[0] += 1
                        for jj in range(G):
                            k.mm(py.v(), htg[sl][jj][:, r * 128:(r + 1) * 128], w2g[sl][:, jj, hf * 512:(hf + 1) * 512],
                                 start=(jj == 0), stop=(jj == G - 1))
                        ya = yacc[s_ % 2][r][hf]
                        if g == 0:
                            k.copy("dve", ya.v(), py.v())
                        else:
                            k.tt("dve", ya.v(), py.v(), ya.v(), ALU.add)
                if g == NG - 1:
                    for r in range(4):
                        k._deps("sp", [yacc[s_ % 2][r][1].v()], [])
                        k.dma("sp", ys_d[s_ * 512 + r * 128:s_ * 512 + (r + 1) * 128, :],
                              View(yacc_t[s_ % 2][:, r, :], yacc[s_ % 2][r][0]))

            load_x(0)
            loadA(0)
            loadB(0)
            loadA(1)
            loadB(1)
            loadA(2)
            phase1(0)
            for u in range(U):
                s_, g = units[u]
                if g == 2 and s_ + 1 < NST:
                    load_x(s_ + 1)
                if u + 1 < U:
                    phase1(u + 1)
                if u + 3 < U:
                    loadA(u + 3)
                phase2(u)
                if u + 2 < U:
                    loadB(u + 2)
            k.barrier()
        with ExitStack() as st4:
            gb, bb = load_ln_params(P, st4, View(g_d.ap[li:li + 1, :], g_d), View(b_d.ap[li:li + 1, :], b_d), "ffn_ln")
            tmp = alloc_ln_tmp(P, st4, "ffn_")
            xin = [Buf(k.sb(st4, f"ffn_xin{i}", [128, D], F32)[:, :]) for i in range(3)]
            y1 = [Buf(k.sb(st4, f"ffn_y1{i}", [128, D], F32)[:, :]) for i in range(3)]
            y2 = [Buf(k.sb(st4, f"ffn_y2{i}", [128, D], F32)[:, :]) for i in range(3)]

            def fetch(t):
                k.dma("sp", xin[t % 3].v(), P.hres[t * 128:(t + 1) * 128, :])
                k.dma_indirect(out=y1[t % 3].v(), out_off=None, in_=ys_d.v(), in_off=slot_i[:, t, 0:1])
                k.dma_indirect(out=y2[t % 3].v(), out_off=None, in_=ys_d.v(), in_off=slot_i[:, t, 1:2])

            fetch(0)
            fetch(1)
            for t in range(NT):
                if t + 2 < NT:
                    fetch(t + 2)
                xi, a, b = xin[t % 3], y1[t % 3], y2[t % 3]
                k.act(a.v(), a.v(), AF.Identity, scale=gs[:, t, 0:1])
                k.stt("dve", a.v(), b.v(), gs[:, t, 1:2], a.v(), ALU.mult, ALU.add)
                k.stt("dve", xi.v(), xi.v(), ALPHA, a.v(), ALU.mult, ALU.add)
                emit_ln_tail(P, st4, t, xi, gb, bb, dst, tmp)
            flush_ln_tail(P, tmp)
            k.barrier()


def load_w_bf16(P, st, name, src_view, kchunks, ncols, col0=0):
    k = P.k
    b = Buf(k.sb(st, name, [128, kchunks, ncols], BF16)[:, :, :], name)
    k.dma("pool", b.v(), View(src_view.ap.rearrange("(kc p) f -> p kc f", p=128)[:, :, col0:col0 + ncols], src_view.buf))
    return b


def emit_mix_out(P, li, oT, wo_view, dst):
    k = P.k
    g_d = P.inp("ln_mix_g", [DEPTH, D])
    b_d = P.inp("ln_mix_b", [DEPTH, D])
    with ExitStack() as st:
        wo = load_w_bf16(P, st, "wo_sb", wo_view, KC, D)
        gb, bb = load_ln_params(P, st, View(g_d.ap[li:li + 1, :], g_d), View(b_d.ap[li:li + 1, :], b_d), "mix_ln")
        tmp = alloc_ln_tmp(P, st, "mix_")
        xin = [Buf(k.sb(st, f"mix_xin{i}", [128, D], F32)[:, :]) for i in range(3)]
        k.dma("sp", xin[0].v(), P.hres[0:128, :])
        k.dma("sp", xin[1].v(), P.hres[128:256, :])
        c = 0
        for t in range(NT):
            if t + 2 < NT:
                k.dma("sp", xin[(t + 2) % 3].v(), P.hres[(t + 2) * 128:(t + 3) * 128, :])
            xi = xin[t % 3]
            for hf in range(2):
                py = P.ps[c % 4]
                c += 1
                for kc in range(KC):
                    k.mm(py.v(), oT[kc][:, t * 128:(t + 1) * 128], wo[:, kc, hf * 512:(hf + 1) * 512], start=(kc == 0), stop=(kc == KC - 1))
                k.stt("dve", xi[:, hf * 512:(hf + 1) * 512], xi[:, hf * 512:(hf + 1) * 512], ALPHA, py.v(), ALU.mult, ALU.add)
            emit_ln_tail(P, st, t, xi, gb, bb, dst, tmp)
        flush_ln_tail(P, tmp)
        k.barrier()


def emit_mix_gmlp(P, li, dst):
    k = P.k
    nc = k.nc
    j = li // 4
    w_in = P.inp("sg_w_in", [1, D, 2 * D])
    vg_d = P.inp("sg_v_norm_g", [1, D])
    vb_d = P.inp("sg_v_norm_b", [1, D])
    ws_d = P.inp("sg_w_s", [1, 8, 128, 128])
    bs_d = P.inp("sg_b_s", [1, 8, 128])
    wout = P.inp("sg_w_out", [1, D, D])
    with ExitStack() as st:
        uT_t = k.sb(st, "uT", [128, KC, S], BF16)
        uT = [[Buf(uT_t[:, c, tb * 512:(tb + 1) * 512]) for tb in range(4)] for c in range(KC)]
        vln_t = k.sb(st, "vln", [128, NT, D], BF16)
        vln = [Buf(vln_t[:, t, :]) for t in range(NT)]
        wsT = Buf(k.sb(st, "wsT", [128, 8, 128], BF16)[:, :, :], "wsT")
        bs4 = Buf(k.sb(st, "bs4", [1, 8, 512], BF16)[:, :, :], "bs4")
        with ExitStack() as st2:
            win = load_w_bf16(P, st2, "w_in_sb", View(w_in.ap[j], w_in), KC, 2 * D)
            ws_sb = Buf(k.sb(st2, "ws_sb", [128, 8, 128], BF16)[:, :, :])
            k.dma("pool", ws_sb.v(), View(ws_d.ap[j].rearrange("g i j -> i g j"), ws_d))
            for g in range(8):
                k.tr(P.ps_tr[:, g * 128:(g + 1) * 128], ws_sb[:, g, :], P.ident.v())
            k.copy("dve", wsT.v(), View(P.ps_tr.ap.rearrange("p (k c) -> p k c", c=128), P.ps_tr))
            k.memset("pool", wsT[64:128, :, 0:64], 0.0)
            bs_f = Buf(k.sb(st2, "bs_f", [1, 8, 128], F32)[:, :, :])
            k.dma("sp", bs_f.v(), View(bs_d.ap[j:j + 1], bs_d))
            for r in range(4):
                k.copy("dve", bs4[:, :, r * 128:(r + 1) * 128], bs_f.v())
            vg, vb = load_ln_params(P, st2, View(vg_d.ap[j:j + 1, :], vg_d), View(vb_d.ap[j:j + 1, :], vb_d), "sg_ln")
            c2 = 0
            for c in range(KC):
                for tb in range(4):
                    pp = P.ps[c2 % 4]
                    c2 += 1
                    for kc in range(KC):
                        k.mm(pp.v(), win[:, kc, c * 128:(c + 1) * 128], P.hT[tb][:, kc, :], start=(kc == 0), stop=(kc == KC - 1))
                    k.act(uT[c][tb].v(), pp.v(), AF.Gelu_apprx_tanh)
            vt = [Buf(k.sb(st2, f"sg_v{i}", [128, D], F32)[:, :]) for i in range(2)]
            stats = Buf(k.sb(st2, "sg_stats", [128, 12], F32)[:, :])
            mv = Buf(k.sb(st2, "sg_mv", [128, 2], F32)[:, :])
            sc = Buf(k.sb(st2, "sg_sc", [128, 4], F32)[:, :])
            eps = Buf(k.sb(st2, "sg_eps", [128, 1], F32)[:, :])
            k.memset("pool", eps.v(), LN_EPS)
            for t in range(NT):
                tb, tt_ = t // 4, t % 4
                v = vt[t % 2]
                for hf in range(2):
                    pp = P.ps[4 + c2 % 3]
                    c2 += 1
                    for kc in range(KC):
                        k.mm(pp.v(), P.hT[tb][:, kc, tt_ * 128:(tt_ + 1) * 128], win[:, kc, D + hf * 512:D + (hf + 1) * 512],
                             start=(kc == 0), stop=(kc == KC - 1))
                    k.act(v[:, hf * 512:(hf + 1) * 512], pp.v(), AF.Gelu_apprx_tanh)
                    k.op("dve", lambda hf=hf, v=v: nc.vector.bn_stats(out=stats.ap[:, hf * 6:(hf + 1) * 6], in_=v.ap[:, hf * 512:(hf + 1) * 512]),
                         [v.v()], [stats.v()])
                k.op("dve", lambda: nc.vector.bn_aggr(out=mv.ap, in_=stats.ap), [stats.v()], [mv.v()])
                k.act(sc[:, 0:1], mv[:, 1:2], AF.Sqrt, bias=eps[:, 0:1])
                k.recip(sc[:, 1:2], sc[:, 0:1])
                k.stt("dve", sc[:, 2:3], mv[:, 0:1], -1.0, sc[:, 1:2], ALU.mult, ALU.mult)
                k.act(v.v(), v.v(), AF.Identity, scale=sc[:, 1:2], bias=sc[:, 2:3])
                k.tt("pool", v.v(), v.v(), vg.v(), ALU.mult)
                k.tt("pool", vln[t].v(), v.v(), vb.v(), ALU.add)
            k.barrier()
        c3 = 0
        for g in range(8):
            for tb in range(4):
                pp = P.ps[c3 % 4]
                c3 += 1
                k.mm(pp.v(), P.ones[0:1, :], bs4[0:1, g, :], start=True, stop=False)
                for r in range(4):
                    t = tb * 4 + r
                    k.mm(pp[:, r * 128:(r + 1) * 128], vln[t][:, g * 128:(g + 1) * 128], wsT[:, g, :], start=False, stop=(r == 3))
                k.tt("dve", uT[g][tb].v(), uT[g][tb].v(), pp.v(), ALU.mult)
        sT = [Buf(uT_t[:, c, :]) for c in range(KC)]
        k.barrier()
        emit_mix_out(P, li, sT, View(wout.ap[j], wout), dst)


class Rot:
    def __init__(self, n):
        self.n = n
        self.i = 0

    def nxt(self):
        v = self.i % self.n
        self.i += 1
        return v


class AttnPipe:
    def __init__(self, depth=2):
        self.depth = depth
        self.q = []
        self.deferred = []

    def push(self, cfn, after=None):
        self.q.append((cfn, after))
        ready = [fn for (n, fn) in self.deferred if n <= 1]
        self.deferred = [(n - 1, fn) for (n, fn) in self.deferred if n > 1]
        for fn in ready:
            fn()
        while len(self.q) > self.depth:
            self._pop()

    def _pop(self):
        cfn, after = self.q.pop(0)
        cfn()
        if after is not None:
            after()

    def defer(self, n, fn):
        self.deferred.append((n, fn))

    def flush(self):
        while self.q:
            self._pop()
        while self.deferred:
            d = self.deferred
            self.deferred = []
            for (_, fn) in d:
                fn()


def run_blocks(P, pipe, blocks, q_of, k_of, v_of, ones_v, psO, psD, pts, rs, rp, scale, bias_of=None, after=None):
    k = P.k
    n = len(blocks)
    for bi, (kb, c0, N, zero, extra) in enumerate(blocks):
        pS = P.ps[rs.nxt()]
        k.mm(pS[:, 0:N], k_of(kb), q_of(c0, N), start=True, stop=(bias_of is None))
        if bias_of is not None:
            k.mm(pS[:, 0:N], P.ident.v(), bias_of(extra, N), start=False, stop=True)
        pt = pts[rp.nxt()]
        k.act(pt[:, 0:N], pS[:, 0:N], AF.Exp, scale=scale)
        if zero is not None:
            (r0, r1, z0, z1) = zero
            k.memset("pool", pt[r0:r1, z0:z1], 0.0)

        def cfn(o=psO[:, c0:c0 + N], dn=psD[:, c0:c0 + N], vv=v_of(kb), pv=pt[:, 0:N], st_=(bi == 0), sp_=(bi == n - 1)):
            k.mm(o, vv, pv, start=st_, stop=sp_)
            k.mm(dn, ones_v, pv, start=st_, stop=sp_)

        pipe.push(cfn, after if bi == n - 1 else None)


def causal_blocks(qb):
    bl = []
    for kb in range(4 * qb + 4):
        r = kb - 4 * qb
        if r <= 0:
            bl.append((kb, 0, 512, (64, 128, 0, 64) if r == 0 else None, None))
        else:
            bl.append((kb, 128 * r, 512 - 128 * r, (64, 128, 0, 64), None))
    return bl


def emit_mix_diff(P, li, dst):
    k = P.k
    nc = k.nc
    j = li // 4
    lam_init = 0.8 - 0.6 * math.exp(-0.3 * li)
    wq = P.inp("diff_wq", [1, D, D])
    wk = P.inp("diff_wk", [1, D, D])
    wv = P.inp("diff_wv", [1, D, D])
    wo = P.inp("diff_wo", [1, D, D])
    lqk = [P.inp(n, [1, 64]) for n in ("diff_lq1", "diff_lk1", "diff_lq2", "diff_lk2")]
    subg = P.inp("diff_sub_g", [1, 128])
    scale = 64 ** -0.5
    with ExitStack() as st:
        oT_t = k.sb(st, "oT", [128, KC, S], BF16)
        oTb = [[Buf(oT_t[:, c, qb * 512:(qb + 1) * 512]) for qb in range(4)] for c in range(KC)]
        sti = ExitStack()
        V_t = k.sb(sti, "Vall", [128, NT, D], BF16)
        V = [Buf(V_t[:, t, :]) for t in range(NT)]
        nlam = Buf(k.sb(sti, "nlam", [128, 1], F32)[:, :])
        gsc = Buf(k.sb(sti, "gsc", [128, 1], F32)[:, :])
        eps5 = Buf(k.sb(sti, "eps5", [128, 1], F32)[:, :])
        k.memset("pool", eps5.v(), 1e-5)
        with ExitStack() as st2:
            lt = Buf(k.sb(st2, "lqk", [128, 4, 64], F32)[:, :, :])
            for i in range(4):
                k.dma("sp", lt[:, i, :], View(lqk[i].ap[j:j + 1, :].partition_broadcast(128), lqk[i]))
            junk = Buf(k.sb(st2, "ljunk", [128, 64], F32)[:, :])
            acc = Buf(k.sb(st2, "lacc", [128, 4], F32)[:, :])
            k.stt("dve", junk.v(), lt[:, 0, :], 1.0, lt[:, 1, :], ALU.mult, ALU.mult, accum=acc[:, 0:1])
            k.stt("dve", junk.v(), lt[:, 2, :], 1.0, lt[:, 3, :], ALU.mult, ALU.mult, accum=acc[:, 1:2])
            k.act(acc[:, 2:3], acc[:, 0:1], AF.Exp)
            k.act(acc[:, 3:4], acc[:, 1:2], AF.Exp)
            k.tt("dve", nlam.v(), acc[:, 3:4], acc[:, 2:3], ALU.subtract)
            k.ts("dve", nlam.v(), nlam.v(), -lam_init, ALU.add)
            k.dma("sp", gsc.v(), View(subg.ap[j:j + 1, :].rearrange("o d -> d o"), subg))
            k.ts("dve", gsc.v(), gsc.v(), 1.0 - lam_init, ALU.mult)
            wv_sb = load_w_bf16(P, st2, "wv_sb", View(wv.ap[j], wv), KC, D)
            c = 0
            for t in range(NT):
                tb, tt_ = t // 4, t % 4
                for hf in range(2):
                    pp = P.ps[c % 4]
                    c += 1
                    for kc in range(KC):
                        k.mm(pp.v(), P.hT[tb][:, kc, tt_ * 128:(tt_ + 1) * 128], wv_sb[:, kc, hf * 512:(hf + 1) * 512],
                             start=(kc == 0), stop=(kc == KC - 1))
                    k.copy("act", V[t][:, hf * 512:(hf + 1) * 512], pp.v())
            k.barrier()
        with ExitStack() as st3:
            wqh = [Buf(k.sb(st3, f"wqh{i}", [128, KC, 128], BF16)[:, :, :]) for i in range(2)]
            wkh = [Buf(k.sb(st3, f"wkh{i}", [128, KC, 128], BF16)[:, :, :]) for i in range(2)]
            QT_t = [k.sb(st3, f"QT{i}", [128, S], BF16) for i in range(2)]
            KT_t = [k.sb(st3, f"KT{i}", [128, S], BF16) for i in range(2)]
            QT = [[Buf(QT_t[i][:, tb * 512:(tb + 1) * 512]) for tb in range(4)] for i in range(2)]
            KT = [[Buf(KT_t[i][:, tb * 512:(tb + 1) * 512]) for tb in range(4)] for i in range(2)]
            pts = [Buf(k.sb(st3, f"pt{i}", [128, 512], BF16)[:, :]) for i in range(4)]
            rd = Buf(k.sb(st3, "f_rd", [128, 512], F32)[:, :])
            o0 = Buf(k.sb(st3, "f_o0", [128, 512], F32)[:, :])
            o1 = Buf(k.sb(st3, "f_o1", [128, 512], F32)[:, :])
            sq = Buf(k.sb(st3, "f_sq", [128, 512], BF16)[:, :])
            rs, rp = Rot(3), Rot(4)

            def load_head(h):
                s_ = h % 2
                k.dma("pool", wqh[s_].v(), View(wq.ap[j].rearrange("(kc p) f -> p kc f", p=128)[:, :, h * 128:(h + 1) * 128], wq))
                k.dma("pool", wkh[s_].v(), View(wk.ap[j].rearrange("(kc p) f -> p kc f", p=128)[:, :, h * 128:(h + 1) * 128], wk))

            def proj_head(h):
                s_ = h % 2
                for (W, T) in ((wqh[s_], QT[s_]), (wkh[s_], KT[s_])):
                    for tb in range(4):
                        pp = P.ps[rs.nxt()]
                        for kc in range(KC):
                            k.mm(pp.v(), W[:, kc, :], P.hT[tb][:, kc, :], start=(kc == 0), stop=(kc == KC - 1))
                        k.copy("dve", T[tb].v(), pp.v())

            pipe = AttnPipe(2)
            unit = [0]
            o0b = [Buf(k.sb(st3, f"f_o0b{i}", [128, 512], F32)[:, :]) for i in range(2)]
            sqb = [Buf(k.sb(st3, f"f_sqb{i}", [128, 512], BF16)[:, :]) for i in range(2)]
            rdb = [Buf(k.sb(st3, f"f_rdb{i}", [128, 512], F32)[:, :]) for i in range(2)]

            def attn_head(h):
                s_ = h % 2
                for qb in range(4):
                    bl = causal_blocks(qb)
                    par = (h * 4 + qb) % 2
                    for m in range(2):
                        r0, r1 = m * 64, (m + 1) * 64
                        u = unit[0] % 2
                        unit[0] += 1
                        psO, psD = P.ps[3 + 2 * u], P.ps[4 + 2 * u]

                        def fin(m=m, psO=psO, psD=psD, par=par, h=h, qb=qb):
                            if m == 0:
                                k.recip(rd.v(), psD.v())
                                k.tt("dve", o0b[par].v(), psO.v(), rd.v(), ALU.mult)
                            else:
                                k.recip(rd.v(), psD.v())
                                k.tt("dve", o1.v(), psO.v(), rd.v(), ALU.mult)
                                k.stt("dve", o0b[par].v(), o1.v(), nlam[:, 0:1], o0b[par].v(), ALU.mult, ALU.add)
                                k.tt("dve", sqb[par].v(), o0b[par].v(), o0b[par].v(), ALU.mult)

                                def fin2():
                                    pS = P.ps[rs.nxt()]
                                    k.mm(pS.v(), P.ones.v(), sqb[par].v(), start=True, stop=True)
                                    k.act(rdb[par].v(), pS.v(), AF.Ln, scale=1.0 / 128.0, bias=eps5[:, 0:1])
                                    k.act(rdb[par].v(), rdb[par].v(), AF.Exp, scale=-0.5)
                                    k.stt("dve", oTb[h][qb].v(), o0b[par].v(), gsc[:, 0:1], rdb[par].v(), ALU.mult, ALU.mult)

                                pipe.defer(4, fin2)

                        run_blocks(P, pipe, bl,
                                   q_of=lambda c0, N: QT[s_][qb][r0:r1, c0:c0 + N],
                                   k_of=lambda kb: KT[s_][kb // 4][r0:r1, (kb % 4) * 128:(kb % 4 + 1) * 128],
                                   v_of=lambda kb: V[kb][:, h * 128:(h + 1) * 128],
                                   ones_v=P.ones.v(), psO=psO, psD=psD, pts=pts, rs=rs, rp=rp, scale=scale, after=fin)

            load_head(0)
            load_head(1)
            proj_head(0)
            for h in range(8):
                if h + 1 < 8:
                    proj_head(h + 1)
                if h + 2 < 8:
                    load_head(h + 2)
                attn_head(h)
            pipe.flush()
            k.barrier()
        sti.close()
        oT = [Buf(oT_t[:, c, :]) for c in range(KC)]
        emit_mix_out(P, li, oT, View(wo.ap[j], wo), dst)


def band_blocks(qb):
    bl = []
    for r in (4, 5, 6, 7, 3, 2, 1, 0):
        if r >= 4:
            rp_ = r - 4
            bl.append((4 * qb + rp_, 128 * rp_, 512 - 128 * rp_, (64, 128, 0, 64), 0))
        elif qb > 0:
            N = 128 * (r + 1)
            bl.append((4 * qb - 4 + r, 0, N, (0, 64, N - 64, N), 512 - 128 * r))
    return bl


def emit_mix_band(P, li, dst):
    k = P.k
    j = li // 4
    wqkv = P.inp("ca_w_qkv", [1, D, 3 * D])
    bt_d = P.inp("ca_bt", [16, 128, 640])
    wo = P.inp("ca_wo", [1, D, D])
    scale = 64 ** -0.5
    with ExitStack() as st:
        oT_t = k.sb(st, "oT", [128, KC, S], BF16)
        oTb = [[Buf(oT_t[:, c, qb * 512:(qb + 1) * 512]) for qb in range(4)] for c in range(KC)]
        sti = ExitStack()
        V_t = k.sb(sti, "Vall", [128, NT, D], BF16)
        V = [Buf(V_t[:, t, :]) for t in range(NT)]
        BT = Buf(k.sb(sti, "BT", [128, 16, 640], BF16)[:, :, :], "BT")
        k.dma("pool", BT.v(), View(bt_d.ap.rearrange("h p x -> p h x"), bt_d))
        k.ts("pool", BT.v(), BT.v(), 1.0 / scale, ALU.mult)
        with ExitStack() as st2:
            wv_sb = load_w_bf16(P, st2, "wv_sb", View(wqkv.ap[j], wqkv), KC, D, col0=2 * D)
            c = 0
            for t in range(NT):
                tb, tt_ = t // 4, t % 4
                for hf in range(2):
                    pp = P.ps[c % 4]
                    c += 1
                    for kc in range(KC):
                        k.mm(pp.v(), P.hT[tb][:, kc, tt_ * 128:(tt_ + 1) * 128], wv_sb[:, kc, hf * 512:(hf + 1) * 512],
                             start=(kc == 0), stop=(kc == KC - 1))
                    k.copy("act", V[t][:, hf * 512:(hf + 1) * 512], pp.v())
            k.barrier()
        with ExitStack() as st3:
            wqh = [Buf(k.sb(st3, f"wqh{i}", [128, KC, 128], BF16)[:, :, :]) for i in range(2)]
            wkh = [Buf(k.sb(st3, f"wkh{i}", [128, KC, 128], BF16)[:, :, :]) for i in range(2)]
            QT_t = [k.sb(st3, f"QT{i}", [128, S], BF16) for i in range(2)]
            KT_t = [k.sb(st3, f"KT{i}", [128, S], BF16) for i in range(2)]
            QT = [[Buf(QT_t[i][:, tb * 512:(tb + 1) * 512]) for tb in range(4)] for i in range(2)]
            KT = [[Buf(KT_t[i][:, tb * 512:(tb + 1) * 512]) for tb in range(4)] for i in range(2)]
            pts = [Buf(k.sb(st3, f"pt{i}", [128, 512], BF16)[:, :]) for i in range(4)]
            rd = [Buf(k.sb(st3, f"f_rd{i}", [128, 512], F32)[:, :]) for i in range(2)]
            rs, rp = Rot(3), Rot(4)
            wq_v = View(wqkv.ap[j].rearrange("(kc p) f -> p kc f", p=128), wqkv)

            def load_pair(p):
                s_ = p % 2
                k.dma("pool", wqh[s_].v(), wq_v[:, :, p * 128:(p + 1) * 128])
                k.dma("pool", wkh[s_].v(), wq_v[:, :, D + p * 128:D + (p + 1) * 128])

            def proj_pair(p):
                s_ = p % 2
                for (W, T) in ((wqh[s_], QT[s_]), (wkh[s_], KT[s_])):
                    for tb in range(4):
                        pp = P.ps[rs.nxt()]
                        for kc in range(KC):
                            k.mm(pp.v(), W[:, kc, :], P.hT[tb][:, kc, :], start=(kc == 0), stop=(kc == KC - 1))
                        k.copy("dve", T[tb].v(), pp.v())

            unit = [0]
            pipe = AttnPipe(2)

            def attn_pair(p):
                s_ = p % 2
                for hh in range(2):
                    h = 2 * p + hh
                    r0, r1 = hh * 64, (hh + 1) * 64
                    for qb in range(4):
                        u = unit[0] % 2
                        unit[0] += 1
                        psO, psD = P.ps[3 + 2 * u], P.ps[4 + 2 * u]
                        def fin(u=u, psO=psO, psD=psD, r0=r0, r1=r1, p=p, qb=qb):
                            k.recip(rd[u][r0:r1, :], psD[r0:r1, :])
                            k.tt("dve", oTb[p][qb][r0:r1, :], psO[r0:r1, :], rd[u][r0:r1, :], ALU.mult)

                        run_blocks(P, pipe, band_blocks(qb),
                                   q_of=lambda c0, N: QT[s_][qb][r0:r1, c0:c0 + N],
                                   k_of=lambda kb: KT[s_][kb // 4][r0:r1, (kb % 4) * 128:(kb % 4 + 1) * 128],
                                   v_of=lambda kb: V[kb][:, p * 128:(p + 1) * 128],
                                   ones_v=P.ones.v(), psO=psO, psD=psD, pts=pts, rs=rs, rp=rp, scale=scale,
                                   bias_of=lambda off, N: BT[:, h, off:off + N], after=fin)

            load_pair(0)
            load_pair(1)
            proj_pair(0)
            for p in range(8):
                if p + 1 < 8:
                    proj_pair(p + 1)
                if p + 2 < 8:
                    load_pair(p + 2)
                attn_pair(p)
            pipe.flush()
            k.barrier()
        sti.close()
        oT = [Buf(oT_t[:, c, :]) for c in range(KC)]
        emit_mix_out(P, li, oT, View(wo.ap[j], wo), dst)


MLA_STAGE = [9]


def emit_mix_mla(P, li, dst):
    k = P.k
    nc = k.nc
    j = li // 4
    QR, KVR, NH = 384, 256, 16
    w_dq = P.inp("mla_w_dq", [1, D, QR])
    qg_d = P.inp("mla_q_norm_g", [1, QR])
    w_uq = P.inp("mla_w_uq", [1, QR, NH * 96])
    w_dkv = P.inp("mla_w_dkv", [1, D, KVR + 32])
    kvg_d = P.inp("mla_kv_norm_g", [1, KVR])
    w_ukv = P.inp("mla_w_ukv", [1, KVR, NH * 128])
    wo = P.inp("mla_wo", [1, D, D])
    cs_d = P.inp("rope_cs", [2, 32, S])
    scale = 96 ** -0.5
    with ExitStack() as st:
        oT_t = k.sb(st, "oT", [128, KC, S], BF16)
        oTb = [[Buf(oT_t[:, c, qb * 512:(qb + 1) * 512]) for qb in range(4)] for c in range(KC)]
        sti = ExitStack()
        V_t = k.sb(sti, "Vall", [128, NT, D], BF16)
        V = [Buf(V_t[:, t, :]) for t in range(NT)]
        cqT_t = k.sb(sti, "cqT", [128, 3, S], BF16)
        cqT = [Buf(cqT_t[:, :, tb * 512:(tb + 1) * 512]) for tb in range(4)]
        ckvT_t = k.sb(sti, "ckvT", [128, 2, S], BF16)
        ckvT = [Buf(ckvT_t[:, :, tb * 512:(tb + 1) * 512]) for tb in range(4)]
        cs = Buf(k.sb(sti, "cs", [128, 2, S], F32)[:, :, :], "cs")
        k.dma("sp", cs[64:96, :, :], View(cs_d.ap.rearrange("c p s -> p c s"), cs_d))
        wuq = Buf(k.sb(sti, "wuq", [128, 3, NH * 96 + 32], BF16)[:, :, :], "wuq")
        wuqR = Buf(k.sb(sti, "wuqR", [128, 3, NH * 96 + 32], BF16)[:, :, :], "wuqR")
        wkn = Buf(k.sb(sti, "wkn", [128, 2, NH * 64 + 64], BF16)[:, :, :], "wkn")
        k.memset("pool", wuq.v(), 0.0)
        k.memset("pool", wkn.v(), 0.0)
        KR_t = k.sb(sti, "KR", [128, S], BF16)
        KR = [Buf(KR_t[:, tb * 512:(tb + 1) * 512]) for tb in range(4)]
        eps6 = Buf(k.sb(sti, "eps6", [128, 1], F32)[:, :])
        k.memset("pool", eps6.v(), 1e-6)
        with ExitStack() as st2:
            wdq = load_w_bf16(P, st2, "wdq", View(w_dq.ap[j], w_dq), KC, QR)
            wdkc = load_w_bf16(P, st2, "wdkc", View(w_dkv.ap[j], w_dkv), KC, KVR)
            wvv = Buf(k.sb(st2, "wvv", [128, 2, NH * 64], BF16)[:, :, :], "wvv")
            wkr = Buf(k.sb(st2, "wkr", [128, KC, 128], BF16)[:, :, :], "wkr")
            wkrR = Buf(k.sb(st2, "wkrR", [128, KC, 128], BF16)[:, :, :], "wkrR")
            wkst = Buf(k.sb(st2, "wkst", [128, KC, 32], F32)[:, :, :], "wkst")
            dkv_v = View(w_dkv.ap[j].rearrange("(kc p) f -> p kc f", p=128), w_dkv)
            k.memset("pool", wkr.v(), 0.0)
            k.memset("pool", wkrR.v(), 0.0)
            k.dma("sp", wkst.v(), dkv_v[:, :, KVR:KVR + 32])
            k.copy("pool", wkr[:, :, 64:96], wkst.v())
            k.ts("pool", wkrR[:, :, 64:80], wkst[:, :, 16:32], -1.0, ALU.mult)
            k.copy("pool", wkrR[:, :, 80:96], wkst[:, :, 0:16])
            gq = Buf(k.sb(st2, "gq", [128, 3], F32)[:, :])
            gkv = Buf(k.sb(st2, "gkv", [128, 2], F32)[:, :])
            for kc in range(3):
                k.dma("sp", gq[:, kc:kc + 1], View(qg_d.ap[j:j + 1, kc * 128:(kc + 1) * 128].rearrange("o d -> d o"), qg_d))
            for kc in range(2):
                k.dma("sp", gkv[:, kc:kc + 1], View(kvg_d.ap[j:j + 1, kc * 128:(kc + 1) * 128].rearrange("o d -> d o"), kvg_d))
            with ExitStack() as st2a:
                stg = Buf(k.sb(st2a, "stg_uq", [128, 3, NH * 96], F32)[:, :, :])
                k.dma("sp", stg.v(), View(w_uq.ap[j].rearrange("(kc p) f -> p kc f", p=128), w_uq))
                for kc in range(3):
                    k.act(wuq[:, kc, 0:NH * 96], stg[:, kc, :], AF.Identity, scale=gq[:, kc:kc + 1])
                k.memset("pool", wuqR.v(), 0.0)
                w4 = View(wuq.ap[:, :, 0:NH * 96].rearrange("p k (h c) -> p k h c", c=96), wuq)
                r4 = View(wuqR.ap[:, :, 0:NH * 96].rearrange("p k (h c) -> p k h c", c=96), wuqR)
                for kc in range(3):
                    k.ts("pool", r4[:, kc, :, 64:80], w4[:, kc, :, 80:96], -1.0, ALU.mult)
                    k.copy("pool", r4[:, kc, :, 80:96], w4[:, kc, :, 64:80])
                k.barrier()
            with ExitStack() as st2b:
                stg = Buf(k.sb(st2b, "stg_ukv", [128, 2, NH * 128], F32)[:, :, :])
                k.dma("sp", stg.v(), View(w_ukv.ap[j].rearrange("(kc p) f -> p kc f", p=128), w_ukv))
                s4 = View(stg.ap.rearrange("p k (h c) -> p k h c", c=128), stg)
                kn4 = View(wkn.ap[:, :, 0:NH * 64].rearrange("p k (h c) -> p k h c", c=64), wkn)
                vv4 = View(wvv.ap.rearrange("p k (h c) -> p k h c", c=64), wvv)
                for kc in range(2):
                    k.act(kn4[:, kc, :, :], s4[:, kc, :, 0:64], AF.Identity, scale=gkv[:, kc:kc + 1])
                    k.act(vv4[:, kc, :, :], s4[:, kc, :, 64:128], AF.Identity, scale=gkv[:, kc:kc + 1])
                k.barrier()
            junk = Buf(k.sb(st2, "mjunk", [128, QR], F32)[:, :])
            NT_A = NT if MLA_STAGE[0] >= 2 else 0
            cqn = [Buf(k.sb(st2, f"cqn{i}", [128, QR], BF16)[:, :]) for i in range(2)]
            cqf = [Buf(k.sb(st2, f"cqf{i}", [128, QR], F32)[:, :]) for i in range(2)]
            ckf = [Buf(k.sb(st2, f"ckf{i}", [128, KVR], F32)[:, :]) for i in range(2)]
            ckn = [Buf(k.sb(st2, f"ckn{i}", [128, KVR], BF16)[:, :]) for i in range(2)]
            sc = [Buf(k.sb(st2, f"msc{i}", [128, 8], F32)[:, :]) for i in range(2)]
            ptr3 = View(P.ps_tr.ap.rearrange("p (k c) -> p k c", c=128), P.ps_tr)
            for t in range(NT_A):
                tb, tt_ = t // 4, t % 4
                u = t % 2
                pq, pk = P.ps[2 * u], P.ps[2 * u + 1]
                for kc in range(KC):
                    k.mm(pq[:, 0:QR], P.hT[tb][:, kc, tt_ * 128:(tt_ + 1) * 128], wdq[:, kc, :], start=(kc == 0), stop=(kc == KC - 1))
                for kc in range(KC):
                    k.mm(pk[:, 0:KVR], P.hT[tb][:, kc, tt_ * 128:(tt_ + 1) * 128], wdkc[:, kc, :], start=(kc == 0), stop=(kc == KC - 1))
                k.copy("act", cqf[u].v(), pq[:, 0:QR])
                k.copy("act", ckf[u].v(), pk[:, 0:KVR])
                k.stt("dve", junk[:, 0:QR], cqf[u].v(), 1.0, cqf[u].v(), ALU.mult, ALU.mult, accum=sc[u][:, 0:1])
                k.stt("dve", junk[:, 0:KVR], ckf[u].v(), 1.0, ckf[u].v(), ALU.mult, ALU.mult, accum=sc[u][:, 1:2])
                k.act(sc[u][:, 2:3], sc[u][:, 0:1], AF.Sqrt, scale=1.0 / QR, bias=eps6[:, 0:1])
                k.act(sc[u][:, 3:4], sc[u][:, 1:2], AF.Sqrt, scale=1.0 / KVR, bias=eps6[:, 0:1])
                k.recip(sc[u][:, 4:6], sc[u][:, 2:4])
                k.ts("dve", cqn[u].v(), cqf[u].v(), sc[u][:, 4:5], ALU.mult)
                k.ts("dve", ckn[u].v(), ckf[u].v(), sc[u][:, 5:6], ALU.mult)
                for kc in range(3):
                    k.tr(P.ps_tr[:, kc * 128:(kc + 1) * 128], cqn[u][:, kc * 128:(kc + 1) * 128], P.ident.v())
                for kc in range(2):
                    k.tr(P.ps_tr[:, (3 + kc) * 128:(4 + kc) * 128], ckn[u][:, kc * 128:(kc + 1) * 128], P.ident.v())
                k.copy("dve", cqT[tb][:, :, tt_ * 128:(tt_ + 1) * 128], ptr3[:, 0:3, :])
                k.copy("dve", ckvT[tb][:, :, tt_ * 128:(tt_ + 1) * 128], ptr3[:, 3:5, :])
            ta = Buf(k.sb(st2, "rk_a", [128, 512], F32)[:, :])
            tb_ = Buf(k.sb(st2, "rk_b", [128, 512], F32)[:, :])
            for tb in range(4 if MLA_STAGE[0] >= 3 else 0):
                p1, p2 = P.ps[4], P.ps[5]
                for kc in range(KC):
                    k.mm(p1.v(), wkr[:, kc, :], P.hT[tb][:, kc, :], start=(kc == 0), stop=(kc == KC - 1))
                for kc in range(KC):
                    k.mm(p2.v(), wkrR[:, kc, :], P.hT[tb][:, kc, :], start=(kc == 0), stop=(kc == KC - 1))
                k.tt("dve", ta[64:96, :], p1[64:96, :], cs[64:96, 0, tb * 512:(tb + 1) * 512], ALU.mult)
                k.tt("dve", tb_[64:96, :], p2[64:96, :], cs[64:96, 1, tb * 512:(tb + 1) * 512], ALU.mult)
                k.tt("dve", KR[tb][64:96, :], ta[64:96, :], tb_[64:96, :], ALU.add)
            c = 0
            for t in range(NT if MLA_STAGE[0] >= 4 else 0):
                tb, tt_ = t // 4, t % 4
                for hf in range(2):
                    pp = P.ps[c % 4]
                    c += 1
                    for kc in range(2):
                        k.mm(pp.v(), ckvT[tb][:, kc, tt_ * 128:(tt_ + 1) * 128], wvv[:, kc, hf * 512:(hf + 1) * 512],
                             start=(kc == 0), stop=(kc == 1))
                    k.copy("act", V[t][:, hf * 512:(hf + 1) * 512], pp.v())
            k.barrier()
        with ExitStack() as st3:
            QT_t = [k.sb(st3, f"QT{i}", [128, S], BF16) for i in range(2)]
            KT_t = [k.sb(st3, f"KT{i}", [128, S], BF16) for i in range(2)]
            QT = [[Buf(QT_t[i][:, tb * 512:(tb + 1) * 512]) for tb in range(4)] for i in range(2)]
            KT = [[Buf(KT_t[i][:, tb * 512:(tb + 1) * 512]) for tb in range(4)] for i in range(2)]
            pts = [Buf(k.sb(st3, f"pt{i}", [128, 512], BF16)[:, :]) for i in range(4)]
            rd = [Buf(k.sb(st3, f"f_rd{i}", [128, 512], F32)[:, :]) for i in range(2)]
            ta = Buf(k.sb(st3, "rq_a", [128, 512], F32)[:, :])
            tb2 = Buf(k.sb(st3, "rq_b", [128, 512], F32)[:, :])
            rs, rp = Rot(3), Rot(4)
            for i in range(2):
                for tb in range(4):
                    k.memset("pool", QT[i][tb].v(), 0.0)
                    k.memset("pool", KT[i][tb].v(), 0.0)
                    k.copy("pool", KT[i][tb][64:96, :], KR[tb][64:96, :])

            def proj_head(h):
                s_ = h % 2
                for tb in range(4):
                    p1 = P.ps[rs.nxt()]
                    for kc in range(3):
                        k.mm(p1.v(), wuq[:, kc, h * 96:h * 96 + 128], cqT[tb][:, kc, :], start=(kc == 0), stop=(kc == 2))
                    p2 = P.ps[rs.nxt()]
                    for kc in range(3):
                        k.mm(p2.v(), wuqR[:, kc, h * 96:h * 96 + 128], cqT[tb][:, kc, :], start=(kc == 0), stop=(kc == 2))
                    k.copy("dve", QT[s_][tb][0:64, :], p1[0:64, :])
                    k.tt("dve", ta[64:96, :], p1[64:96, :], cs[64:96, 0, tb * 512:(tb + 1) * 512], ALU.mult)
                    k.tt("dve", tb2[64:96, :], p2[64:96, :], cs[64:96, 1, tb * 512:(tb + 1) * 512], ALU.mult)
                    k.tt("dve", QT[s_][tb][64:96, :], ta[64:96, :], tb2[64:96, :], ALU.add)
                    p3 = P.ps[rs.nxt()]
                    for kc in range(2):
                        k.mm(p3.v(), wkn[:, kc, h * 64:h * 64 + 128], ckvT[tb][:, kc, :], start=(kc == 0), stop=(kc == 1))
                    k.copy("dve", KT[s_][tb][0:64, :], p3[0:64, :])

            unit = [0]
            pipe = AttnPipe(2)

            def attn_head(h):
                s_ = h % 2
                p, hh = h // 2, h % 2
                r0, r1 = hh * 64, (hh + 1) * 64
                for qb in range(4):
                    u = unit[0] % 2
                    unit[0] += 1
                    psO, psD = P.ps[3 + 2 * u], P.ps[4 + 2 * u]
                    def fin(u=u, psO=psO, psD=psD, r0=r0, r1=r1, p=p, qb=qb):
                        k.recip(rd[u][r0:r1, :], psD[r0:r1, :])
                        k.tt("dve", oTb[p][qb][r0:r1, :], psO[r0:r1, :], rd[u][r0:r1, :], ALU.mult)

                    run_blocks(P, pipe, causal_blocks(qb),
                               q_of=lambda c0, N: QT[s_][qb][:, c0:c0 + N],
                               k_of=lambda kb: KT[s_][kb // 4][:, (kb % 4) * 128:(kb % 4 + 1) * 128],
                               v_of=lambda kb: V[kb][:, p * 128:(p + 1) * 128],
                               ones_v=P.ones.v(), psO=psO, psD=psD, pts=pts, rs=rs, rp=rp, scale=scale, after=fin)

            NHX = NH if MLA_STAGE[0] >= 6 else (1 if MLA_STAGE[0] >= 5 else 0)
            if NHX:
                proj_head(0)
            for h in range(NHX):
                if h + 1 < NHX:
                    proj_head(h + 1)
                if MLA_STAGE[0] != 5:
                    attn_head(h)
            pipe.flush()
            k.barrier()
        sti.close()
        oT = [Buf(oT_t[:, c, :]) for c in range(KC)]
        emit_mix_out(P, li, oT, View(wo.ap[j], wo), dst)


def build(plan):
    k = K()
    P = Prog(k, plan)
    emit_consts(P)
    emit_prologue(P)
    n = len(plan)
    for i, name in enumerate(plan):
        dst = P.y if i == n - 1 else P.hres
        kind, li = name[:3], int(name[3:])
        if kind == "ffn":
            if li % 2 == 1 and SPARSE_MOE:
                emit_ffn_sparse(P, li, dst)
            else:
                emit_ffn(P, li, moe=(li % 2 == 1), dst=dst)
        elif kind == "mix":
            [emit_mix_diff, emit_mix_band, emit_mix_mla, emit_mix_gmlp][li % 4](P, li, dst)
        else:
            raise ValueError(name)
    k.final_wait()
    return k, P


def host_inputs(P, inputs):
    m = {}
    for name in P.ins:
        if name == "ca_bt":
            rb = np.asarray(inputs["ca_rel_bias"], np.float32)[0]
            idx = np.minimum(np.arange(640)[None, :] - np.arange(128)[:, None] + 128, 256)
            m[name] = np.ascontiguousarray(rb[:, idx])
        elif name == "rope_cs":
            half = 16
            inv_freq = (10000.0 ** (-np.arange(half, dtype=np.float32) / half)).astype(np.float32)
            ang = np.arange(S, dtype=np.float32)[None, :] * inv_freq[:, None]
            cos = np.concatenate([np.cos(ang), np.cos(ang)], axis=0)
            sin = np.concatenate([np.sin(ang), np.sin(ang)], axis=0)
            m[name] = np.ascontiguousarray(np.stack([cos, sin], axis=0).astype(np.float32))
        elif name in ("moe_w1r", "moe_w3r"):
            w = np.asarray(inputs["moe_w1" if name == "moe_w1r" else "moe_w3"], np.float32)
            w = w.reshape(2, NE, 2, 4, 128, NG, 512).transpose(0, 1, 5, 4, 2, 3, 6)
            m[name] = np.ascontiguousarray(w).reshape(2 * NE * NG * 128 * 2, WROW)
        elif name == "moe_w2r":
            w = np.asarray(inputs["moe_w2"], np.float32)
            w = w.reshape(2, NE, NG, 2, 2, 128, D).transpose(0, 1, 2, 5, 3, 4, 6)
            m[name] = np.ascontiguousarray(w).reshape(2 * NE * NG * 128 * 2, WROW)
        elif name == "tri_c":
            m[name] = np.triu(np.ones((128, 128), np.float32), 1)
        elif name == "iota2_c":
            m[name] = (2.0 * np.arange(128, dtype=np.float32)).reshape(128, 1)
        elif name == "moe_wrT":
            m[name] = np.ascontiguousarray(np.transpose(np.asarray(inputs["moe_w_router"], np.float32), (0, 2, 1)))
        else:
            m[name] = np.ascontiguousarray(np.asarray(inputs[name], np.float32))
    m["ident_c"] = np.eye(128, dtype=np.float32)
    return m


SPARSE_MOE = True
FULL_PLAN = ["mix0", "ffn0", "mix1", "ffn1", "mix2", "ffn2", "mix3", "ffn3"]


def run_plan(plan, inputs, xs, trace=False):
    k, P = build(plan)
    shared = host_inputs(P, inputs)
    in_maps = []
    for xc in xs:
        mcore = dict(shared)
        mcore["x"] = np.ascontiguousarray(xc, dtype=np.float32)
        in_maps.append(mcore)
    res = run_bass_kernel_spmd(k.nc, in_maps, core_ids=list(range(len(xs))), trace=trace)
    return [r["y"] for r in res.results], res


def kernel(**inputs):
    x = np.asarray(inputs["x"], np.float32)
    outs, _ = run_plan(FULL_PLAN, inputs, [x[b] for b in range(x.shape[0])])
    return np.stack(outs, axis=0).astype(np.float32)
```

```python
import math
from contextlib import ExitStack

import numpy as np
import concourse.bass as bass
import concourse.mybir as mybir
from concourse.bass_utils import run_bass_kernel_spmd

F32 = mybir.dt.float32
BF16 = mybir.dt.bfloat16
AF = mybir.ActivationFunctionType
ALU = mybir.AluOpType
AX = mybir.AxisListType

S = 2048
D = 1024
NT = S // 128
KC = D // 128
DEPTH = 4
ALPHA = (2 * DEPTH) ** 0.25
LN_EPS = 1e-5
D_FF = 2816
D_FFE = 3584
NE = 8
NDS = 16


class Buf:
    def __init__(self, ap, name=""):
        self.ap = ap
        self.w = None
        self.r = {}
        self.name = name

    def __getitem__(self, idx):
        return View(self.ap[idx], self)

    def v(self):
        return View(self.ap, self)


class View:
    def __init__(self, ap, buf):
        self.ap = ap
        self.buf = buf

    def __getitem__(self, idx):
        return View(self.ap[idx], self.buf)


def _ap(x):
    return x.ap if isinstance(x, View) else x


class K:
    def __init__(self):
        nc = bass.Bass("TRN2", target_bir_lowering=False)
        self.nc = nc
        self.eng = {"pe": nc.tensor, "act": nc.scalar, "dve": nc.vector, "pool": nc.gpsimd, "sp": nc.sync}
        self.sem = {e: nc.alloc_semaphore("s_" + e) for e in ("pe", "act", "dve", "pool")}
        self.cnt = {e: 0 for e in self.sem}
        self.dsem = [nc.alloc_semaphore(f"s_d{i}") for i in range(NDS)]
        self.dcnt = [0] * NDS
        self.dnext = 0
        self.dnext_pool = 0
        self.seen = {e: {} for e in self.eng}
        self.n_inst = 0

    def _semh(self, key):
        return self.sem[key] if isinstance(key, str) else self.dsem[key[1]]

    def _wait(self, e, key, val):
        if self.seen[e].get(key, 0) >= val:
            return
        self.eng[e].wait_ge(self._semh(key), val)
        self.seen[e][key] = val

    def _deps(self, e, reads, writes):
        deps = {}

        def add(t):
            if t is None:
                return
            k, v = t
            if deps.get(k, 0) < v:
                deps[k] = v

        for v in reads:
            add(v.buf.w)
        for v in writes:
            add(v.buf.w)
            for kk, val in v.buf.r.items():
                add((kk, val))
        for kk, val in deps.items():
            if kk == e and e == "pe":
                continue
            self._wait(e, kk, val)

    def _done(self, key, val, reads, writes):
        for v in writes:
            v.buf.w = (key, val)
            v.buf.r = {}
        for v in reads:
            if v.buf.r.get(key, 0) < val:
                v.buf.r[key] = val

    def op(self, e, fn, reads, writes):
        reads = [r for r in reads if isinstance(r, View)]
        self._deps(e, reads, writes)
        inst = fn()
        self.cnt[e] += 1
        inst.then_inc(self.sem[e], 1)
        self._done(e, self.cnt[e], reads, writes)
        self.n_inst += 1
        return inst

    def dma(self, q, out, in_, **kw):
        self._deps(q, [in_], [out])
        half = NDS // 2
        if q == "pool":
            i = half + self.dnext_pool
            self.dnext_pool = (self.dnext_pool + 1) % half
        else:
            i = self.dnext
            self.dnext = (self.dnext + 1) % half
        key = ("d", i)
        if self.dcnt[i] > 0:
            self._wait(q, key, 16 * self.dcnt[i])
        inst = self.eng[q].dma_start(out=out.ap, in_=in_.ap, **kw)
        self.dcnt[i] += 1
        inst.then_inc(self.dsem[i], 16)
        self._done(key, 16 * self.dcnt[i], [in_], [out])
        self.n_inst += 1

    def dma_indirect(self, out, out_off, in_, in_off):
        q = "pool"
        idx = in_off if in_off is not None else out_off
        self._deps(q, [in_, idx], [out])
        half = NDS // 2
        i = half + self.dnext_pool
        self.dnext_pool = (self.dnext_pool + 1) % half
        key = ("d", i)
        if self.dcnt[i] > 0:
            self._wait(q, key, 16 * self.dcnt[i])
        oo = bass.IndirectOffsetOnAxis(ap=out_off.ap, axis=0) if out_off is not None else None
        io = bass.IndirectOffsetOnAxis(ap=in_off.ap, axis=0) if in_off is not None else None
        inst = self.nc.gpsimd.indirect_dma_start(out=out.ap, out_offset=oo, in_=in_.ap, in_offset=io)
        self.dcnt[i] += 1
        inst.then_inc(self.dsem[i], 16)
        self._done(key, 16 * self.dcnt[i], [in_, idx], [out])
        self.n_inst += 1

    def barrier(self):
        keys = [(e, self.cnt[e]) for e in self.sem] + [(("d", i), 16 * self.dcnt[i]) for i in range(NDS)]
        for e in self.eng:
            for kk, val in keys:
                if val > 0 and kk != e:
                    self._wait(e, kk, val)

    def final_wait(self):
        for i in range(NDS):
            if self.dcnt[i] > 0:
                self._wait("sp", ("d", i), 16 * self.dcnt[i])

    def mm(self, out, lhsT, rhs, start=True, stop=True, **kw):
        return self.op("pe", lambda: self.nc.tensor.matmul(out.ap, lhsT.ap, rhs.ap, start=start, stop=stop, **kw),
                       [lhsT, rhs], [out])

    def tr(self, out, in_, ident):
        return self.op("pe", lambda: self.nc.tensor.transpose(out.ap, in_.ap, ident.ap), [in_, ident], [out])

    def act(self, out, in_, func, bias=None, scale=None, accum=None):
        kw = {}
        reads = [in_]
        writes = [out]
        if bias is not None:
            kw["bias"] = _ap(bias)
            reads.append(bias)
        if scale is not None:
            kw["scale"] = _ap(scale)
            reads.append(scale)
        if accum is not None:
            kw["accum_out"] = accum.ap
            writes.append(accum)
        return self.op("act", lambda: self.nc.scalar.activation(out=out.ap, in_=in_.ap, func=func, **kw), reads, writes)

    def tt(self, e, out, in0, in1, op):
        return self.op(e, lambda: self.eng[e].tensor_tensor(out=out.ap, in0=in0.ap, in1=in1.ap, op=op), [in0, in1], [out])

    def ts(self, e, out, in0, s1, op0, s2=None, op1=None, accum=None):
        kw = {}
        writes = [out]
        if op1 is not None:
            kw["op1"] = op1
        if accum is not None:
            kw["accum_out"] = accum.ap
            writes.append(accum)
        return self.op(e, lambda: self.eng[e].tensor_scalar(out=out.ap, in0=in0.ap, scalar1=_ap(s1), scalar2=_ap(s2), op0=op0, **kw),
                       [in0, s1, s2], writes)

    def stt(self, e, out, in0, scalar, in1, op0, op1, accum=None):
        kw = {}
        writes = [out]
        if accum is not None:
            kw["accum_out"] = accum.ap
            writes.append(accum)
        return self.op(e, lambda: self.eng[e].scalar_tensor_tensor(out=out.ap, in0=in0.ap, scalar=_ap(scalar), in1=in1.ap, op0=op0, op1=op1, **kw),
                       [in0, scalar, in1], writes)

    def copy(self, e, out, in_):
        if e == "act":
            return self.act(out, in_, AF.Copy)
        return self.op(e, lambda: self.eng[e].tensor_copy(out=out.ap, in_=in_.ap), [in_], [out])

    def memset(self, e, out, val):
        return self.op(e, lambda: self.eng[e].memset(out.ap, val), [], [out])

    def recip(self, out, in_):
        return self.op("dve", lambda: self.nc.vector.reciprocal(out=out.ap, in_=in_.ap), [in_], [out])

    def sb(self, st, name, shape, dtype):
        self.n_alloc = getattr(self, "n_alloc", 0) + 1
        return st.enter_context(self.nc.sbuf_tensor(f"{name}_{self.n_alloc}", list(shape), dtype))

    def dram_in(self, name, shape, dtype=F32):
        return Buf(self.nc.dram_tensor(name, list(shape), dtype, kind="ExternalInput").ap(), name)

    def dram_out(self, name, shape, dtype=F32):
        return Buf(self.nc.dram_tensor(name, list(shape), dtype, kind="ExternalOutput").ap(), name)

    def dram_tmp(self, name, shape, dtype=F32):
        return Buf(self.nc.dram_tensor(name, list(shape), dtype, kind="Internal").ap(), name)


class Prog:
    def __init__(self, k, plan):
        self.k = k
        nc = k.nc
        self.plan = plan
        self.st = ExitStack()
        st = self.st
        hT_t = k.sb(st, "hT", [128, KC, S], BF16)
        self.hT = [Buf(hT_t[:, :, tb * 512:(tb + 1) * 512], f"hT{tb}") for tb in range(4)]
        ident_t = k.sb(st, "ident", [128, 128], BF16)
        self.ident = Buf(ident_t[:, :], "ident")
        ones_t = k.sb(st, "ones", [128, 128], BF16)
        self.ones = Buf(ones_t[:, :], "ones")
        small_t = k.sb(st, "small", [128, 64], F32)
        self.small_t = small_t
        self.ps = [Buf(nc.alloc_psum_tensor(f"ps{i}", [128, 512], F32)[:, :], f"ps{i}") for i in range(7)]
        pst = nc.alloc_psum_tensor("pstr", [128, 1024], BF16)
        self.ps_tr = Buf(pst[:, :], "pstr")
        self.x = k.dram_in("x", [S, D])
        self.y = k.dram_out("y", [S, D])
        self.hres = k.dram_tmp("hres", [S, D])
        self.ident_d = k.dram_in("ident_c", [128, 128])
        self.ins = {}

    def inp(self, name, shape):
        if name not in self.ins:
            self.ins[name] = self.k.dram_in(name, shape)
        return self.ins[name]


def emit_consts(P):
    k = P.k
    k.dma("pool", P.ident.v(), P.ident_d.v())
    k.memset("pool", P.ones.v(), 1.0)


def emit_ln_tail(P, st, t, xs, gb, bb, dst, tmp):
    k = P.k
    ts_ = tmp["sets"][t % len(tmp["sets"])]
    stats, mv, sc, xn, xb = ts_["stats"], ts_["mv"], ts_["sc"], ts_["xn"], ts_["xb"]
    for hf in range(2):
        k.op("dve", lambda hf=hf: k.nc.vector.bn_stats(out=stats.ap[:, hf * 6:(hf + 1) * 6], in_=xs.ap[:, hf * 512:(hf + 1) * 512]),
             [xs.v()], [stats.v()])
    k.op("dve", lambda: k.nc.vector.bn_aggr(out=mv.ap, in_=stats.ap), [stats.v()], [mv.v()])
    k.act(sc[:, 0:1], mv[:, 1:2], AF.Sqrt, bias=tmp["eps"][:, 0:1])
    k.recip(sc[:, 1:2], sc[:, 0:1])
    k.stt("dve", sc[:, 2:3], mv[:, 0:1], -1.0, sc[:, 1:2], ALU.mult, ALU.mult)
    k.act(xn.v(), xs.v(), AF.Identity, scale=sc[:, 1:2], bias=sc[:, 2:3])
    k.tt("pool", xn.v(), xn.v(), gb.v(), ALU.mult)
    k.tt("pool", xn.v(), xn.v(), bb.v(), ALU.add)
    k.dma("sp", dst[t * 128:(t + 1) * 128, :], xn.v())
    k.copy("act", xb.v(), xn.v())

    def part_b(t=t, xb=xb):
        for kc in range(KC):
            k.tr(P.ps_tr[:, kc * 128:(kc + 1) * 128], xb[:, kc * 128:(kc + 1) * 128], P.ident.v())
        tb, tt_ = t // 4, t % 4
        k.copy("dve", P.hT[tb][:, :, tt_ * 128:(tt_ + 1) * 128],
               View(P.ps_tr.ap.rearrange("p (k c) -> p k c", c=128), P.ps_tr))

    tmp["pending"].append(part_b)
    while len(tmp["pending"]) > len(tmp["sets"]) - 1:
        tmp["pending"].pop(0)()
    return xn


def flush_ln_tail(P, tmp):
    while tmp["pending"]:
        tmp["pending"].pop(0)()


def alloc_ln_tmp(P, st, pfx, nset=4):
    k = P.k
    tmp = {"sets": [], "pending": []}
    for i in range(nset):
        d = {}
        d["stats"] = Buf(k.sb(st, pfx + f"stats{i}", [128, 12], F32)[:, :])
        d["mv"] = Buf(k.sb(st, pfx + f"mv{i}", [128, 2], F32)[:, :])
        d["sc"] = Buf(k.sb(st, pfx + f"sc{i}", [128, 4], F32)[:, :])
        d["xn"] = Buf(k.sb(st, pfx + f"xn{i}", [128, D], F32)[:, :])
        d["xb"] = Buf(k.sb(st, pfx + f"xb{i}", [128, D], BF16)[:, :])
        tmp["sets"].append(d)
    tmp["eps"] = Buf(k.sb(st, pfx + "eps", [128, 1], F32)[:, :])
    k.memset("pool", tmp["eps"].v(), LN_EPS)
    return tmp


def emit_prologue(P):
    k = P.k
    with ExitStack() as st:
        xin = [Buf(k.sb(st, f"pro_x{i}", [128, D], F32)[:, :]) for i in range(2)]
        xb = [Buf(k.sb(st, f"pro_xb{i}", [128, D], BF16)[:, :]) for i in range(2)]
        for t in range(NT):
            xi = xin[t % 2]
            k.dma("sp", xi.v(), P.x[t * 128:(t + 1) * 128, :])
            k.dma("sp", P.hres[t * 128:(t + 1) * 128, :], xi.v())
            k.copy("act", xb[t % 2].v(), xi.v())
            for kc in range(KC):
                k.tr(P.ps_tr[:, kc * 128:(kc + 1) * 128], xb[t % 2][:, kc * 128:(kc + 1) * 128], P.ident.v())
            tb, tt_ = t // 4, t % 4
            k.copy("dve", P.hT[tb][:, :, tt_ * 128:(tt_ + 1) * 128],
                   View(P.ps_tr.ap.rearrange("p (k c) -> p k c", c=128), P.ps_tr))
        k.barrier()


def load_ln_params(P, st, g_d, b_d, pfx):
    k = P.k
    gb = Buf(k.sb(st, pfx + "g", [128, D], F32)[:, :])
    bb = Buf(k.sb(st, pfx + "b", [128, D], F32)[:, :])
    k.dma("sp", gb.v(), View(g_d.ap.partition_broadcast(128), g_d.buf))
    k.dma("sp", bb.v(), View(b_d.ap.partition_broadcast(128), b_d.buf))
    return gb, bb


def emit_ffn(P, li, moe, dst):
    k = P.k
    nc = k.nc
    j = li // 2
    if moe:
        E, F = NE, D_FFE
        w1 = P.inp("moe_w1", [2, NE, D, D_FFE])
        w3 = P.inp("moe_w3", [2, NE, D, D_FFE])
        w2 = P.inp("moe_w2", [2, NE, D_FFE, D])
        wr = P.inp("moe_wrT", [2, NE, D])
        w1v = lambda e: View(w1.ap[j, e], w1)
        w3v = lambda e: View(w3.ap[j, e], w3)
        w2v = lambda e: View(w2.ap[j, e], w2)
    else:
        E, F = 1, D_FF
        w1 = P.inp("ffn_w1", [2, D, D_FF])
        w3 = P.inp("ffn_w3", [2, D, D_FF])
        w2 = P.inp("ffn_w2", [2, D_FF, D])
        w1v = lambda e: View(w1.ap[j], w1)
        w3v = lambda e: View(w3.ap[j], w3)
        w2v = lambda e: View(w2.ap[j], w2)
    g_d = P.inp("ln_ffn_g", [DEPTH, D])
    b_d = P.inp("ln_ffn_b", [DEPTH, D])
    nj = F // 128
    G = 4
    units = [(e, j0, min(G, nj - j0)) for e in range(E) for j0 in range(0, nj, G)]
    U = len(units)
    with ExitStack() as st:
        comb_t = k.sb(st, "comb", [128, NT, NE], F32)
        comb = Buf(comb_t[:, :, :], "comb")
        if moe:
            with ExitStack() as st2:
                wrb = Buf(k.sb(st2, "wrb", [128, NE, D], F32)[:, :, :], "wrb")
                k.dma("sp", wrb.v(), View(wr.ap[j].partition_broadcast(128), wr))
                hin = [Buf(k.sb(st2, f"rt_h{i}", [128, D], F32)[:, :]) for i in range(2)]
                junk = Buf(k.sb(st2, "rt_junk", [128, D], F32)[:, :])
                lg = Buf(k.sb(st2, "rt_lg", [128, NE], F32)[:, :])
                l2 = Buf(k.sb(st2, "rt_l2", [128, NE], F32)[:, :])
                m1 = Buf(k.sb(st2, "rt_m1", [128, NE], F32)[:, :])
                m2 = Buf(k.sb(st2, "rt_m2", [128, NE], F32)[:, :])
                sc = Buf(k.sb(st2, "rt_sc", [128, 8], F32)[:, :])
                for t in range(NT):
                    hi = hin[t % 2]
                    k.dma("sp", hi.v(), P.hres[t * 128:(t + 1) * 128, :])
                    for e in range(NE):
                        k.stt("dve", junk.v(), hi.v(), 1.0, wrb[:, e, :], ALU.mult, ALU.mult, accum=lg[:, e:e + 1])
                    k.op("dve", lambda: nc.vector.reduce_max(out=sc.ap[:, 0:1], in_=lg.ap, axis=AX.X), [lg.v()], [sc.v()])
                    k.ts("dve", m1.v(), lg.v(), sc[:, 0:1], ALU.is_equal)
                    k.stt("dve", l2.v(), m1.v(), -1e30, lg.v(), ALU.mult, ALU.add)
                    k.op("dve", lambda: nc.vector.reduce_max(out=sc.ap[:, 1:2], in_=l2.ap, axis=AX.X), [l2.v()], [sc.v()])
                    k.ts("dve", m2.v(), l2.v(), sc[:, 1:2], ALU.is_equal)
                    k.tt("dve", sc[:, 2:3], sc[:, 1:2], sc[:, 0:1], ALU.subtract)
                    k.act(sc[:, 3:4], sc[:, 2:3], AF.Exp)
                    k.ts("dve", sc[:, 4:5], sc[:, 3:4], 1.0, ALU.add)
                    k.recip(sc[:, 5:6], sc[:, 4:5])
                    k.tt("dve", sc[:, 6:7], sc[:, 3:4], sc[:, 5:6], ALU.mult)
                    k.ts("dve", m1.v(), m1.v(), sc[:, 5:6], ALU.mult)
                    k.stt("dve", comb[:, t, :], m2.v(), sc[:, 6:7], m1.v(), ALU.mult, ALU.add)
                k.barrier()
        yacc_t = k.sb(st, "yacc", [128, NT, D], F32)
        yacc = [[Buf(yacc_t[:, t, hf * 512:(hf + 1) * 512]) for hf in range(2)] for t in range(NT)]
        stc = ExitStack()
        htg_t = [k.sb(stc, f"htg{i}", [128, G, S], BF16) for i in range(2)]
        htg = [[[Buf(htg_t[i][:, jj, tb * 512:(tb + 1) * 512]) for tb in range(4)] for jj in range(G)] for i in range(2)]
        w1g = [Buf(k.sb(stc, f"w1g{i}", [128, KC, G * 128], BF16)[:, :, :]) for i in range(2)]
        w3g = [Buf(k.sb(stc, f"w3g{i}", [128, KC, G * 128], BF16)[:, :, :]) for i in range(2)]
        w2g = [Buf(k.sb(stc, f"w2g{i}", [128, G, D], BF16)[:, :, :]) for i in range(2)]
        sil = [Buf(k.sb(stc, f"sil{i}", [128, 512], F32)[:, :]) for i in range(2)]

        def loadA(u):
            e, j0, n = units[u]
            s = u % 2
            k.dma("pool", w1g[s][:, :, 0:n * 128],
                  View(w1v(e).ap.rearrange("(kc p) f -> p kc f", p=128)[:, :, j0 * 128:(j0 + n) * 128], w1))
            k.dma("pool", w3g[s][:, :, 0:n * 128],
                  View(w3v(e).ap.rearrange("(kc p) f -> p kc f", p=128)[:, :, j0 * 128:(j0 + n) * 128], w3))

        def loadB(u):
            e, j0, n = units[u]
            s = u % 2
            k.dma("pool", w2g[s][:, 0:n, :],
                  View(w2v(e).ap[j0 * 128:(j0 + n) * 128, :].rearrange("(j p) d -> p j d", p=128), w2))

        cnt1 = [0]

        def phase1(u):
            e, j0, n = units[u]
            s = u % 2
            for jj in range(n):
                for tb in range(4):
                    c = cnt1[0] % 2
                    cnt1[0] += 1
                    pa, pb = P.ps[2 * c], P.ps[2 * c + 1]
                    for (W, pp) in ((w1g[s], pa), (w3g[s], pb)):
                        for kc in range(KC):
                            k.mm(pp.v(), W[:, kc, jj * 128:(jj + 1) * 128], P.hT[tb][:, kc, :], start=(kc == 0), stop=(kc == KC - 1))
                    k.act(sil[c].v(), pa.v(), AF.Silu)
                    k.tt("dve", htg[s][jj][tb].v(), sil[c].v(), pb.v(), ALU.mult)

        cnt2 = [0]

        def phase2(u):
            e, j0, n = units[u]
            s = u % 2
            for t in range(NT):
                tb, tt_ = t // 4, t % 4
                for hf in range(2):
                    py = P.ps[4 + cnt2[0] % 3]
                    cnt2[0] += 1
                    for jj in range(n):
                        k.mm(py.v(), htg[s][jj][tb][:, tt_ * 128:(tt_ + 1) * 128], w2g[s][:, jj, hf * 512:(hf + 1) * 512],
                             start=(jj == 0), stop=(jj == n - 1))
                    ya = yacc[t][hf]
                    if moe:
                        if u == 0:
                            k.ts("dve", ya.v(), py.v(), comb[:, t, e:e + 1], ALU.mult)
                        else:
                            k.stt("dve", ya.v(), py.v(), comb[:, t, e:e + 1], ya.v(), ALU.mult, ALU.add)
                    else:
                        if u == 0:
                            k.copy("dve", ya.v(), py.v())
                        else:
                            k.tt("dve", ya.v(), py.v(), ya.v(), ALU.add)

        loadA(0)
        loadB(0)
        if U > 1:
            loadA(1)
            loadB(1)
        phase1(0)
        for u in range(U):
            if u + 1 < U:
                phase1(u + 1)
            if u + 2 < U:
                loadA(u + 2)
            phase2(u)
            if u + 2 < U:
                loadB(u + 2)
        k.barrier()
        stc.close()
        gb, bb = load_ln_params(P, st, View(g_d.ap[li:li + 1, :], g_d), View(b_d.ap[li:li + 1, :], b_d), "ffn_ln")
        tmp = alloc_ln_tmp(P, st, "ffn_")
        xin = [Buf(k.sb(st, f"ffn_xin{i}", [128, D], F32)[:, :]) for i in range(3)]
        k.dma("sp", xin[0].v(), P.hres[0:128, :])
        k.dma("sp", xin[1].v(), P.hres[128:256, :])
        for t in range(NT):
            if t + 2 < NT:
                k.dma("sp", xin[(t + 2) % 3].v(), P.hres[(t + 2) * 128:(t + 3) * 128, :])
            xi = xin[t % 3]
            for hf in range(2):
                k.stt("dve", xi[:, hf * 512:(hf + 1) * 512], xi[:, hf * 512:(hf + 1) * 512], ALPHA, yacc[t][hf].v(), ALU.mult, ALU.add)
            emit_ln_tail(P, st, t, xi, gb, bb, dst, tmp)
        flush_ln_tail(P, tmp)
        k.barrier()


NG = 7
NST = 15
WROW = 2048


def emit_ffn_sparse(P, li, dst):
    k = P.k
    nc = k.nc
    j = li // 2
    I32 = mybir.dt.int32
    nrows = 2 * NE * NG * 128 * 2
    w1r = P.inp("moe_w1r", [nrows, WROW])
    w3r = P.inp("moe_w3r", [nrows, WROW])
    w2r = P.inp("moe_w2r", [nrows, WROW])
    if ROUTER_PE:
        wrn = P.inp("moe_w_router", [2, D, NE])
    else:
        wr = P.inp("moe_wrT", [2, NE, D])
    tri_d = P.inp("tri_c", [128, 128])
    io_d = P.inp("iota2_c", [128, 1])
    g_d = P.inp("ln_ffn_g", [DEPTH, D])
    b_d = P.inp("ln_ffn_b", [DEPTH, D])
    if not hasattr(P, "xs_d"):
        P.xs_d = k.dram_tmp("xs_sorted", [NST * 512, D], BF16)
        P.ys_d = k.dram_tmp("ys_sorted", [NST * 512, D], F32)
    xs_d, ys_d = P.xs_d, P.ys_d
    with ExitStack() as st:
        gs = Buf(k.sb(st, "gs", [128, NT, 2], F32)[:, :, :], "gs")
        slot_i = Buf(k.sb(st, "slot_i", [128, NT, 2], I32)[:, :, :], "slot_i")
        idxw = Buf(k.sb(st, "idxw", [128, NST, NG, 2], I32)[:, :, :, :], "idxw")
        with ExitStack() as st2:
            if not ROUTER_PE:
                wrb = Buf(k.sb(st2, "wrb", [128, NE, D], F32)[:, :, :], "wrb")
                k.dma("sp", wrb.v(), View(wr.ap[j].partition_broadcast(128), wr))
            tri = Buf(k.sb(st2, "tri", [128, 128], BF16)[:, :], "tri")
            k.dma("pool", tri.v(), tri_d.v())
            io2 = Buf(k.sb(st2, "io2", [128, 1], F32)[:, :], "io2")
            k.dma("sp", io2.v(), io_d.v())
            zt = Buf(k.sb(st2, "zt", [128, 4096], BF16)[:, :], "zt")
            k.memset("pool", zt.v(), 0.0)
            xs_flat = View(xs_d.ap.rearrange("(p r) d -> p (r d)", p=128), xs_d)
            for i in range(NST * 4 * D // 4096):
                k.dma("sp", xs_flat[:, i * 4096:(i + 1) * 4096], zt.v())
            m1s = Buf(k.sb(st2, "m1s", [128, NT, NE], F32)[:, :, :], "m1s")
            m2s = Buf(k.sb(st2, "m2s", [128, NT, NE], F32)[:, :, :], "m2s")
            abf = Buf(k.sb(st2, "abf", [128, NT, NE], BF16)[:, :, :], "abf")
            hin = [Buf(k.sb(st2, f"rt_h{i}", [128, D], F32)[:, :]) for i in range(2)]
            xbt_t = k.sb(st2, "rt_xb", [128, NT, D], BF16)
            xbt = [Buf(xbt_t[:, t, :]) for t in range(NT)]
            junk = Buf(k.sb(st2, "rt_junk", [128, D], F32)[:, :])
            lg = Buf(k.sb(st2, "rt_lg", [128, NE], F32)[:, :])
            l2 = Buf(k.sb(st2, "rt_l2", [128, NE], F32)[:, :])
            sc = Buf(k.sb(st2, "rt_sc", [128, 8], F32)[:, :])
            lga = Buf(k.sb(st2, "rt_lga", [128, NT, NE], F32)[:, :, :], "lga")
            l2a = Buf(k.sb(st2, "rt_l2a", [128, NT, NE], F32)[:, :, :], "l2a")
            mxa = Buf(k.sb(st2, "rt_mxa", [128, 4, NT], F32)[:, :, :], "mxa")
            if ROUTER_PE:
                id32 = Buf(k.sb(st2, "id32", [128, 128], F32)[:, :], "id32")
                k.dma("sp", id32.v(), P.ident_d.v())
                wr32 = Buf(k.sb(st2, "wr32", [128, KC, NE], F32)[:, :, :], "wr32")
                k.dma("sp", wr32.v(), View(wrn.ap[j].rearrange("(kc p) e -> p kc e", p=128), wrn))
                h32 = [Buf(k.sb(st2, f"h32_{i}", [128, KC, 128], F32)[:, :, :]) for i in range(2)]
                k.dma("sp", hin[0].v(), P.hres[0:128, :])
                for t in range(NT):
                    hi = hin[t % 2]
                    if t + 1 < NT:
                        k.dma("sp", hin[(t + 1) % 2].v(), P.hres[(t + 1) * 128:(t + 2) * 128, :])
                    k.copy("act", xbt[t].v(), hi.v())
                    pa, pb = P.ps[2 * (t % 2)], P.ps[2 * (t % 2) + 1]
                    for kc in range(KC):
                        pq = pa if kc < 4 else pb
                        k.tr(pq[:, (kc % 4) * 128:(kc % 4 + 1) * 128], hi[:, kc * 128:(kc + 1) * 128], id32.v())
                    hv = View(h32[t % 2].ap.rearrange("p k c -> p (k c)"), h32[t % 2])
                    k.copy("dve", hv[:, 0:512], pa.v())
                    k.copy("act", hv[:, 512:1024], pb.v())
                    for kc in range(KC):
                        k.mm(P.ps[4][:, t * 8:(t + 1) * 8], h32[t % 2][:, kc, :], wr32[:, kc, :], start=(kc == 0), stop=(kc == KC - 1))
                k.copy("dve", View(lga.ap.rearrange("p t e -> p (t e)"), lga), P.ps[4][:, 0:NT * NE])
            else:
                for t in range(NT):
                    hi = hin[t % 2]
                    k.dma("sp", hi.v(), P.hres[t * 128:(t + 1) * 128, :])
                    k.copy("act", xbt[t].v(), hi.v())
                    for e in range(NE):
                        k.stt("dve", junk.v(), hi.v(), 1.0, wrb[:, e, :], ALU.mult, ALU.mult, accum=lga[:, t, e:e + 1])
            bc = lambda v: View(v.ap.unsqueeze(2).broadcast_to([128, NT, NE]), v.buf)
            k.op("dve", lambda: nc.vector.reduce_max(out=mxa.ap[:, 0, :], in_=lga.ap, axis=AX.X), [lga.v()], [mxa.v()])
            k.tt("dve", m1s.v(), lga.v(), bc(mxa[:, 0, :]), ALU.is_equal)
            k.stt("dve", l2a.v(), m1s.v(), -1e30, lga.v(), ALU.mult, ALU.add)
            k.op("dve", lambda: nc.vector.reduce_max(out=mxa.ap[:, 1, :], in_=l2a.ap, axis=AX.X), [l2a.v()], [mxa.v()])
            k.tt("dve", m2s.v(), l2a.v(), bc(mxa[:, 1, :]), ALU.is_equal)
            k.tt("dve", mxa[:, 2, :], mxa[:, 1, :], mxa[:, 0, :], ALU.subtract)
            k.act(mxa[:, 2, :], mxa[:, 2, :], AF.Exp)
            k.ts("dve", mxa[:, 3, :], mxa[:, 2, :], 1.0, ALU.add)
            k.recip(gs[:, :, 0], mxa[:, 3, :])
            k.tt("dve", gs[:, :, 1], mxa[:, 2, :], gs[:, :, 0], ALU.mult)
            k.tt("dve", abf.v(), m1s.v(), m2s.v(), ALU.add)
            pp = P.ps[0]
            for t in range(NT):
                for tp in range(t):
                    k.mm(pp[:, t * 8:(t + 1) * 8], P.ones.v(), abf[:, tp, :], start=(tp == 0), stop=False)
                k.mm(pp[:, t * 8:(t + 1) * 8], tri.v(), abf[:, t, :], start=(t == 0), stop=True)
            for t in range(NT):
                k.mm(pp[:, 128:136], P.ones.v(), abf[:, t, :], start=(t == 0), stop=(t == NT - 1))
            posf = Buf(k.sb(st2, "posf", [128, 136], F32)[:, :], "posf")
            k.copy("dve", posf.v(), pp[:, 0:136])
            w8 = Buf(k.sb(st2, "w8", [128, 8, 8], F32)[:, :, :], "w8")
            for m in range(4):
                k.ts("dve", w8[:, m, :], posf[:, 128:136], 512.0 * m, ALU.is_gt)
            k.tt("dve", w8[:, 0, :], w8[:, 0, :], w8[:, 1, :], ALU.add)
            k.tt("dve", w8[:, 2, :], w8[:, 2, :], w8[:, 3, :], ALU.add)
            k.tt("dve", w8[:, 0, :], w8[:, 0, :], w8[:, 2, :], ALU.add)
            k.ts("dve", w8[:, 4, :], w8[:, 0, :], 512.0, ALU.mult)
            k.memset("pool", w8[:, 5, :], 0.0)
            for e in range(1, NE):
                k.tt("dve", w8[:, 5, e:e + 1], w8[:, 5, e - 1:e], w8[:, 4, e - 1:e], ALU.add)
            k.tt("dve", w8[:, 6, :], w8[:, 5, :], w8[:, 4, :], ALU.add)
            sf = Buf(k.sb(st2, "sf", [128, NT, NE], F32)[:, :, :], "sf")
            slotf = Buf(k.sb(st2, "slotf", [128, 2, NT], F32)[:, :, :], "slotf")
            j8 = Buf(k.sb(st2, "j8", [128, NE], F32)[:, :], "j8")
            pos3 = View(posf.ap[:, 0:128].rearrange("p (t e) -> p t e", e=NE), posf)
            k.tt("dve", sf.v(), pos3, View(w8.ap[:, 5, :].unsqueeze(1).broadcast_to([128, NT, NE]), w8), ALU.add)
            k.tt("dve", l2a.v(), m1s.v(), sf.v(), ALU.mult)
            k.op("dve", lambda: nc.vector.reduce_sum(out=slotf.ap[:, 0, :], in_=l2a.ap, axis=AX.X), [l2a.v()], [slotf.v()])
            k.tt("dve", l2a.v(), m2s.v(), sf.v(), ALU.mult)
            k.op("dve", lambda: nc.vector.reduce_sum(out=slotf.ap[:, 1, :], in_=l2a.ap, axis=AX.X), [l2a.v()], [slotf.v()])
            k.copy("dve", slot_i[:, :, 0], slotf[:, 0, :])
            k.copy("dve", slot_i[:, :, 1], slotf[:, 1, :])
            esf = Buf(k.sb(st2, "esf", [128, NST], F32)[:, :], "esf")
            for s_ in range(NST):
                k.ts("dve", j8.v(), w8[:, 6, :], 512.0 * s_, ALU.is_le, s2=0.0, op1=ALU.add, accum=esf[:, s_:s_ + 1])
            k.ts("dve", esf.v(), esf.v(), float(NE - 1), ALU.min)
            k.ts("dve", esf.v(), esf.v(), float(NG * 128 * 2), ALU.mult, s2=io2[:, 0:1], op1=ALU.add)
            idxf = Buf(k.sb(st2, "idxf", [128, NST, NG, 2], F32)[:, :, :, :], "idxf")
            for g in range(NG):
                for hf in range(2):
                    k.ts("dve", idxf[:, :, g, hf], esf.v(), float(((j * NE) * NG + g) * 128 * 2 + hf), ALU.add)
            k.copy("dve", idxw.v(), idxf.v())
            k.barrier()
            for t in range(NT):
                for sl in range(2):
                    k.dma_indirect(out=xs_d.v(), out_off=slot_i[:, t, sl:sl + 1], in_=xbt[t].v(), in_off=None)
            k.barrier()
        with ExitStack() as st3:
            G = 4
            xsT = [Buf(k.sb(st3, f"xsT{i}", [128, KC, 512], BF16)[:, :, :]) for i in range(2)]
            xrow = [Buf(k.sb(st3, f"xrow{i}", [128, D], BF16)[:, :]) for i in range(2)]
            yacc_t = [k.sb(st3, f"yacc{i}", [128, 4, D], F32) for i in range(2)]
            yacc = [[[Buf(yacc_t[i][:, r, hf * 512:(hf + 1) * 512]) for hf in range(2)] for r in range(4)] for i in range(2)]
            htg = [[Buf(k.sb(st3, f"htg{i}_{jj}", [128, 512], BF16)[:, :]) for jj in range(G)] for i in range(2)]
            w1g = [Buf(k.sb(st3, f"w1g{i}", [128, KC, 512], BF16)[:, :, :]) for i in range(3)]
            w3g = [Buf(k.sb(st3, f"w3g{i}", [128, KC, 512], BF16)[:, :, :]) for i in range(3)]
            w2g = [Buf(k.sb(st3, f"w2g{i}", [128, G, D], BF16)[:, :, :]) for i in range(2)]
            sil = [Buf(k.sb(st3, f"sil{i}", [128, 512], F32)[:, :]) for i in range(2)]
            units = [(s_, g) for s_ in range(NST) for g in range(NG)]
            U = len(units)
            ptr3 = View(P.ps_tr.ap.rearrange("p (k c) -> p k c", c=128), P.ps_tr)
            xc = [0]

            def load_x(s_):
                for r in range(4):
                    xr = xrow[xc[0] % 2]
                    xc[0] += 1
                    k.dma("sp", xr.v(), xs_d[s_ * 512 + r * 128:s_ * 512 + (r + 1) * 128, :])
                    for kc in range(KC):
                        k.tr(P.ps_tr[:, kc * 128:(kc + 1) * 128], xr[:, kc * 128:(kc + 1) * 128], P.ident.v())
                    k.copy("dve", xsT[s_ % 2][:, :, r * 128:(r + 1) * 128], ptr3)

            def loadA(u):
                s_, g = units[u]
                sl = u % 3
                for hf in range(2):
                    k.dma_indirect(out=View(w1g[sl].ap[:, hf * 4:(hf + 1) * 4, :].rearrange("p k f -> p (k f)"), w1g[sl]),
                                   out_off=None, in_=w1r.v(), in_off=idxw[:, s_, g, hf:hf + 1])
                    k.dma_indirect(out=View(w3g[sl].ap[:, hf * 4:(hf + 1) * 4, :].rearrange("p k f -> p (k f)"), w3g[sl]),
                                   out_off=None, in_=w3r.v(), in_off=idxw[:, s_, g, hf:hf + 1])

            def loadB(u):
                s_, g = units[u]
                sl = u % 2
                for hf in range(2):
                    k.dma_indirect(out=View(w2g[sl].ap[:, hf * 2:(hf + 1) * 2, :].rearrange("p k f -> p (k f)"), w2g[sl]),
                                   out_off=None, in_=w2r.v(), in_off=idxw[:, s_, g, hf:hf + 1])

            cnt1 = [0]

            def phase1(u):
                s_, g = units[u]
                sl = u % 2
                sa = u % 3
                for jj in range(G):
                    c = cnt1[0] % 2
                    cnt1[0] += 1
                    pa, pb = P.ps[2 * c], P.ps[2 * c + 1]
                    for (W, pq) in ((w1g[sa], pa), (w3g[sa], pb)):
                        for kc in range(KC):
                            k.mm(pq.v(), W[:, kc, jj * 128:(jj + 1) * 128], xsT[s_ % 2][:, kc, :], start=(kc == 0), stop=(kc == KC - 1))
                    k.act(sil[c].v(), pa.v(), AF.Silu)
                    k.tt("dve", htg[sl][jj].v(), sil[c].v(), pb.v(), ALU.mult)

            cnt2 = [0]

            def phase2(u):
                s_, g = units[u]
                sl = u % 2
                for r in range(4):
                    for hf in range(2):
                        py = P.ps[4 + cnt2[0] % 3]
                        cnt2[0] += 1
                        for jj in range(G):
                            k.mm(py.v(), htg[sl][jj][:, r * 128:(r + 1) * 128], w2g[sl][:, jj, hf * 512:(hf + 1) * 512],
                                 start=(jj == 0), stop=(jj == G - 1))
                        ya = yacc[s_ % 2][r][hf]
                        if g == 0:
                            k.copy("dve", ya.v(), py.v())
                        else:
                            k.tt("dve", ya.v(), py.v(), ya.v(), ALU.add)
                if g == NG - 1:
                    for r in range(4):
                        k._deps("sp", [yacc[s_ % 2][r][1].v()], [])
                        k.dma("sp", ys_d[s_ * 512 + r * 128:s_ * 512 + (r + 1) * 128, :],
                              View(yacc_t[s_ % 2][:, r, :], yacc[s_ % 2][r][0]))

            load_x(0)
            loadA(0)
            loadB(0)
            loadA(1)
            loadB(1)
            loadA(2)
            phase1(0)
            for u in range(U):
                s_, g = units[u]
                if g == 2 and s_ + 1 < NST:
                    load_x(s_ + 1)
                if u + 1 < U:
                    phase1(u + 1)
                if u + 3 < U:
                    loadA(u + 3)
                phase2(u)
                if u + 2 < U:
                    loadB(u + 2)
            k.barrier()
        with ExitStack() as st4:
            gb, bb = load_ln_params(P, st4, View(g_d.ap[li:li + 1, :], g_d), View(b_d.ap[li:li + 1, :], b_d), "ffn_ln")
            tmp = alloc_ln_tmp(P, st4, "ffn_")
            xin = [Buf(k.sb(st4, f"ffn_xin{i}", [128, D], F32)[:, :]) for i in range(3)]
            y1 = [Buf(k.sb(st4, f"ffn_y1{i}", [128, D], F32)[:, :]) for i in range(3)]
            y2 = [Buf(k.sb(st4, f"ffn_y2{i}", [128, D], F32)[:, :]) for i in range(3)]

            def fetch(t):
                k.dma("sp", xin[t % 3].v(), P.hres[t * 128:(t + 1) * 128, :])
                k.dma_indirect(out=y1[t % 3].v(), out_off=None, in_=ys_d.v(), in_off=slot_i[:, t, 0:1])
                k.dma_indirect(out=y2[t % 3].v(), out_off=None, in_=ys_d.v(), in_off=slot_i[:, t, 1:2])

            fetch(0)
            fetch(1)
            for t in range(NT):
                if t + 2 < NT:
                    fetch(t + 2)
                xi, a, b = xin[t % 3], y1[t % 3], y2[t % 3]
                k.act(a.v(), a.v(), AF.Identity, scale=gs[:, t, 0:1])
                k.stt("dve", a.v(), b.v(), gs[:, t, 1:2], a.v(), ALU.mult, ALU.add)
                k.stt("dve", xi.v(), xi.v(), ALPHA, a.v(), ALU.mult, ALU.add)
                emit_ln_tail(P, st4, t, xi, gb, bb, dst, tmp)
            flush_ln_tail(P, tmp)
            k.barrier()


def load_w_bf16(P, st, name, src_view, kchunks, ncols, col0=0):
    k = P.k
    b = Buf(k.sb(st, name, [128, kchunks, ncols], BF16)[:, :, :], name)
    k.dma("pool", b.v(), View(src_view.ap.rearrange("(kc p) f -> p kc f", p=128)[:, :, col0:col0 + ncols], src_view.buf))
    return b


def emit_mix_out(P, li, oT, wo_view, dst):
    k = P.k
    g_d = P.inp("ln_mix_g", [DEPTH, D])
    b_d = P.inp("ln_mix_b", [DEPTH, D])
    with ExitStack() as st:
        wo = load_w_bf16(P, st, "wo_sb", wo_view, KC, D)
        gb, bb = load_ln_params(P, st, View(g_d.ap[li:li + 1, :], g_d), View(b_d.ap[li:li + 1, :], b_d), "mix_ln")
        tmp = alloc_ln_tmp(P, st, "mix_")
        xin = [Buf(k.sb(st, f"mix_xin{i}", [128, D], F32)[:, :]) for i in range(3)]
        k.dma("sp", xin[0].v(), P.hres[0:128, :])
        k.dma("sp", xin[1].v(), P.hres[128:256, :])
        c = 0
        for t in range(NT):
            if t + 2 < NT:
                k.dma("sp", xin[(t + 2) % 3].v(), P.hres[(t + 2) * 128:(t + 3) * 128, :])
            xi = xin[t % 3]
            for hf in range(2):
                py = P.ps[c % 4]
                c += 1
                for kc in range(KC):
                    k.mm(py.v(), oT[kc][:, t * 128:(t + 1) * 128], wo[:, kc, hf * 512:(hf + 1) * 512], start=(kc == 0), stop=(kc == KC - 1))
                k.stt("dve", xi[:, hf * 512:(hf + 1) * 512], xi[:, hf * 512:(hf + 1) * 512], ALPHA, py.v(), ALU.mult, ALU.add)
            emit_ln_tail(P, st, t, xi, gb, bb, dst, tmp)
        flush_ln_tail(P, tmp)
        k.barrier()


def emit_mix_gmlp(P, li, dst):
    k = P.k
    nc = k.nc
    j = li // 4
    w_in = P.inp("sg_w_in", [1, D, 2 * D])
    vg_d = P.inp("sg_v_norm_g", [1, D])
    vb_d = P.inp("sg_v_norm_b", [1, D])
    ws_d = P.inp("sg_w_s", [1, 8, 128, 128])
    bs_d = P.inp("sg_b_s", [1, 8, 128])
    wout = P.inp("sg_w_out", [1, D, D])
    with ExitStack() as st:
        uT_t = k.sb(st, "uT", [128, KC, S], BF16)
        uT = [[Buf(uT_t[:, c, tb * 512:(tb + 1) * 512]) for tb in range(4)] for c in range(KC)]
        vln_t = k.sb(st, "vln", [128, NT, D], BF16)
        vln = [Buf(vln_t[:, t, :]) for t in range(NT)]
        wsT = Buf(k.sb(st, "wsT", [128, 8, 128], BF16)[:, :, :], "wsT")
        bs4 = Buf(k.sb(st, "bs4", [1, 8, 512], BF16)[:, :, :], "bs4")
        with ExitStack() as st2:
            win = load_w_bf16(P, st2, "w_in_sb", View(w_in.ap[j], w_in), KC, 2 * D)
            ws_sb = Buf(k.sb(st2, "ws_sb", [128, 8, 128], BF16)[:, :, :])
            k.dma("pool", ws_sb.v(), View(ws_d.ap[j].rearrange("g i j -> i g j"), ws_d))
            for g in range(8):
                k.tr(P.ps_tr[:, g * 128:(g + 1) * 128], ws_sb[:, g, :], P.ident.v())
            k.copy("dve", wsT.v(), View(P.ps_tr.ap.rearrange("p (k c) -> p k c", c=128), P.ps_tr))
            k.memset("pool", wsT[64:128, :, 0:64], 0.0)
            bs_f = Buf(k.sb(st2, "bs_f", [1, 8, 128], F32)[:, :, :])
            k.dma("sp", bs_f.v(), View(bs_d.ap[j:j + 1], bs_d))
            for r in range(4):
                k.copy("dve", bs4[:, :, r * 128:(r + 1) * 128], bs_f.v())
            vg, vb = load_ln_params(P, st2, View(vg_d.ap[j:j + 1, :], vg_d), View(vb_d.ap[j:j + 1, :], vb_d), "sg_ln")
            c2 = 0
            for c in range(KC):
                for tb in range(4):
                    pp = P.ps[c2 % 4]
                    c2 += 1
                    for kc in range(KC):
                        k.mm(pp.v(), win[:, kc, c * 128:(c + 1) * 128], P.hT[tb][:, kc, :], start=(kc == 0), stop=(kc == KC - 1))
                    k.act(uT[c][tb].v(), pp.v(), AF.Gelu_apprx_tanh)
            vt = [Buf(k.sb(st2, f"sg_v{i}", [128, D], F32)[:, :]) for i in range(2)]
            stats = Buf(k.sb(st2, "sg_stats", [128, 12], F32)[:, :])
            mv = Buf(k.sb(st2, "sg_mv", [128, 2], F32)[:, :])
            sc = Buf(k.sb(st2, "sg_sc", [128, 4], F32)[:, :])
            eps = Buf(k.sb(st2, "sg_eps", [128, 1], F32)[:, :])
            k.memset("pool", eps.v(), LN_EPS)
            for t in range(NT):
                tb, tt_ = t // 4, t % 4
                v = vt[t % 2]
                for hf in range(2):
                    pp = P.ps[4 + c2 % 3]
                    c2 += 1
                    for kc in range(KC):
                        k.mm(pp.v(), P.hT[tb][:, kc, tt_ * 128:(tt_ + 1) * 128], win[:, kc, D + hf * 512:D + (hf + 1) * 512],
                             start=(kc == 0), stop=(kc == KC - 1))
                    k.act(v[:, hf * 512:(hf + 1) * 512], pp.v(), AF.Gelu_apprx_tanh)
                    k.op("dve", lambda hf=hf, v=v: nc.vector.bn_stats(out=stats.ap[:, hf * 6:(hf + 1) * 6], in_=v.ap[:, hf * 512:(hf + 1) * 512]),
                         [v.v()], [stats.v()])
                k.op("dve", lambda: nc.vector.bn_aggr(out=mv.ap, in_=stats.ap), [stats.v()], [mv.v()])
                k.act(sc[:, 0:1], mv[:, 1:2], AF.Sqrt, bias=eps[:, 0:1])
                k.recip(sc[:, 1:2], sc[:, 0:1])
                k.stt("dve", sc[:, 2:3], mv[:, 0:1], -1.0, sc[:, 1:2], ALU.mult, ALU.mult)
                k.act(v.v(), v.v(), AF.Identity, scale=sc[:, 1:2], bias=sc[:, 2:3])
                k.tt("pool", v.v(), v.v(), vg.v(), ALU.mult)
                k.tt("pool", vln[t].v(), v.v(), vb.v(), ALU.add)
            k.barrier()
        c3 = 0
        for g in range(8):
            for tb in range(4):
                pp = P.ps[c3 % 4]
                c3 += 1
                k.mm(pp.v(), P.ones[0:1, :], bs4[0:1, g, :], start=True, stop=False)
                for r in range(4):
                    t = tb * 4 + r
                    k.mm(pp[:, r * 128:(r + 1) * 128], vln[t][:, g * 128:(g + 1) * 128], wsT[:, g, :], start=False, stop=(r == 3))
                k.tt("dve", uT[g][tb].v(), uT[g][tb].v(), pp.v(), ALU.mult)
        sT = [Buf(uT_t[:, c, :]) for c in range(KC)]
        k.barrier()
        emit_mix_out(P, li, sT, View(wout.ap[j], wout), dst)


class Rot:
    def __init__(self, n):
        self.n = n
        self.i = 0

    def nxt(self):
        v = self.i % self.n
        self.i += 1
        return v


class AttnPipe:
    def __init__(self, depth=2):
        self.depth = depth
        self.q = []
        self.deferred = []

    def push(self, cfn, after=None):
        self.q.append((cfn, after))
        ready = [fn for (n, fn) in self.deferred if n <= 1]
        self.deferred = [(n - 1, fn) for (n, fn) in self.deferred if n > 1]
        for fn in ready:
            fn()
        while len(self.q) > self.depth:
            self._pop()

    def _pop(self):
        cfn, after = self.q.pop(0)
        cfn()
        if after is not None:
            after()

    def defer(self, n, fn):
        self.deferred.append((n, fn))

    def flush(self):
        while self.q:
            self._pop()
        while self.deferred:
            d = self.deferred
            self.deferred = []
            for (_, fn) in d:
                fn()


def run_blocks(P, pipe, blocks, q_of, k_of, v_of, ones_v, psO, psD, pts, rs, rp, scale, bias_of=None, after=None):
    k = P.k
    n = len(blocks)
    for bi, (kb, c0, N, zero, extra) in enumerate(blocks):
        pS = P.ps[rs.nxt()]
        k.mm(pS[:, 0:N], k_of(kb), q_of(c0, N), start=True, stop=(bias_of is None))
        if bias_of is not None:
            k.mm(pS[:, 0:N], P.ident.v(), bias_of(extra, N), start=False, stop=True)
        pt = pts[rp.nxt()]
        k.act(pt[:, 0:N], pS[:, 0:N], AF.Exp, scale=scale)
        if zero is not None:
            (r0, r1, z0, z1) = zero
            k.memset("pool", pt[r0:r1, z0:z1], 0.0)

        def cfn(o=psO[:, c0:c0 + N], dn=psD[:, c0:c0 + N], vv=v_of(kb), pv=pt[:, 0:N], st_=(bi == 0), sp_=(bi == n - 1)):
            k.mm(o, vv, pv, start=st_, stop=sp_)
            k.mm(dn, ones_v, pv, start=st_, stop=sp_)

        pipe.push(cfn, after if bi == n - 1 else None)


def causal_blocks(qb):
    bl = []
    for kb in range(4 * qb + 4):
        r = kb - 4 * qb
        if r <= 0:
            bl.append((kb, 0, 512, (64, 128, 0, 64) if r == 0 else None, None))
        else:
            bl.append((kb, 128 * r, 512 - 128 * r, (64, 128, 0, 64), None))
    return bl


def emit_mix_diff(P, li, dst):
    k = P.k
    nc = k.nc
    j = li // 4
    lam_init = 0.8 - 0.6 * math.exp(-0.3 * li)
    wq = P.inp("diff_wq", [1, D, D])
    wk = P.inp("diff_wk", [1, D, D])
    wv = P.inp("diff_wv", [1, D, D])
    wo = P.inp("diff_wo", [1, D, D])
    lqk = [P.inp(n, [1, 64]) for n in ("diff_lq1", "diff_lk1", "diff_lq2", "diff_lk2")]
    subg = P.inp("diff_sub_g", [1, 128])
    scale = 64 ** -0.5
    with ExitStack() as st:
        oT_t = k.sb(st, "oT", [128, KC, S], BF16)
        oTb = [[Buf(oT_t[:, c, qb * 512:(qb + 1) * 512]) for qb in range(4)] for c in range(KC)]
        sti = ExitStack()
        V_t = k.sb(sti, "Vall", [128, NT, D], BF16)
        V = [Buf(V_t[:, t, :]) for t in range(NT)]
        nlam = Buf(k.sb(sti, "nlam", [128, 1], F32)[:, :])
        gsc = Buf(k.sb(sti, "gsc", [128, 1], F32)[:, :])
        eps5 = Buf(k.sb(sti, "eps5", [128, 1], F32)[:, :])
        k.memset("pool", eps5.v(), 1e-5)
        with ExitStack() as st2:
            lt = Buf(k.sb(st2, "lqk", [128, 4, 64], F32)[:, :, :])
            for i in range(4):
                k.dma("sp", lt[:, i, :], View(lqk[i].ap[j:j + 1, :].partition_broadcast(128), lqk[i]))
            junk = Buf(k.sb(st2, "ljunk", [128, 64], F32)[:, :])
            acc = Buf(k.sb(st2, "lacc", [128, 4], F32)[:, :])
            k.stt("dve", junk.v(), lt[:, 0, :], 1.0, lt[:, 1, :], ALU.mult, ALU.mult, accum=acc[:, 0:1])
            k.stt("dve", junk.v(), lt[:, 2, :], 1.0, lt[:, 3, :], ALU.mult, ALU.mult, accum=acc[:, 1:2])
            k.act(acc[:, 2:3], acc[:, 0:1], AF.Exp)
            k.act(acc[:, 3:4], acc[:, 1:2], AF.Exp)
            k.tt("dve", nlam.v(), acc[:, 3:4], acc[:, 2:3], ALU.subtract)
            k.ts("dve", nlam.v(), nlam.v(), -lam_init, ALU.add)
            k.dma("sp", gsc.v(), View(subg.ap[j:j + 1, :].rearrange("o d -> d o"), subg))
            k.ts("dve", gsc.v(), gsc.v(), 1.0 - lam_init, ALU.mult)
            wv_sb = load_w_bf16(P, st2, "wv_sb", View(wv.ap[j], wv), KC, D)
            c = 0
            for t in range(NT):
                tb, tt_ = t // 4, t % 4
                for hf in range(2):
                    pp = P.ps[c % 4]
                    c += 1
                    for kc in range(KC):
                        k.mm(pp.v(), P.hT[tb][:, kc, tt_ * 128:(tt_ + 1) * 128], wv_sb[:, kc, hf * 512:(hf + 1) * 512],
                             start=(kc == 0), stop=(kc == KC - 1))
                    k.copy("act", V[t][:, hf * 512:(hf + 1) * 512], pp.v())
            k.barrier()
        with ExitStack() as st3:
            wqh = [Buf(k.sb(st3, f"wqh{i}", [128, KC, 128], BF16)[:, :, :]) for i in range(2)]
            wkh = [Buf(k.sb(st3, f"wkh{i}", [128, KC, 128], BF16)[:, :, :]) for i in range(2)]
            QT_t = [k.sb(st3, f"QT{i}", [128, S], BF16) for i in range(2)]
            KT_t = [k.sb(st3, f"KT{i}", [128, S], BF16) for i in range(2)]
            QT = [[Buf(QT_t[i][:, tb * 512:(tb + 1) * 512]) for tb in range(4)] for i in range(2)]
            KT = [[Buf(KT_t[i][:, tb * 512:(tb + 1) * 512]) for tb in range(4)] for i in range(2)]
            pts = [Buf(k.sb(st3, f"pt{i}", [128, 512], BF16)[:, :]) for i in range(4)]
            rd = Buf(k.sb(st3, "f_rd", [128, 512], F32)[:, :])
            o0 = Buf(k.sb(st3, "f_o0", [128, 512], F32)[:, :])
            o1 = Buf(k.sb(st3, "f_o1", [128, 512], F32)[:, :])
            sq = Buf(k.sb(st3, "f_sq", [128, 512], BF16)[:, :])
            rs, rp = Rot(3), Rot(4)

            def load_head(h):
                s_ = h % 2
                k.dma("pool", wqh[s_].v(), View(wq.ap[j].rearrange("(kc p) f -> p kc f", p=128)[:, :, h * 128:(h + 1) * 128], wq))
                k.dma("pool", wkh[s_].v(), View(wk.ap[j].rearrange("(kc p) f -> p kc f", p=128)[:, :, h * 128:(h + 1) * 128], wk))

            def proj_head(h):
                s_ = h % 2
                for (W, T) in ((wqh[s_], QT[s_]), (wkh[s_], KT[s_])):
                    for tb in range(4):
                        pp = P.ps[rs.nxt()]
                        for kc in range(KC):
                            k.mm(pp.v(), W[:, kc, :], P.hT[tb][:, kc, :], start=(kc == 0), stop=(kc == KC - 1))
                        k.copy("dve", T[tb].v(), pp.v())

            pipe = AttnPipe(3)
            unit = [0]
            o0b = [Buf(k.sb(st3, f"f_o0b{i}", [128, 512], F32)[:, :]) for i in range(2)]
            sqb = [Buf(k.sb(st3, f"f_sqb{i}", [128, 512], BF16)[:, :]) for i in range(2)]
            rdb = [Buf(k.sb(st3, f"f_rdb{i}", [128, 512], F32)[:, :]) for i in range(2)]

            def attn_head(h):
                s_ = h % 2
                for qb in range(4):
                    bl = causal_blocks(qb)
                    par = (h * 4 + qb) % 2
                    for m in range(2):
                        r0, r1 = m * 64, (m + 1) * 64
                        u = unit[0] % 2
                        unit[0] += 1
                        psO, psD = P.ps[3 + 2 * u], P.ps[4 + 2 * u]

                        def fin(m=m, psO=psO, psD=psD, par=par, h=h, qb=qb):
                            if m == 0:
                                k.recip(rd.v(), psD.v())
                                k.tt("dve", o0b[par].v(), psO.v(), rd.v(), ALU.mult)
                            else:
                                k.recip(rd.v(), psD.v())
                                k.tt("dve", o1.v(), psO.v(), rd.v(), ALU.mult)
                                k.stt("dve", o0b[par].v(), o1.v(), nlam[:, 0:1], o0b[par].v(), ALU.mult, ALU.add)
                                k.tt("dve", sqb[par].v(), o0b[par].v(), o0b[par].v(), ALU.mult)

                                def fin2():
                                    pS = P.ps[rs.nxt()]
                                    k.mm(pS.v(), P.ones.v(), sqb[par].v(), start=True, stop=True)
                                    k.act(rdb[par].v(), pS.v(), AF.Ln, scale=1.0 / 128.0, bias=eps5[:, 0:1])
                                    k.act(rdb[par].v(), rdb[par].v(), AF.Exp, scale=-0.5)
                                    k.stt("dve", oTb[h][qb].v(), o0b[par].v(), gsc[:, 0:1], rdb[par].v(), ALU.mult, ALU.mult)

                                pipe.defer(4, fin2)

                        run_blocks(P, pipe, bl,
                                   q_of=lambda c0, N: QT[s_][qb][r0:r1, c0:c0 + N],
                                   k_of=lambda kb: KT[s_][kb // 4][r0:r1, (kb % 4) * 128:(kb % 4 + 1) * 128],
                                   v_of=lambda kb: V[kb][:, h * 128:(h + 1) * 128],
                                   ones_v=P.ones.v(), psO=psO, psD=psD, pts=pts, rs=rs, rp=rp, scale=scale, after=fin)

            load_head(0)
            load_head(1)
            proj_head(0)
            for h in range(8):
                if h + 1 < 8:
                    proj_head(h + 1)
                if h + 2 < 8:
                    load_head(h + 2)
                attn_head(h)
            pipe.flush()
            k.barrier()
        sti.close()
        oT = [Buf(oT_t[:, c, :]) for c in range(KC)]
        emit_mix_out(P, li, oT, View(wo.ap[j], wo), dst)


def band_blocks(qb):
    bl = []
    for r in (4, 5, 6, 7, 3, 2, 1, 0):
        if r >= 4:
            rp_ = r - 4
            bl.append((4 * qb + rp_, 128 * rp_, 512 - 128 * rp_, (64, 128, 0, 64), 0))
        elif qb > 0:
            N = 128 * (r + 1)
            bl.append((4 * qb - 4 + r, 0, N, (0, 64, N - 64, N), 512 - 128 * r))
    return bl


def emit_mix_band(P, li, dst):
    k = P.k
    j = li // 4
    wqkv = P.inp("ca_w_qkv", [1, D, 3 * D])
    bt_d = P.inp("ca_bt", [16, 128, 640])
    wo = P.inp("ca_wo", [1, D, D])
    scale = 64 ** -0.5
    with ExitStack() as st:
        oT_t = k.sb(st, "oT", [128, KC, S], BF16)
        oTb = [[Buf(oT_t[:, c, qb * 512:(qb + 1) * 512]) for qb in range(4)] for c in range(KC)]
        sti = ExitStack()
        V_t = k.sb(sti, "Vall", [128, NT, D], BF16)
        V = [Buf(V_t[:, t, :]) for t in range(NT)]
        BT = Buf(k.sb(sti, "BT", [128, 16, 640], BF16)[:, :, :], "BT")
        k.dma("pool", BT.v(), View(bt_d.ap.rearrange("h p x -> p h x"), bt_d))
        k.ts("pool", BT.v(), BT.v(), 1.0 / scale, ALU.mult)
        with ExitStack() as st2:
            wv_sb = load_w_bf16(P, st2, "wv_sb", View(wqkv.ap[j], wqkv), KC, D, col0=2 * D)
            c = 0
            for t in range(NT):
                tb, tt_ = t // 4, t % 4
                for hf in range(2):
                    pp = P.ps[c % 4]
                    c += 1
                    for kc in range(KC):
                        k.mm(pp.v(), P.hT[tb][:, kc, tt_ * 128:(tt_ + 1) * 128], wv_sb[:, kc, hf * 512:(hf + 1) * 512],
                             start=(kc == 0), stop=(kc == KC - 1))
                    k.copy("act", V[t][:, hf * 512:(hf + 1) * 512], pp.v())
            k.barrier()
        with ExitStack() as st3:
            wqh = [Buf(k.sb(st3, f"wqh{i}", [128, KC, 128], BF16)[:, :, :]) for i in range(2)]
            wkh = [Buf(k.sb(st3, f"wkh{i}", [128, KC, 128], BF16)[:, :, :]) for i in range(2)]
            QT_t = [k.sb(st3, f"QT{i}", [128, S], BF16) for i in range(2)]
            KT_t = [k.sb(st3, f"KT{i}", [128, S], BF16) for i in range(2)]
            QT = [[Buf(QT_t[i][:, tb * 512:(tb + 1) * 512]) for tb in range(4)] for i in range(2)]
            KT = [[Buf(KT_t[i][:, tb * 512:(tb + 1) * 512]) for tb in range(4)] for i in range(2)]
            pts = [Buf(k.sb(st3, f"pt{i}", [128, 512], BF16)[:, :]) for i in range(4)]
            rd = [Buf(k.sb(st3, f"f_rd{i}", [128, 512], F32)[:, :]) for i in range(2)]
            rs, rp = Rot(3), Rot(4)
            wq_v = View(wqkv.ap[j].rearrange("(kc p) f -> p kc f", p=128), wqkv)

            def load_pair(p):
                s_ = p % 2
                k.dma("pool", wqh[s_].v(), wq_v[:, :, p * 128:(p + 1) * 128])
                k.dma("pool", wkh[s_].v(), wq_v[:, :, D + p * 128:D + (p + 1) * 128])

            def proj_pair(p):
                s_ = p % 2
                for (W, T) in ((wqh[s_], QT[s_]), (wkh[s_], KT[s_])):
                    for tb in range(4):
                        pp = P.ps[rs.nxt()]
                        for kc in range(KC):
                            k.mm(pp.v(), W[:, kc, :], P.hT[tb][:, kc, :], start=(kc == 0), stop=(kc == KC - 1))
                        k.copy("dve", T[tb].v(), pp.v())

            unit = [0]
            pipe = AttnPipe(3)

            def attn_pair(p):
                s_ = p % 2
                for hh in range(2):
                    h = 2 * p + hh
                    r0, r1 = hh * 64, (hh + 1) * 64
                    for qb in range(4):
                        u = unit[0] % 2
                        unit[0] += 1
                        psO, psD = P.ps[3 + 2 * u], P.ps[4 + 2 * u]
                        def fin(u=u, psO=psO, psD=psD, r0=r0, r1=r1, p=p, qb=qb):
                            k.recip(rd[u][r0:r1, :], psD[r0:r1, :])
                            k.tt("dve", oTb[p][qb][r0:r1, :], psO[r0:r1, :], rd[u][r0:r1, :], ALU.mult)

                        run_blocks(P, pipe, band_blocks(qb),
                                   q_of=lambda c0, N: QT[s_][qb][r0:r1, c0:c0 + N],
                                   k_of=lambda kb: KT[s_][kb // 4][r0:r1, (kb % 4) * 128:(kb % 4 + 1) * 128],
                                   v_of=lambda kb: V[kb][:, p * 128:(p + 1) * 128],
                                   ones_v=P.ones.v(), psO=psO, psD=psD, pts=pts, rs=rs, rp=rp, scale=scale,
                                   bias_of=lambda off, N: BT[:, h, off:off + N], after=fin)

            load_pair(0)
            load_pair(1)
            proj_pair(0)
            for p in range(8):
                if p + 1 < 8:
                    proj_pair(p + 1)
                if p + 2 < 8:
                    load_pair(p + 2)
                attn_pair(p)
            pipe.flush()
            k.barrier()
        sti.close()
        oT = [Buf(oT_t[:, c, :]) for c in range(KC)]
        emit_mix_out(P, li, oT, View(wo.ap[j], wo), dst)


MLA_STAGE = [9]


def emit_mix_mla(P, li, dst):
    k = P.k
    nc = k.nc
    j = li // 4
    QR, KVR, NH = 384, 256, 16
    w_dq = P.inp("mla_w_dq", [1, D, QR])
    qg_d = P.inp("mla_q_norm_g", [1, QR])
    w_uq = P.inp("mla_w_uq", [1, QR, NH * 96])
    w_dkv = P.inp("mla_w_dkv", [1, D, KVR + 32])
    kvg_d = P.inp("mla_kv_norm_g", [1, KVR])
    w_ukv = P.inp("mla_w_ukv", [1, KVR, NH * 128])
    wo = P.inp("mla_wo", [1, D, D])
    cs_d = P.inp("rope_cs", [2, 32, S])
    scale = 96 ** -0.5
    with ExitStack() as st:
        oT_t = k.sb(st, "oT", [128, KC, S], BF16)
        oTb = [[Buf(oT_t[:, c, qb * 512:(qb + 1) * 512]) for qb in range(4)] for c in range(KC)]
        sti = ExitStack()
        V_t = k.sb(sti, "Vall", [128, NT, D], BF16)
        V = [Buf(V_t[:, t, :]) for t in range(NT)]
        cqT_t = k.sb(sti, "cqT", [128, 3, S], BF16)
        cqT = [Buf(cqT_t[:, :, tb * 512:(tb + 1) * 512]) for tb in range(4)]
        ckvT_t = k.sb(sti, "ckvT", [128, 2, S], BF16)
        ckvT = [Buf(ckvT_t[:, :, tb * 512:(tb + 1) * 512]) for tb in range(4)]
        cs = Buf(k.sb(sti, "cs", [128, 2, S], F32)[:, :, :], "cs")
        k.dma("sp", cs[64:96, :, :], View(cs_d.ap.rearrange("c p s -> p c s"), cs_d))
        wuq = Buf(k.sb(sti, "wuq", [128, 3, NH * 96 + 32], BF16)[:, :, :], "wuq")
        wuqR = Buf(k.sb(sti, "wuqR", [128, 3, NH * 96 + 32], BF16)[:, :, :], "wuqR")
        wkn = Buf(k.sb(sti, "wkn", [128, 2, NH * 64 + 64], BF16)[:, :, :], "wkn")
        k.memset("pool", wuq.v(), 0.0)
        k.memset("pool", wkn.v(), 0.0)
        KR_t = k.sb(sti, "KR", [128, S], BF16)
        KR = [Buf(KR_t[:, tb * 512:(tb + 1) * 512]) for tb in range(4)]
        eps6 = Buf(k.sb(sti, "eps6", [128, 1], F32)[:, :])
        k.memset("pool", eps6.v(), 1e-6)
        with ExitStack() as st2:
            wdq = load_w_bf16(P, st2, "wdq", View(w_dq.ap[j], w_dq), KC, QR)
            wdkc = load_w_bf16(P, st2, "wdkc", View(w_dkv.ap[j], w_dkv), KC, KVR)
            wvv = Buf(k.sb(st2, "wvv", [128, 2, NH * 64], BF16)[:, :, :], "wvv")
            wkr = Buf(k.sb(st2, "wkr", [128, KC, 128], BF16)[:, :, :], "wkr")
            wkrR = Buf(k.sb(st2, "wkrR", [128, KC, 128], BF16)[:, :, :], "wkrR")
            wkst = Buf(k.sb(st2, "wkst", [128, KC, 32], F32)[:, :, :], "wkst")
            dkv_v = View(w_dkv.ap[j].rearrange("(kc p) f -> p kc f", p=128), w_dkv)
            k.memset("pool", wkr.v(), 0.0)
            k.memset("pool", wkrR.v(), 0.0)
            k.dma("sp", wkst.v(), dkv_v[:, :, KVR:KVR + 32])
            k.copy("pool", wkr[:, :, 64:96], wkst.v())
            k.ts("pool", wkrR[:, :, 64:80], wkst[:, :, 16:32], -1.0, ALU.mult)
            k.copy("pool", wkrR[:, :, 80:96], wkst[:, :, 0:16])
            gq = Buf(k.sb(st2, "gq", [128, 3], F32)[:, :])
            gkv = Buf(k.sb(st2, "gkv", [128, 2], F32)[:, :])
            for kc in range(3):
                k.dma("sp", gq[:, kc:kc + 1], View(qg_d.ap[j:j + 1, kc * 128:(kc + 1) * 128].rearrange("o d -> d o"), qg_d))
            for kc in range(2):
                k.dma("sp", gkv[:, kc:kc + 1], View(kvg_d.ap[j:j + 1, kc * 128:(kc + 1) * 128].rearrange("o d -> d o"), kvg_d))
            with ExitStack() as st2a:
                stg = Buf(k.sb(st2a, "stg_uq", [128, 3, NH * 96], F32)[:, :, :])
                k.dma("sp", stg.v(), View(w_uq.ap[j].rearrange("(kc p) f -> p kc f", p=128), w_uq))
                for kc in range(3):
                    k.act(wuq[:, kc, 0:NH * 96], stg[:, kc, :], AF.Identity, scale=gq[:, kc:kc + 1])
                k.memset("pool", wuqR.v(), 0.0)
                w4 = View(wuq.ap[:, :, 0:NH * 96].rearrange("p k (h c) -> p k h c", c=96), wuq)
                r4 = View(wuqR.ap[:, :, 0:NH * 96].rearrange("p k (h c) -> p k h c", c=96), wuqR)
                for kc in range(3):
                    k.ts("pool", r4[:, kc, :, 64:80], w4[:, kc, :, 80:96], -1.0, ALU.mult)
                    k.copy("pool", r4[:, kc, :, 80:96], w4[:, kc, :, 64:80])
                k.barrier()
            with ExitStack() as st2b:
                stg = Buf(k.sb(st2b, "stg_ukv", [128, 2, NH * 128], F32)[:, :, :])
                k.dma("sp", stg.v(), View(w_ukv.ap[j].rearrange("(kc p) f -> p kc f", p=128), w_ukv))
                s4 = View(stg.ap.rearrange("p k (h c) -> p k h c", c=128), stg)
                kn4 = View(wkn.ap[:, :, 0:NH * 64].rearrange("p k (h c) -> p k h c", c=64), wkn)
                vv4 = View(wvv.ap.rearrange("p k (h c) -> p k h c", c=64), wvv)
                for kc in range(2):
                    k.act(kn4[:, kc, :, :], s4[:, kc, :, 0:64], AF.Identity, scale=gkv[:, kc:kc + 1])
                    k.act(vv4[:, kc, :, :], s4[:, kc, :, 64:128], AF.Identity, scale=gkv[:, kc:kc + 1])
                k.barrier()
            junk = Buf(k.sb(st2, "mjunk", [128, QR], F32)[:, :])
            NT_A = NT if MLA_STAGE[0] >= 2 else 0
            cqn = [Buf(k.sb(st2, f"cqn{i}", [128, QR], BF16)[:, :]) for i in range(2)]
            cqf = [Buf(k.sb(st2, f"cqf{i}", [128, QR], F32)[:, :]) for i in range(2)]
            ckf = [Buf(k.sb(st2, f"ckf{i}", [128, KVR], F32)[:, :]) for i in range(2)]
            ckn = [Buf(k.sb(st2, f"ckn{i}", [128, KVR], BF16)[:, :]) for i in range(2)]
            sc = [Buf(k.sb(st2, f"msc{i}", [128, 8], F32)[:, :]) for i in range(2)]
            ptr3 = View(P.ps_tr.ap.rearrange("p (k c) -> p k c", c=128), P.ps_tr)
            for t in range(NT_A):
                tb, tt_ = t // 4, t % 4
                u = t % 2
                pq, pk = P.ps[2 * u], P.ps[2 * u + 1]
                for kc in range(KC):
                    k.mm(pq[:, 0:QR], P.hT[tb][:, kc, tt_ * 128:(tt_ + 1) * 128], wdq[:, kc, :], start=(kc == 0), stop=(kc == KC - 1))
                for kc in range(KC):
                    k.mm(pk[:, 0:KVR], P.hT[tb][:, kc, tt_ * 128:(tt_ + 1) * 128], wdkc[:, kc, :], start=(kc == 0), stop=(kc == KC - 1))
                k.copy("act", cqf[u].v(), pq[:, 0:QR])
                k.copy("act", ckf[u].v(), pk[:, 0:KVR])
                k.stt("dve", junk[:, 0:QR], cqf[u].v(), 1.0, cqf[u].v(), ALU.mult, ALU.mult, accum=sc[u][:, 0:1])
                k.stt("dve", junk[:, 0:KVR], ckf[u].v(), 1.0, ckf[u].v(), ALU.mult, ALU.mult, accum=sc[u][:, 1:2])
                k.act(sc[u][:, 2:3], sc[u][:, 0:1], AF.Sqrt, scale=1.0 / QR, bias=eps6[:, 0:1])
                k.act(sc[u][:, 3:4], sc[u][:, 1:2], AF.Sqrt, scale=1.0 / KVR, bias=eps6[:, 0:1])
                k.recip(sc[u][:, 4:6], sc[u][:, 2:4])
                k.ts("dve", cqn[u].v(), cqf[u].v(), sc[u][:, 4:5], ALU.mult)
                k.ts("dve", ckn[u].v(), ckf[u].v(), sc[u][:, 5:6], ALU.mult)
                for kc in range(3):
                    k.tr(P.ps_tr[:, kc * 128:(kc + 1) * 128], cqn[u][:, kc * 128:(kc + 1) * 128], P.ident.v())
                for kc in range(2):
                    k.tr(P.ps_tr[:, (3 + kc) * 128:(4 + kc) * 128], ckn[u][:, kc * 128:(kc + 1) * 128], P.ident.v())
                k.copy("dve", cqT[tb][:, :, tt_ * 128:(tt_ + 1) * 128], ptr3[:, 0:3, :])
                k.copy("dve", ckvT[tb][:, :, tt_ * 128:(tt_ + 1) * 128], ptr3[:, 3:5, :])
            ta = Buf(k.sb(st2, "rk_a", [128, 512], F32)[:, :])
            tb_ = Buf(k.sb(st2, "rk_b", [128, 512], F32)[:, :])
            for tb in range(4 if MLA_STAGE[0] >= 3 else 0):
                p1, p2 = P.ps[4], P.ps[5]
                for kc in range(KC):
                    k.mm(p1.v(), wkr[:, kc, :], P.hT[tb][:, kc, :], start=(kc == 0), stop=(kc == KC - 1))
                for kc in range(KC):
                    k.mm(p2.v(), wkrR[:, kc, :], P.hT[tb][:, kc, :], start=(kc == 0), stop=(kc == KC - 1))
                k.tt("dve", ta[64:96, :], p1[64:96, :], cs[64:96, 0, tb * 512:(tb + 1) * 512], ALU.mult)
                k.tt("dve", tb_[64:96, :], p2[64:96, :], cs[64:96, 1, tb * 512:(tb + 1) * 512], ALU.mult)
                k.tt("dve", KR[tb][64:96, :], ta[64:96, :], tb_[64:96, :], ALU.add)
            c = 0
            for t in range(NT if MLA_STAGE[0] >= 4 else 0):
                tb, tt_ = t // 4, t % 4
                for hf in range(2):
                    pp = P.ps[c % 4]
                    c += 1
                    for kc in range(2):
                        k.mm(pp.v(), ckvT[tb][:, kc, tt_ * 128:(tt_ + 1) * 128], wvv[:, kc, hf * 512:(hf + 1) * 512],
                             start=(kc == 0), stop=(kc == 1))
                    k.copy("act", V[t][:, hf * 512:(hf + 1) * 512], pp.v())
            k.barrier()
        with ExitStack() as st3:
            QT_t = [k.sb(st3, f"QT{i}", [128, S], BF16) for i in range(2)]
            KT_t = [k.sb(st3, f"KT{i}", [128, S], BF16) for i in range(2)]
            QT = [[Buf(QT_t[i][:, tb * 512:(tb + 1) * 512]) for tb in range(4)] for i in range(2)]
            KT = [[Buf(KT_t[i][:, tb * 512:(tb + 1) * 512]) for tb in range(4)] for i in range(2)]
            pts = [Buf(k.sb(st3, f"pt{i}", [128, 512], BF16)[:, :]) for i in range(4)]
            rd = [Buf(k.sb(st3, f"f_rd{i}", [128, 512], F32)[:, :]) for i in range(2)]
            ta = Buf(k.sb(st3, "rq_a", [128, 512], F32)[:, :])
            tb2 = Buf(k.sb(st3, "rq_b", [128, 512], F32)[:, :])
            rs, rp = Rot(3), Rot(4)
            for i in range(2):
                for tb in range(4):
                    k.memset("pool", QT[i][tb].v(), 0.0)
                    k.memset("pool", KT[i][tb].v(), 0.0)
                    k.copy("pool", KT[i][tb][64:96, :], KR[tb][64:96, :])

            def proj_head(h):
                s_ = h % 2
                for tb in range(4):
                    p1 = P.ps[rs.nxt()]
                    for kc in range(3):
                        k.mm(p1.v(), wuq[:, kc, h * 96:h * 96 + 128], cqT[tb][:, kc, :], start=(kc == 0), stop=(kc == 2))
                    p2 = P.ps[rs.nxt()]
                    for kc in range(3):
                        k.mm(p2.v(), wuqR[:, kc, h * 96:h * 96 + 128], cqT[tb][:, kc, :], start=(kc == 0), stop=(kc == 2))
                    k.copy("dve", QT[s_][tb][0:64, :], p1[0:64, :])
                    k.tt("dve", ta[64:96, :], p1[64:96, :], cs[64:96, 0, tb * 512:(tb + 1) * 512], ALU.mult)
                    k.tt("dve", tb2[64:96, :], p2[64:96, :], cs[64:96, 1, tb * 512:(tb + 1) * 512], ALU.mult)
                    k.tt("dve", QT[s_][tb][64:96, :], ta[64:96, :], tb2[64:96, :], ALU.add)
                    p3 = P.ps[rs.nxt()]
                    for kc in range(2):
                        k.mm(p3.v(), wkn[:, kc, h * 64:h * 64 + 128], ckvT[tb][:, kc, :], start=(kc == 0), stop=(kc == 1))
                    k.copy("dve", KT[s_][tb][0:64, :], p3[0:64, :])

            unit = [0]
            pipe = AttnPipe(3)

            def attn_head(h):
                s_ = h % 2
                p, hh = h // 2, h % 2
                r0, r1 = hh * 64, (hh + 1) * 64
                for qb in range(4):
                    u = unit[0] % 2
                    unit[0] += 1
                    psO, psD = P.ps[3 + 2 * u], P.ps[4 + 2 * u]
                    def fin(u=u, psO=psO, psD=psD, r0=r0, r1=r1, p=p, qb=qb):
                        k.recip(rd[u][r0:r1, :], psD[r0:r1, :])
                        k.tt("dve", oTb[p][qb][r0:r1, :], psO[r0:r1, :], rd[u][r0:r1, :], ALU.mult)

                    run_blocks(P, pipe, causal_blocks(qb),
                               q_of=lambda c0, N: QT[s_][qb][:, c0:c0 + N],
                               k_of=lambda kb: KT[s_][kb // 4][:, (kb % 4) * 128:(kb % 4 + 1) * 128],
                               v_of=lambda kb: V[kb][:, p * 128:(p + 1) * 128],
                               ones_v=P.ones.v(), psO=psO, psD=psD, pts=pts, rs=rs, rp=rp, scale=scale, after=fin)

            NHX = NH if MLA_STAGE[0] >= 6 else (1 if MLA_STAGE[0] >= 5 else 0)
            if NHX:
                proj_head(0)
            for h in range(NHX):
                if h + 1 < NHX:
                    proj_head(h + 1)
                if MLA_STAGE[0] != 5:
                    attn_head(h)
            pipe.flush()
            k.barrier()
        sti.close()
        oT = [Buf(oT_t[:, c, :]) for c in range(KC)]
        emit_mix_out(P, li, oT, View(wo.ap[j], wo), dst)


def build(plan):
    k = K()
    P = Prog(k, plan)
    emit_consts(P)
    emit_prologue(P)
    n = len(plan)
    for i, name in enumerate(plan):
        dst = P.y if i == n - 1 else P.hres
        kind, li = name[:3], int(name[3:])
        if kind == "ffn":
            if li % 2 == 1 and SPARSE_MOE:
                emit_ffn_sparse(P, li, dst)
            else:
                emit_ffn(P, li, moe=(li % 2 == 1), dst=dst)
        elif kind == "mix":
            [emit_mix_diff, emit_mix_band, emit_mix_mla, emit_mix_gmlp][li % 4](P, li, dst)
        else:
            raise ValueError(name)
    k.final_wait()
    return k, P


def host_inputs(P, inputs):
    m = {}
    for name in P.ins:
        if name == "ca_bt":
            rb = np.asarray(inputs["ca_rel_bias"], np.float32)[0]
            idx = np.minimum(np.arange(640)[None, :] - np.arange(128)[:, None] + 128, 256)
            m[name] = np.ascontiguousarray(rb[:, idx])
        elif name == "rope_cs":
            half = 16
            inv_freq = (10000.0 ** (-np.arange(half, dtype=np.float32) / half)).astype(np.float32)
            ang = np.arange(S, dtype=np.float32)[None, :] * inv_freq[:, None]
            cos = np.concatenate([np.cos(ang), np.cos(ang)], axis=0)
            sin = np.concatenate([np.sin(ang), np.sin(ang)], axis=0)
            m[name] = np.ascontiguousarray(np.stack([cos, sin], axis=0).astype(np.float32))
        elif name in ("moe_w1r", "moe_w3r"):
            w = np.asarray(inputs["moe_w1" if name == "moe_w1r" else "moe_w3"], np.float32)
            w = w.reshape(2, NE, 2, 4, 128, NG, 512).transpose(0, 1, 5, 4, 2, 3, 6)
            m[name] = np.ascontiguousarray(w).reshape(2 * NE * NG * 128 * 2, WROW)
        elif name == "moe_w2r":
            w = np.asarray(inputs["moe_w2"], np.float32)
            w = w.reshape(2, NE, NG, 2, 2, 128, D).transpose(0, 1, 2, 5, 3, 4, 6)
            m[name] = np.ascontiguousarray(w).reshape(2 * NE * NG * 128 * 2, WROW)
        elif name == "tri_c":
            m[name] = np.triu(np.ones((128, 128), np.float32), 1)
        elif name == "iota2_c":
            m[name] = (2.0 * np.arange(128, dtype=np.float32)).reshape(128, 1)
        elif name == "moe_wrT":
            m[name] = np.ascontiguousarray(np.transpose(np.asarray(inputs["moe_w_router"], np.float32), (0, 2, 1)))
        else:
            m[name] = np.ascontiguousarray(np.asarray(inputs[name], np.float32))
    m["ident_c"] = np.eye(128, dtype=np.float32)
    return m


SPARSE_MOE = True
ROUTER_PE = True
FULL_PLAN = ["mix0", "ffn0", "mix1", "ffn1", "mix2", "ffn2", "mix3", "ffn3"]


def run_plan(plan, inputs, xs, trace=False):
    k, P = build(plan)
    shared = host_inputs(P, inputs)
    in_maps = []
    for xc in xs:
        mcore = dict(shared)
        mcore["x"] = np.ascontiguousarray(xc, dtype=np.float32)
        in_maps.append(mcore)
    res = run_bass_kernel_spmd(k.nc, in_maps, core_ids=list(range(len(xs))), trace=trace)
    return [r["y"] for r in res.results], res


def kernel(**inputs):
    x = np.asarray(inputs["x"], np.float32)
    outs, _ = run_plan(FULL_PLAN, inputs, [x[b] for b in range(x.shape[0])])
    return np.stack(outs, axis=0).astype(np.float32)
```

```python
import math
from contextlib import ExitStack

import numpy as np
import concourse.bass as bass
import concourse.mybir as mybir
from concourse.bass_utils import run_bass_kernel_spmd

F32 = mybir.dt.float32
BF16 = mybir.dt.bfloat16
AF = mybir.ActivationFunctionType
ALU = mybir.AluOpType
AX = mybir.AxisListType

S = 2048
D = 1024
NT = S // 128
KC = D // 128
DEPTH = 4
ALPHA = (2 * DEPTH) ** 0.25
LN_EPS = 1e-5
D_FF = 2816
D_FFE = 3584
NE = 8
NDS = 16


class Buf:
    def __init__(self, ap, name=""):
        self.ap = ap
        self.w = None
        self.r = {}
        self.name = name

    def __getitem__(self, idx):
        return View(self.ap[idx], self)

    def v(self):
        return View(self.ap, self)


class View:
    def __init__(self, ap, buf):
        self.ap = ap
        self.buf = buf

    def __getitem__(self, idx):
        return View(self.ap[idx], self.buf)


def _ap(x):
    return x.ap if isinstance(x, View) else x


class K:
    def __init__(self):
        nc = bass.Bass("TRN2", target_bir_lowering=False)
        self.nc = nc
        self.eng = {"pe": nc.tensor, "act": nc.scalar, "dve": nc.vector, "pool": nc.gpsimd, "sp": nc.sync}
        self.sem = {e: nc.alloc_semaphore("s_" + e) for e in ("pe", "act", "dve", "pool")}
        self.cnt = {e: 0 for e in self.sem}
        self.dsem = [nc.alloc_semaphore(f"s_d{i}") for i in range(NDS)]
        self.dcnt = [0] * NDS
        self.dnext = 0
        self.dnext_pool = 0
        self.seen = {e: {} for e in self.eng}
        self.n_inst = 0

    def _semh(self, key):
        return self.sem[key] if isinstance(key, str) else self.dsem[key[1]]

    def _wait(self, e, key, val):
        if self.seen[e].get(key, 0) >= val:
            return
        self.eng[e].wait_ge(self._semh(key), val)
        self.seen[e][key] = val

    def _deps(self, e, reads, writes):
        deps = {}

        def add(t):
            if t is None:
                return
            k, v = t
            if deps.get(k, 0) < v:
                deps[k] = v

        for v in reads:
            add(v.buf.w)
        for v in writes:
            add(v.buf.w)
            for kk, val in v.buf.r.items():
                add((kk, val))
        for kk, val in deps.items():
            if kk == e and e == "pe":
                continue
            self._wait(e, kk, val)

    def _done(self, key, val, reads, writes):
        for v in writes:
            v.buf.w = (key, val)
            v.buf.r = {}
        for v in reads:
            if v.buf.r.get(key, 0) < val:
                v.buf.r[key] = val

    def op(self, e, fn, reads, writes):
        reads = [r for r in reads if isinstance(r, View)]
        self._deps(e, reads, writes)
        inst = fn()
        self.cnt[e] += 1
        inst.then_inc(self.sem[e], 1)
        self._done(e, self.cnt[e], reads, writes)
        self.n_inst += 1
        return inst

    def dma(self, q, out, in_, **kw):
        self._deps(q, [in_], [out])
        half = NDS // 2
        if q == "pool":
            i = half + self.dnext_pool
            self.dnext_pool = (self.dnext_pool + 1) % half
        else:
            i = self.dnext
            self.dnext = (self.dnext + 1) % half
        key = ("d", i)
        if self.dcnt[i] > 0:
            self._wait(q, key, 16 * self.dcnt[i])
        inst = self.eng[q].dma_start(out=out.ap, in_=in_.ap, **kw)
        self.dcnt[i] += 1
        inst.then_inc(self.dsem[i], 16)
        self._done(key, 16 * self.dcnt[i], [in_], [out])
        self.n_inst += 1

    def dma_indirect(self, out, out_off, in_, in_off):
        q = "pool"
        idx = in_off if in_off is not None else out_off
        self._deps(q, [in_, idx], [out])
        half = NDS // 2
        i = half + self.dnext_pool
        self.dnext_pool = (self.dnext_pool + 1) % half
        key = ("d", i)
        if self.dcnt[i] > 0:
            self._wait(q, key, 16 * self.dcnt[i])
        oo = bass.IndirectOffsetOnAxis(ap=out_off.ap, axis=0) if out_off is not None else None
        io = bass.IndirectOffsetOnAxis(ap=in_off.ap, axis=0) if in_off is not None else None
        inst = self.nc.gpsimd.indirect_dma_start(out=out.ap, out_offset=oo, in_=in_.ap, in_offset=io)
        self.dcnt[i] += 1
        inst.then_inc(self.dsem[i], 16)
        self._done(key, 16 * self.dcnt[i], [in_, idx], [out])
        self.n_inst += 1

    def barrier(self):
        keys = [(e, self.cnt[e]) for e in self.sem] + [(("d", i), 16 * self.dcnt[i]) for i in range(NDS)]
        for e in self.eng:
            for kk, val in keys:
                if val > 0 and kk != e:
                    self._wait(e, kk, val)

    def final_wait(self):
        for i in range(NDS):
            if self.dcnt[i] > 0:
                self._wait("sp", ("d", i), 16 * self.dcnt[i])

    def mm(self, out, lhsT, rhs, start=True, stop=True, **kw):
        return self.op("pe", lambda: self.nc.tensor.matmul(out.ap, lhsT.ap, rhs.ap, start=start, stop=stop, **kw),
                       [lhsT, rhs], [out])

    def tr(self, out, in_, ident):
        return self.op("pe", lambda: self.nc.tensor.transpose(out.ap, in_.ap, ident.ap), [in_, ident], [out])

    def act(self, out, in_, func, bias=None, scale=None, accum=None):
        kw = {}
        reads = [in_]
        writes = [out]
        if bias is not None:
            kw["bias"] = _ap(bias)
            reads.append(bias)
        if scale is not None:
            kw["scale"] = _ap(scale)
            reads.append(scale)
        if accum is not None:
            kw["accum_out"] = accum.ap
            writes.append(accum)
        return self.op("act", lambda: self.nc.scalar.activation(out=out.ap, in_=in_.ap, func=func, **kw), reads, writes)

    def tt(self, e, out, in0, in1, op):
        return self.op(e, lambda: self.eng[e].tensor_tensor(out=out.ap, in0=in0.ap, in1=in1.ap, op=op), [in0, in1], [out])

    def ts(self, e, out, in0, s1, op0, s2=None, op1=None, accum=None):
        kw = {}
        writes = [out]
        if op1 is not None:
            kw["op1"] = op1
        if accum is not None:
            kw["accum_out"] = accum.ap
            writes.append(accum)
        return self.op(e, lambda: self.eng[e].tensor_scalar(out=out.ap, in0=in0.ap, scalar1=_ap(s1), scalar2=_ap(s2), op0=op0, **kw),
                       [in0, s1, s2], writes)

    def stt(self, e, out, in0, scalar, in1, op0, op1, accum=None):
        kw = {}
        writes = [out]
        if accum is not None:
            kw["accum_out"] = accum.ap
            writes.append(accum)
        return self.op(e, lambda: self.eng[e].scalar_tensor_tensor(out=out.ap, in0=in0.ap, scalar=_ap(scalar), in1=in1.ap, op0=op0, op1=op1, **kw),
                       [in0, scalar, in1], writes)

    def copy(self, e, out, in_):
        if e == "act":
            return self.act(out, in_, AF.Copy)
        return self.op(e, lambda: self.eng[e].tensor_copy(out=out.ap, in_=in_.ap), [in_], [out])

    def memset(self, e, out, val):
        return self.op(e, lambda: self.eng[e].memset(out.ap, val), [], [out])

    def recip(self, out, in_):
        return self.op("dve", lambda: self.nc.vector.reciprocal(out=out.ap, in_=in_.ap), [in_], [out])

    def sb(self, st, name, shape, dtype):
        self.n_alloc = getattr(self, "n_alloc", 0) + 1
        return st.enter_context(self.nc.sbuf_tensor(f"{name}_{self.n_alloc}", list(shape), dtype))

    def dram_in(self, name, shape, dtype=F32):
        return Buf(self.nc.dram_tensor(name, list(shape), dtype, kind="ExternalInput").ap(), name)

    def dram_out(self, name, shape, dtype=F32):
        return Buf(self.nc.dram_tensor(name, list(shape), dtype, kind="ExternalOutput").ap(), name)

    def dram_tmp(self, name, shape, dtype=F32):
        return Buf(self.nc.dram_tensor(name, list(shape), dtype, kind="Internal").ap(), name)


class Prog:
    def __init__(self, k, plan):
        self.k = k
        nc = k.nc
        self.plan = plan
        self.st = ExitStack()
        st = self.st
        hT_t = k.sb(st, "hT", [128, KC, S], BF16)
        self.hT = [Buf(hT_t[:, :, tb * 512:(tb + 1) * 512], f"hT{tb}") for tb in range(4)]
        ident_t = k.sb(st, "ident", [128, 128], BF16)
        self.ident = Buf(ident_t[:, :], "ident")
        ones_t = k.sb(st, "ones", [128, 128], BF16)
        self.ones = Buf(ones_t[:, :], "ones")
        small_t = k.sb(st, "small", [128, 64], F32)
        self.small_t = small_t
        self.ps = [Buf(nc.alloc_psum_tensor(f"ps{i}", [128, 512], F32)[:, :], f"ps{i}") for i in range(7)]
        pst = nc.alloc_psum_tensor("pstr", [128, 1024], BF16)
        self.ps_tr = Buf(pst[:, :], "pstr")
        self.x = k.dram_in("x", [S, D])
        self.y = k.dram_out("y", [S, D])
        self.hres = k.dram_tmp("hres", [S, D])
        self.ident_d = k.dram_in("ident_c", [128, 128])
        self.ins = {}

    def inp(self, name, shape):
        if name not in self.ins:
            self.ins[name] = self.k.dram_in(name, shape)
        return self.ins[name]


def emit_consts(P):
    k = P.k
    k.dma("pool", P.ident.v(), P.ident_d.v())
    k.memset("pool", P.ones.v(), 1.0)


def emit_ln_tail(P, st, t, xs, gb, bb, dst, tmp):
    k = P.k
    ln_tail_drain(P, tmp, 2)
    ts_ = tmp["sets"][t % len(tmp["sets"])]
    stats, mv, sc, xn, xb = ts_["stats"], ts_["mv"], ts_["sc"], ts_["xn"], ts_["xb"]
    for hf in range(2):
        k.op("dve", lambda hf=hf: k.nc.vector.bn_stats(out=stats.ap[:, hf * 6:(hf + 1) * 6], in_=xs.ap[:, hf * 512:(hf + 1) * 512]),
             [xs.v()], [stats.v()])
    k.op("dve", lambda: k.nc.vector.bn_aggr(out=mv.ap, in_=stats.ap), [stats.v()], [mv.v()])
    k.act(sc[:, 0:1], mv[:, 1:2], AF.Sqrt, bias=tmp["eps"][:, 0:1])
    k.recip(sc[:, 1:2], sc[:, 0:1])
    k.stt("dve", sc[:, 2:3], mv[:, 0:1], -1.0, sc[:, 1:2], ALU.mult, ALU.mult)
    k.act(xn.v(), xs.v(), AF.Identity, scale=sc[:, 1:2], bias=sc[:, 2:3])
    k.tt("pool", xn.v(), xn.v(), gb.v(), ALU.mult)
    k.tt("pool", xn.v(), xn.v(), bb.v(), ALU.add)
    k.dma("sp", dst[t * 128:(t + 1) * 128, :], xn.v())

    def part_b2(t=t):
        tb, tt_ = t // 4, t % 4
        k.copy("dve", P.hT[tb][:, :, tt_ * 128:(tt_ + 1) * 128],
               View(P.ps_tr.ap.rearrange("p (k c) -> p k c", c=128), P.ps_tr))

    def part_b1(t=t, xb=xb, xn=xn):
        k.copy("act", xb.v(), xn.v())
        for kc in range(KC):
            k.tr(P.ps_tr[:, kc * 128:(kc + 1) * 128], xb[:, kc * 128:(kc + 1) * 128], P.ident.v())
        tmp["b2"].append(part_b2)

    tmp["b1"].append(part_b1)
    return xn


def ln_tail_drain(P, tmp, keep):
    if tmp["b2"]:
        tmp["b2"].pop(0)()
    while len(tmp["b1"]) > keep - 1 and tmp["b1"]:
        if tmp["b2"]:
            tmp["b2"].pop(0)()
        tmp["b1"].pop(0)()


def flush_ln_tail(P, tmp):
    while tmp["b1"] or tmp["b2"]:
        if tmp["b2"]:
            tmp["b2"].pop(0)()
        if tmp["b1"]:
            tmp["b1"].pop(0)()


def alloc_ln_tmp(P, st, pfx, nset=4):
    k = P.k
    tmp = {"sets": [], "b1": [], "b2": []}
    for i in range(nset):
        d = {}
        d["stats"] = Buf(k.sb(st, pfx + f"stats{i}", [128, 12], F32)[:, :])
        d["mv"] = Buf(k.sb(st, pfx + f"mv{i}", [128, 2], F32)[:, :])
        d["sc"] = Buf(k.sb(st, pfx + f"sc{i}", [128, 4], F32)[:, :])
        d["xn"] = Buf(k.sb(st, pfx + f"xn{i}", [128, D], F32)[:, :])
        d["xb"] = Buf(k.sb(st, pfx + f"xb{i}", [128, D], BF16)[:, :])
        tmp["sets"].append(d)
    tmp["eps"] = Buf(k.sb(st, pfx + "eps", [128, 1], F32)[:, :])
    k.memset("pool", tmp["eps"].v(), LN_EPS)
    return tmp


def emit_prologue(P):
    k = P.k
    with ExitStack() as st:
        xin = [Buf(k.sb(st, f"pro_x{i}", [128, D], F32)[:, :]) for i in range(2)]
        xb = [Buf(k.sb(st, f"pro_xb{i}", [128, D], BF16)[:, :]) for i in range(2)]
        for t in range(NT):
            xi = xin[t % 2]
            k.dma("sp", xi.v(), P.x[t * 128:(t + 1) * 128, :])
            k.dma("sp", P.hres[t * 128:(t + 1) * 128, :], xi.v())
            k.copy("act", xb[t % 2].v(), xi.v())
            for kc in range(KC):
                k.tr(P.ps_tr[:, kc * 128:(kc + 1) * 128], xb[t % 2][:, kc * 128:(kc + 1) * 128], P.ident.v())
            tb, tt_ = t // 4, t % 4
            k.copy("dve", P.hT[tb][:, :, tt_ * 128:(tt_ + 1) * 128],
                   View(P.ps_tr.ap.rearrange("p (k c) -> p k c", c=128), P.ps_tr))
        k.barrier()


def load_ln_params(P, st, g_d, b_d, pfx):
    k = P.k
    gb = Buf(k.sb(st, pfx + "g", [128, D], F32)[:, :])
    bb = Buf(k.sb(st, pfx + "b", [128, D], F32)[:, :])
    k.dma("sp", gb.v(), View(g_d.ap.partition_broadcast(128), g_d.buf))
    k.dma("sp", bb.v(), View(b_d.ap.partition_broadcast(128), b_d.buf))
    return gb, bb


def emit_ffn(P, li, moe, dst):
    k = P.k
    nc = k.nc
    j = li // 2
    if moe:
        E, F = NE, D_FFE
        w1 = P.inp("moe_w1", [2, NE, D, D_FFE])
        w3 = P.inp("moe_w3", [2, NE, D, D_FFE])
        w2 = P.inp("moe_w2", [2, NE, D_FFE, D])
        wr = P.inp("moe_wrT", [2, NE, D])
        w1v = lambda e: View(w1.ap[j, e], w1)
        w3v = lambda e: View(w3.ap[j, e], w3)
        w2v = lambda e: View(w2.ap[j, e], w2)
    else:
        E, F = 1, D_FF
        w1 = P.inp("ffn_w1", [2, D, D_FF])
        w3 = P.inp("ffn_w3", [2, D, D_FF])
        w2 = P.inp("ffn_w2", [2, D_FF, D])
        w1v = lambda e: View(w1.ap[j], w1)
        w3v = lambda e: View(w3.ap[j], w3)
        w2v = lambda e: View(w2.ap[j], w2)
    g_d = P.inp("ln_ffn_g", [DEPTH, D])
    b_d = P.inp("ln_ffn_b", [DEPTH, D])
    nj = F // 128
    G = 4
    units = [(e, j0, min(G, nj - j0)) for e in range(E) for j0 in range(0, nj, G)]
    U = len(units)
    with ExitStack() as st:
        comb_t = k.sb(st, "comb", [128, NT, NE], F32)
        comb = Buf(comb_t[:, :, :], "comb")
        if moe:
            with ExitStack() as st2:
                wrb = Buf(k.sb(st2, "wrb", [128, NE, D], F32)[:, :, :], "wrb")
                k.dma("sp", wrb.v(), View(wr.ap[j].partition_broadcast(128), wr))
                hin = [Buf(k.sb(st2, f"rt_h{i}", [128, D], F32)[:, :]) for i in range(2)]
                junk = Buf(k.sb(st2, "rt_junk", [128, D], F32)[:, :])
                lg = Buf(k.sb(st2, "rt_lg", [128, NE], F32)[:, :])
                l2 = Buf(k.sb(st2, "rt_l2", [128, NE], F32)[:, :])
                m1 = Buf(k.sb(st2, "rt_m1", [128, NE], F32)[:, :])
                m2 = Buf(k.sb(st2, "rt_m2", [128, NE], F32)[:, :])
                sc = Buf(k.sb(st2, "rt_sc", [128, 8], F32)[:, :])
                for t in range(NT):
                    hi = hin[t % 2]
                    k.dma("sp", hi.v(), P.hres[t * 128:(t + 1) * 128, :])
                    for e in range(NE):
                        k.stt("dve", junk.v(), hi.v(), 1.0, wrb[:, e, :], ALU.mult, ALU.mult, accum=lg[:, e:e + 1])
                    k.op("dve", lambda: nc.vector.reduce_max(out=sc.ap[:, 0:1], in_=lg.ap, axis=AX.X), [lg.v()], [sc.v()])
                    k.ts("dve", m1.v(), lg.v(), sc[:, 0:1], ALU.is_equal)
                    k.stt("dve", l2.v(), m1.v(), -1e30, lg.v(), ALU.mult, ALU.add)
                    k.op("dve", lambda: nc.vector.reduce_max(out=sc.ap[:, 1:2], in_=l2.ap, axis=AX.X), [l2.v()], [sc.v()])
                    k.ts("dve", m2.v(), l2.v(), sc[:, 1:2], ALU.is_equal)
                    k.tt("dve", sc[:, 2:3], sc[:, 1:2], sc[:, 0:1], ALU.subtract)
                    k.act(sc[:, 3:4], sc[:, 2:3], AF.Exp)
                    k.ts("dve", sc[:, 4:5], sc[:, 3:4], 1.0, ALU.add)
                    k.recip(sc[:, 5:6], sc[:, 4:5])
                    k.tt("dve", sc[:, 6:7], sc[:, 3:4], sc[:, 5:6], ALU.mult)
                    k.ts("dve", m1.v(), m1.v(), sc[:, 5:6], ALU.mult)
                    k.stt("dve", comb[:, t, :], m2.v(), sc[:, 6:7], m1.v(), ALU.mult, ALU.add)
                k.barrier()
        yacc_t = k.sb(st, "yacc", [128, NT, D], F32)
        yacc = [[Buf(yacc_t[:, t, hf * 512:(hf + 1) * 512]) for hf in range(2)] for t in range(NT)]
        stc = ExitStack()
        htg_t = [k.sb(stc, f"htg{i}", [128, G, S], BF16) for i in range(2)]
        htg = [[[Buf(htg_t[i][:, jj, tb * 512:(tb + 1) * 512]) for tb in range(4)] for jj in range(G)] for i in range(2)]
        w1g = [Buf(k.sb(stc, f"w1g{i}", [128, KC, G * 128], BF16)[:, :, :]) for i in range(2)]
        w3g = [Buf(k.sb(stc, f"w3g{i}", [128, KC, G * 128], BF16)[:, :, :]) for i in range(2)]
        w2g = [Buf(k.sb(stc, f"w2g{i}", [128, G, D], BF16)[:, :, :]) for i in range(2)]
        sil = [Buf(k.sb(stc, f"sil{i}", [128, 512], F32)[:, :]) for i in range(2)]

        def loadA(u):
            e, j0, n = units[u]
            s = u % 2
            k.dma("pool", w1g[s][:, :, 0:n * 128],
                  View(w1v(e).ap.rearrange("(kc p) f -> p kc f", p=128)[:, :, j0 * 128:(j0 + n) * 128], w1))
            k.dma("pool", w3g[s][:, :, 0:n * 128],
                  View(w3v(e).ap.rearrange("(kc p) f -> p kc f", p=128)[:, :, j0 * 128:(j0 + n) * 128], w3))

        def loadB(u):
            e, j0, n = units[u]
            s = u % 2
            k.dma("pool", w2g[s][:, 0:n, :],
                  View(w2v(e).ap[j0 * 128:(j0 + n) * 128, :].rearrange("(j p) d -> p j d", p=128), w2))

        cnt1 = [0]

        def phase1(u):
            e, j0, n = units[u]
            s = u % 2
            for jj in range(n):
                for tb in range(4):
                    c = cnt1[0] % 2
                    cnt1[0] += 1
                    pa, pb = P.ps[2 * c], P.ps[2 * c + 1]
                    for (W, pp) in ((w1g[s], pa), (w3g[s], pb)):
                        for kc in range(KC):
                            k.mm(pp.v(), W[:, kc, jj * 128:(jj + 1) * 128], P.hT[tb][:, kc, :], start=(kc == 0), stop=(kc == KC - 1))
                    k.act(sil[c].v(), pa.v(), AF.Silu)
                    k.tt("dve", htg[s][jj][tb].v(), sil[c].v(), pb.v(), ALU.mult)

        cnt2 = [0]

        def phase2(u):
            e, j0, n = units[u]
            s = u % 2
            for t in range(NT):
                tb, tt_ = t // 4, t % 4
                for hf in range(2):
                    py = P.ps[4 + cnt2[0] % 3]
                    cnt2[0] += 1
                    for jj in range(n):
                        k.mm(py.v(), htg[s][jj][tb][:, tt_ * 128:(tt_ + 1) * 128], w2g[s][:, jj, hf * 512:(hf + 1) * 512],
                             start=(jj == 0), stop=(jj == n - 1))
                    ya = yacc[t][hf]
                    if moe:
                        if u == 0:
                            k.ts("dve", ya.v(), py.v(), comb[:, t, e:e + 1], ALU.mult)
                        else:
                            k.stt("dve", ya.v(), py.v(), comb[:, t, e:e + 1], ya.v(), ALU.mult, ALU.add)
                    else:
                        if u == 0:
                            k.copy("dve", ya.v(), py.v())
                        else:
                            k.tt("dve", ya.v(), py.v(), ya.v(), ALU.add)

        loadA(0)
        loadB(0)
        if U > 1:
            loadA(1)
            loadB(1)
        phase1(0)
        for u in range(U):
            if u + 1 < U:
                phase1(u + 1)
            if u + 2 < U:
                loadA(u + 2)
            phase2(u)
            if u + 2 < U:
                loadB(u + 2)
        k.barrier()
        stc.close()
        gb, bb = load_ln_params(P, st, View(g_d.ap[li:li + 1, :], g_d), View(b_d.ap[li:li + 1, :], b_d), "ffn_ln")
        tmp = alloc_ln_tmp(P, st, "ffn_")
        xin = [Buf(k.sb(st, f"ffn_xin{i}", [128, D], F32)[:, :]) for i in range(3)]
        k.dma("sp", xin[0].v(), P.hres[0:128, :])
        k.dma("sp", xin[1].v(), P.hres[128:256, :])
        for t in range(NT):
            if t + 2 < NT:
                k.dma("sp", xin[(t + 2) % 3].v(), P.hres[(t + 2) * 128:(t + 3) * 128, :])
            xi = xin[t % 3]
            for hf in range(2):
                k.stt("dve", xi[:, hf * 512:(hf + 1) * 512], xi[:, hf * 512:(hf + 1) * 512], ALPHA, yacc[t][hf].v(), ALU.mult, ALU.add)
            emit_ln_tail(P, st, t, xi, gb, bb, dst, tmp)
        flush_ln_tail(P, tmp)
        k.barrier()


NG = 7
NST = 15
WROW = 2048


def emit_ffn_sparse(P, li, dst):
    k = P.k
    nc = k.nc
    j = li // 2
    I32 = mybir.dt.int32
    nrows = 2 * NE * NG * 128 * 2
    w1r = P.inp("moe_w1r", [nrows, WROW])
    w3r = P.inp("moe_w3r", [nrows, WROW])
    w2r = P.inp("moe_w2r", [nrows, WROW])
    if ROUTER_PE:
        wrn = P.inp("moe_w_router", [2, D, NE])
    else:
        wr = P.inp("moe_wrT", [2, NE, D])
    tri_d = P.inp("tri_c", [128, 128])
    io_d = P.inp("iota2_c", [128, 1])
    g_d = P.inp("ln_ffn_g", [DEPTH, D])
    b_d = P.inp("ln_ffn_b", [DEPTH, D])
    if not hasattr(P, "xs_d"):
        P.xs_d = k.dram_tmp("xs_sorted", [NST * 512, D], BF16)
        P.ys_d = k.dram_tmp("ys_sorted", [NST * 512, D], F32)
    xs_d, ys_d = P.xs_d, P.ys_d
    with ExitStack() as st:
        gs = Buf(k.sb(st, "gs", [128, NT, 2], F32)[:, :, :], "gs")
        slot_i = Buf(k.sb(st, "slot_i", [128, NT, 2], I32)[:, :, :], "slot_i")
        idxw = Buf(k.sb(st, "idxw", [128, NST, NG, 2], I32)[:, :, :, :], "idxw")
        with ExitStack() as st2:
            if not ROUTER_PE:
                wrb = Buf(k.sb(st2, "wrb", [128, NE, D], F32)[:, :, :], "wrb")
                k.dma("sp", wrb.v(), View(wr.ap[j].partition_broadcast(128), wr))
            tri = Buf(k.sb(st2, "tri", [128, 128], BF16)[:, :], "tri")
            k.dma("pool", tri.v(), tri_d.v())
            io2 = Buf(k.sb(st2, "io2", [128, 1], F32)[:, :], "io2")
            k.dma("sp", io2.v(), io_d.v())
            zt = Buf(k.sb(st2, "zt", [128, 4096], BF16)[:, :], "zt")
            k.memset("pool", zt.v(), 0.0)
            xs_flat = View(xs_d.ap.rearrange("(p r) d -> p (r d)", p=128), xs_d)
            for i in range(NST * 4 * D // 4096):
                k.dma("sp", xs_flat[:, i * 4096:(i + 1) * 4096], zt.v())
            m1s = Buf(k.sb(st2, "m1s", [128, NT, NE], F32)[:, :, :], "m1s")
            m2s = Buf(k.sb(st2, "m2s", [128, NT, NE], F32)[:, :, :], "m2s")
            abf = Buf(k.sb(st2, "abf", [128, NT, NE], BF16)[:, :, :], "abf")
            hin = [Buf(k.sb(st2, f"rt_h{i}", [128, D], F32)[:, :]) for i in range(2)]
            xbt_t = k.sb(st2, "rt_xb", [128, NT, D], BF16)
            xbt = [Buf(xbt_t[:, t, :]) for t in range(NT)]
            junk = Buf(k.sb(st2, "rt_junk", [128, D], F32)[:, :])
            lg = Buf(k.sb(st2, "rt_lg", [128, NE], F32)[:, :])
            l2 = Buf(k.sb(st2, "rt_l2", [128, NE], F32)[:, :])
            sc = Buf(k.sb(st2, "rt_sc", [128, 8], F32)[:, :])
            lga = Buf(k.sb(st2, "rt_lga", [128, NT, NE], F32)[:, :, :], "lga")
            l2a = Buf(k.sb(st2, "rt_l2a", [128, NT, NE], F32)[:, :, :], "l2a")
            mxa = Buf(k.sb(st2, "rt_mxa", [128, 4, NT], F32)[:, :, :], "mxa")
            if ROUTER_PE:
                id32 = Buf(k.sb(st2, "id32", [128, 128], F32)[:, :], "id32")
                k.dma("sp", id32.v(), P.ident_d.v())
                wr32 = Buf(k.sb(st2, "wr32", [128, KC, NE], F32)[:, :, :], "wr32")
                k.dma("sp", wr32.v(), View(wrn.ap[j].rearrange("(kc p) e -> p kc e", p=128), wrn))
                h32 = [Buf(k.sb(st2, f"h32_{i}", [128, KC, 128], F32)[:, :, :]) for i in range(2)]
                k.dma("sp", hin[0].v(), P.hres[0:128, :])
                for t in range(NT):
                    hi = hin[t % 2]
                    if t + 1 < NT:
                        k.dma("sp", hin[(t + 1) % 2].v(), P.hres[(t + 1) * 128:(t + 2) * 128, :])
                    k.copy("act", xbt[t].v(), hi.v())
                    pa, pb = P.ps[2 * (t % 2)], P.ps[2 * (t % 2) + 1]
                    for kc in range(KC):
                        pq = pa if kc < 4 else pb
                        k.tr(pq[:, (kc % 4) * 128:(kc % 4 + 1) * 128], hi[:, kc * 128:(kc + 1) * 128], id32.v())
                    hv = View(h32[t % 2].ap.rearrange("p k c -> p (k c)"), h32[t % 2])
                    k.copy("dve", hv[:, 0:512], pa.v())
                    k.copy("act", hv[:, 512:1024], pb.v())
                    for kc in range(KC):
                        k.mm(P.ps[4][:, t * 8:(t + 1) * 8], h32[t % 2][:, kc, :], wr32[:, kc, :], start=(kc == 0), stop=(kc == KC - 1))
                k.copy("dve", View(lga.ap.rearrange("p t e -> p (t e)"), lga), P.ps[4][:, 0:NT * NE])
            else:
                for t in range(NT):
                    hi = hin[t % 2]
                    k.dma("sp", hi.v(), P.hres[t * 128:(t + 1) * 128, :])
                    k.copy("act", xbt[t].v(), hi.v())
                    for e in range(NE):
                        k.stt("dve", junk.v(), hi.v(), 1.0, wrb[:, e, :], ALU.mult, ALU.mult, accum=lga[:, t, e:e + 1])
            bc = lambda v: View(v.ap.unsqueeze(2).broadcast_to([128, NT, NE]), v.buf)
            k.op("dve", lambda: nc.vector.reduce_max(out=mxa.ap[:, 0, :], in_=lga.ap, axis=AX.X), [lga.v()], [mxa.v()])
            k.tt("dve", m1s.v(), lga.v(), bc(mxa[:, 0, :]), ALU.is_equal)
            k.stt("dve", l2a.v(), m1s.v(), -1e30, lga.v(), ALU.mult, ALU.add)
            k.op("dve", lambda: nc.vector.reduce_max(out=mxa.ap[:, 1, :], in_=l2a.ap, axis=AX.X), [l2a.v()], [mxa.v()])
            k.tt("dve", m2s.v(), l2a.v(), bc(mxa[:, 1, :]), ALU.is_equal)
            k.tt("dve", mxa[:, 2, :], mxa[:, 1, :], mxa[:, 0, :], ALU.subtract)
            k.act(mxa[:, 2, :], mxa[:, 2, :], AF.Exp)
            k.ts("dve", mxa[:, 3, :], mxa[:, 2, :], 1.0, ALU.add)
            k.recip(gs[:, :, 0], mxa[:, 3, :])
            k.tt("dve", gs[:, :, 1], mxa[:, 2, :], gs[:, :, 0], ALU.mult)
            k.tt("dve", abf.v(), m1s.v(), m2s.v(), ALU.add)
            pp = P.ps[0]
            for t in range(NT):
                for tp in range(t):
                    k.mm(pp[:, t * 8:(t + 1) * 8], P.ones.v(), abf[:, tp, :], start=(tp == 0), stop=False)
                k.mm(pp[:, t * 8:(t + 1) * 8], tri.v(), abf[:, t, :], start=(t == 0), stop=True)
            for t in range(NT):
                k.mm(pp[:, 128:136], P.ones.v(), abf[:, t, :], start=(t == 0), stop=(t == NT - 1))
            posf = Buf(k.sb(st2, "posf", [128, 136], F32)[:, :], "posf")
            k.copy("dve", posf.v(), pp[:, 0:136])
            w8 = Buf(k.sb(st2, "w8", [128, 8, 8], F32)[:, :, :], "w8")
            for m in range(4):
                k.ts("dve", w8[:, m, :], posf[:, 128:136], 512.0 * m, ALU.is_gt)
            k.tt("dve", w8[:, 0, :], w8[:, 0, :], w8[:, 1, :], ALU.add)
            k.tt("dve", w8[:, 2, :], w8[:, 2, :], w8[:, 3, :], ALU.add)
            k.tt("dve", w8[:, 0, :], w8[:, 0, :], w8[:, 2, :], ALU.add)
            k.ts("dve", w8[:, 4, :], w8[:, 0, :], 512.0, ALU.mult)
            k.memset("pool", w8[:, 5, :], 0.0)
            for e in range(1, NE):
                k.tt("dve", w8[:, 5, e:e + 1], w8[:, 5, e - 1:e], w8[:, 4, e - 1:e], ALU.add)
            k.tt("dve", w8[:, 6, :], w8[:, 5, :], w8[:, 4, :], ALU.add)
            sf = Buf(k.sb(st2, "sf", [128, NT, NE], F32)[:, :, :], "sf")
            slotf = Buf(k.sb(st2, "slotf", [128, 2, NT], F32)[:, :, :], "slotf")
            j8 = Buf(k.sb(st2, "j8", [128, NE], F32)[:, :], "j8")
            pos3 = View(posf.ap[:, 0:128].rearrange("p (t e) -> p t e", e=NE), posf)
            k.tt("dve", sf.v(), pos3, View(w8.ap[:, 5, :].unsqueeze(1).broadcast_to([128, NT, NE]), w8), ALU.add)
            k.tt("dve", l2a.v(), m1s.v(), sf.v(), ALU.mult)
            k.op("dve", lambda: nc.vector.reduce_sum(out=slotf.ap[:, 0, :], in_=l2a.ap, axis=AX.X), [l2a.v()], [slotf.v()])
            k.tt("dve", l2a.v(), m2s.v(), sf.v(), ALU.mult)
            k.op("dve", lambda: nc.vector.reduce_sum(out=slotf.ap[:, 1, :], in_=l2a.ap, axis=AX.X), [l2a.v()], [slotf.v()])
            k.copy("dve", slot_i[:, :, 0], slotf[:, 0, :])
            k.copy("dve", slot_i[:, :, 1], slotf[:, 1, :])
            esf = Buf(k.sb(st2, "esf", [128, NST], F32)[:, :], "esf")
            for s_ in range(NST):
                k.ts("dve", j8.v(), w8[:, 6, :], 512.0 * s_, ALU.is_le, s2=0.0, op1=ALU.add, accum=esf[:, s_:s_ + 1])
            k.ts("dve", esf.v(), esf.v(), float(NE - 1), ALU.min)
            k.ts("dve", esf.v(), esf.v(), float(NG * 128 * 2), ALU.mult, s2=io2[:, 0:1], op1=ALU.add)
            idxf = Buf(k.sb(st2, "idxf", [128, NST, NG, 2], F32)[:, :, :, :], "idxf")
            for g in range(NG):
                for hf in range(2):
                    k.ts("dve", idxf[:, :, g, hf], esf.v(), float(((j * NE) * NG + g) * 128 * 2 + hf), ALU.add)
            k.copy("dve", idxw.v(), idxf.v())
            k.barrier()
            for t in range(NT):
                for sl in range(2):
                    k.dma_indirect(out=xs_d.v(), out_off=slot_i[:, t, sl:sl + 1], in_=xbt[t].v(), in_off=None)
            k.barrier()
        with ExitStack() as st3:
            G = 4
            xsT = [Buf(k.sb(st3, f"xsT{i}", [128, KC, 512], BF16)[:, :, :]) for i in range(2)]
            xrow = [Buf(k.sb(st3, f"xrow{i}", [128, D], BF16)[:, :]) for i in range(2)]
            yacc_t = [k.sb(st3, f"yacc{i}", [128, 4, D], F32) for i in range(2)]
            yacc = [[[Buf(yacc_t[i][:, r, hf * 512:(hf + 1) * 512]) for hf in range(2)] for r in range(4)] for i in range(2)]
            htg = [[Buf(k.sb(st3, f"htg{i}_{jj}", [128, 512], BF16)[:, :]) for jj in range(G)] for i in range(2)]
            w1g = [Buf(k.sb(st3, f"w1g{i}", [128, KC, 512], BF16)[:, :, :]) for i in range(3)]
            w3g = [Buf(k.sb(st3, f"w3g{i}", [128, KC, 512], BF16)[:, :, :]) for i in range(3)]
            w2g = [Buf(k.sb(st3, f"w2g{i}", [128, G, D], BF16)[:, :, :]) for i in range(2)]
            sil = [Buf(k.sb(st3, f"sil{i}", [128, 512], F32)[:, :]) for i in range(2)]
            units = [(s_, g) for s_ in range(NST) for g in range(NG)]
            U = len(units)
            ptr3 = View(P.ps_tr.ap.rearrange("p (k c) -> p k c", c=128), P.ps_tr)
            xc = [0]

            def load_x(s_):
                for r in range(4):
                    xr = xrow[xc[0] % 2]
                    xc[0] += 1
                    k.dma("sp", xr.v(), xs_d[s_ * 512 + r * 128:s_ * 512 + (r + 1) * 128, :])
                    for kc in range(KC):
                        k.tr(P.ps_tr[:, kc * 128:(kc + 1) * 128], xr[:, kc * 128:(kc + 1) * 128], P.ident.v())
                    k.copy("dve", xsT[s_ % 2][:, :, r * 128:(r + 1) * 128], ptr3)

            def loadA(u):
                s_, g = units[u]
                sl = u % 3
                for hf in range(2):
                    k.dma_indirect(out=View(w1g[sl].ap[:, hf * 4:(hf + 1) * 4, :].rearrange("p k f -> p (k f)"), w1g[sl]),
                                   out_off=None, in_=w1r.v(), in_off=idxw[:, s_, g, hf:hf + 1])
                    k.dma_indirect(out=View(w3g[sl].ap[:, hf * 4:(hf + 1) * 4, :].rearrange("p k f -> p (k f)"), w3g[sl]),
                                   out_off=None, in_=w3r.v(), in_off=idxw[:, s_, g, hf:hf + 1])

            def loadB(u):
                s_, g = units[u]
                sl = u % 2
                for hf in range(2):
                    k.dma_indirect(out=View(w2g[sl].ap[:, hf * 2:(hf + 1) * 2, :].rearrange("p k f -> p (k f)"), w2g[sl]),
                                   out_off=None, in_=w2r.v(), in_off=idxw[:, s_, g, hf:hf + 1])

            cnt1 = [0]

            def phase1(u):
                s_, g = units[u]
                sl = u % 2
                sa = u % 3
                for jj in range(G):
                    c = cnt1[0] % 2
                    cnt1[0] += 1
                    pa, pb = P.ps[2 * c], P.ps[2 * c + 1]
                    for (W, pq) in ((w1g[sa], pa), (w3g[sa], pb)):
                        for kc in range(KC):
                            k.mm(pq.v(), W[:, kc, jj * 128:(jj + 1) * 128], xsT[s_ % 2][:, kc, :], start=(kc == 0), stop=(kc == KC - 1))
                    k.act(sil[c].v(), pa.v(), AF.Silu)
                    k.tt("dve", htg[sl][jj].v(), sil[c].v(), pb.v(), ALU.mult)

            cnt2 = [0]

            def phase2(u):
                s_, g = units[u]
                sl = u % 2
                for r in range(4):
                    for hf in range(2):
                        py = P.ps[4 + cnt2[0] % 3]
                        cnt2[0] += 1
                        for jj in range(G):
                            k.mm(py.v(), htg[sl][jj][:, r * 128:(r + 1) * 128], w2g[sl][:, jj, hf * 512:(hf + 1) * 512],
                                 start=(jj == 0), stop=(jj == G - 1))
                        ya = yacc[s_ % 2][r][hf]
                        if g == 0:
                            k.copy("dve", ya.v(), py.v())
                        else:
                            k.tt("dve", ya.v(), py.v(), ya.v(), ALU.add)
                if g == NG - 1:
                    for r in range(4):
                        k._deps("sp", [yacc[s_ % 2][r][1].v()], [])
                        k.dma("sp", ys_d[s_ * 512 + r * 128:s_ * 512 + (r + 1) * 128, :],
                              View(yacc_t[s_ % 2][:, r, :], yacc[s_ % 2][r][0]))

            load_x(0)
            loadA(0)
            loadB(0)
            loadA(1)
            loadB(1)
            loadA(2)
            phase1(0)
            for u in range(U):
                s_, g = units[u]
                if g == 2 and s_ + 1 < NST:
                    load_x(s_ + 1)
                if u + 1 < U:
                    phase1(u + 1)
                if u + 3 < U:
                    loadA(u + 3)
                phase2(u)
                if u + 2 < U:
                    loadB(u + 2)
            k.barrier()
        with ExitStack() as st4:
            gb, bb = load_ln_params(P, st4, View(g_d.ap[li:li + 1, :], g_d), View(b_d.ap[li:li + 1, :], b_d), "ffn_ln")
            tmp = alloc_ln_tmp(P, st4, "ffn_")
            xin = [Buf(k.sb(st4, f"ffn_xin{i}", [128, D], F32)[:, :]) for i in range(3)]
            y1 = [Buf(k.sb(st4, f"ffn_y1{i}", [128, D], F32)[:, :]) for i in range(3)]
            y2 = [Buf(k.sb(st4, f"ffn_y2{i}", [128, D], F32)[:, :]) for i in range(3)]

            def fetch(t):
                k.dma("sp", xin[t % 3].v(), P.hres[t * 128:(t + 1) * 128, :])
                k.dma_indirect(out=y1[t % 3].v(), out_off=None, in_=ys_d.v(), in_off=slot_i[:, t, 0:1])
                k.dma_indirect(out=y2[t % 3].v(), out_off=None, in_=ys_d.v(), in_off=slot_i[:, t, 1:2])

            fetch(0)
            fetch(1)
            for t in range(NT):
                if t + 2 < NT:
                    fetch(t + 2)
                xi, a, b = xin[t % 3], y1[t % 3], y2[t % 3]
                k.act(a.v(), a.v(), AF.Identity, scale=gs[:, t, 0:1])
                k.stt("dve", a.v(), b.v(), gs[:, t, 1:2], a.v(), ALU.mult, ALU.add)
                k.stt("dve", xi.v(), xi.v(), ALPHA, a.v(), ALU.mult, ALU.add)
                emit_ln_tail(P, st4, t, xi, gb, bb, dst, tmp)
            flush_ln_tail(P, tmp)
            k.barrier()


def load_w_bf16(P, st, name, src_view, kchunks, ncols, col0=0):
    k = P.k
    b = Buf(k.sb(st, name, [128, kchunks, ncols], BF16)[:, :, :], name)
    k.dma("pool", b.v(), View(src_view.ap.rearrange("(kc p) f -> p kc f", p=128)[:, :, col0:col0 + ncols], src_view.buf))
    return b


def emit_mix_out(P, li, oT, wo_view, dst):
    k = P.k
    g_d = P.inp("ln_mix_g", [DEPTH, D])
    b_d = P.inp("ln_mix_b", [DEPTH, D])
    with ExitStack() as st:
        wo = load_w_bf16(P, st, "wo_sb", wo_view, KC, D)
        gb, bb = load_ln_params(P, st, View(g_d.ap[li:li + 1, :], g_d), View(b_d.ap[li:li + 1, :], b_d), "mix_ln")
        tmp = alloc_ln_tmp(P, st, "mix_")
        xin = [Buf(k.sb(st, f"mix_xin{i}", [128, D], F32)[:, :]) for i in range(3)]
        k.dma("sp", xin[0].v(), P.hres[0:128, :])
        k.dma("sp", xin[1].v(), P.hres[128:256, :])
        c = 0
        for t in range(NT):
            if t + 2 < NT:
                k.dma("sp", xin[(t + 2) % 3].v(), P.hres[(t + 2) * 128:(t + 3) * 128, :])
            xi = xin[t % 3]
            for hf in range(2):
                py = P.ps[c % 4]
                c += 1
                for kc in range(KC):
                    k.mm(py.v(), oT[kc][:, t * 128:(t + 1) * 128], wo[:, kc, hf * 512:(hf + 1) * 512], start=(kc == 0), stop=(kc == KC - 1))
                k.stt("dve", xi[:, hf * 512:(hf + 1) * 512], xi[:, hf * 512:(hf + 1) * 512], ALPHA, py.v(), ALU.mult, ALU.add)
            emit_ln_tail(P, st, t, xi, gb, bb, dst, tmp)
        flush_ln_tail(P, tmp)
        k.barrier()


def emit_mix_gmlp(P, li, dst):
    k = P.k
    nc = k.nc
    j = li // 4
    w_in = P.inp("sg_w_in", [1, D, 2 * D])
    vg_d = P.inp("sg_v_norm_g", [1, D])
    vb_d = P.inp("sg_v_norm_b", [1, D])
    ws_d = P.inp("sg_w_s", [1, 8, 128, 128])
    bs_d = P.inp("sg_b_s", [1, 8, 128])
    wout = P.inp("sg_w_out", [1, D, D])
    with ExitStack() as st:
        uT_t = k.sb(st, "uT", [128, KC, S], BF16)
        uT = [[Buf(uT_t[:, c, tb * 512:(tb + 1) * 512]) for tb in range(4)] for c in range(KC)]
        vln_t = k.sb(st, "vln", [128, NT, D], BF16)
        vln = [Buf(vln_t[:, t, :]) for t in range(NT)]
        wsT = Buf(k.sb(st, "wsT", [128, 8, 128], BF16)[:, :, :], "wsT")
        bs4 = Buf(k.sb(st, "bs4", [1, 8, 512], BF16)[:, :, :], "bs4")
        with ExitStack() as st2:
            win = load_w_bf16(P, st2, "w_in_sb", View(w_in.ap[j], w_in), KC, 2 * D)
            ws_sb = Buf(k.sb(st2, "ws_sb", [128, 8, 128], BF16)[:, :, :])
            k.dma("pool", ws_sb.v(), View(ws_d.ap[j].rearrange("g i j -> i g j"), ws_d))
            for g in range(8):
                k.tr(P.ps_tr[:, g * 128:(g + 1) * 128], ws_sb[:, g, :], P.ident.v())
            k.copy("dve", wsT.v(), View(P.ps_tr.ap.rearrange("p (k c) -> p k c", c=128), P.ps_tr))
            k.memset("pool", wsT[64:128, :, 0:64], 0.0)
            bs_f = Buf(k.sb(st2, "bs_f", [1, 8, 128], F32)[:, :, :])
            k.dma("sp", bs_f.v(), View(bs_d.ap[j:j + 1], bs_d))
            for r in range(4):
                k.copy("dve", bs4[:, :, r * 128:(r + 1) * 128], bs_f.v())
            vg, vb = load_ln_params(P, st2, View(vg_d.ap[j:j + 1, :], vg_d), View(vb_d.ap[j:j + 1, :], vb_d), "sg_ln")
            c2 = 0
            for c in range(KC):
                for tb in range(4):
                    pp = P.ps[c2 % 4]
                    c2 += 1
                    for kc in range(KC):
                        k.mm(pp.v(), win[:, kc, c * 128:(c + 1) * 128], P.hT[tb][:, kc, :], start=(kc == 0), stop=(kc == KC - 1))
                    k.act(uT[c][tb].v(), pp.v(), AF.Gelu_apprx_tanh)
            vt = [Buf(k.sb(st2, f"sg_v{i}", [128, D], F32)[:, :]) for i in range(2)]
            stats = Buf(k.sb(st2, "sg_stats", [128, 12], F32)[:, :])
            mv = Buf(k.sb(st2, "sg_mv", [128, 2], F32)[:, :])
            sc = Buf(k.sb(st2, "sg_sc", [128, 4], F32)[:, :])
            eps = Buf(k.sb(st2, "sg_eps", [128, 1], F32)[:, :])
            k.memset("pool", eps.v(), LN_EPS)
            for t in range(NT):
                tb, tt_ = t // 4, t % 4
                v = vt[t % 2]
                for hf in range(2):
                    pp = P.ps[4 + c2 % 3]
                    c2 += 1
                    for kc in range(KC):
                        k.mm(pp.v(), P.hT[tb][:, kc, tt_ * 128:(tt_ + 1) * 128], win[:, kc, D + hf * 512:D + (hf + 1) * 512],
                             start=(kc == 0), stop=(kc == KC - 1))
                    k.act(v[:, hf * 512:(hf + 1) * 512], pp.v(), AF.Gelu_apprx_tanh)
                    k.op("dve", lambda hf=hf, v=v: nc.vector.bn_stats(out=stats.ap[:, hf * 6:(hf + 1) * 6], in_=v.ap[:, hf * 512:(hf + 1) * 512]),
                         [v.v()], [stats.v()])
                k.op("dve", lambda: nc.vector.bn_aggr(out=mv.ap, in_=stats.ap), [stats.v()], [mv.v()])
                k.act(sc[:, 0:1], mv[:, 1:2], AF.Sqrt, bias=eps[:, 0:1])
                k.recip(sc[:, 1:2], sc[:, 0:1])
                k.stt("dve", sc[:, 2:3], mv[:, 0:1], -1.0, sc[:, 1:2], ALU.mult, ALU.mult)
                k.act(v.v(), v.v(), AF.Identity, scale=sc[:, 1:2], bias=sc[:, 2:3])
                k.tt("pool", v.v(), v.v(), vg.v(), ALU.mult)
                k.tt("pool", vln[t].v(), v.v(), vb.v(), ALU.add)
            k.barrier()
        c3 = 0
        for g in range(8):
            for tb in range(4):
                pp = P.ps[c3 % 4]
                c3 += 1
                k.mm(pp.v(), P.ones[0:1, :], bs4[0:1, g, :], start=True, stop=False)
                for r in range(4):
                    t = tb * 4 + r
                    k.mm(pp[:, r * 128:(r + 1) * 128], vln[t][:, g * 128:(g + 1) * 128], wsT[:, g, :], start=False, stop=(r == 3))
                k.tt("dve", uT[g][tb].v(), uT[g][tb].v(), pp.v(), ALU.mult)
        sT = [Buf(uT_t[:, c, :]) for c in range(KC)]
        k.barrier()
        emit_mix_out(P, li, sT, View(wout.ap[j], wout), dst)


class Rot:
    def __init__(self, n):
        self.n = n
        self.i = 0

    def nxt(self):
        v = self.i % self.n
        self.i += 1
        return v


class AttnPipe:
    def __init__(self, depth=2):
        self.depth = depth
        self.q = []
        self.deferred = []

    def push(self, cfn, after=None):
        self.q.append((cfn, after))
        ready = [fn for (n, fn) in self.deferred if n <= 1]
        self.deferred = [(n - 1, fn) for (n, fn) in self.deferred if n > 1]
        for fn in ready:
            fn()
        while len(self.q) > self.depth:
            self._pop()

    def _pop(self):
        cfn, after = self.q.pop(0)
        cfn()
        if after is not None:
            after()

    def defer(self, n, fn):
        self.deferred.append((n, fn))

    def flush(self):
        while self.q:
            self._pop()
        while self.deferred:
            d = self.deferred
            self.deferred = []
            for (_, fn) in d:
                fn()


def run_blocks(P, pipe, blocks, q_of, k_of, v_of, ones_v, psO, psD, pts, rs, rp, scale, bias_of=None, after=None):
    k = P.k
    n = len(blocks)
    for bi, (kb, c0, N, zero, extra) in enumerate(blocks):
        pS = P.ps[rs.nxt()]
        k.mm(pS[:, 0:N], k_of(kb), q_of(c0, N), start=True, stop=(bias_of is None))
        if bias_of is not None:
            k.mm(pS[:, 0:N], P.ident.v(), bias_of(extra, N), start=False, stop=True)
        pt = pts[rp.nxt()]
        k.act(pt[:, 0:N], pS[:, 0:N], AF.Exp, scale=scale)
        if zero is not None:
            (r0, r1, z0, z1) = zero
            k.memset("pool", pt[r0:r1, z0:z1], 0.0)

        def cfn(o=psO[:, c0:c0 + N], dn=psD[:, c0:c0 + N], vv=v_of(kb), pv=pt[:, 0:N], st_=(bi == 0), sp_=(bi == n - 1)):
            k.mm(o, vv, pv, start=st_, stop=sp_)
            k.mm(dn, ones_v, pv, start=st_, stop=sp_)

        pipe.push(cfn, after if bi == n - 1 else None)


def causal_blocks(qb):
    bl = []
    for kb in range(4 * qb + 4):
        r = kb - 4 * qb
        if r <= 0:
            bl.append((kb, 0, 512, (64, 128, 0, 64) if r == 0 else None, None))
        else:
            bl.append((kb, 128 * r, 512 - 128 * r, (64, 128, 0, 64), None))
    return bl


def emit_mix_diff(P, li, dst):
    k = P.k
    nc = k.nc
    j = li // 4
    lam_init = 0.8 - 0.6 * math.exp(-0.3 * li)
    wq = P.inp("diff_wq", [1, D, D])
    wk = P.inp("diff_wk", [1, D, D])
    wv = P.inp("diff_wv", [1, D, D])
    wo = P.inp("diff_wo", [1, D, D])
    lqk = [P.inp(n, [1, 64]) for n in ("diff_lq1", "diff_lk1", "diff_lq2", "diff_lk2")]
    subg = P.inp("diff_sub_g", [1, 128])
    scale = 64 ** -0.5
    with ExitStack() as st:
        oT_t = k.sb(st, "oT", [128, KC, S], BF16)
        oTb = [[Buf(oT_t[:, c, qb * 512:(qb + 1) * 512]) for qb in range(4)] for c in range(KC)]
        sti = ExitStack()
        V_t = k.sb(sti, "Vall", [128, NT, D], BF16)
        V = [Buf(V_t[:, t, :]) for t in range(NT)]
        nlam = Buf(k.sb(sti, "nlam", [128, 1], F32)[:, :])
        gsc = Buf(k.sb(sti, "gsc", [128, 1], F32)[:, :])
        eps5 = Buf(k.sb(sti, "eps5", [128, 1], F32)[:, :])
        k.memset("pool", eps5.v(), 1e-5)
        with ExitStack() as st2:
            lt = Buf(k.sb(st2, "lqk", [128, 4, 64], F32)[:, :, :])
            for i in range(4):
                k.dma("sp", lt[:, i, :], View(lqk[i].ap[j:j + 1, :].partition_broadcast(128), lqk[i]))
            junk = Buf(k.sb(st2, "ljunk", [128, 64], F32)[:, :])
            acc = Buf(k.sb(st2, "lacc", [128, 4], F32)[:, :])
            k.stt("dve", junk.v(), lt[:, 0, :], 1.0, lt[:, 1, :], ALU.mult, ALU.mult, accum=acc[:, 0:1])
            k.stt("dve", junk.v(), lt[:, 2, :], 1.0, lt[:, 3, :], ALU.mult, ALU.mult, accum=acc[:, 1:2])
            k.act(acc[:, 2:3], acc[:, 0:1], AF.Exp)
            k.act(acc[:, 3:4], acc[:, 1:2], AF.Exp)
            k.tt("dve", nlam.v(), acc[:, 3:4], acc[:, 2:3], ALU.subtract)
            k.ts("dve", nlam.v(), nlam.v(), -lam_init, ALU.add)
            k.dma("sp", gsc.v(), View(subg.ap[j:j + 1, :].rearrange("o d -> d o"), subg))
            k.ts("dve", gsc.v(), gsc.v(), 1.0 - lam_init, ALU.mult)
            wv_sb = load_w_bf16(P, st2, "wv_sb", View(wv.ap[j], wv), KC, D)
            c = 0
            for t in range(NT):
                tb, tt_ = t // 4, t % 4
                for hf in range(2):
                    pp = P.ps[c % 4]
                    c += 1
                    for kc in range(KC):
                        k.mm(pp.v(), P.hT[tb][:, kc, tt_ * 128:(tt_ + 1) * 128], wv_sb[:, kc, hf * 512:(hf + 1) * 512],
                             start=(kc == 0), stop=(kc == KC - 1))
                    k.copy("act", V[t][:, hf * 512:(hf + 1) * 512], pp.v())
            k.barrier()
        with ExitStack() as st3:
            wqh = [Buf(k.sb(st3, f"wqh{i}", [128, KC, 128], BF16)[:, :, :]) for i in range(2)]
            wkh = [Buf(k.sb(st3, f"wkh{i}", [128, KC, 128], BF16)[:, :, :]) for i in range(2)]
            QT_t = [k.sb(st3, f"QT{i}", [128, S], BF16) for i in range(2)]
            KT_t = [k.sb(st3, f"KT{i}", [128, S], BF16) for i in range(2)]
            QT = [[Buf(QT_t[i][:, tb * 512:(tb + 1) * 512]) for tb in range(4)] for i in range(2)]
            KT = [[Buf(KT_t[i][:, tb * 512:(tb + 1) * 512]) for tb in range(4)] for i in range(2)]
            pts = [Buf(k.sb(st3, f"pt{i}", [128, 512], BF16)[:, :]) for i in range(4)]
            rd = Buf(k.sb(st3, "f_rd", [128, 512], F32)[:, :])
            o0 = Buf(k.sb(st3, "f_o0", [128, 512], F32)[:, :])
            o1 = Buf(k.sb(st3, "f_o1", [128, 512], F32)[:, :])
            sq = Buf(k.sb(st3, "f_sq", [128, 512], BF16)[:, :])
            rs, rp = Rot(3), Rot(4)

            def load_head(h):
                s_ = h % 2
                k.dma("pool", wqh[s_].v(), View(wq.ap[j].rearrange("(kc p) f -> p kc f", p=128)[:, :, h * 128:(h + 1) * 128], wq))
                k.dma("pool", wkh[s_].v(), View(wk.ap[j].rearrange("(kc p) f -> p kc f", p=128)[:, :, h * 128:(h + 1) * 128], wk))

            def proj_head(h):
                s_ = h % 2
                for (W, T) in ((wqh[s_], QT[s_]), (wkh[s_], KT[s_])):
                    for tb in range(4):
                        pp = P.ps[rs.nxt()]
                        for kc in range(KC):
                            k.mm(pp.v(), W[:, kc, :], P.hT[tb][:, kc, :], start=(kc == 0), stop=(kc == KC - 1))
                        k.copy("dve", T[tb].v(), pp.v())

            pipe = AttnPipe(3)
            unit = [0]
            o0b = [Buf(k.sb(st3, f"f_o0b{i}", [128, 512], F32)[:, :]) for i in range(2)]
            sqb = [Buf(k.sb(st3, f"f_sqb{i}", [128, 512], BF16)[:, :]) for i in range(2)]
            rdb = [Buf(k.sb(st3, f"f_rdb{i}", [128, 512], F32)[:, :]) for i in range(2)]

            def attn_head(h):
                s_ = h % 2
                for qb in range(4):
                    bl = causal_blocks(qb)
                    par = (h * 4 + qb) % 2
                    for m in range(2):
                        r0, r1 = m * 64, (m + 1) * 64
                        u = unit[0] % 2
                        unit[0] += 1
                        psO, psD = P.ps[3 + 2 * u], P.ps[4 + 2 * u]

                        def fin(m=m, psO=psO, psD=psD, par=par, h=h, qb=qb):
                            if m == 0:
                                k.recip(rd.v(), psD.v())
                                k.tt("dve", o0b[par].v(), psO.v(), rd.v(), ALU.mult)
                            else:
                                k.recip(rd.v(), psD.v())
                                k.tt("dve", o1.v(), psO.v(), rd.v(), ALU.mult)
                                k.stt("dve", o0b[par].v(), o1.v(), nlam[:, 0:1], o0b[par].v(), ALU.mult, ALU.add)
                                k.tt("dve", sqb[par].v(), o0b[par].v(), o0b[par].v(), ALU.mult)

                                def fin2():
                                    pS = P.ps[rs.nxt()]
                                    k.mm(pS.v(), P.ones.v(), sqb[par].v(), start=True, stop=True)
                                    k.act(rdb[par].v(), pS.v(), AF.Ln, scale=1.0 / 128.0, bias=eps5[:, 0:1])
                                    k.act(rdb[par].v(), rdb[par].v(), AF.Exp, scale=-0.5)
                                    k.stt("dve", oTb[h][qb].v(), o0b[par].v(), gsc[:, 0:1], rdb[par].v(), ALU.mult, ALU.mult)

                                pipe.defer(4, fin2)

                        run_blocks(P, pipe, bl,
                                   q_of=lambda c0, N: QT[s_][qb][r0:r1, c0:c0 + N],
                                   k_of=lambda kb: KT[s_][kb // 4][r0:r1, (kb % 4) * 128:(kb % 4 + 1) * 128],
                                   v_of=lambda kb: V[kb][:, h * 128:(h + 1) * 128],
                                   ones_v=P.ones.v(), psO=psO, psD=psD, pts=pts, rs=rs, rp=rp, scale=scale, after=fin)

            load_head(0)
            load_head(1)
            proj_head(0)
            for h in range(8):
                if h + 1 < 8:
                    proj_head(h + 1)
                if h + 2 < 8:
                    load_head(h + 2)
                attn_head(h)
            pipe.flush()
            k.barrier()
        sti.close()
        oT = [Buf(oT_t[:, c, :]) for c in range(KC)]
        emit_mix_out(P, li, oT, View(wo.ap[j], wo), dst)


def band_blocks(qb):
    bl = []
    for r in (4, 5, 6, 7, 3, 2, 1, 0):
        if r >= 4:
            rp_ = r - 4
            bl.append((4 * qb + rp_, 128 * rp_, 512 - 128 * rp_, (64, 128, 0, 64), 0))
        elif qb > 0:
            N = 128 * (r + 1)
            bl.append((4 * qb - 4 + r, 0, N, (0, 64, N - 64, N), 512 - 128 * r))
    return bl


def emit_mix_band(P, li, dst):
    k = P.k
    j = li // 4
    wqkv = P.inp("ca_w_qkv", [1, D, 3 * D])
    bt_d = P.inp("ca_bt", [16, 128, 640])
    wo = P.inp("ca_wo", [1, D, D])
    scale = 64 ** -0.5
    with ExitStack() as st:
        oT_t = k.sb(st, "oT", [128, KC, S], BF16)
        oTb = [[Buf(oT_t[:, c, qb * 512:(qb + 1) * 512]) for qb in range(4)] for c in range(KC)]
        sti = ExitStack()
        V_t = k.sb(sti, "Vall", [128, NT, D], BF16)
        V = [Buf(V_t[:, t, :]) for t in range(NT)]
        BT = Buf(k.sb(sti, "BT", [128, 16, 640], BF16)[:, :, :], "BT")
        k.dma("pool", BT.v(), View(bt_d.ap.rearrange("h p x -> p h x"), bt_d))
        k.ts("pool", BT.v(), BT.v(), 1.0 / scale, ALU.mult)
        with ExitStack() as st2:
            wv_sb = load_w_bf16(P, st2, "wv_sb", View(wqkv.ap[j], wqkv), KC, D, col0=2 * D)
            c = 0
            for t in range(NT):
                tb, tt_ = t // 4, t % 4
                for hf in range(2):
                    pp = P.ps[c % 4]
                    c += 1
                    for kc in range(KC):
                        k.mm(pp.v(), P.hT[tb][:, kc, tt_ * 128:(tt_ + 1) * 128], wv_sb[:, kc, hf * 512:(hf + 1) * 512],
                             start=(kc == 0), stop=(kc == KC - 1))
                    k.copy("act", V[t][:, hf * 512:(hf + 1) * 512], pp.v())
            k.barrier()
        with ExitStack() as st3:
            wqh = [Buf(k.sb(st3, f"wqh{i}", [128, KC, 128], BF16)[:, :, :]) for i in range(2)]
            wkh = [Buf(k.sb(st3, f"wkh{i}", [128, KC, 128], BF16)[:, :, :]) for i in range(2)]
            QT_t = [k.sb(st3, f"QT{i}", [128, S], BF16) for i in range(2)]
            KT_t = [k.sb(st3, f"KT{i}", [128, S], BF16) for i in range(2)]
            QT = [[Buf(QT_t[i][:, tb * 512:(tb + 1) * 512]) for tb in range(4)] for i in range(2)]
            KT = [[Buf(KT_t[i][:, tb * 512:(tb + 1) * 512]) for tb in range(4)] for i in range(2)]
            pts = [Buf(k.sb(st3, f"pt{i}", [128, 512], BF16)[:, :]) for i in range(4)]
            rd = [Buf(k.sb(st3, f"f_rd{i}", [128, 512], F32)[:, :]) for i in range(2)]
            rs, rp = Rot(3), Rot(4)
            wq_v = View(wqkv.ap[j].rearrange("(kc p) f -> p kc f", p=128), wqkv)

            def load_pair(p):
                s_ = p % 2
                k.dma("pool", wqh[s_].v(), wq_v[:, :, p * 128:(p + 1) * 128])
                k.dma("pool", wkh[s_].v(), wq_v[:, :, D + p * 128:D + (p + 1) * 128])

            def proj_pair(p):
                s_ = p % 2
                for (W, T) in ((wqh[s_], QT[s_]), (wkh[s_], KT[s_])):
                    for tb in range(4):
                        pp = P.ps[rs.nxt()]
                        for kc in range(KC):
                            k.mm(pp.v(), W[:, kc, :], P.hT[tb][:, kc, :], start=(kc == 0), stop=(kc == KC - 1))
                        k.copy("dve", T[tb].v(), pp.v())

            unit = [0]
            pipe = AttnPipe(3)

            def attn_pair(p):
                s_ = p % 2
                for hh in range(2):
                    h = 2 * p + hh
                    r0, r1 = hh * 64, (hh + 1) * 64
                    for qb in range(4):
                        u = unit[0] % 2
                        unit[0] += 1
                        psO, psD = P.ps[3 + 2 * u], P.ps[4 + 2 * u]
                        def fin(u=u, psO=psO, psD=psD, r0=r0, r1=r1, p=p, qb=qb):
                            k.recip(rd[u][r0:r1, :], psD[r0:r1, :])
                            k.tt("dve", oTb[p][qb][r0:r1, :], psO[r0:r1, :], rd[u][r0:r1, :], ALU.mult)

                        run_blocks(P, pipe, band_blocks(qb),
                                   q_of=lambda c0, N: QT[s_][qb][r0:r1, c0:c0 + N],
                                   k_of=lambda kb: KT[s_][kb // 4][r0:r1, (kb % 4) * 128:(kb % 4 + 1) * 128],
                                   v_of=lambda kb: V[kb][:, p * 128:(p + 1) * 128],
                                   ones_v=P.ones.v(), psO=psO, psD=psD, pts=pts, rs=rs, rp=rp, scale=scale,
                                   bias_of=lambda off, N: BT[:, h, off:off + N], after=fin)

            load_pair(0)
            load_pair(1)
            proj_pair(0)
            for p in range(8):
                if p + 1 < 8:
                    proj_pair(p + 1)
                if p + 2 < 8:
                    load_pair(p + 2)
                attn_pair(p)
            pipe.flush()
            k.barrier()
        sti.close()
        oT = [Buf(oT_t[:, c, :]) for c in range(KC)]
        emit_mix_out(P, li, oT, View(wo.ap[j], wo), dst)


MLA_STAGE = [9]


def emit_mix_mla(P, li, dst):
    k = P.k
    nc = k.nc
    j = li // 4
    QR, KVR, NH = 384, 256, 16
    w_dq = P.inp("mla_w_dq", [1, D, QR])
    qg_d = P.inp("mla_q_norm_g", [1, QR])
    w_uq = P.inp("mla_w_uq", [1, QR, NH * 96])
    w_dkv = P.inp("mla_w_dkv", [1, D, KVR + 32])
    kvg_d = P.inp("mla_kv_norm_g", [1, KVR])
    w_ukv = P.inp("mla_w_ukv", [1, KVR, NH * 128])
    wo = P.inp("mla_wo", [1, D, D])
    cs_d = P.inp("rope_cs", [2, 32, S])
    scale = 96 ** -0.5
    with ExitStack() as st:
        oT_t = k.sb(st, "oT", [128, KC, S], BF16)
        oTb = [[Buf(oT_t[:, c, qb * 512:(qb + 1) * 512]) for qb in range(4)] for c in range(KC)]
        sti = ExitStack()
        V_t = k.sb(sti, "Vall", [128, NT, D], BF16)
        V = [Buf(V_t[:, t, :]) for t in range(NT)]
        cqT_t = k.sb(sti, "cqT", [128, 3, S], BF16)
        cqT = [Buf(cqT_t[:, :, tb * 512:(tb + 1) * 512]) for tb in range(4)]
        ckvT_t = k.sb(sti, "ckvT", [128, 2, S], BF16)
        ckvT = [Buf(ckvT_t[:, :, tb * 512:(tb + 1) * 512]) for tb in range(4)]
        cs = Buf(k.sb(sti, "cs", [128, 2, S], F32)[:, :, :], "cs")
        k.dma("sp", cs[64:96, :, :], View(cs_d.ap.rearrange("c p s -> p c s"), cs_d))
        wuq = Buf(k.sb(sti, "wuq", [128, 3, NH * 96 + 32], BF16)[:, :, :], "wuq")
        wuqR = Buf(k.sb(sti, "wuqR", [128, 3, NH * 96 + 32], BF16)[:, :, :], "wuqR")
        wkn = Buf(k.sb(sti, "wkn", [128, 2, NH * 64 + 64], BF16)[:, :, :], "wkn")
        k.memset("pool", wuq.v(), 0.0)
        k.memset("pool", wkn.v(), 0.0)
        KR_t = k.sb(sti, "KR", [128, S], BF16)
        KR = [Buf(KR_t[:, tb * 512:(tb + 1) * 512]) for tb in range(4)]
        eps6 = Buf(k.sb(sti, "eps6", [128, 1], F32)[:, :])
        k.memset("pool", eps6.v(), 1e-6)
        with ExitStack() as st2:
            wdq = load_w_bf16(P, st2, "wdq", View(w_dq.ap[j], w_dq), KC, QR)
            wdkc = load_w_bf16(P, st2, "wdkc", View(w_dkv.ap[j], w_dkv), KC, KVR)
            wvv = Buf(k.sb(st2, "wvv", [128, 2, NH * 64], BF16)[:, :, :], "wvv")
            wkr = Buf(k.sb(st2, "wkr", [128, KC, 128], BF16)[:, :, :], "wkr")
            wkrR = Buf(k.sb(st2, "wkrR", [128, KC, 128], BF16)[:, :, :], "wkrR")
            wkst = Buf(k.sb(st2, "wkst", [128, KC, 32], F32)[:, :, :], "wkst")
            dkv_v = View(w_dkv.ap[j].rearrange("(kc p) f -> p kc f", p=128), w_dkv)
            k.memset("pool", wkr.v(), 0.0)
            k.memset("pool", wkrR.v(), 0.0)
            k.dma("sp", wkst.v(), dkv_v[:, :, KVR:KVR + 32])
            k.copy("pool", wkr[:, :, 64:96], wkst.v())
            k.ts("pool", wkrR[:, :, 64:80], wkst[:, :, 16:32], -1.0, ALU.mult)
            k.copy("pool", wkrR[:, :, 80:96], wkst[:, :, 0:16])
            gq = Buf(k.sb(st2, "gq", [128, 3], F32)[:, :])
            gkv = Buf(k.sb(st2, "gkv", [128, 2], F32)[:, :])
            for kc in range(3):
                k.dma("sp", gq[:, kc:kc + 1], View(qg_d.ap[j:j + 1, kc * 128:(kc + 1) * 128].rearrange("o d -> d o"), qg_d))
            for kc in range(2):
                k.dma("sp", gkv[:, kc:kc + 1], View(kvg_d.ap[j:j + 1, kc * 128:(kc + 1) * 128].rearrange("o d -> d o"), kvg_d))
            with ExitStack() as st2a:
                stg = Buf(k.sb(st2a, "stg_uq", [128, 3, NH * 96], F32)[:, :, :])
                k.dma("sp", stg.v(), View(w_uq.ap[j].rearrange("(kc p) f -> p kc f", p=128), w_uq))
                for kc in range(3):
                    k.act(wuq[:, kc, 0:NH * 96], stg[:, kc, :], AF.Identity, scale=gq[:, kc:kc + 1])
                k.memset("pool", wuqR.v(), 0.0)
                w4 = View(wuq.ap[:, :, 0:NH * 96].rearrange("p k (h c) -> p k h c", c=96), wuq)
                r4 = View(wuqR.ap[:, :, 0:NH * 96].rearrange("p k (h c) -> p k h c", c=96), wuqR)
                for kc in range(3):
                    k.ts("pool", r4[:, kc, :, 64:80], w4[:, kc, :, 80:96], -1.0, ALU.mult)
                    k.copy("pool", r4[:, kc, :, 80:96], w4[:, kc, :, 64:80])
                k.barrier()
            with ExitStack() as st2b:
                stg = Buf(k.sb(st2b, "stg_ukv", [128, 2, NH * 128], F32)[:, :, :])
                k.dma("sp", stg.v(), View(w_ukv.ap[j].rearrange("(kc p) f -> p kc f", p=128), w_ukv))
                s4 = View(stg.ap.rearrange("p k (h c) -> p k h c", c=128), stg)
                kn4 = View(wkn.ap[:, :, 0:NH * 64].rearrange("p k (h c) -> p k h c", c=64), wkn)
                vv4 = View(wvv.ap.rearrange("p k (h c) -> p k h c", c=64), wvv)
                for kc in range(2):
                    k.act(kn4[:, kc, :, :], s4[:, kc, :, 0:64], AF.Identity, scale=gkv[:, kc:kc + 1])
                    k.act(vv4[:, kc, :, :], s4[:, kc, :, 64:128], AF.Identity, scale=gkv[:, kc:kc + 1])
                k.barrier()
            junk = Buf(k.sb(st2, "mjunk", [128, QR], F32)[:, :])
            NT_A = NT if MLA_STAGE[0] >= 2 else 0
            cqn = [Buf(k.sb(st2, f"cqn{i}", [128, QR], BF16)[:, :]) for i in range(2)]
            cqf = [Buf(k.sb(st2, f"cqf{i}", [128, QR], F32)[:, :]) for i in range(2)]
            ckf = [Buf(k.sb(st2, f"ckf{i}", [128, KVR], F32)[:, :]) for i in range(2)]
            ckn = [Buf(k.sb(st2, f"ckn{i}", [128, KVR], BF16)[:, :]) for i in range(2)]
            sc = [Buf(k.sb(st2, f"msc{i}", [128, 8], F32)[:, :]) for i in range(2)]
            ptr3 = View(P.ps_tr.ap.rearrange("p (k c) -> p k c", c=128), P.ps_tr)
            for t in range(NT_A):
                tb, tt_ = t // 4, t % 4
                u = t % 2
                pq, pk = P.ps[2 * u], P.ps[2 * u + 1]
                for kc in range(KC):
                    k.mm(pq[:, 0:QR], P.hT[tb][:, kc, tt_ * 128:(tt_ + 1) * 128], wdq[:, kc, :], start=(kc == 0), stop=(kc == KC - 1))
                for kc in range(KC):
                    k.mm(pk[:, 0:KVR], P.hT[tb][:, kc, tt_ * 128:(tt_ + 1) * 128], wdkc[:, kc, :], start=(kc == 0), stop=(kc == KC - 1))
                k.copy("act", cqf[u].v(), pq[:, 0:QR])
                k.copy("act", ckf[u].v(), pk[:, 0:KVR])
                k.stt("dve", junk[:, 0:QR], cqf[u].v(), 1.0, cqf[u].v(), ALU.mult, ALU.mult, accum=sc[u][:, 0:1])
                k.stt("dve", junk[:, 0:KVR], ckf[u].v(), 1.0, ckf[u].v(), ALU.mult, ALU.mult, accum=sc[u][:, 1:2])
                k.act(sc[u][:, 2:3], sc[u][:, 0:1], AF.Sqrt, scale=1.0 / QR, bias=eps6[:, 0:1])
                k.act(sc[u][:, 3:4], sc[u][:, 1:2], AF.Sqrt, scale=1.0 / KVR, bias=eps6[:, 0:1])
                k.recip(sc[u][:, 4:6], sc[u][:, 2:4])
                k.ts("dve", cqn[u].v(), cqf[u].v(), sc[u][:, 4:5], ALU.mult)
                k.ts("dve", ckn[u].v(), ckf[u].v(), sc[u][:, 5:6], ALU.mult)
                for kc in range(3):
                    k.tr(P.ps_tr[:, kc * 128:(kc + 1) * 128], cqn[u][:, kc * 128:(kc + 1) * 128], P.ident.v())
                for kc in range(2):
                    k.tr(P.ps_tr[:, (3 + kc) * 128:(4 + kc) * 128], ckn[u][:, kc * 128:(kc + 1) * 128], P.ident.v())
                k.copy("dve", cqT[tb][:, :, tt_ * 128:(tt_ + 1) * 128], ptr3[:, 0:3, :])
                k.copy("dve", ckvT[tb][:, :, tt_ * 128:(tt_ + 1) * 128], ptr3[:, 3:5, :])
            ta = Buf(k.sb(st2, "rk_a", [128, 512], F32)[:, :])
            tb_ = Buf(k.sb(st2, "rk_b", [128, 512], F32)[:, :])
            for tb in range(4 if MLA_STAGE[0] >= 3 else 0):
                p1, p2 = P.ps[4], P.ps[5]
                for kc in range(KC):
                    k.mm(p1.v(), wkr[:, kc, :], P.hT[tb][:, kc, :], start=(kc == 0), stop=(kc == KC - 1))
                for kc in range(KC):
                    k.mm(p2.v(), wkrR[:, kc, :], P.hT[tb][:, kc, :], start=(kc == 0), stop=(kc == KC - 1))
                k.tt("dve", ta[64:96, :], p1[64:96, :], cs[64:96, 0, tb * 512:(tb + 1) * 512], ALU.mult)
                k.tt("dve", tb_[64:96, :], p2[64:96, :], cs[64:96, 1, tb * 512:(tb + 1) * 512], ALU.mult)
                k.tt("dve", KR[tb][64:96, :], ta[64:96, :], tb_[64:96, :], ALU.add)
            c = 0
            for t in range(NT if MLA_STAGE[0] >= 4 else 0):
                tb, tt_ = t // 4, t % 4
                for hf in range(2):
                    pp = P.ps[c % 4]
                    c += 1
                    for kc in range(2):
                        k.mm(pp.v(), ckvT[tb][:, kc, tt_ * 128:(tt_ + 1) * 128], wvv[:, kc, hf * 512:(hf + 1) * 512],
                             start=(kc == 0), stop=(kc == 1))
                    k.copy("act", V[t][:, hf * 512:(hf + 1) * 512], pp.v())
            k.barrier()
        with ExitStack() as st3:
            QT_t = [k.sb(st3, f"QT{i}", [128, S], BF16) for i in range(2)]
            KT_t = [k.sb(st3, f"KT{i}", [128, S], BF16) for i in range(2)]
            QT = [[Buf(QT_t[i][:, tb * 512:(tb + 1) * 512]) for tb in range(4)] for i in range(2)]
            KT = [[Buf(KT_t[i][:, tb * 512:(tb + 1) * 512]) for tb in range(4)] for i in range(2)]
            pts = [Buf(k.sb(st3, f"pt{i}", [128, 512], BF16)[:, :]) for i in range(4)]
            rd = [Buf(k.sb(st3, f"f_rd{i}", [128, 512], F32)[:, :]) for i in range(2)]
            ta = Buf(k.sb(st3, "rq_a", [128, 512], F32)[:, :])
            tb2 = Buf(k.sb(st3, "rq_b", [128, 512], F32)[:, :])
            rs, rp = Rot(3), Rot(4)
            for i in range(2):
                for tb in range(4):
                    k.memset("pool", QT[i][tb].v(), 0.0)
                    k.memset("pool", KT[i][tb].v(), 0.0)
                    k.copy("pool", KT[i][tb][64:96, :], KR[tb][64:96, :])

            def proj_head(h):
                s_ = h % 2
                for tb in range(4):
                    p1 = P.ps[rs.nxt()]
                    for kc in range(3):
                        k.mm(p1.v(), wuq[:, kc, h * 96:h * 96 + 128], cqT[tb][:, kc, :], start=(kc == 0), stop=(kc == 2))
                    p2 = P.ps[rs.nxt()]
                    for kc in range(3):
                        k.mm(p2.v(), wuqR[:, kc, h * 96:h * 96 + 128], cqT[tb][:, kc, :], start=(kc == 0), stop=(kc == 2))
                    k.copy("dve", QT[s_][tb][0:64, :], p1[0:64, :])
                    k.tt("dve", ta[64:96, :], p1[64:96, :], cs[64:96, 0, tb * 512:(tb + 1) * 512], ALU.mult)
                    k.tt("dve", tb2[64:96, :], p2[64:96, :], cs[64:96, 1, tb * 512:(tb + 1) * 512], ALU.mult)
                    k.tt("dve", QT[s_][tb][64:96, :], ta[64:96, :], tb2[64:96, :], ALU.add)
                    p3 = P.ps[rs.nxt()]
                    for kc in range(2):
                        k.mm(p3.v(), wkn[:, kc, h * 64:h * 64 + 128], ckvT[tb][:, kc, :], start=(kc == 0), stop=(kc == 1))
                    k.copy("dve", KT[s_][tb][0:64, :], p3[0:64, :])

            unit = [0]
            pipe = AttnPipe(3)

            def attn_head(h):
                s_ = h % 2
                p, hh = h // 2, h % 2
                r0, r1 = hh * 64, (hh + 1) * 64
                for qb in range(4):
                    u = unit[0] % 2
                    unit[0] += 1
                    psO, psD = P.ps[3 + 2 * u], P.ps[4 + 2 * u]
                    def fin(u=u, psO=psO, psD=psD, r0=r0, r1=r1, p=p, qb=qb):
                        k.recip(rd[u][r0:r1, :], psD[r0:r1, :])
                        k.tt("dve", oTb[p][qb][r0:r1, :], psO[r0:r1, :], rd[u][r0:r1, :], ALU.mult)

                    run_blocks(P, pipe, causal_blocks(qb),
                               q_of=lambda c0, N: QT[s_][qb][:, c0:c0 + N],
                               k_of=lambda kb: KT[s_][kb // 4][:, (kb % 4) * 128:(kb % 4 + 1) * 128],
                               v_of=lambda kb: V[kb][:, p * 128:(p + 1) * 128],
                               ones_v=P.ones.v(), psO=psO, psD=psD, pts=pts, rs=rs, rp=rp, scale=scale, after=fin)

            NHX = NH if MLA_STAGE[0] >= 6 else (1 if MLA_STAGE[0] >= 5 else 0)
            if NHX:
                proj_head(0)
            for h in range(NHX):
                if h + 1 < NHX:
                    proj_head(h + 1)
                if MLA_STAGE[0] != 5:
                    attn_head(h)
            pipe.flush()
            k.barrier()
        sti.close()
        oT = [Buf(oT_t[:, c, :]) for c in range(KC)]
        emit_mix_out(P, li, oT, View(wo.ap[j], wo), dst)


def build(plan):
    k = K()
    P = Prog(k, plan)
    emit_consts(P)
    emit_prologue(P)
    n = len(plan)
    for i, name in enumerate(plan):
        dst = P.y if i == n - 1 else P.hres
        kind, li = name[:3], int(name[3:])
        if kind == "ffn":
            if li % 2 == 1 and SPARSE_MOE:
                emit_ffn_sparse(P, li, dst)
            else:
                emit_ffn(P, li, moe=(li % 2 == 1), dst=dst)
        elif kind == "mix":
            [emit_mix_diff, emit_mix_band, emit_mix_mla, emit_mix_gmlp][li % 4](P, li, dst)
        else:
            raise ValueError(name)
    k.final_wait()
    return k, P


def host_inputs(P, inputs):
    m = {}
    for name in P.ins:
        if name == "ca_bt":
            rb = np.asarray(inputs["ca_rel_bias"], np.float32)[0]
            idx = np.minimum(np.arange(640)[None, :] - np.arange(128)[:, None] + 128, 256)
            m[name] = np.ascontiguousarray(rb[:, idx])
        elif name == "rope_cs":
            half = 16
            inv_freq = (10000.0 ** (-np.arange(half, dtype=np.float32) / half)).astype(np.float32)
            ang = np.arange(S, dtype=np.float32)[None, :] * inv_freq[:, None]
            cos = np.concatenate([np.cos(ang), np.cos(ang)], axis=0)
            sin = np.concatenate([np.sin(ang), np.sin(ang)], axis=0)
            m[name] = np.ascontiguousarray(np.stack([cos, sin], axis=0).astype(np.float32))
        elif name in ("moe_w1r", "moe_w3r"):
            w = np.asarray(inputs["moe_w1" if name == "moe_w1r" else "moe_w3"], np.float32)
            w = w.reshape(2, NE, 2, 4, 128, NG, 512).transpose(0, 1, 5, 4, 2, 3, 6)
            m[name] = np.ascontiguousarray(w).reshape(2 * NE * NG * 128 * 2, WROW)
        elif name == "moe_w2r":
            w = np.asarray(inputs["moe_w2"], np.float32)
            w = w.reshape(2, NE, NG, 2, 2, 128, D).transpose(0, 1, 2, 5, 3, 4, 6)
            m[name] = np.ascontiguousarray(w).reshape(2 * NE * NG * 128 * 2, WROW)
        elif name == "tri_c":
            m[name] = np.triu(np.ones((128, 128), np.float32), 1)
        elif name == "iota2_c":
            m[name] = (2.0 * np.arange(128, dtype=np.float32)).reshape(128, 1)
        elif name == "moe_wrT":
            m[name] = np.ascontiguousarray(np.transpose(np.asarray(inputs["moe_w_router"], np.float32), (0, 2, 1)))
        else:
            m[name] = np.ascontiguousarray(np.asarray(inputs[name], np.float32))
    m["ident_c"] = np.eye(128, dtype=np.float32)
    return m


SPARSE_MOE = True
ROUTER_PE = True
FULL_PLAN = ["mix0", "ffn0", "mix1", "ffn1", "mix2", "ffn2", "mix3", "ffn3"]


def run_plan(plan, inputs, xs, trace=False):
    k, P = build(plan)
    shared = host_inputs(P, inputs)
    in_maps = []
    for xc in xs:
        mcore = dict(shared)
        mcore["x"] = np.ascontiguousarray(xc, dtype=np.float32)
        in_maps.append(mcore)
    res = run_bass_kernel_spmd(k.nc, in_maps, core_ids=list(range(len(xs))), trace=trace)
    return [r["y"] for r in res.results], res


def kernel(**inputs):
    x = np.asarray(inputs["x"], np.float32)
    outs, _ = run_plan(FULL_PLAN, inputs, [x[b] for b in range(x.shape[0])])
    return np.stack(outs, axis=0).astype(np.float32)
```
